# Optimizing a Trainium2 kernel written in Bass

```python
import math
import jax, jax.numpy as jnp
from jax import lax
import numpy as np

D_MODEL = 1024
BATCH = 8
SEQ = 4096
DEPTH = 2

D_MIX = D_MODEL
N_GROUPS = 4
GROUP = D_MIX // N_GROUPS
HEAD_DIM = 64
LRU_BLOCKS = GROUP // HEAD_DIM
LRU_BLOCK_DIM = GROUP // LRU_BLOCKS
LRU_CONV = 4
LRU_C = 8.0
SC_CONV = 3
FOX_HEADS = GROUP // HEAD_DIM
NSA_HEADS = GROUP // HEAD_DIM
CMP_LEN = 32
CMP_STRIDE = 16
CMP_HIDDEN = 128
SLC_BLOCK = 64
SLC_TOPK = 16
WINDOW = 512
Q_BLOCK = 128
REL_BUCKETS = 32
REL_MAX_DIST = 128
D_FF = ((8 * D_MODEL // 3 + 255) // 256) * 256
NEG_INF = -1e30
BIG = 1e30
RMS_EPS = 1e-6

C_LRU_X = 0
C_LRU_G = C_LRU_X + GROUP
C_SC_B = C_LRU_G + GROUP
C_SC_C = C_SC_B + GROUP
C_SC_X = C_SC_C + GROUP
C_FOX_Q = C_SC_X + GROUP
C_FOX_K = C_FOX_Q + GROUP
C_FOX_V = C_FOX_K + GROUP
C_FOX_F = C_FOX_V + GROUP
C_NSA_Q = C_FOX_F + FOX_HEADS
C_NSA_KV = C_NSA_Q + GROUP
C_NSA_G = C_NSA_KV + 6 * HEAD_DIM
N_IN = C_NSA_G + 3 * NSA_HEADS

kernel_name = "hymba_style_hybrid_lru_conv_fox_nsa"


def rms_norm(x, g):
    xf = x.astype(jnp.float32)
    y = xf * lax.rsqrt(jnp.mean(xf * xf, axis=-1, keepdims=True) + RMS_EPS)
    return (y * g.astype(jnp.float32)).astype(x.dtype)


def cols(z, start, width):
    return z[..., start:start + width]


def causal_dwconv(x, w):
    k_len, ch = w.shape
    return lax.conv_general_dilated(x, w[:, None, :], window_strides=(1,), padding=[(k_len - 1, 0)],
                                    dimension_numbers=('NWC', 'WIO', 'NWC'), feature_group_count=ch)


def masked_softmax(s, mask):
    p = jax.nn.softmax(jnp.where(mask, s, NEG_INF), axis=-1)
    return jnp.where(mask, p, 0.0)


def rel_bucket(dist):
    max_exact = REL_BUCKETS // 2
    d = jnp.maximum(dist, 0)
    large = max_exact + (jnp.log(jnp.maximum(d, 1).astype(jnp.float32) / max_exact)
                         / math.log(REL_MAX_DIST / max_exact) * (REL_BUCKETS - max_exact)).astype(jnp.int32)
    large = jnp.minimum(large, REL_BUCKETS - 1)
    return jnp.where(d < max_exact, d, large)


def rg_lru_mixer(xr, gate, conv_w, conv_b, w_gates, b_gates, lam):
    bsz, s_len, _ = xr.shape
    xc = causal_dwconv(xr, conv_w) + conv_b
    xb = xc.reshape(bsz, s_len, LRU_BLOCKS, LRU_BLOCK_DIM)
    r = jax.nn.sigmoid(jnp.einsum('bshi,hij->bshj', xb, w_gates[0]).reshape(bsz, s_len, GROUP) + b_gates[0])
    i = jax.nn.sigmoid(jnp.einsum('bshi,hij->bshj', xb, w_gates[1]).reshape(bsz, s_len, GROUP) + b_gates[1])
    log_a = (-LRU_C * jax.nn.softplus(-lam.astype(jnp.float32))) * r.astype(jnp.float32)
    a = jnp.exp(log_a)
    b = jnp.sqrt(-jnp.expm1(2.0 * log_a)) * (i * xc).astype(jnp.float32)

    def combine(left, right):
        a1, b1 = left
        a2, b2 = right
        return a1 * a2, a2 * b1 + b2

    _, h = lax.associative_scan(combine, (a, b), axis=1)
    return h.astype(xr.dtype) * jax.nn.gelu(gate)


def short_conv_mixer(bg, cg, xs, conv_w):
    return bg * causal_dwconv(cg * xs, conv_w)


def fox_mixer(q, k, v, f_logit, f_bias, qk_gain):
    bsz, s_len, _ = q.shape
    h, d = FOX_HEADS, HEAD_DIM
    q = rms_norm(q.reshape(bsz, s_len, h, d), qk_gain[0])
    k = rms_norm(k.reshape(bsz, s_len, h, d), qk_gain[1])
    v = v.reshape(bsz, s_len, h, d)
    logf = jax.nn.log_sigmoid((f_logit + f_bias).astype(jnp.float32))
    c = lax.cumsum(logf, axis=1).transpose(0, 2, 1)
    nb = s_len // Q_BLOCK
    qb = q.reshape(bsz, nb, Q_BLOCK, h, d).swapaxes(0, 1)
    cb = c.reshape(bsz, h, nb, Q_BLOCK).transpose(2, 0, 1, 3)
    kpos = jnp.arange(s_len)
    scale = HEAD_DIM ** -0.5

    def block(args):
        qi, ci, bi = args
        t = bi * Q_BLOCK + jnp.arange(Q_BLOCK)
        s = jnp.einsum('bqhd,bkhd->bhqk', qi, k).astype(jnp.float32) * scale + (ci[..., :, None] - c[..., None, :])
        p = masked_softmax(s, kpos[None, :] <= t[:, None])
        return jnp.einsum('bhqk,bkhd->bqhd', p.astype(v.dtype), v)

    o = lax.map(block, (qb, cb, jnp.arange(nb)))
    return o.swapaxes(0, 1).reshape(bsz, s_len, GROUP)


def compress(kx, pos, w1, w2):
    s_len = kx.shape[1]
    n_cmp = (s_len - CMP_LEN) // CMP_STRIDE + 1
    idx = jnp.arange(n_cmp)[:, None] * CMP_STRIDE + jnp.arange(CMP_LEN)[None, :]
    blocks = kx[:, idx] + pos
    hid = jax.nn.gelu(jnp.einsum('bnld,ldm->bnm', blocks, w1))
    return hid @ w2


def nsa_mixer(q, kc_in, vc_in, ks_in, vs_in, kw_in, vw_in, g_logit, g_bias, qk_gain, cmp_pos, cmp_w1, cmp_w2, rel_bias):
    bsz, s_len, _ = q.shape
    h, d = NSA_HEADS, HEAD_DIM
    f32 = jnp.float32
    q = rms_norm(q.reshape(bsz, s_len, h, d), qk_gain[0])
    kc = rms_norm(compress(kc_in, cmp_pos[0], cmp_w1[0], cmp_w2[0]), qk_gain[1])
    vc = compress(vc_in, cmp_pos[1], cmp_w1[1], cmp_w2[1])
    ks_ = rms_norm(ks_in, qk_gain[2])
    kw = rms_norm(kw_in, qk_gain[3])
    gates = jax.nn.sigmoid(g_logit + g_bias).reshape(bsz, s_len, 3, h)
    n_cmp = kc.shape[1]
    n_slc = s_len // SLC_BLOCK
    top = min(SLC_TOPK, n_slc)
    cmp_start = jnp.arange(n_cmp) * CMP_STRIDE
    cmp_end = cmp_start + CMP_LEN - 1
    slc_start = jnp.arange(n_slc) * SLC_BLOCK
    overlap = jnp.clip(jnp.minimum(cmp_start[:, None] + CMP_LEN, slc_start[None, :] + SLC_BLOCK)
                       - jnp.maximum(cmp_start[:, None], slc_start[None, :]), 0, CMP_LEN).astype(f32)
    ks_blk = ks_.reshape(bsz, n_slc, SLC_BLOCK, d)
    vs_blk = vs_in.reshape(bsz, n_slc, SLC_BLOCK, d)
    kw_pad = jnp.pad(kw, ((0, 0), (WINDOW, 0), (0, 0)))
    vw_pad = jnp.pad(vw_in, ((0, 0), (WINDOW, 0), (0, 0)))
    nb = s_len // Q_BLOCK
    qb = q.reshape(bsz, nb, Q_BLOCK, h, d).swapaxes(0, 1)
    gb = gates.reshape(bsz, nb, Q_BLOCK, 3, h).swapaxes(0, 1)
    bidx = jnp.arange(bsz)[:, None, None]
    jblk = jnp.arange(n_slc)
    scale = HEAD_DIM ** -0.5

    def block(args):
        qi, gi, bi = args
        t = bi * Q_BLOCK + jnp.arange(Q_BLOCK)
        dist_c = t[:, None] - cmp_end[None, :]
        s_c = (jnp.einsum('bqhd,bnd->bhqn', qi, kc).astype(f32) * scale
               + rel_bias[rel_bucket(dist_c)].transpose(2, 0, 1))
        p_c = masked_softmax(s_c, dist_c >= 0)
        o_c = jnp.einsum('bhqn,bnd->bqhd', p_c.astype(vc.dtype), vc)
        imp = jnp.einsum('bhqn,nm->bqm', p_c, overlap)
        cur = t // SLC_BLOCK
        forced = (jblk[None, :] == 0) | (jblk[None, :] == cur[:, None]) | (jblk[None, :] == cur[:, None] - 1)
        imp = jnp.where(forced, BIG, imp)
        imp = jnp.where(jblk[None, :] <= cur[:, None], imp, -jnp.inf)
        _, sel = lax.top_k(imp, top)
        k_sel = ks_blk[bidx, sel].reshape(bsz, Q_BLOCK, top * SLC_BLOCK, d)
        v_sel = vs_blk[bidx, sel].reshape(bsz, Q_BLOCK, top * SLC_BLOCK, d)
        pos_sel = (sel[..., None] * SLC_BLOCK + jnp.arange(SLC_BLOCK)).reshape(bsz, Q_BLOCK, top * SLC_BLOCK)
        dist_s = t[None, :, None] - pos_sel
        s_s = (jnp.einsum('bqhd,bqnd->bhqn', qi, k_sel).astype(f32) * scale
               + rel_bias[rel_bucket(dist_s)].transpose(0, 3, 1, 2))
        p_s = masked_softmax(s_s, (dist_s >= 0)[:, None])
        o_s = jnp.einsum('bhqn,bqnd->bqhd', p_s.astype(v_sel.dtype), v_sel)
        k_win = lax.dynamic_slice_in_dim(kw_pad, bi * Q_BLOCK, WINDOW + Q_BLOCK, axis=1)
        v_win = lax.dynamic_slice_in_dim(vw_pad, bi * Q_BLOCK, WINDOW + Q_BLOCK, axis=1)
        pos_w = bi * Q_BLOCK - WINDOW + jnp.arange(WINDOW + Q_BLOCK)
        dist_w = t[:, None] - pos_w[None, :]
        mask_w = (dist_w >= 0) & (dist_w < WINDOW) & (pos_w[None, :] >= 0)
        s_w = (jnp.einsum('bqhd,bnd->bhqn', qi, k_win).astype(f32) * scale
               + rel_bias[rel_bucket(dist_w)].transpose(2, 0, 1))
        p_w = masked_softmax(s_w, mask_w)
        o_w = jnp.einsum('bhqn,bnd->bqhd', p_w.astype(v_win.dtype), v_win)
        return gi[:, :, 0, :, None] * o_c + gi[:, :, 1, :, None] * o_s + gi[:, :, 2, :, None] * o_w

    o = lax.map(block, (qb, gb, jnp.arange(nb)))
    return o.swapaxes(0, 1).reshape(bsz, s_len, GROUP)


def setup_inputs(seed: int = 0) -> dict:
    key = jax.random.key(seed)
    ks = jax.random.split(key, 24)
    f32 = jnp.float32
    nl = DEPTH

    def nrm(k, shape, scale):
        return jax.random.normal(k, shape, f32) * scale

    u = jax.random.uniform(ks[7], (nl, GROUP), f32, 0.9, 0.999)
    return {
        'x': jax.random.normal(ks[0], (BATCH, SEQ, D_MODEL), f32),
        'norm_mix': 1.0 + nrm(ks[1], (nl, D_MODEL), 0.02),
        'w_in': nrm(ks[2], (nl, D_MODEL, N_IN), D_MODEL ** -0.5),
        'lru_conv_w': nrm(ks[3], (nl, LRU_CONV, GROUP), LRU_CONV ** -0.5),
        'lru_conv_b': nrm(ks[4], (nl, GROUP), 0.02),
        'lru_w_gates': nrm(ks[5], (nl, 2, LRU_BLOCKS, LRU_BLOCK_DIM, LRU_BLOCK_DIM), LRU_BLOCK_DIM ** -0.5),
        'lru_b_gates': nrm(ks[6], (nl, 2, GROUP), 0.02),
        'lru_lambda': jnp.log(u) - jnp.log1p(-u),
        'sc_conv_w': nrm(ks[8], (nl, SC_CONV, GROUP), SC_CONV ** -0.5),
        'fox_f_bias': 3.0 + nrm(ks[9], (nl, FOX_HEADS), 0.1),
        'fox_qk_norm': 1.0 + nrm(ks[10], (nl, 2, HEAD_DIM), 0.02),
        'nsa_qk_norm': 1.0 + nrm(ks[11], (nl, 4, HEAD_DIM), 0.02),
        'nsa_cmp_pos': nrm(ks[12], (nl, 2, CMP_LEN, HEAD_DIM), 0.1),
        'nsa_cmp_w1': nrm(ks[13], (nl, 2, CMP_LEN, HEAD_DIM, CMP_HIDDEN), (CMP_LEN * HEAD_DIM) ** -0.5),
        'nsa_cmp_w2': nrm(ks[14], (nl, 2, CMP_HIDDEN, HEAD_DIM), CMP_HIDDEN ** -0.5),
        'nsa_gate_bias': nrm(ks[15], (nl, 3 * NSA_HEADS), 0.02),
        'rel_bias': nrm(ks[16], (REL_BUCKETS, NSA_HEADS), 0.2),
        'out_norm': 1.0 + nrm(ks[17], (nl, N_GROUPS, GROUP), 0.02),
        'w_out': nrm(ks[18], (nl, D_MIX, D_MODEL), D_MIX ** -0.5),
        'norm_ffn': 1.0 + nrm(ks[19], (nl, D_MODEL), 0.02),
        'w_gate_up': nrm(ks[20], (nl, D_MODEL, 2 * D_FF), D_MODEL ** -0.5),
        'w_down': nrm(ks[21], (nl, D_FF, D_MODEL), D_FF ** -0.5),
    }


def reference(x, norm_mix, w_in, lru_conv_w, lru_conv_b, lru_w_gates, lru_b_gates, lru_lambda, sc_conv_w,
              fox_f_bias, fox_qk_norm, nsa_qk_norm, nsa_cmp_pos, nsa_cmp_w1, nsa_cmp_w2, nsa_gate_bias,
              rel_bias, out_norm, w_out, norm_ffn, w_gate_up, w_down):
    for l in range(DEPTH):
        z = rms_norm(x, norm_mix[l]) @ w_in[l]
        y_a = rg_lru_mixer(cols(z, C_LRU_X, GROUP), cols(z, C_LRU_G, GROUP), lru_conv_w[l], lru_conv_b[l],
                           lru_w_gates[l], lru_b_gates[l], lru_lambda[l])
        y_b = short_conv_mixer(cols(z, C_SC_B, GROUP), cols(z, C_SC_C, GROUP), cols(z, C_SC_X, GROUP), sc_conv_w[l])
        y_c = fox_mixer(cols(z, C_FOX_Q, GROUP), cols(z, C_FOX_K, GROUP), cols(z, C_FOX_V, GROUP),
                        cols(z, C_FOX_F, FOX_HEADS), fox_f_bias[l], fox_qk_norm[l])
        kv = [cols(z, C_NSA_KV + j * HEAD_DIM, HEAD_DIM) for j in range(6)]
        y_d = nsa_mixer(cols(z, C_NSA_Q, GROUP), kv[0], kv[1], kv[2], kv[3], kv[4], kv[5],
                        cols(z, C_NSA_G, 3 * NSA_HEADS), nsa_gate_bias[l], nsa_qk_norm[l],
                        nsa_cmp_pos[l], nsa_cmp_w1[l], nsa_cmp_w2[l], rel_bias)
        mixed = jnp.concatenate([rms_norm(y_a, out_norm[l, 0]), rms_norm(y_b, out_norm[l, 1]),
                                 rms_norm(y_c, out_norm[l, 2]), rms_norm(y_d, out_norm[l, 3])], axis=-1)
        x = x + mixed @ w_out[l]
        gu = rms_norm(x, norm_ffn[l]) @ w_gate_up[l]
        x = x + (jax.nn.silu(gu[..., :D_FF]) * gu[..., D_FF:]) @ w_down[l]
    return x
```

```python
import numpy as np
import ml_dtypes
from contextlib import ExitStack
import concourse.bass as bass
import concourse.mybir as mybir
from concourse.bass_utils import run_bass_kernel_spmd

F32 = mybir.dt.float32
BF16 = mybir.dt.bfloat16
AF = mybir.ActivationFunctionType
ALU = mybir.AluOpType

S = 4096
D = 1024
NL = 2
DFF = 2816
NFM = 2308
NTM = 396
EPS = 1e-6
NEGM = -30000.0
R_LRU_X, R_LRU_G, R_SC_B, R_SC_C, R_SC_X = 0, 256, 512, 768, 1024
R_FOX_Q, R_FOX_K, R_NSA_Q, R_KC, R_VC, R_KS, R_KW, R_FOX_F = 1280, 1536, 1792, 2048, 2112, 2176, 2240, 2304
T_FOX_V, T_VS, T_VW, T_G = 0, 256, 320, 384
ZC = 4112
ZW = 128


class U:
    __slots__ = ("lastw", "readers")

    def __init__(self):
        self.lastw = None
        self.readers = []


class T:
    def __init__(self, t):
        self.t = t
        self.u = U()

    def __getitem__(self, k):
        return self.t[k]


class FW:
    def __init__(self, nc, es, ndma=32):
        self.nc = nc
        self.engs = {}
        for name, h in [("pe", nc.tensor), ("dve", nc.vector), ("act", nc.scalar),
                        ("pool", nc.gpsimd), ("sp", nc.sync)]:
            sem = es.enter_context(nc.semaphore("sem_" + name))
            self.engs[name] = dict(h=h, sem=sem, count=0, known={})
        self.dsems = [[es.enter_context(nc.semaphore("dq%d" % i)), 0] for i in range(ndma)]
        self.drr = 0
        self.ninstr = 0

    def _wait(self, en, toks):
        e = self.engs[en]
        need = {}
        for (sem, val) in toks:
            k = id(sem)
            if k not in need or need[k][1] < val:
                need[k] = (sem, val)
        for k, (sem, val) in need.items():
            if e["known"].get(k, 0) < val:
                e["h"].wait_ge(sem, val)
                e["known"][k] = val
                self.ninstr += 1

    def _deps(self, en, reads, writes):
        toks = []
        for u in reads:
            t = u.lastw
            if t is not None:
                if t[2] == en and en == "pe":
                    continue
                toks.append((t[0], t[1]))
        for u in writes:
            t = u.lastw
            if t is not None and (t[2] != en or en == "dma"):
                toks.append((t[0], t[1]))
            for r in u.readers:
                if r[2] != en or en == "dma":
                    toks.append((r[0], r[1]))
        return toks

    def _commit(self, tok, reads, writes):
        for u in writes:
            u.lastw = tok
            u.readers = []
        for u in reads:
            if u in writes:
                continue
            u.readers.append(tok)
            if len(u.readers) > 48:
                best = {}
                for r in u.readers:
                    k = id(r[0])
                    if k not in best or best[k][1] < r[1]:
                        best[k] = r
                u.readers = list(best.values())

    def op(self, en, fn, reads=(), writes=()):
        reads = [x.u if isinstance(x, T) else x for x in reads]
        writes = [x.u if isinstance(x, T) else x for x in writes]
        e = self.engs[en]
        self._wait(en, self._deps(en, reads, writes))
        ins = fn(e["h"])
        e["count"] += 1
        ins.then_inc(e["sem"], 1)
        self.ninstr += 1
        tok = (e["sem"], e["count"], en)
        self._commit(tok, reads, writes)
        return tok

    def dma(self, en, out, in_, reads=(), writes=(), **kw):
        reads = [x.u if isinstance(x, T) else x for x in reads]
        writes = [x.u if isinstance(x, T) else x for x in writes]
        e = self.engs[en]
        slot = self.dsems[self.drr % len(self.dsems)]
        self.drr += 1
        toks = self._deps("dma", reads, writes)
        toks.append((slot[0], slot[1]))
        self._wait(en, toks)
        ins = e["h"].dma_start(out=out, in_=in_, **kw)
        slot[1] += 16
        ins.then_inc(slot[0], 16)
        self.ninstr += 1
        tok = (slot[0], slot[1], "dma")
        self._commit(tok, reads, writes)
        return tok

    def barrier(self):
        toks = []
        for n, e in self.engs.items():
            if e["count"] > 0:
                toks.append((e["sem"], e["count"]))
        for s in self.dsems:
            if s[1] > 0:
                toks.append((s[0], s[1]))
        for n in self.engs:
            self._wait(n, toks)


def build(dbg=False, nlayers=NL, stop=99):
    nc = bass.Bass("TRN2", target_bir_lowering=False)

    def din(name, shape, dt=F32):
        return nc.dram_tensor(name, list(shape), dt, kind="ExternalInput").ap()

    kind_s = "ExternalOutput" if dbg else "Internal"

    def dscr(name, shape, dt=F32):
        return nc.dram_tensor(name, list(shape), dt, kind=kind_s)

    xT = din("xT", [D, S])
    w_in = din("w_in", [NL, D, 2704])
    w_out = din("w_out", [NL, D, D])
    w_gu = din("w_gu", [NL, D, 2 * DFF])
    w_dn = din("w_dn", [NL, DFF, D])
    pvec_d = din("pvec", [NL, 128, 64])
    brow_d = din("brow", [NL, 1, 524])
    wbd_d = din("wbd", [NL, 2, 2, 128, 128])
    cw1_d = din("cw1", [NL, 2, 32, 64, 128])
    cw2_d = din("cw2", [NL, 2, 128, 64])
    cposT_d = din("cposT", [NL, 2, 64, 32])
    rb33_d = din("rb33", [33, 4])
    ohc_d = din("ohc", [33, 8208], BF16)
    ohw_d = din("ohw", [33, 1152], BF16)
    cident_d = din("cident", [128, 128], BF16)
    cJ_d = din("cJ", [128, 128], BF16)
    ctri_d = din("ctri", [128, 128], BF16)
    cE_d = din("cE", [64, 32 * 128], BF16)
    covl_d = din("covl", [128, 128], BF16)
    out_d = nc.dram_tensor("out", [D, S], F32, kind="ExternalOutput").ap()
    zT_h = dscr("zT", [NFM, S])
    ztok_h = dscr("ztok", [S, NTM])
    mixT_h = dscr("mixT", [D, S], BF16)
    xres_h = dscr("xres", [D, S])
    ftc_h = dscr("ftc", [4, 8208], BF16)
    ftw_h = dscr("ftw", [4, 1152], BF16)
    zT, ztok, mixT, xres, ftc, ftw = (h.ap() for h in (zT_h, ztok_h, mixT_h, xres_h, ftc_h, ftw_h))
    u_zT = [[U() for _ in range(8)] for _ in range(19)]
    u_ztok = [U() for _ in range(32)]
    u_mix = [[U() for _ in range(8)] for _ in range(8)]
    u_xres = [U() for _ in range(8)]
    u_ft = U()

    with ExitStack() as es:
        fw = FW(nc, es)

        uid = [0]

        def sb(st, name, shape, dt=F32):
            uid[0] += 1
            return T(st.enter_context(nc.sbuf_tensor("%s_%d" % (name, uid[0]), list(shape), dt)))

        def psm(st, name, shape, dt=F32):
            return T(st.enter_context(nc.psum_tensor(name, list(shape), dt)))

        def act(out, in_, func, reads, writes, **kw):
            return fw.op("act", lambda e: e.activation(out=out, in_=in_, func=func, **kw), reads, writes)

        def mm(out, lhsT, rhs, start, stop, reads, writes, **kw):
            return fw.op("pe", lambda e: e.matmul(out, lhsT=lhsT, rhs=rhs, start=start, stop=stop, **kw), reads, writes)

        def tt(en, out, in0, in1, op, reads, writes):
            return fw.op(en, lambda e: e.tensor_tensor(out=out, in0=in0, in1=in1, op=op), reads, writes)

        def ts(en, out, in0, s1, s2, op0, op1, reads, writes):
            if op1 is None:
                return fw.op(en, lambda e: e.tensor_scalar(out=out, in0=in0, scalar1=s1, scalar2=None, op0=op0), reads, writes)
            return fw.op(en, lambda e: e.tensor_scalar(out=out, in0=in0, scalar1=s1, scalar2=s2, op0=op0, op1=op1), reads, writes)

        def stt(out, in0, scalar, in1, op0, op1, reads, writes):
            return fw.op("dve", lambda e: e.scalar_tensor_tensor(out=out, in0=in0, scalar=scalar, in1=in1, op0=op0, op1=op1), reads, writes)

        def cp(en, out, in_, reads, writes):
            return fw.op(en, lambda e: e.tensor_copy(out=out, in_=in_), reads, writes)

        def mset(en, ap, val, writes):
            return fw.op(en, lambda e: e.memset(ap, val), (), writes)

        ident = sb(es, "ident", [128, 128], BF16)
        Jm = sb(es, "Jm", [128, 128], BF16)
        tri = sb(es, "tri", [128, 128], BF16)
        Emat = sb(es, "Emat", [64, 32 * 128], BF16)
        ovl = sb(es, "ovl", [128, 128], BF16)
        ones1024 = sb(es, "ones1024", [128, 128], BF16)
        ones256 = sb(es, "ones256", [128, 128], BF16)
        ones64 = sb(es, "ones64", [64, 64], BF16)
        zeros = sb(es, "zeros", [128, 512], BF16)
        slab_s = sb(es, "slab_s", [128, 4, 640], BF16)
        slab_w = sb(es, "slab_w", [128, 4, 1024], BF16)
        pv_all = sb(es, "pv_all", [128, NL, 64])
        brow_all = sb(es, "brow_all", [128, NL, 524])
        fw.dma("sp", ident[:], cident_d[:, :], writes=[ident])
        fw.dma("sp", Jm[:], cJ_d[:, :], writes=[Jm])
        fw.dma("sp", tri[:], ctri_d[:, :], writes=[tri])
        fw.dma("sp", Emat[:], cE_d[:, :], writes=[Emat])
        fw.dma("sp", ovl[:], covl_d[:, :], writes=[ovl])
        for l in range(NL):
            fw.dma("sp", pv_all[:, l, :], pvec_d[l], writes=[pv_all])
            fw.dma("sp", brow_all[:, l, :], brow_d[l].to_broadcast([128, 524]), writes=[brow_all])
        mset("pool", ones1024[:], 1.0 / 1024, [ones1024])
        mset("pool", ones256[:], 1.0 / 256, [ones256])
        mset("pool", ones64[:], 1.0 / 64, [ones64])
        mset("pool", zeros[:], 0.0, [zeros])

        PS = [psm(es, "ps%d" % i, [128, 512]) for i in range(7)]
        PSB = psm(es, "psb", [128, 1024], BF16)
        rb31 = sb(es, "rb31", [128, 4])
        fw.dma("sp", rb31[:], rb33_d[31:32, :].to_broadcast([128, 4]), writes=[rb31])

        with ExitStack() as ph:
            rb_f = sb(ph, "rb_f", [33, 4])
            rb_b = sb(ph, "rb_b", [33, 4], BF16)
            oh = sb(ph, "oh", [33, 8208], BF16)
            ohw_s = sb(ph, "ohw_s", [33, 1152], BF16)
            ftab = sb(ph, "ftab", [4, 8208 + 1152], BF16)
            fw.dma("sp", rb_f[:], rb33_d[:, :], writes=[rb_f])
            fw.dma("sp", oh[:], ohc_d[:, :], writes=[oh])
            fw.dma("sp", ohw_s[:], ohw_d[:, :], writes=[ohw_s])
            cp("dve", rb_b[:], rb_f[:], [rb_f], [rb_b])
            pieces = [(oh, c0, min(512, 8208 - c0), c0) for c0 in range(0, 8208, 512)]
            pieces += [(ohw_s, c0, min(512, 1152 - c0), 8208 + c0) for c0 in range(0, 1152, 512)]
            for i, (src, c0, n, dst0) in enumerate(pieces):
                ps = PS[i % 2]
                mm(ps[0:4, 0:n], rb_b[:, 0:4], src[:, c0:c0 + n], True, True, [rb_b, src], [ps])
                cp("dve", ftab[:, dst0:dst0 + n], ps[0:4, 0:n], [ps], [ftab])
            fw.dma("sp", ftc[:, :], ftab[:, 0:8208], reads=[ftab], writes=[u_ft])
            fw.dma("sp", ftw[:, :], ftab[:, 8208:8208 + 1152], reads=[ftab], writes=[u_ft])
            for h in range(4):
                src = bass.AP(tensor=ftc_h, offset=h * 8208 + ZC - 127, ap=[[1, 128], [1, 640]])
                fw.dma("sp", slab_s[:, h, :], src, reads=[u_ft], writes=[slab_s])
                src = bass.AP(tensor=ftw_h, offset=h * 1152 + ZW - 127, ap=[[1, 128], [1, 1024]])
                fw.dma("sp", slab_w[:, h, :], src, reads=[u_ft], writes=[slab_w])
            fw.barrier()

        def rstd_from(ps_ap, out_ap, tmp_ap, reads, tmpT, outT, scale=1.0):
            act(tmp_ap, ps_ap, AF.Ln, reads, [tmpT], bias=EPS, scale=scale)
            act(out_ap, tmp_ap, AF.Exp, [tmpT], [outT], scale=-0.5)

        for l in range(nlayers):
            x_src = xT if l == 0 else xres
            x_dst = out_d if l == nlayers - 1 else xres
            pv = lambda c0, c1=None, p0=0, p1=128: pv_all[p0:p1, l, c0:(c0 + 1 if c1 is None else c1)]

            with ExitStack() as ph:
                w_sb = sb(ph, "w_sb", [128, 8, 2704], BF16)
                wst = [sb(ph, "wst%d" % i, [128, 2704]) for i in range(2)]
                for c in range(8):
                    st_ = wst[c % 2]
                    fw.dma("sp", st_[:], w_in[l, c * 128:(c + 1) * 128, :], writes=[st_])
                    ts("pool" if c % 2 else "dve", w_sb[:, c, :], st_[:], pv(c), None, ALU.mult, None, [st_, pv_all], [w_sb])
                xt = [sb(ph, "xt%d" % i, [128, 8, 512]) for i in range(2)]
                xn = [sb(ph, "xn%d" % i, [128, 8, 512], BF16) for i in range(2)]
                sq = sb(ph, "sq", [128, 8, 512], BF16)
                lnb = sb(ph, "lnb", [128, 512])
                rstd = sb(ph, "rstd", [128, 512])
                zst = [sb(ph, "zst%d" % i, [128, 512]) for i in range(4)]
                ev = 0
                for tt_ in range(8):
                    t0 = tt_ * 512
                    xt_, xn_ = xt[tt_ % 2], xn[tt_ % 2]
                    rd = [u_xres[tt_]] if l > 0 else []
                    fw.dma("sp", xt_[:], x_src[:, t0:t0 + 512].rearrange("(c p) t -> p c t", p=128), reads=rd, writes=[xt_])
                    act(sq[:], xt_[:], AF.Square, [xt_], [sq])
                    for c in range(8):
                        mm(PS[0][:], ones1024[:], sq[:, c, :], c == 0, c == 7, [ones1024, sq], [PS[0]])
                    rstd_from(PS[0][:], rstd[:], lnb[:], [PS[0]], lnb, rstd)
                    tt("dve", xn_[:], xt_[:], rstd[:].unsqueeze(1).to_broadcast([128, 8, 512]), ALU.mult, [xt_, rstd], [xn_])
                    for oc in range(19):
                        m = 128 if oc < 18 else 4
                        ps = PS[1 + ev % 3]
                        for c in range(8):
                            mm(ps[0:m, :], w_sb[:, c, oc * 128:oc * 128 + m], xn_[:, c, :], c == 0, c == 7, [w_sb, xn_], [ps])
                        st_ = zst[ev % 4]
                        if ev % 2 == 0:
                            act(st_[0:m, :], ps[0:m, :], AF.Copy, [ps], [st_])
                        else:
                            cp("dve", st_[0:m, :], ps[0:m, :], [ps], [st_])
                        fw.dma("pool", zT[oc * 128:oc * 128 + m, t0:t0 + 512], st_[0:m, :], reads=[st_], writes=[u_zT[oc][tt_]])
                        ev += 1
                    for s in range(4):
                        ps = PS[1 + ev % 3]
                        for c in range(8):
                            mm(ps[:, 0:NTM], xn_[:, c, s * 128:(s + 1) * 128], w_sb[:, c, NFM:NFM + NTM], c == 0, c == 7, [w_sb, xn_], [ps])
                        st_ = zst[ev % 4]
                        if ev % 2 == 0:
                            act(st_[:, 0:NTM], ps[:, 0:NTM], AF.Copy, [ps], [st_])
                        else:
                            cp("dve", st_[:, 0:NTM], ps[:, 0:NTM], [ps], [st_])
                        fw.dma("pool", ztok[t0 + s * 128:t0 + (s + 1) * 128, :], st_[:, 0:NTM], reads=[st_], writes=[u_ztok[tt_ * 4 + s]])
                        ev += 1
                fw.barrier()
            if stop <= 1:
                break

            with ExitStack() as ph:
                NB = 4100
                X = sb(ph, "X", [128, NB])
                C = sb(ph, "C", [128, NB])
                R = sb(ph, "R", [128, NB])
                I = sb(ph, "I", [128, NB])
                A2 = sb(ph, "A2", [128, NB])
                Y0 = sb(ph, "Y0", [128, S])
                xcb = sb(ph, "xcb", [128, S], BF16)
                sq0 = sb(ph, "sq0", [128, S], BF16)
                sq1 = sb(ph, "sq1", [128, S], BF16)
                wg_f = sb(ph, "wg_f", [128, 4, 128])
                wg_b = sb(ph, "wg_b", [128, 4, 128], BF16)
                cst = sb(ph, "cst", [128, 8])
                lnb = sb(ph, "lnb2", [128, 512])
                rstd = sb(ph, "rstd2", [128, 512])
                mst = [sb(ph, "mst%d" % i, [128, 512], BF16) for i in range(3)]
                for g in range(2):
                    for j in range(2):
                        fw.dma("sp", wg_f[:, g * 2 + j, :], wbd_d[l, g, j], writes=[wg_f])
                cp("dve", wg_b[:], wg_f[:], [wg_f], [wg_b])
                act(cst[:, 4:6], pv(30, 32), AF.Exp, [pv_all], [cst], scale=-1.0)
                act(cst[:, 6:8], cst[:, 4:6], AF.Ln, [cst], [cst], bias=1.0)
                ts("dve", cst[:, 0:2], cst[:, 6:8], -8.0, None, ALU.mult, None, [cst], [cst])
                ts("dve", cst[:, 2:4], cst[:, 6:8], -16.0, None, ALU.mult, None, [cst], [cst])

                def outnorm_fm(Ys, sqs, grp):
                    for j in range(2):
                        act(sqs[j][:], Ys[j][:, 0:S], AF.Square, [Ys[j]], [sqs[j]])
                    for tt_ in range(8):
                        t0 = tt_ * 512
                        ps = PS[tt_ % 2]
                        mm(ps[:], ones256[:], sqs[0][:, t0:t0 + 512], True, False, [ones256, sqs[0]], [ps])
                        mm(ps[:], ones256[:], sqs[1][:, t0:t0 + 512], False, True, [ones256, sqs[1]], [ps])
                        rstd_from(ps[:], rstd[:], lnb[:], [ps], lnb, rstd)
                        for j in range(2):
                            m_ = mst[(tt_ * 2 + j) % 3]
                            stt(m_[:], Ys[j][:, t0:t0 + 512], pv(38 + grp * 2 + j), rstd[:], ALU.mult, ALU.mult, [Ys[j], pv_all, rstd], [m_])
                            fw.dma("pool", mixT[grp * 256 + j * 128:grp * 256 + (j + 1) * 128, t0:t0 + 512], m_[:], reads=[m_], writes=[u_mix[grp * 2 + j][tt_]])

                for j in range(2):
                    mset("pool", X[:, 0:3], 0.0, [X])
                    fw.dma("sp", X[:, 3:3 + S], zT[R_LRU_X + j * 128:R_LRU_X + (j + 1) * 128, :], reads=u_zT[0 + j], writes=[X])
                    wc = 16 + j * 4
                    ts("dve", C[:, 0:S], X[:, 0:S], pv(wc), pv(24 + j), ALU.mult, ALU.add, [X, pv_all], [C])
                    for k in range(1, 4):
                        stt(C[:, 0:S], X[:, k:k + S], pv(wc + k), C[:, 0:S], ALU.mult, ALU.add, [X, pv_all, C], [C])
                    cp("pool", xcb[:], C[:, 0:S], [C], [xcb])
                    for tt_ in range(8):
                        t0 = tt_ * 512
                        for g, dst in ((0, R), (1, I)):
                            ps = PS[(tt_ * 2 + g) % 4]
                            mm(ps[:], wg_b[:, g * 2 + j, :], xcb[:, t0:t0 + 512], True, True, [wg_b, xcb], [ps])
                            act(dst[:, t0:t0 + 512], ps[:], AF.Sigmoid, [ps, pv_all], [dst], bias=pv(26 + g * 2 + j))
                    act(A2[:, 0:S], R[:, 0:S], AF.Exp, [R, cst], [A2], scale=cst[:, 2 + j:3 + j])
                    act(R[:, 0:S], R[:, 0:S], AF.Exp, [R, cst], [R], scale=cst[:, j:j + 1])
                    act(A2[:, 0:S], A2[:, 0:S], AF.Sqrt, [A2], [A2], scale=-1.0, bias=1.0)
                    tt("dve", I[:, 0:S], I[:, 0:S], C[:, 0:S], ALU.mult, [I, C], [I])
                    tt("dve", I[:, 0:S], I[:, 0:S], A2[:, 0:S], ALU.mult, [I, A2], [I])
                    fw.op("dve", lambda e: e.tensor_tensor_scan(out=C[:, 0:S], data0=R[:, 0:S], data1=I[:, 0:S], initial=0.0, op0=ALU.mult, op1=ALU.add), [R, I], [C])
                    fw.dma("sp", X[:, 0:S], zT[R_LRU_G + j * 128:R_LRU_G + (j + 1) * 128, :], reads=u_zT[2 + j], writes=[X])
                    act(A2[:, 0:S], X[:, 0:S], AF.Gelu_apprx_tanh, [X], [A2])
                    Yd = Y0 if j == 0 else C
                    tt("dve", Yd[:, 0:S], C[:, 0:S], A2[:, 0:S], ALU.mult, [C, A2], [Yd])
                outnorm_fm([Y0, C], [sq0, sq1], 0)
                for j in range(2):
                    Yd = Y0 if j == 0 else C
                    mset("pool", X[:, 0:2], 0.0, [X])
                    fw.dma("sp", R[:, 0:S], zT[R_SC_C + j * 128:R_SC_C + (j + 1) * 128, :], reads=u_zT[6 + j], writes=[R])
                    fw.dma("sp", I[:, 0:S], zT[R_SC_X + j * 128:R_SC_X + (j + 1) * 128, :], reads=u_zT[8 + j], writes=[I])
                    fw.dma("sp", A2[:, 0:S], zT[R_SC_B + j * 128:R_SC_B + (j + 1) * 128, :], reads=u_zT[4 + j], writes=[A2])
                    tt("dve", X[:, 2:2 + S], R[:, 0:S], I[:, 0:S], ALU.mult, [R, I], [X])
                    wc = 32 + j * 3
                    ts("dve", R[:, 0:S], X[:, 0:S], pv(wc), None, ALU.mult, None, [X, pv_all], [R])
                    for k in range(1, 3):
                        stt(R[:, 0:S], X[:, k:k + S], pv(wc + k), R[:, 0:S], ALU.mult, ALU.add, [X, pv_all, R], [R])
                    tt("dve", Yd[:, 0:S], R[:, 0:S], A2[:, 0:S], ALU.mult, [R, A2], [Yd])
                outnorm_fm([Y0, C], [sq0, sq1], 1)
                fw.barrier()
            if stop <= 2:
                break

            def headnorm(tl, src_ap, src_units, gcol, scale, dst, ntok):
                hq, hsq, gs, lnb, rstd = tl
                if src_ap is not None:
                    fw.dma("sp", hq[0:64, 0:ntok], src_ap, reads=src_units, writes=[hq])
                ts("dve", gs[:, 0:1], pv(gcol, p1=64), float(scale), None, ALU.mult, None, [pv_all], [gs])
                act(hsq[0:64, 0:ntok], hq[0:64, 0:ntok], AF.Square, [hq], [hsq])
                for t0 in range(0, ntok, 512):
                    n = min(512, ntok - t0)
                    ps = PS[5 + (t0 // 512) % 2]
                    mm(ps[0:64, 0:n], ones64[:], hsq[0:64, t0:t0 + n], True, True, [ones64, hsq], [ps])
                    rstd_from(ps[0:64, 0:n], rstd[0:64, 0:n], lnb[0:64, 0:n], [ps], lnb, rstd)
                    stt(dst[0:64, t0:t0 + n], hq[0:64, t0:t0 + n], gs[:, 0:1], rstd[0:64, 0:n], ALU.mult, ALU.mult, [hq, gs, rstd], [dst])

            def run_attention(tiles, pts):
                n_t = len(tiles)
                ring = PS[0:3]

                def emit_pre(i):
                    if i < n_t and tiles[i].get("pre") is not None:
                        bt, src = tiles[i]["pre"]
                        fw.dma("sp", bt[:], src, reads=[u_ft], writes=[bt])

                emit_pre(0)

                def emit_qk(i):
                    tl = tiles[i]
                    ps = ring[i % 3]
                    emit_pre(i + 1)
                    np_ = tl["np"]
                    last = len(tl["mms"]) - 1
                    for idx, (c0, ncol, lhsT, rhs, rds) in enumerate(tl["mms"]):
                        mm(ps[0:np_, c0:c0 + ncol], lhsT, rhs, idx == 0, idx == last, rds, [ps], skip_group_check=True)

                def emit_rest(i):
                    tl = tiles[i]
                    ps = ring[i % 3]
                    pt = pts[i % len(pts)]
                    np_, n = tl["np"], tl["n"]
                    if tl.get("bias") is not None:
                        act(pt[0:np_, 0:n], ps[0:np_, 0:n], AF.Exp, [ps] + tl["bias_reads"], [pt], bias=tl["bias"])
                    else:
                        act(pt[0:np_, 0:n], ps[0:np_, 0:n], AF.Exp, [ps], [pt])
                    if tl.get("init") is not None:
                        accT, w = tl["init"]
                        mm(accT[:, 0:w], zeros[:, 0:128], zeros[:, 0:w], True, True, [zeros], [accT])
                    for (accT, acc_ap, pc0, V_ap, rds) in tl["pv"]:
                        mm(acc_ap, pt[0:np_, pc0:pc0 + 128], V_ap, False, True, [pt] + rds, [accT], skip_group_check=True)
                    if tl.get("fin") is not None:
                        tl["fin"]()

                for i in range(n_t + 1):
                    if i < n_t:
                        emit_qk(i)
                    if i >= 1:
                        emit_rest(i - 1)

            def outnorm_tok(tl, Q, yv, yT, gain_ap, rowbase):
                junk, ssq, lnq, rs, ym, mst2 = tl
                q0 = Q * 512
                for s in range(4):
                    act(junk[:, 0:256], yv(s), AF.Square, [yT], [junk, ssq], accum_out=ssq[:, s:s + 1])
                act(lnq[:, 0:4], ssq[:, 0:4], AF.Ln, [ssq], [lnq], scale=1.0 / 256, bias=EPS)
                act(rs[:, 0:4], lnq[:, 0:4], AF.Exp, [lnq], [rs], scale=-0.5)
                for s in range(4):
                    stt(ym[:, s, :], yv(s), rs[:, s:s + 1], gain_ap, ALU.mult, ALU.mult, [yT, rs, brow_all], [ym])
                for j in range(2):
                    for s in range(4):
                        fw.op("pe", lambda e, s=s, j=j: e.transpose(out=PSB[:, s * 128:(s + 1) * 128], in_=ym[:, s, j * 128:(j + 1) * 128], identity=ident[:]), [ym, ident], [PSB])
                    m_ = mst2[j]
                    cp("dve", m_[:], PSB[:, 0:512], [PSB], [m_])
                    fw.dma("pool", mixT[rowbase + j * 128:rowbase + (j + 1) * 128, q0:q0 + 512], m_[:], reads=[m_], writes=[u_mix[rowbase // 128 + j][Q]])

            with ExitStack() as ph:
                Qp = [sb(ph, "Qp%d" % h, [128, S], BF16) for h in range(4)]
                Kp = [sb(ph, "Kp%d" % h, [128, S], BF16) for h in range(4)]
                with ExitStack() as ph2:
                    fr = sb(ph2, "fr", [4, S])
                    e1 = sb(ph2, "e1", [4, S])
                    cc = sb(ph2, "cc", [4, S])
                    ones4 = sb(ph2, "ones4", [4, S], BF16)
                    nb = sb(ph2, "nb", [4, 1])
                    csp = sb(ph2, "csp", [4, 3, S], BF16)
                    ncsp = sb(ph2, "ncsp", [4, 3, S], BF16)
                    fw.dma("sp", fr[:], zT[R_FOX_F:R_FOX_F + 4, :], reads=u_zT[18], writes=[fr])
                    ts("dve", nb[:], pv(48, p1=4), -1.0, None, ALU.mult, None, [pv_all], [nb])
                    act(e1[:], fr[:], AF.Exp, [fr, nb], [e1], scale=-1.0, bias=nb[:])
                    act(e1[:], e1[:], AF.Ln, [e1], [e1], bias=1.0)
                    ts("dve", e1[:], e1[:], -1.0, None, ALU.mult, None, [e1], [e1])
                    mset("pool", ones4[:], 1.0, [ones4])
                    fw.op("dve", lambda e: e.tensor_tensor_scan(out=cc[:], data0=ones4[:], data1=e1[:], initial=0.0, op0=ALU.mult, op1=ALU.add), [ones4, e1], [cc])
                    cp("dve", csp[:, 0, :], cc[:], [cc], [csp])
                    cp("dve", fr[:], csp[:, 0, :], [csp], [fr])
                    tt("dve", cc[:], cc[:], fr[:], ALU.subtract, [cc, fr], [cc])
                    cp("dve", csp[:, 1, :], cc[:], [cc], [csp])
                    cp("dve", fr[:], csp[:, 1, :], [csp], [fr])
                    tt("dve", cc[:], cc[:], fr[:], ALU.subtract, [cc, fr], [cc])
                    cp("dve", csp[:, 2, :], cc[:], [cc], [csp])
                    ts("dve", ncsp[:], csp[:], -1.0, None, ALU.mult, None, [csp], [ncsp])
                    for h in range(4):
                        mset("pool", Qp[h][64:128, :], 0.0, [Qp[h]])
                        mset("pool", Kp[h][64:128, :], 0.0, [Kp[h]])
                        for i in range(3):
                            fw.dma("sp", Qp[h][64 + i:65 + i, :], csp[h:h + 1, i, :], reads=[csp], writes=[Qp[h]])
                            fw.dma("sp", Kp[h][96 + i:97 + i, :], ncsp[h:h + 1, i, :], reads=[ncsp], writes=[Kp[h]])
                        fw.dma("sp", Qp[h][96:99, :], ones4[0:3, :], reads=[ones4], writes=[Qp[h]])
                        fw.dma("sp", Kp[h][64:67, :], ones4[0:3, :], reads=[ones4], writes=[Kp[h]])
                    fw.barrier()
                V1 = sb(ph, "V1", [128, 32, 4, 65], BF16)
                hq = sb(ph, "hq", [64, S])
                hsq = sb(ph, "hsq", [64, S], BF16)
                gs = sb(ph, "gs", [64, 1])
                lnb = sb(ph, "lnb3", [64, 512])
                rstd = sb(ph, "rstd3", [64, 512])
                hn = (hq, hsq, gs, lnb, rstd)
                vst = sb(ph, "vst", [128, 8, 256])
                pts = [sb(ph, "pt%d" % i, [128, 512], BF16) for i in range(3)]
                rden = [sb(ph, "rden%d" % i, [128, 4]) for i in range(2)]
                ytk = [sb(ph, "ytk%d" % i, [128, 4, 256]) for i in range(2)]
                on_tl = (sb(ph, "junk", [128, 256]), sb(ph, "ssq", [128, 4]), sb(ph, "lnq", [128, 4]), sb(ph, "rs", [128, 4]),
                         sb(ph, "ym", [128, 4, 256], BF16), [sb(ph, "mst2_%d" % i, [128, 512], BF16) for i in range(2)])
                for h in range(4):
                    headnorm(hn, zT[R_FOX_Q + h * 64:R_FOX_Q + (h + 1) * 64, :], u_zT[10 + h // 2], 42, 0.125, Qp[h], S)
                    headnorm(hn, zT[R_FOX_K + h * 64:R_FOX_K + (h + 1) * 64, :], u_zT[12 + h // 2], 43, 1.0, Kp[h], S)
                mset("pool", V1[:, :, :, 64:65], 1.0, [V1])
                for g in range(4):
                    fw.dma("sp", vst[:], ztok[g * 1024:(g + 1) * 1024, T_FOX_V:T_FOX_V + 256].rearrange("(k p) c -> p k c", p=128), reads=u_ztok[g * 8:(g + 1) * 8], writes=[vst])
                    for h in range(4):
                        cp("pool" if h % 2 else "dve", V1[:, g * 8:(g + 1) * 8, h, 0:64], vst[:, :, h * 64:(h + 1) * 64], [vst], [V1])
                accn = 0
                for Q in range(8):
                    q0 = Q * 512
                    yT = ytk[Q % 2]
                    tiles = []
                    for h in range(4):
                        accT = PS[3 + accn % 2]
                        rd_ = rden[accn % 2]
                        accn += 1
                        nk = 4 * Q + 4
                        for kt in range(nk):
                            k0 = kt * 128
                            d = kt - 4 * Q
                            dd = max(d, 0)
                            n = 512 - 128 * dd
                            mms = [(0, n, Kp[h][:, k0:k0 + 128], Qp[h][:, q0 + 128 * dd:q0 + 512], [Kp[h], Qp[h]])]
                            if d >= 0:
                                mms.append((0, 128, ident[:], tri[:], [ident, tri]))
                            tl = dict(np=128, n=n, mms=mms, pv=[])
                            for s in range(dd, 4):
                                tl["pv"].append((accT, accT[:, s * 65:(s + 1) * 65], (s - dd) * 128, V1[:, kt, h, :], [V1]))
                            if kt == 0:
                                tl["init"] = (accT, 260)
                            if kt == nk - 1:
                                def fin(accT=accT, rd_=rd_, h=h, yT=yT):
                                    accv = accT[:, 0:260].rearrange("p (s c) -> p s c", c=65)
                                    fw.op("dve", lambda e: e.reciprocal(out=rd_[:, 0:4], in_=accv[:, :, 64]), [accT], [rd_])
                                    for s in range(4):
                                        ts("dve", yT[:, s, h * 64:(h + 1) * 64], accv[:, s, 0:64], rd_[:, s:s + 1], None, ALU.mult, None, [accT, rd_], [yT])
                                tl["fin"] = fin
                            tiles.append(tl)
                    run_attention(tiles, pts)
                    outnorm_tok(on_tl, Q, lambda s, yT=yT: yT[:, s, :], yT, brow_all[:, l, 0:256], 512)
                fw.barrier()
            if stop <= 3:
                break

            with ExitStack() as ph:
                Qn = [sb(ph, "Qn%d" % h, [64, S], BF16) for h in range(4)]
                KsT = sb(ph, "KsT", [64, S], BF16)
                KwT = sb(ph, "KwT", [64, S], BF16)
                KcT = sb(ph, "KcT", [64, 256], BF16)
                VS1 = sb(ph, "VS1", [128, 32, 65], BF16)
                VW1 = sb(ph, "VW1", [128, 32, 65], BF16)
                VC1 = sb(ph, "VC1", [128, 2, 128], BF16)
                selT = sb(ph, "selT", [64, S], BF16)
                G = sb(ph, "G", [128, 32, 12])
                yd = sb(ph, "yd", [128, 32, 256])
                hq = sb(ph, "hq5", [64, 4112])
                hsq = sb(ph, "hsq5", [64, 4112], BF16)
                gs = sb(ph, "gs5", [64, 1])
                lnb = sb(ph, "lnb5", [64, 512])
                rstd = sb(ph, "rstd5", [64, 512])
                hn = (hq, hsq, gs, lnb, rstd)
                w1b = sb(ph, "w1b", [64, 32, 128], BF16)
                posb = sb(ph, "posb", [64, 32], BF16)
                w2b = sb(ph, "w2b", [128, 64], BF16)
                hbias = sb(ph, "hbias", [128, 1])
                hidb = sb(ph, "hidb", [128, 256], BF16)
                vst2 = sb(ph, "vst2", [128, 32, 64])
                gst = sb(ph, "gst", [128, 32, 12])
                pts = [sb(ph, "ptn%d" % i, [128, 512], BF16) for i in range(3)]
                btile = [sb(ph, "btile%d" % i, [128, 512], BF16) for i in range(3)]
                den = [sb(ph, "den%d" % i, [128, 4]) for i in range(2)]
                rden = [sb(ph, "rdenn%d" % i, [128, 4]) for i in range(2)]
                coef = [sb(ph, "coef%d" % i, [128, 4]) for i in range(2)]
                impacc = sb(ph, "impacc", [128, 4, 64])
                imp2 = sb(ph, "imp2", [128, 64])
                m8 = sb(ph, "m8", [128, 8])
                m8b = sb(ph, "m8b", [128, 8])
                self_ = sb(ph, "self", [128, 64])
                selb = sb(ph, "selb", [128, 64], BF16)
                on_tl = (sb(ph, "junk5", [128, 256]), sb(ph, "ssq5", [128, 4]), sb(ph, "lnq5", [128, 4]), sb(ph, "rs5", [128, 4]),
                         sb(ph, "ym5", [128, 4, 256], BF16), [sb(ph, "mst5_%d" % i, [128, 512], BF16) for i in range(2)])
                for h in range(4):
                    headnorm(hn, zT[R_NSA_Q + h * 64:R_NSA_Q + (h + 1) * 64, :], u_zT[14 + h // 2], 44, 0.125, Qn[h], S)
                headnorm(hn, zT[R_KS:R_KS + 64, :], u_zT[17], 46, 1.0, KsT, S)
                headnorm(hn, zT[R_KW:R_KW + 64, :], u_zT[17], 47, 1.0, KwT, S)
                for kv in range(2):
                    rows = R_KC if kv == 0 else R_VC
                    fw.dma("sp", hq[0:64, 0:S], zT[rows:rows + 64, :], reads=u_zT[16], writes=[hq])
                    mset("pool", hq[0:64, S:4112], 0.0, [hq])
                    cp("dve", hsq[0:64, :], hq[0:64, :], [hq], [hsq])
                    fw.dma("pool", w1b[:], cw1_d[l, kv].rearrange("l d m -> d l m"), writes=[w1b])
                    fw.dma("pool", posb[:], cposT_d[l, kv], writes=[posb])
                    fw.dma("pool", w2b[:], cw2_d[l, kv], writes=[w2b])
                    ps = PS[5]
                    for li in range(32):
                        mm(ps[:, 0:1], w1b[:, li, :], posb[:, li:li + 1], li == 0, li == 31, [w1b, posb], [ps])
                    cp("dve", hbias[:], ps[:, 0:1], [ps], [hbias])
                    ps = PS[6]
                    for li in range(32):
                        if li < 16:
                            rhs = hsq[0:64, 0:4096].rearrange("p (n r) -> p n r", r=16)[:, :, li]
                        else:
                            rhs = hsq[0:64, 16:4112].rearrange("p (n r) -> p n r", r=16)[:, :, li - 16]
                        mm(ps[:, 0:256], w1b[:, li, :], rhs, li == 0, li == 31, [w1b, hsq], [ps])
                    act(hidb[:], ps[:, 0:256], AF.Gelu_apprx_tanh, [ps, hbias], [hidb], bias=hbias[:])
                    if kv == 0:
                        ps = PS[5]
                        mm(ps[0:64, 0:256], w2b[:], hidb[:], True, True, [w2b, hidb], [ps])
                        cp("dve", hq[0:64, 0:256], ps[0:64, 0:256], [ps], [hq])
                        headnorm(hn, None, None, 45, 1.0, KcT, 256)
                    else:
                        for c in range(2):
                            ps = PS[5]
                            mm(ps[:, 0:64], hidb[:, c * 128:(c + 1) * 128], w2b[:], True, True, [w2b, hidb], [ps])
                            cp("dve", VC1[:, c, 0:64], ps[:, 0:64], [ps], [VC1])
                            cp("dve", VC1[:, c, 64:128], ovl[:, c * 64:(c + 1) * 64], [ovl], [VC1])
                for (VT, col) in ((VS1, T_VS), (VW1, T_VW)):
                    fw.dma("sp", vst2[:], ztok[:, col:col + 64].rearrange("(k p) c -> p k c", p=128), reads=u_ztok, writes=[vst2])
                    cp("dve", VT[:, :, 0:64], vst2[:], [vst2], [VT])
                    mset("pool", VT[:, :, 64:65], 1.0, [VT])
                fw.dma("sp", gst[:], ztok[:, T_G:T_G + 12].rearrange("(k p) c -> p k c", p=128), reads=u_ztok, writes=[gst])
                tt("dve", gst[:], gst[:], brow_all[:, l, 512:524].unsqueeze(1).to_broadcast([128, 32, 12]), ALU.add, [gst, brow_all], [gst])
                act(G[:], gst[:], AF.Sigmoid, [gst], [G])
                mset("pool", selT[:], 0.0, [selT])

                accn = 0
                bn = 0
                for Q in range(8):
                    q0 = Q * 512

                    def fin_generic(accT, w, h, branch, first, dn, rd_, cf_, Q=Q):
                        accv = accT[:, 0:4 * w].rearrange("p (s c) -> p s c", c=w)
                        if branch == 0:
                            fw.op("dve", lambda e: e.tensor_reduce(out=dn[:, 0:4], in_=accv[:, :, 64:128], axis=mybir.AxisListType.X, op=ALU.add), [accT], [dn])
                            ts("dve", dn[:, 0:4], dn[:, 0:4], 1.0 / 32, 1e-30, ALU.mult, ALU.max, [dn], [dn])
                            fw.op("dve", lambda e: e.reciprocal(out=rd_[:, 0:4], in_=dn[:, 0:4]), [dn], [rd_])
                        else:
                            fw.op("dve", lambda e: e.reciprocal(out=rd_[:, 0:4], in_=accv[:, :, 64]), [accT], [rd_])
                        tt("dve", cf_[:, 0:4], rd_[:, 0:4], G[:, 4 * Q:4 * Q + 4, branch * 4 + h], ALU.mult, [rd_, G], [cf_])
                        for s in range(4):
                            ydv = yd[:, 4 * Q + s, h * 64:(h + 1) * 64]
                            if first:
                                ts("dve", ydv, accv[:, s, 0:64], cf_[:, s:s + 1], None, ALU.mult, None, [accT, cf_], [yd])
                            else:
                                stt(ydv, accv[:, s, 0:64], cf_[:, s:s + 1], ydv, ALU.mult, ALU.add, [accT, cf_, yd], [yd])
                        if branch == 0:
                            for s in range(4):
                                if h == 0:
                                    ts("dve", impacc[:, s, :], accv[:, s, 64:128], rd_[:, s:s + 1], None, ALU.mult, None, [accT, rd_], [impacc])
                                else:
                                    stt(impacc[:, s, :], accv[:, s, 64:128], rd_[:, s:s + 1], impacc[:, s, :], ALU.mult, ALU.add, [accT, rd_, impacc], [impacc])

                    tiles = []
                    for h in range(4):
                        accT = PS[3 + accn % 2]
                        dn, rd_, cf_ = den[accn % 2], rden[accn % 2], coef[accn % 2]
                        accn += 1
                        chunks = [0, 1] if Q >= 4 else [0]
                        for c in chunks:
                            bt = btile[bn % 3]
                            bn += 1
                            off = h * 8208 + ZC + q0 - 31 - 16 * (c * 128 + 127)
                            src = bass.AP(tensor=ftc_h, offset=off, ap=[[16, 128], [1, 512]])
                            tl = dict(np=128, n=512, pv=[])
                            tl["pre"] = (bt, src)
                            tl["mms"] = [(0, 512, KcT[0:64, c * 128:(c + 1) * 128], Qn[h][0:64, q0:q0 + 512], [KcT, Qn[h]]),
                                         (0, 512, Jm[:], bt[:], [Jm, bt])]
                            for s in range(4):
                                tl["pv"].append((accT, accT[:, s * 128:(s + 1) * 128], s * 128, VC1[:, c, :], [VC1]))
                            if c == 0:
                                tl["init"] = (accT, 512)
                            if c == chunks[-1]:
                                tl["fin"] = (lambda accT=accT, h=h, dn=dn, rd_=rd_, cf_=cf_: fin_generic(accT, 128, h, 0, True, dn, rd_, cf_))
                            tiles.append(tl)
                    run_attention(tiles, pts)

                    tiles = []
                    for h in range(4):
                        accT = PS[3 + accn % 2]
                        dn, rd_, cf_ = den[accn % 2], rden[accn % 2], coef[accn % 2]
                        accn += 1
                        kts = [kt for kt in range(4 * Q - 4, 4 * Q + 4) if kt >= 0]
                        for kt in kts:
                            k0 = kt * 128
                            d = kt - 4 * Q
                            dd = max(d, 0)
                            n = 512 - 128 * dd
                            sc0 = 128 * (-d) if d < 0 else 0
                            tl = dict(np=128, n=n, pv=[])
                            tl["mms"] = [(0, n, KwT[0:64, k0:k0 + 128], Qn[h][0:64, q0 + 128 * dd:q0 + 512], [KwT, Qn[h]]),
                                         (0, n, Jm[:], slab_w[:, h, sc0:sc0 + n], [Jm, slab_w])]
                            for s in range(dd, 4):
                                tl["pv"].append((accT, accT[:, s * 65:(s + 1) * 65], (s - dd) * 128, VW1[:, kt, :], [VW1]))
                            if kt == kts[0]:
                                tl["init"] = (accT, 260)
                            if kt == kts[-1]:
                                tl["fin"] = (lambda accT=accT, h=h, dn=dn, rd_=rd_, cf_=cf_: fin_generic(accT, 65, h, 2, False, dn, rd_, cf_))
                            tiles.append(tl)
                    run_attention(tiles, pts)

                    for s in range(4):
                        i_ = 4 * Q + s
                        iv = impacc[:, s, :]
                        if 2 * i_ + 2 < 64:
                            mset("dve", impacc[:, s, 2 * i_ + 2:64], -1e30, [impacc])
                        mset("dve", impacc[0:64, s, 2 * i_ + 1:2 * i_ + 2], -1e30, [impacc])
                        mset("dve", impacc[64:128, s, 2 * i_ + 1:2 * i_ + 2], 1e30, [impacc])
                        mset("dve", impacc[:, s, 2 * i_:2 * i_ + 1], 1e30, [impacc])
                        if i_ >= 1:
                            mset("dve", impacc[0:64, s, 2 * i_ - 1:2 * i_], 1e30, [impacc])
                        mset("dve", impacc[:, s, 0:1], 1e30, [impacc])
                        fw.op("dve", lambda e, iv=iv: e.max(out=m8[:], in_=iv), [impacc], [m8])
                        fw.op("dve", lambda e, iv=iv: e.match_replace(out=imp2[:], in_to_replace=m8[:], in_values=iv, imm_value=-3e38), [impacc, m8], [imp2])
                        fw.op("dve", lambda e: e.max(out=m8b[:], in_=imp2[:]), [imp2], [m8b])
                        ts("dve", self_[:], iv, m8b[:, 7:8], -1.0, ALU.is_ge, ALU.add, [impacc, m8b], [self_])
                        ts("dve", selb[:], self_[:], -NEGM, None, ALU.mult, None, [self_], [selb])
                        fw.op("pe", lambda e, s=s: e.transpose(out=PSB[0:64, s * 128:(s + 1) * 128], in_=selb[:], identity=ident[:]), [selb, ident], [PSB])
                    cp("dve", selT[0:64, q0:q0 + 512], PSB[0:64, 0:512], [PSB], [selT])

                    tiles = []
                    for h in range(4):
                        accT = PS[3 + accn % 2]
                        dn, rd_, cf_ = den[accn % 2], rden[accn % 2], coef[accn % 2]
                        accn += 1
                        nk = 4 * Q + 4
                        for kt in range(nk):
                            k0 = kt * 128
                            d = kt - 4 * Q
                            dd = max(d, 0)
                            n = 512 - 128 * dd
                            tl = dict(np=128, n=n, pv=[])
                            tl["mms"] = [(0, n, KsT[0:64, k0:k0 + 128], Qn[h][0:64, q0 + 128 * dd:q0 + 512], [KsT, Qn[h]]),
                                         (0, n, Emat[0:64, kt * 128:(kt + 1) * 128], selT[0:64, q0 + 128 * dd:q0 + 512], [Emat, selT])]
                            if d >= -1:
                                sc0 = 128 if d == -1 else 0
                                tl["mms"].append((0, n, Jm[:], slab_s[:, h, sc0:sc0 + n], [Jm, slab_s]))
                            else:
                                tl["bias"] = rb31[:, h:h + 1]
                                tl["bias_reads"] = [rb31]
                            for s in range(dd, 4):
                                tl["pv"].append((accT, accT[:, s * 65:(s + 1) * 65], (s - dd) * 128, VS1[:, kt, :], [VS1]))
                            if kt == 0:
                                tl["init"] = (accT, 260)
                            if kt == nk - 1:
                                tl["fin"] = (lambda accT=accT, h=h, dn=dn, rd_=rd_, cf_=cf_: fin_generic(accT, 65, h, 1, False, dn, rd_, cf_))
                            tiles.append(tl)
                    run_attention(tiles, pts)
                    outnorm_tok(on_tl, Q, lambda s, Q=Q: yd[:, 4 * Q + s, :], yd, brow_all[:, l, 256:512], 768)
                fw.barrier()
            if stop <= 4:
                break

            with ExitStack() as ph:
                wo = sb(ph, "wo", [128, 8, 1024], BF16)
                for c in range(8):
                    fw.dma("pool", wo[:, c, :], w_out[l, c * 128:(c + 1) * 128, :], writes=[wo])
                xt = [sb(ph, "xto%d" % i, [128, 8, 512]) for i in range(2)]
                mt = [sb(ph, "mt%d" % i, [128, 8, 512], BF16) for i in range(2)]
                xo = [sb(ph, "xo%d" % i, [128, 512]) for i in range(4)]
                ev = 0
                for tt_ in range(8):
                    t0 = tt_ * 512
                    xt_, mt_ = xt[tt_ % 2], mt[tt_ % 2]
                    rd = [u_xres[tt_]] if l > 0 else []
                    fw.dma("sp", xt_[:], x_src[:, t0:t0 + 512].rearrange("(c p) t -> p c t", p=128), reads=rd, writes=[xt_])
                    fw.dma("sp", mt_[:], mixT[:, t0:t0 + 512].rearrange("(c p) t -> p c t", p=128), reads=[u_mix[c][tt_] for c in range(8)], writes=[mt_])
                    for n_ in range(8):
                        ps = PS[ev % 3]
                        for c in range(8):
                            mm(ps[:], wo[:, c, n_ * 128:(n_ + 1) * 128], mt_[:, c, :], c == 0, c == 7, [wo, mt_], [ps])
                        xo_ = xo[ev % 4]
                        tt("dve", xo_[:], xt_[:, n_, :], ps[:], ALU.add, [xt_, ps], [xo_])
                        fw.dma("pool", xres[n_ * 128:(n_ + 1) * 128, t0:t0 + 512], xo_[:], reads=[xo_], writes=[u_xres[tt_]])
                        ev += 1
                fw.barrier()
            if stop <= 5:
                break

            with ExitStack() as ph:
                wgu = sb(ph, "wgu", [128, 8, 2 * DFF], BF16)
                wdn = sb(ph, "wdn", [128, 22, 1024], BF16)
                with ExitStack() as ph2:
                    wst = [sb(ph2, "wstf%d" % i, [128, DFF]) for i in range(2)]
                    k_ = 0
                    for c in range(8):
                        for hh in range(2):
                            st_ = wst[k_ % 2]
                            fw.dma("sp", st_[:], w_gu[l, c * 128:(c + 1) * 128, hh * DFF:(hh + 1) * DFF], writes=[st_])
                            ts("pool" if k_ % 2 else "dve", wgu[:, c, hh * DFF:(hh + 1) * DFF], st_[:], pv(8 + c), None, ALU.mult, None, [st_, pv_all], [wgu])
                            k_ += 1
                    fw.barrier()
                for c in range(22):
                    fw.dma("pool", wdn[:, c, :], w_dn[l, c * 128:(c + 1) * 128, :], writes=[wdn])
                NT = 256
                xt = [sb(ph, "xtf%d" % i, [128, 8, NT]) for i in range(2)]
                xn = sb(ph, "xnf", [128, 8, NT], BF16)
                sq = sb(ph, "sqf", [128, 8, NT], BF16)
                lnb = sb(ph, "lnbf", [128, NT])
                rstd = sb(ph, "rstdf", [128, NT])
                hT = sb(ph, "hT", [128, 22, NT], BF16)
                sg = [sb(ph, "sg%d" % i, [128, NT]) for i in range(2)]
                xo = [sb(ph, "xof%d" % i, [128, NT]) for i in range(4)]
                ev = 0
                for tt_ in range(S // NT):
                    t0 = tt_ * NT
                    xt_ = xt[tt_ % 2]
                    ux = u_xres[t0 // 512]
                    fw.dma("sp", xt_[:], xres[:, t0:t0 + NT].rearrange("(c p) t -> p c t", p=128), reads=[ux], writes=[xt_])
                    act(sq[:], xt_[:], AF.Square, [xt_], [sq])
                    for c in range(8):
                        mm(PS[6][:, 0:NT], ones1024[:], sq[:, c, :], c == 0, c == 7, [ones1024, sq], [PS[6]])
                    rstd_from(PS[6][:, 0:NT], rstd[:], lnb[:], [PS[6]], lnb, rstd)
                    tt("dve", xn[:], xt_[:], rstd[:].unsqueeze(1).to_broadcast([128, 8, NT]), ALU.mult, [xt_, rstd], [xn])
                    for j in range(22):
                        psg = PS[(2 * j) % 4]
                        psu = PS[(2 * j + 1) % 4]
                        for c in range(8):
                            mm(psg[:, 0:NT], wgu[:, c, j * 128:(j + 1) * 128], xn[:, c, :], c == 0, c == 7, [wgu, xn], [psg])
                        for c in range(8):
                            mm(psu[:, 0:NT], wgu[:, c, DFF + j * 128:DFF + (j + 1) * 128], xn[:, c, :], c == 0, c == 7, [wgu, xn], [psu])
                        sg_ = sg[j % 2]
                        act(sg_[:], psg[:, 0:NT], AF.Silu, [psg], [sg_])
                        tt("dve", hT[:, j, :], sg_[:], psu[:, 0:NT], ALU.mult, [sg_, psu], [hT])
                    for n_ in range(8):
                        ps = PS[4 + n_ % 2]
                        for j in range(22):
                            mm(ps[:, 0:NT], wdn[:, j, n_ * 128:(n_ + 1) * 128], hT[:, j, :], j == 0, j == 21, [wdn, hT], [ps])
                        xo_ = xo[ev % 4]
                        tt("dve", xo_[:], xt_[:, n_, :], ps[:, 0:NT], ALU.add, [xt_, ps], [xo_])
                        fw.dma("pool", x_dst[n_ * 128:(n_ + 1) * 128, t0:t0 + NT], xo_[:], reads=[xo_], writes=[ux] if x_dst is xres else [])
                        ev += 1
                fw.barrier()

        fw.barrier()
    return nc


def _bucket(d):
    d = np.maximum(d, 0)
    large = 16 + (np.log(np.maximum(d, 1).astype(np.float32) / np.float32(16)) / np.float32(np.log(128 / 16)) * np.float32(16)).astype(np.int32)
    large = np.minimum(large, 31)
    return np.where(d < 16, d, large)


def _host_consts():
    bf = ml_dtypes.bfloat16
    c = {}
    c["cident"] = np.eye(128, dtype=np.float32).astype(bf)
    c["cJ"] = np.eye(128, dtype=np.float32)[::-1].copy().astype(bf)
    kl = np.arange(128)[:, None]
    ql = np.arange(128)[None, :]
    c["ctri"] = np.where(ql >= kl, 0.0, NEGM).astype(np.float32).astype(bf)
    E = np.zeros((64, 32, 128), np.float32)
    for kt in range(32):
        for k in range(128):
            E[2 * kt + k // 64, kt, k] = 1.0
    c["cE"] = E.reshape(64, 32 * 128).astype(bf)
    n = np.arange(256)
    m = np.arange(64)
    cs = n[:, None] * 16
    ss = m[None, :] * 64
    ov = np.clip(np.minimum(cs + 32, ss + 64) - np.maximum(cs, ss), 0, 32).astype(np.float32)
    ov[255] = 0
    c["covl"] = np.concatenate([ov[0:128], ov[128:256]], axis=1).astype(bf)
    d = np.arange(8208) - ZC
    oh = np.zeros((33, 8208), np.float32)
    b = _bucket(d)
    oh[b[d >= 0], np.nonzero(d >= 0)[0]] = 1.0
    oh[32, d < 0] = 1.0
    c["ohc"] = oh.astype(bf)
    d = np.arange(1152) - ZW
    oh = np.zeros((33, 1152), np.float32)
    b = _bucket(d)
    ok = (d >= 0) & (d < 512)
    oh[b[ok], np.nonzero(ok)[0]] = 1.0
    oh[32, ~ok] = 1.0
    c["ohw"] = oh.astype(bf)
    return c


def _host_layout(inp):
    f = np.float32
    C_LRU_X, C_LRU_G, C_SC_B, C_SC_C, C_SC_X = 0, 256, 512, 768, 1024
    C_FOX_Q, C_FOX_K, C_FOX_V, C_FOX_F, C_NSA_Q, C_NSA_KV, C_NSA_G = 1280, 1536, 1792, 2048, 2052, 2308, 2692
    r = lambda a, n: list(range(a, a + n))
    perm = (r(C_LRU_X, 256) + r(C_LRU_G, 256) + r(C_SC_B, 256) + r(C_SC_C, 256) + r(C_SC_X, 256)
            + r(C_FOX_Q, 256) + r(C_FOX_K, 256) + r(C_NSA_Q, 256)
            + r(C_NSA_KV + 0, 64) + r(C_NSA_KV + 64, 64) + r(C_NSA_KV + 128, 64) + r(C_NSA_KV + 256, 64)
            + r(C_FOX_F, 4)
            + r(C_FOX_V, 256) + r(C_NSA_KV + 192, 64) + r(C_NSA_KV + 320, 64) + r(C_NSA_G, 12))
    assert len(perm) == 2704 and len(set(perm)) == 2704
    m = {}
    m["w_in"] = np.ascontiguousarray(np.asarray(inp["w_in"], f)[:, :, perm])
    m["w_out"] = np.ascontiguousarray(np.asarray(inp["w_out"], f))
    m["w_gu"] = np.ascontiguousarray(np.asarray(inp["w_gate_up"], f))
    m["w_dn"] = np.ascontiguousarray(np.asarray(inp["w_down"], f))
    pvec = np.zeros((NL, 128, 64), f)
    brow = np.zeros((NL, 1, 524), f)
    wbd = np.zeros((NL, 2, 2, 128, 128), f)
    for l in range(NL):
        pvec[l, :, 0:8] = np.asarray(inp["norm_mix"][l]).reshape(8, 128).T
        pvec[l, :, 8:16] = np.asarray(inp["norm_ffn"][l]).reshape(8, 128).T
        for j in range(2):
            sl = slice(j * 128, (j + 1) * 128)
            pvec[l, :, 16 + j * 4:20 + j * 4] = np.asarray(inp["lru_conv_w"][l])[:, sl].T
            pvec[l, :, 24 + j] = np.asarray(inp["lru_conv_b"][l])[sl]
            for g in range(2):
                pvec[l, :, 26 + g * 2 + j] = np.asarray(inp["lru_b_gates"][l])[g, sl]
                for bb in range(2):
                    wbd[l, g, j, bb * 64:(bb + 1) * 64, bb * 64:(bb + 1) * 64] = np.asarray(inp["lru_w_gates"][l])[g, 2 * j + bb]
            pvec[l, :, 30 + j] = np.asarray(inp["lru_lambda"][l])[sl]
            pvec[l, :, 32 + j * 3:35 + j * 3] = np.asarray(inp["sc_conv_w"][l])[:, sl].T
            for grp in range(2):
                pvec[l, :, 38 + grp * 2 + j] = np.asarray(inp["out_norm"][l])[grp, sl]
        pvec[l, 0:64, 42] = np.asarray(inp["fox_qk_norm"][l])[0]
        pvec[l, 0:64, 43] = np.asarray(inp["fox_qk_norm"][l])[1]
        for i in range(4):
            pvec[l, 0:64, 44 + i] = np.asarray(inp["nsa_qk_norm"][l])[i]
        pvec[l, 0:4, 48] = np.asarray(inp["fox_f_bias"][l])
        brow[l, 0, 0:256] = np.asarray(inp["out_norm"][l])[2]
        brow[l, 0, 256:512] = np.asarray(inp["out_norm"][l])[3]
        brow[l, 0, 512:524] = np.asarray(inp["nsa_gate_bias"][l])
    m["pvec"] = pvec
    m["brow"] = brow
    m["wbd"] = wbd
    m["cw1"] = np.ascontiguousarray(np.asarray(inp["nsa_cmp_w1"], f))
    m["cw2"] = np.ascontiguousarray(np.asarray(inp["nsa_cmp_w2"], f))
    m["cposT"] = np.ascontiguousarray(np.asarray(inp["nsa_cmp_pos"], f).transpose(0, 1, 3, 2))
    rb = np.zeros((33, 4), f)
    rb[0:32] = np.asarray(inp["rel_bias"], f)
    rb[32] = NEGM
    m["rb33"] = rb
    m.update(_host_consts())
    return m


def kernel(**inputs):
    x = np.asarray(inputs["x"], np.float32)
    shared = _host_layout(inputs)
    nc = build()
    in_maps = []
    for b in range(8):
        d = dict(shared)
        d["xT"] = np.ascontiguousarray(x[b].T)
        in_maps.append(d)
    res = run_bass_kernel_spmd(nc, in_maps, core_ids=list(range(8)))
    out = np.stack([np.ascontiguousarray(res.results[b]["out"].T) for b in range(8)], axis=0)
    return out.astype(np.float32)
```

```python
import numpy as np
import ml_dtypes
from contextlib import ExitStack
import concourse.bass as bass
import concourse.mybir as mybir
from concourse.bass_utils import run_bass_kernel_spmd

F32 = mybir.dt.float32
BF16 = mybir.dt.bfloat16
AF = mybir.ActivationFunctionType
ALU = mybir.AluOpType

S = 4096
D = 1024
NL = 2
DFF = 2816
NFM = 2308
NTM = 396
EPS = 1e-6
NEGM = -30000.0
R_LRU_X, R_LRU_G, R_SC_B, R_SC_C, R_SC_X = 0, 256, 512, 768, 1024
R_FOX_Q, R_FOX_K, R_NSA_Q, R_KC, R_VC, R_KS, R_KW, R_FOX_F = 1280, 1536, 1792, 2048, 2112, 2176, 2240, 2304
T_FOX_V, T_VS, T_VW, T_G = 0, 256, 320, 384
ZC = 4112
ZW = 128


class U:
    __slots__ = ("lastw", "readers")

    def __init__(self):
        self.lastw = None
        self.readers = []


class T:
    def __init__(self, t):
        self.t = t
        self.u = U()

    def __getitem__(self, k):
        return self.t[k]


class FW:
    def __init__(self, nc, es, ndma=32):
        self.nc = nc
        self.engs = {}
        for name, h in [("pe", nc.tensor), ("dve", nc.vector), ("act", nc.scalar),
                        ("pool", nc.gpsimd), ("sp", nc.sync)]:
            sem = es.enter_context(nc.semaphore("sem_" + name))
            self.engs[name] = dict(h=h, sem=sem, count=0, known={})
        self.dsems = [[es.enter_context(nc.semaphore("dq%d" % i)), 0] for i in range(ndma)]
        self.drr = 0
        self.ninstr = 0

    def _wait(self, en, toks):
        e = self.engs[en]
        need = {}
        for (sem, val) in toks:
            k = id(sem)
            if k not in need or need[k][1] < val:
                need[k] = (sem, val)
        for k, (sem, val) in need.items():
            if e["known"].get(k, 0) < val:
                e["h"].wait_ge(sem, val)
                e["known"][k] = val
                self.ninstr += 1

    def _deps(self, en, reads, writes):
        toks = []
        for u in reads:
            t = u.lastw
            if t is not None:
                if t[2] == en and en == "pe":
                    continue
                toks.append((t[0], t[1]))
        for u in writes:
            t = u.lastw
            if t is not None and (t[2] != en or en == "dma"):
                toks.append((t[0], t[1]))
            for r in u.readers:
                if r[2] != en or en == "dma":
                    toks.append((r[0], r[1]))
        return toks

    def _commit(self, tok, reads, writes):
        for u in writes:
            u.lastw = tok
            u.readers = []
        for u in reads:
            if u in writes:
                continue
            u.readers.append(tok)
            if len(u.readers) > 48:
                best = {}
                for r in u.readers:
                    k = id(r[0])
                    if k not in best or best[k][1] < r[1]:
                        best[k] = r
                u.readers = list(best.values())

    def op(self, en, fn, reads=(), writes=()):
        reads = [x.u if isinstance(x, T) else x for x in reads]
        writes = [x.u if isinstance(x, T) else x for x in writes]
        e = self.engs[en]
        self._wait(en, self._deps(en, reads, writes))
        ins = fn(e["h"])
        e["count"] += 1
        ins.then_inc(e["sem"], 1)
        self.ninstr += 1
        tok = (e["sem"], e["count"], en)
        self._commit(tok, reads, writes)
        return tok

    def dma(self, en, out, in_, reads=(), writes=(), **kw):
        reads = [x.u if isinstance(x, T) else x for x in reads]
        writes = [x.u if isinstance(x, T) else x for x in writes]
        e = self.engs[en]
        slot = self.dsems[self.drr % len(self.dsems)]
        self.drr += 1
        toks = self._deps("dma", reads, writes)
        toks.append((slot[0], slot[1]))
        self._wait(en, toks)
        ins = e["h"].dma_start(out=out, in_=in_, **kw)
        slot[1] += 16
        ins.then_inc(slot[0], 16)
        self.ninstr += 1
        tok = (slot[0], slot[1], "dma")
        self._commit(tok, reads, writes)
        return tok

    def barrier(self):
        toks = []
        for n, e in self.engs.items():
            if e["count"] > 0:
                toks.append((e["sem"], e["count"]))
        for s in self.dsems:
            if s[1] > 0:
                toks.append((s[0], s[1]))
        for n in self.engs:
            self._wait(n, toks)


def build(dbg=False, nlayers=NL, stop=99):
    nc = bass.Bass("TRN2", target_bir_lowering=False)

    def din(name, shape, dt=F32):
        return nc.dram_tensor(name, list(shape), dt, kind="ExternalInput").ap()

    kind_s = "ExternalOutput" if dbg else "Internal"

    def dscr(name, shape, dt=F32):
        return nc.dram_tensor(name, list(shape), dt, kind=kind_s)

    xT = din("xT", [D, S])
    w_in = din("w_in", [NL, D, 2704])
    w_out = din("w_out", [NL, D, D])
    w_gu = din("w_gu", [NL, D, 2 * DFF])
    w_dn = din("w_dn", [NL, DFF, D])
    pvec_d = din("pvec", [NL, 128, 64])
    brow_d = din("brow", [NL, 1, 524])
    wbd_d = din("wbd", [NL, 2, 2, 128, 128])
    cw1_d = din("cw1", [NL, 2, 32, 64, 128])
    cw2_d = din("cw2", [NL, 2, 128, 64])
    cposT_d = din("cposT", [NL, 2, 64, 32])
    rb33_d = din("rb33", [33, 4])
    ohc_d = din("ohc", [33, 8208], BF16)
    ohw_d = din("ohw", [33, 1152], BF16)
    cident_d = din("cident", [128, 128], BF16)
    cJ_d = din("cJ", [128, 128], BF16)
    ctri_d = din("ctri", [128, 128], BF16)
    cE_d = din("cE", [64, S], BF16)
    covl_d = din("covl", [128, 128], BF16)
    out_d = nc.dram_tensor("out", [D, S], F32, kind="ExternalOutput").ap()
    zT_h = dscr("zT", [NFM, S])
    ztok_h = dscr("ztok", [S, NTM])
    mixT_h = dscr("mixT", [D, S], BF16)
    xres_h = dscr("xres", [D, S])
    ftc_h = dscr("ftc", [4, 8208], BF16)
    ftw_h = dscr("ftw", [4, 1152], BF16)
    zT, ztok, mixT, xres, ftc, ftw = (h.ap() for h in (zT_h, ztok_h, mixT_h, xres_h, ftc_h, ftw_h))
    u_zT = [[U() for _ in range(8)] for _ in range(19)]
    u_ztok = [U() for _ in range(32)]
    u_mix = [[U() for _ in range(8)] for _ in range(8)]
    u_xres = [U() for _ in range(8)]
    u_ft = U()

    with ExitStack() as es:
        fw = FW(nc, es)

        uid = [0]

        def sb(st, name, shape, dt=F32):
            uid[0] += 1
            return T(st.enter_context(nc.sbuf_tensor("%s_%d" % (name, uid[0]), list(shape), dt)))

        def psm(st, name, shape, dt=F32):
            return T(st.enter_context(nc.psum_tensor(name, list(shape), dt)))

        def act(out, in_, func, reads, writes, **kw):
            return fw.op("act", lambda e: e.activation(out=out, in_=in_, func=func, **kw), reads, writes)

        def mm(out, lhsT, rhs, start, stop, reads, writes, **kw):
            return fw.op("pe", lambda e: e.matmul(out, lhsT=lhsT, rhs=rhs, start=start, stop=stop, **kw), reads, writes)

        def tt(en, out, in0, in1, op, reads, writes):
            return fw.op(en, lambda e: e.tensor_tensor(out=out, in0=in0, in1=in1, op=op), reads, writes)

        def ts(en, out, in0, s1, s2, op0, op1, reads, writes):
            if op1 is None:
                return fw.op(en, lambda e: e.tensor_scalar(out=out, in0=in0, scalar1=s1, scalar2=None, op0=op0), reads, writes)
            return fw.op(en, lambda e: e.tensor_scalar(out=out, in0=in0, scalar1=s1, scalar2=s2, op0=op0, op1=op1), reads, writes)

        def stt(out, in0, scalar, in1, op0, op1, reads, writes):
            return fw.op("dve", lambda e: e.scalar_tensor_tensor(out=out, in0=in0, scalar=scalar, in1=in1, op0=op0, op1=op1), reads, writes)

        def cp(en, out, in_, reads, writes):
            return fw.op(en, lambda e: e.tensor_copy(out=out, in_=in_), reads, writes)

        def mset(en, ap, val, writes):
            return fw.op(en, lambda e: e.memset(ap, val), (), writes)

        ident = sb(es, "ident", [128, 128], BF16)
        Jm = sb(es, "Jm", [128, 128], BF16)
        tri = sb(es, "tri", [128, 128], BF16)
        ovl = sb(es, "ovl", [128, 128], BF16)
        ones1024 = sb(es, "ones1024", [128, 128], BF16)
        ones256 = sb(es, "ones256", [128, 128], BF16)
        ones64 = sb(es, "ones64", [64, 64], BF16)
        zeros = sb(es, "zeros", [128, 512], BF16)
        slab_s = sb(es, "slab_s", [128, 4, 640], BF16)
        slab_w = sb(es, "slab_w", [128, 4, 1024], BF16)
        pv_all = sb(es, "pv_all", [128, NL, 64])
        brow_all = sb(es, "brow_all", [128, NL, 524])
        fw.dma("sp", ident[:], cident_d[:, :], writes=[ident])
        fw.dma("sp", Jm[:], cJ_d[:, :], writes=[Jm])
        fw.dma("sp", tri[:], ctri_d[:, :], writes=[tri])
        fw.dma("sp", ovl[:], covl_d[:, :], writes=[ovl])
        for l in range(NL):
            fw.dma("sp", pv_all[:, l, :], pvec_d[l], writes=[pv_all])
            fw.dma("sp", brow_all[:, l, :], brow_d[l].to_broadcast([128, 524]), writes=[brow_all])
        mset("pool", ones1024[:], 1.0 / 1024, [ones1024])
        mset("pool", ones256[:], 1.0 / 256, [ones256])
        mset("pool", ones64[:], 1.0 / 64, [ones64])
        mset("pool", zeros[:], 0.0, [zeros])

        PS = [psm(es, "ps%d" % i, [128, 512]) for i in range(7)]
        PSB = psm(es, "psb", [128, 1024], BF16)
        rb31 = sb(es, "rb31", [128, 4])
        fw.dma("sp", rb31[:], rb33_d[31:32, :].to_broadcast([128, 4]), writes=[rb31])

        with ExitStack() as ph:
            rb_f = sb(ph, "rb_f", [33, 4])
            rb_b = sb(ph, "rb_b", [33, 4], BF16)
            oh = sb(ph, "oh", [33, 8208], BF16)
            ohw_s = sb(ph, "ohw_s", [33, 1152], BF16)
            ftab = sb(ph, "ftab", [4, 8208 + 1152], BF16)
            fw.dma("sp", rb_f[:], rb33_d[:, :], writes=[rb_f])
            fw.dma("sp", oh[:], ohc_d[:, :], writes=[oh])
            fw.dma("sp", ohw_s[:], ohw_d[:, :], writes=[ohw_s])
            cp("dve", rb_b[:], rb_f[:], [rb_f], [rb_b])
            pieces = [(oh, c0, min(512, 8208 - c0), c0) for c0 in range(0, 8208, 512)]
            pieces += [(ohw_s, c0, min(512, 1152 - c0), 8208 + c0) for c0 in range(0, 1152, 512)]
            for i, (src, c0, n, dst0) in enumerate(pieces):
                ps = PS[i % 2]
                mm(ps[0:4, 0:n], rb_b[:, 0:4], src[:, c0:c0 + n], True, True, [rb_b, src], [ps])
                cp("dve", ftab[:, dst0:dst0 + n], ps[0:4, 0:n], [ps], [ftab])
            fw.dma("sp", ftc[:, :], ftab[:, 0:8208], reads=[ftab], writes=[u_ft])
            fw.dma("sp", ftw[:, :], ftab[:, 8208:8208 + 1152], reads=[ftab], writes=[u_ft])
            for h in range(4):
                src = bass.AP(tensor=ftc_h, offset=h * 8208 + ZC - 127, ap=[[1, 128], [1, 640]])
                fw.dma("sp", slab_s[:, h, :], src, reads=[u_ft], writes=[slab_s])
                src = bass.AP(tensor=ftw_h, offset=h * 1152 + ZW - 127, ap=[[1, 128], [1, 1024]])
                fw.dma("sp", slab_w[:, h, :], src, reads=[u_ft], writes=[slab_w])
            fw.barrier()

        def rstd_from(ps_ap, out_ap, tmp_ap, reads, tmpT, outT, scale=1.0):
            act(tmp_ap, ps_ap, AF.Ln, reads, [tmpT], bias=EPS, scale=scale)
            act(out_ap, tmp_ap, AF.Exp, [tmpT], [outT], scale=-0.5)

        for l in range(nlayers):
            x_src = xT if l == 0 else xres
            x_dst = out_d if l == nlayers - 1 else xres
            pv = lambda c0, c1=None, p0=0, p1=128: pv_all[p0:p1, l, c0:(c0 + 1 if c1 is None else c1)]

            with ExitStack() as ph:
                w_sb = sb(ph, "w_sb", [128, 8, 2704], BF16)
                w_u = [U() for _ in range(8)]
                for c in range(8):
                    fw.dma("pool", w_sb[:, c, :], w_in[l, c * 128:(c + 1) * 128, :], writes=[w_u[c]])
                xt = [sb(ph, "xt%d" % i, [128, 8, 512]) for i in range(2)]
                xn = [sb(ph, "xn%d" % i, [128, 8, 512], BF16) for i in range(2)]
                sq = sb(ph, "sq", [128, 8, 512], BF16)
                lnb = sb(ph, "lnb", [128, 512])
                rstd = sb(ph, "rstd", [128, 512])
                zst = [sb(ph, "zst%d" % i, [128, 512]) for i in range(4)]
                ev = 0
                for tt_ in range(8):
                    t0 = tt_ * 512
                    xt_, xn_ = xt[tt_ % 2], xn[tt_ % 2]
                    rd = [u_xres[tt_]] if l > 0 else []
                    fw.dma("sp", xt_[:], x_src[:, t0:t0 + 512].rearrange("(c p) t -> p c t", p=128), reads=rd, writes=[xt_])
                    act(sq[:], xt_[:], AF.Square, [xt_], [sq])
                    for c in range(8):
                        mm(PS[0][:], ones1024[:], sq[:, c, :], c == 0, c == 7, [ones1024, sq], [PS[0]])
                    rstd_from(PS[0][:], rstd[:], lnb[:], [PS[0]], lnb, rstd)
                    for c in range(8):
                        stt(xn_[:, c, :], xt_[:, c, :], pv(c), rstd[:], ALU.mult, ALU.mult, [xt_, rstd, pv_all], [xn_])
                    for oc in range(19):
                        m = 128 if oc < 18 else 4
                        ps = PS[1 + ev % 3]
                        for c in range(8):
                            mm(ps[0:m, :], w_sb[:, c, oc * 128:oc * 128 + m], xn_[:, c, :], c == 0, c == 7, [w_u[c], xn_], [ps])
                        st_ = zst[ev % 4]
                        if ev % 2 == 0:
                            act(st_[0:m, :], ps[0:m, :], AF.Copy, [ps], [st_])
                        else:
                            cp("dve", st_[0:m, :], ps[0:m, :], [ps], [st_])
                        fw.dma("pool", zT[oc * 128:oc * 128 + m, t0:t0 + 512], st_[0:m, :], reads=[st_], writes=[u_zT[oc][tt_]])
                        ev += 1
                    for s in range(4):
                        ps = PS[1 + ev % 3]
                        for c in range(8):
                            mm(ps[:, 0:NTM], xn_[:, c, s * 128:(s + 1) * 128], w_sb[:, c, NFM:NFM + NTM], c == 0, c == 7, [w_u[c], xn_], [ps])
                        st_ = zst[ev % 4]
                        if ev % 2 == 0:
                            act(st_[:, 0:NTM], ps[:, 0:NTM], AF.Copy, [ps], [st_])
                        else:
                            cp("dve", st_[:, 0:NTM], ps[:, 0:NTM], [ps], [st_])
                        fw.dma("pool", ztok[t0 + s * 128:t0 + (s + 1) * 128, :], st_[:, 0:NTM], reads=[st_], writes=[u_ztok[tt_ * 4 + s]])
                        ev += 1
                fw.barrier()
            if stop <= 1:
                break

            with ExitStack() as ph:
                NB = 4100
                X = sb(ph, "X", [128, NB])
                C = sb(ph, "C", [128, NB])
                R = sb(ph, "R", [128, NB])
                I = sb(ph, "I", [128, NB])
                A2 = sb(ph, "A2", [128, NB])
                Y0 = sb(ph, "Y0", [128, S])
                xcb = sb(ph, "xcb", [128, S], BF16)
                sq0 = sb(ph, "sq0", [128, S], BF16)
                sq1 = sb(ph, "sq1", [128, S], BF16)
                wg_f = sb(ph, "wg_f", [128, 4, 128])
                wg_b = sb(ph, "wg_b", [128, 4, 128], BF16)
                cst = sb(ph, "cst", [128, 8])
                lnb = sb(ph, "lnb2", [128, 512])
                rstd = sb(ph, "rstd2", [128, 512])
                mst = [sb(ph, "mst%d" % i, [128, 512], BF16) for i in range(3)]
                for g in range(2):
                    for j in range(2):
                        fw.dma("sp", wg_f[:, g * 2 + j, :], wbd_d[l, g, j], writes=[wg_f])
                cp("dve", wg_b[:], wg_f[:], [wg_f], [wg_b])
                act(cst[:, 4:6], pv(30, 32), AF.Exp, [pv_all], [cst], scale=-1.0)
                act(cst[:, 6:8], cst[:, 4:6], AF.Ln, [cst], [cst], bias=1.0)
                ts("dve", cst[:, 0:2], cst[:, 6:8], -8.0, None, ALU.mult, None, [cst], [cst])
                ts("dve", cst[:, 2:4], cst[:, 6:8], -16.0, None, ALU.mult, None, [cst], [cst])

                def outnorm_fm(Ys, sqs, grp):
                    for j in range(2):
                        act(sqs[j][:], Ys[j][:, 0:S], AF.Square, [Ys[j]], [sqs[j]])
                    for tt_ in range(8):
                        t0 = tt_ * 512
                        ps = PS[tt_ % 2]
                        mm(ps[:], ones256[:], sqs[0][:, t0:t0 + 512], True, False, [ones256, sqs[0]], [ps])
                        mm(ps[:], ones256[:], sqs[1][:, t0:t0 + 512], False, True, [ones256, sqs[1]], [ps])
                        rstd_from(ps[:], rstd[:], lnb[:], [ps], lnb, rstd)
                        for j in range(2):
                            m_ = mst[(tt_ * 2 + j) % 3]
                            stt(m_[:], Ys[j][:, t0:t0 + 512], pv(38 + grp * 2 + j), rstd[:], ALU.mult, ALU.mult, [Ys[j], pv_all, rstd], [m_])
                            fw.dma("pool", mixT[grp * 256 + j * 128:grp * 256 + (j + 1) * 128, t0:t0 + 512], m_[:], reads=[m_], writes=[u_mix[grp * 2 + j][tt_]])

                for j in range(2):
                    mset("pool", X[:, 0:3], 0.0, [X])
                    fw.dma("sp", X[:, 3:3 + S], zT[R_LRU_X + j * 128:R_LRU_X + (j + 1) * 128, :], reads=u_zT[0 + j], writes=[X])
                    wc = 16 + j * 4
                    ts("dve", C[:, 0:S], X[:, 0:S], pv(wc), pv(24 + j), ALU.mult, ALU.add, [X, pv_all], [C])
                    for k in range(1, 4):
                        stt(C[:, 0:S], X[:, k:k + S], pv(wc + k), C[:, 0:S], ALU.mult, ALU.add, [X, pv_all, C], [C])
                    cp("pool", xcb[:], C[:, 0:S], [C], [xcb])
                    for tt_ in range(8):
                        t0 = tt_ * 512
                        for g, dst in ((0, R), (1, I)):
                            ps = PS[(tt_ * 2 + g) % 4]
                            mm(ps[:], wg_b[:, g * 2 + j, :], xcb[:, t0:t0 + 512], True, True, [wg_b, xcb], [ps])
                            act(dst[:, t0:t0 + 512], ps[:], AF.Sigmoid, [ps, pv_all], [dst], bias=pv(26 + g * 2 + j))
                    act(A2[:, 0:S], R[:, 0:S], AF.Exp, [R, cst], [A2], scale=cst[:, 2 + j:3 + j])
                    act(R[:, 0:S], R[:, 0:S], AF.Exp, [R, cst], [R], scale=cst[:, j:j + 1])
                    act(A2[:, 0:S], A2[:, 0:S], AF.Sqrt, [A2], [A2], scale=-1.0, bias=1.0)
                    tt("dve", I[:, 0:S], I[:, 0:S], C[:, 0:S], ALU.mult, [I, C], [I])
                    tt("dve", I[:, 0:S], I[:, 0:S], A2[:, 0:S], ALU.mult, [I, A2], [I])
                    fw.op("dve", lambda e: e.tensor_tensor_scan(out=C[:, 0:S], data0=R[:, 0:S], data1=I[:, 0:S], initial=0.0, op0=ALU.mult, op1=ALU.add), [R, I], [C])
                    fw.dma("sp", X[:, 0:S], zT[R_LRU_G + j * 128:R_LRU_G + (j + 1) * 128, :], reads=u_zT[2 + j], writes=[X])
                    act(A2[:, 0:S], X[:, 0:S], AF.Gelu_apprx_tanh, [X], [A2])
                    Yd = Y0 if j == 0 else C
                    tt("dve", Yd[:, 0:S], C[:, 0:S], A2[:, 0:S], ALU.mult, [C, A2], [Yd])
                outnorm_fm([Y0, C], [sq0, sq1], 0)
                for j in range(2):
                    Yd = Y0 if j == 0 else C
                    mset("pool", X[:, 0:2], 0.0, [X])
                    fw.dma("sp", R[:, 0:S], zT[R_SC_C + j * 128:R_SC_C + (j + 1) * 128, :], reads=u_zT[6 + j], writes=[R])
                    fw.dma("sp", I[:, 0:S], zT[R_SC_X + j * 128:R_SC_X + (j + 1) * 128, :], reads=u_zT[8 + j], writes=[I])
                    fw.dma("sp", A2[:, 0:S], zT[R_SC_B + j * 128:R_SC_B + (j + 1) * 128, :], reads=u_zT[4 + j], writes=[A2])
                    tt("dve", X[:, 2:2 + S], R[:, 0:S], I[:, 0:S], ALU.mult, [R, I], [X])
                    wc = 32 + j * 3
                    ts("dve", R[:, 0:S], X[:, 0:S], pv(wc), None, ALU.mult, None, [X, pv_all], [R])
                    for k in range(1, 3):
                        stt(R[:, 0:S], X[:, k:k + S], pv(wc + k), R[:, 0:S], ALU.mult, ALU.add, [X, pv_all, R], [R])
                    tt("dve", Yd[:, 0:S], R[:, 0:S], A2[:, 0:S], ALU.mult, [R, A2], [Yd])
                outnorm_fm([Y0, C], [sq0, sq1], 1)
                fw.barrier()
            if stop <= 2:
                break

            def headnorm(tl, src_ap, src_units, gcol, scale, dst, ntok):
                hq, hsq, gs, lnb, rstd = tl
                if src_ap is not None:
                    fw.dma("sp", hq[0:64, 0:ntok], src_ap, reads=src_units, writes=[hq])
                ts("dve", gs[:, 0:1], pv(gcol, p1=64), float(scale), None, ALU.mult, None, [pv_all], [gs])
                act(hsq[0:64, 0:ntok], hq[0:64, 0:ntok], AF.Square, [hq], [hsq])
                for t0 in range(0, ntok, 512):
                    n = min(512, ntok - t0)
                    ps = PS[5 + (t0 // 512) % 2]
                    mm(ps[0:64, 0:n], ones64[:], hsq[0:64, t0:t0 + n], True, True, [ones64, hsq], [ps])
                    rstd_from(ps[0:64, 0:n], rstd[0:64, 0:n], lnb[0:64, 0:n], [ps], lnb, rstd)
                    stt(dst[0:64, t0:t0 + n], hq[0:64, t0:t0 + n], gs[:, 0:1], rstd[0:64, 0:n], ALU.mult, ALU.mult, [hq, gs, rstd], [dst])

            def run_attention(tiles, pts):
                n_t = len(tiles)
                ring = PS[0:3]

                def emit_pre(i):
                    if i < n_t and tiles[i].get("pre") is not None:
                        bt, src = tiles[i]["pre"]
                        fw.dma("sp", bt[:], src, reads=[u_ft], writes=[bt])

                emit_pre(0)

                def emit_qk(i):
                    tl = tiles[i]
                    ps = ring[i % 3]
                    emit_pre(i + 1)
                    np_ = tl["np"]
                    last = len(tl["mms"]) - 1
                    for idx, (c0, ncol, lhsT, rhs, rds) in enumerate(tl["mms"]):
                        mm(ps[0:np_, c0:c0 + ncol], lhsT, rhs, idx == 0, idx == last, rds, [ps], skip_group_check=True)

                def emit_rest(i):
                    tl = tiles[i]
                    ps = ring[i % 3]
                    pt = pts[i % len(pts)]
                    np_, n = tl["np"], tl["n"]
                    if tl.get("bias") is not None:
                        act(pt[0:np_, 0:n], ps[0:np_, 0:n], AF.Exp, [ps] + tl["bias_reads"], [pt], bias=tl["bias"])
                    else:
                        act(pt[0:np_, 0:n], ps[0:np_, 0:n], AF.Exp, [ps], [pt])
                    if tl.get("init") is not None:
                        accT, w = tl["init"]
                        mm(accT[:, 0:w], zeros[:, 0:128], zeros[:, 0:w], True, True, [zeros], [accT])
                    for (accT, acc_ap, pc0, V_ap, rds) in tl["pv"]:
                        mm(acc_ap, pt[0:np_, pc0:pc0 + 128], V_ap, False, True, [pt] + rds, [accT], skip_group_check=True)
                    if tl.get("fin") is not None:
                        tl["fin"]()

                LA = 2
                for i in range(n_t + LA):
                    if i < n_t:
                        emit_qk(i)
                    if i >= LA:
                        emit_rest(i - LA)

            def outnorm_tok(tl, Q, yv, yT, gain_ap, rowbase):
                junk, ssq, lnq, rs, ym, mst2 = tl
                q0 = Q * 512
                for s in range(4):
                    act(junk[:, 0:256], yv(s), AF.Square, [yT], [junk, ssq], accum_out=ssq[:, s:s + 1])
                act(lnq[:, 0:4], ssq[:, 0:4], AF.Ln, [ssq], [lnq], scale=1.0 / 256, bias=EPS)
                act(rs[:, 0:4], lnq[:, 0:4], AF.Exp, [lnq], [rs], scale=-0.5)
                for s in range(4):
                    stt(ym[:, s, :], yv(s), rs[:, s:s + 1], gain_ap, ALU.mult, ALU.mult, [yT, rs, brow_all], [ym])
                for j in range(2):
                    for s in range(4):
                        fw.op("pe", lambda e, s=s, j=j: e.transpose(out=PSB[:, s * 128:(s + 1) * 128], in_=ym[:, s, j * 128:(j + 1) * 128], identity=ident[:]), [ym, ident], [PSB])
                    m_ = mst2[j]
                    cp("dve", m_[:], PSB[:, 0:512], [PSB], [m_])
                    fw.dma("pool", mixT[rowbase + j * 128:rowbase + (j + 1) * 128, q0:q0 + 512], m_[:], reads=[m_], writes=[u_mix[rowbase // 128 + j][Q]])

            with ExitStack() as ph:
                Qp = [sb(ph, "Qp%d" % h, [128, S], BF16) for h in range(4)]
                Kp = [sb(ph, "Kp%d" % h, [128, S], BF16) for h in range(4)]
                with ExitStack() as ph2:
                    fr = sb(ph2, "fr", [4, S])
                    e1 = sb(ph2, "e1", [4, S])
                    cc = sb(ph2, "cc", [4, S])
                    ones4 = sb(ph2, "ones4", [4, S], BF16)
                    nb = sb(ph2, "nb", [4, 1])
                    csp = sb(ph2, "csp", [4, 3, S], BF16)
                    ncsp = sb(ph2, "ncsp", [4, 3, S], BF16)
                    fw.dma("sp", fr[:], zT[R_FOX_F:R_FOX_F + 4, :], reads=u_zT[18], writes=[fr])
                    ts("dve", nb[:], pv(48, p1=4), -1.0, None, ALU.mult, None, [pv_all], [nb])
                    act(e1[:], fr[:], AF.Exp, [fr, nb], [e1], scale=-1.0, bias=nb[:])
                    act(e1[:], e1[:], AF.Ln, [e1], [e1], bias=1.0)
                    ts("dve", e1[:], e1[:], -1.0, None, ALU.mult, None, [e1], [e1])
                    mset("pool", ones4[:], 1.0, [ones4])
                    fw.op("dve", lambda e: e.tensor_tensor_scan(out=cc[:], data0=ones4[:], data1=e1[:], initial=0.0, op0=ALU.mult, op1=ALU.add), [ones4, e1], [cc])
                    cp("dve", csp[:, 0, :], cc[:], [cc], [csp])
                    cp("dve", fr[:], csp[:, 0, :], [csp], [fr])
                    tt("dve", cc[:], cc[:], fr[:], ALU.subtract, [cc, fr], [cc])
                    cp("dve", csp[:, 1, :], cc[:], [cc], [csp])
                    cp("dve", fr[:], csp[:, 1, :], [csp], [fr])
                    tt("dve", cc[:], cc[:], fr[:], ALU.subtract, [cc, fr], [cc])
                    cp("dve", csp[:, 2, :], cc[:], [cc], [csp])
                    ts("dve", ncsp[:], csp[:], -1.0, None, ALU.mult, None, [csp], [ncsp])
                    for h in range(4):
                        mset("pool", Qp[h][64:128, :], 0.0, [Qp[h]])
                        mset("pool", Kp[h][64:128, :], 0.0, [Kp[h]])
                        for i in range(3):
                            fw.dma("sp", Qp[h][64 + i:65 + i, :], csp[h:h + 1, i, :], reads=[csp], writes=[Qp[h]])
                            fw.dma("sp", Kp[h][96 + i:97 + i, :], ncsp[h:h + 1, i, :], reads=[ncsp], writes=[Kp[h]])
                        fw.dma("sp", Qp[h][96:99, :], ones4[0:3, :], reads=[ones4], writes=[Qp[h]])
                        fw.dma("sp", Kp[h][64:67, :], ones4[0:3, :], reads=[ones4], writes=[Kp[h]])
                    fw.barrier()
                V1 = sb(ph, "V1", [128, 32, 4, 65], BF16)
                hq = sb(ph, "hq", [64, S])
                hsq = sb(ph, "hsq", [64, S], BF16)
                gs = sb(ph, "gs", [64, 1])
                lnb = sb(ph, "lnb3", [64, 512])
                rstd = sb(ph, "rstd3", [64, 512])
                hn = (hq, hsq, gs, lnb, rstd)
                vst = sb(ph, "vst", [128, 8, 256])
                pts = [sb(ph, "pt%d" % i, [128, 512], BF16) for i in range(3)]
                rden = [sb(ph, "rden%d" % i, [128, 4]) for i in range(2)]
                ytk = [sb(ph, "ytk%d" % i, [128, 4, 256]) for i in range(2)]
                on_tl = (sb(ph, "junk", [128, 256]), sb(ph, "ssq", [128, 4]), sb(ph, "lnq", [128, 4]), sb(ph, "rs", [128, 4]),
                         sb(ph, "ym", [128, 4, 256], BF16), [sb(ph, "mst2_%d" % i, [128, 512], BF16) for i in range(2)])
                for h in range(4):
                    headnorm(hn, zT[R_FOX_Q + h * 64:R_FOX_Q + (h + 1) * 64, :], u_zT[10 + h // 2], 42, 0.125, Qp[h], S)
                    headnorm(hn, zT[R_FOX_K + h * 64:R_FOX_K + (h + 1) * 64, :], u_zT[12 + h // 2], 43, 1.0, Kp[h], S)
                mset("pool", V1[:, :, :, 64:65], 1.0, [V1])
                for g in range(4):
                    fw.dma("sp", vst[:], ztok[g * 1024:(g + 1) * 1024, T_FOX_V:T_FOX_V + 256].rearrange("(k p) c -> p k c", p=128), reads=u_ztok[g * 8:(g + 1) * 8], writes=[vst])
                    for h in range(4):
                        cp("pool" if h % 2 else "dve", V1[:, g * 8:(g + 1) * 8, h, 0:64], vst[:, :, h * 64:(h + 1) * 64], [vst], [V1])
                accn = 0
                for Q in range(8):
                    q0 = Q * 512
                    yT = ytk[Q % 2]
                    tiles = []
                    for h in range(4):
                        accT = PS[3 + accn % 2]
                        rd_ = rden[accn % 2]
                        accn += 1
                        nk = 4 * Q + 4
                        for kt in range(nk):
                            k0 = kt * 128
                            d = kt - 4 * Q
                            dd = max(d, 0)
                            n = 512 - 128 * dd
                            mms = [(0, n, Kp[h][:, k0:k0 + 128], Qp[h][:, q0 + 128 * dd:q0 + 512], [Kp[h], Qp[h]])]
                            if d >= 0:
                                mms.append((0, 128, ident[:], tri[:], [ident, tri]))
                            tl = dict(np=128, n=n, mms=mms, pv=[])
                            for s in range(dd, 4):
                                tl["pv"].append((accT, accT[:, s * 65:(s + 1) * 65], (s - dd) * 128, V1[:, kt, h, :], [V1]))
                            if kt == 0:
                                tl["init"] = (accT, 260)
                            if kt == nk - 1:
                                def fin(accT=accT, rd_=rd_, h=h, yT=yT):
                                    accv = accT[:, 0:260].rearrange("p (s c) -> p s c", c=65)
                                    fw.op("dve", lambda e: e.reciprocal(out=rd_[:, 0:4], in_=accv[:, :, 64]), [accT], [rd_])
                                    for s in range(4):
                                        ts("dve", yT[:, s, h * 64:(h + 1) * 64], accv[:, s, 0:64], rd_[:, s:s + 1], None, ALU.mult, None, [accT, rd_], [yT])
                                tl["fin"] = fin
                            tiles.append(tl)
                    run_attention(tiles, pts)
                    outnorm_tok(on_tl, Q, lambda s, yT=yT: yT[:, s, :], yT, brow_all[:, l, 0:256], 512)
                fw.barrier()
            if stop <= 3:
                break

            with ExitStack() as ph:
                Qn = [sb(ph, "Qn%d" % h, [128, S], BF16) for h in range(4)]
                KsT = sb(ph, "KsT", [128, S], BF16)
                KwT = sb(ph, "KwT", [128, S], BF16)
                KcT = sb(ph, "KcT", [128, 256], BF16)
                for h in range(4):
                    mset("pool", Qn[h][64:128, :], 0.0, [Qn[h]])
                mset("pool", KwT[64:128, :], 0.0, [KwT])
                mset("pool", KcT[64:128, :], 0.0, [KcT])
                fw.dma("sp", KsT[64:128, :], cE_d[:, :], writes=[KsT])
                VS1 = sb(ph, "VS1", [128, 32, 65], BF16)
                VW1 = sb(ph, "VW1", [128, 32, 65], BF16)
                VC1 = sb(ph, "VC1", [128, 2, 128], BF16)
                G = sb(ph, "G", [128, 32, 12])
                yd = sb(ph, "yd", [128, 32, 256])
                hq = sb(ph, "hq5", [64, 4112])
                hsq = sb(ph, "hsq5", [64, 4112], BF16)
                gs = sb(ph, "gs5", [64, 1])
                lnb = sb(ph, "lnb5", [64, 512])
                rstd = sb(ph, "rstd5", [64, 512])
                hn = (hq, hsq, gs, lnb, rstd)
                w1b = sb(ph, "w1b", [64, 32, 128], BF16)
                posb = sb(ph, "posb", [64, 32], BF16)
                w2b = sb(ph, "w2b", [128, 64], BF16)
                hbias = sb(ph, "hbias", [128, 1])
                hidb = sb(ph, "hidb", [128, 256], BF16)
                vst2 = sb(ph, "vst2", [128, 32, 64])
                gst = sb(ph, "gst", [128, 32, 12])
                pts = [sb(ph, "ptn%d" % i, [128, 512], BF16) for i in range(3)]
                btile = [sb(ph, "btile%d" % i, [128, 512], BF16) for i in range(3)]
                den = [sb(ph, "den%d" % i, [128, 4]) for i in range(2)]
                rden = [sb(ph, "rdenn%d" % i, [128, 4]) for i in range(2)]
                coef = [sb(ph, "coef%d" % i, [128, 4]) for i in range(2)]
                impacc = sb(ph, "impacc", [128, 4, 64])
                imp2 = sb(ph, "imp2", [128, 64])
                m8 = sb(ph, "m8", [128, 8])
                m8b = sb(ph, "m8b", [128, 8])
                self_ = sb(ph, "self", [128, 64])
                selb = sb(ph, "selb", [128, 128], BF16)
                on_tl = (sb(ph, "junk5", [128, 256]), sb(ph, "ssq5", [128, 4]), sb(ph, "lnq5", [128, 4]), sb(ph, "rs5", [128, 4]),
                         sb(ph, "ym5", [128, 4, 256], BF16), [sb(ph, "mst5_%d" % i, [128, 512], BF16) for i in range(2)])
                for h in range(4):
                    headnorm(hn, zT[R_NSA_Q + h * 64:R_NSA_Q + (h + 1) * 64, :], u_zT[14 + h // 2], 44, 0.125, Qn[h], S)
                headnorm(hn, zT[R_KS:R_KS + 64, :], u_zT[17], 46, 1.0, KsT, S)
                headnorm(hn, zT[R_KW:R_KW + 64, :], u_zT[17], 47, 1.0, KwT, S)
                for kv in range(2):
                    rows = R_KC if kv == 0 else R_VC
                    fw.dma("sp", hq[0:64, 0:S], zT[rows:rows + 64, :], reads=u_zT[16], writes=[hq])
                    mset("pool", hq[0:64, S:4112], 0.0, [hq])
                    cp("dve", hsq[0:64, :], hq[0:64, :], [hq], [hsq])
                    fw.dma("pool", w1b[:], cw1_d[l, kv].rearrange("l d m -> d l m"), writes=[w1b])
                    fw.dma("pool", posb[:], cposT_d[l, kv], writes=[posb])
                    fw.dma("pool", w2b[:], cw2_d[l, kv], writes=[w2b])
                    ps = PS[5]
                    for li in range(32):
                        mm(ps[:, 0:1], w1b[:, li, :], posb[:, li:li + 1], li == 0, li == 31, [w1b, posb], [ps])
                    cp("dve", hbias[:], ps[:, 0:1], [ps], [hbias])
                    ps = PS[6]
                    for li in range(32):
                        if li < 16:
                            rhs = hsq[0:64, 0:4096].rearrange("p (n r) -> p n r", r=16)[:, :, li]
                        else:
                            rhs = hsq[0:64, 16:4112].rearrange("p (n r) -> p n r", r=16)[:, :, li - 16]
                        mm(ps[:, 0:256], w1b[:, li, :], rhs, li == 0, li == 31, [w1b, hsq], [ps])
                    act(hidb[:], ps[:, 0:256], AF.Gelu_apprx_tanh, [ps, hbias], [hidb], bias=hbias[:])
                    if kv == 0:
                        ps = PS[5]
                        mm(ps[0:64, 0:256], w2b[:], hidb[:], True, True, [w2b, hidb], [ps])
                        cp("dve", hq[0:64, 0:256], ps[0:64, 0:256], [ps], [hq])
                        headnorm(hn, None, None, 45, 1.0, KcT, 256)
                    else:
                        for c in range(2):
                            ps = PS[5]
                            mm(ps[:, 0:64], hidb[:, c * 128:(c + 1) * 128], w2b[:], True, True, [w2b, hidb], [ps])
                            cp("dve", VC1[:, c, 0:64], ps[:, 0:64], [ps], [VC1])
                            cp("dve", VC1[:, c, 64:128], ovl[:, c * 64:(c + 1) * 64], [ovl], [VC1])
                for (VT, col) in ((VS1, T_VS), (VW1, T_VW)):
                    fw.dma("sp", vst2[:], ztok[:, col:col + 64].rearrange("(k p) c -> p k c", p=128), reads=u_ztok, writes=[vst2])
                    cp("dve", VT[:, :, 0:64], vst2[:], [vst2], [VT])
                    mset("pool", VT[:, :, 64:65], 1.0, [VT])
                fw.dma("sp", gst[:], ztok[:, T_G:T_G + 12].rearrange("(k p) c -> p k c", p=128), reads=u_ztok, writes=[gst])
                tt("dve", gst[:], gst[:], brow_all[:, l, 512:524].unsqueeze(1).to_broadcast([128, 32, 12]), ALU.add, [gst, brow_all], [gst])
                act(G[:], gst[:], AF.Sigmoid, [gst], [G])
                mset("pool", selb[:], 0.0, [selb])

                accn = 0
                bn = 0
                for Q in range(8):
                    q0 = Q * 512

                    def fin_generic(accT, w, h, branch, first, dn, rd_, cf_, Q=Q):
                        accv = accT[:, 0:4 * w].rearrange("p (s c) -> p s c", c=w)
                        if branch == 0:
                            fw.op("dve", lambda e: e.tensor_reduce(out=dn[:, 0:4], in_=accv[:, :, 64:128], axis=mybir.AxisListType.X, op=ALU.add), [accT], [dn])
                            ts("dve", dn[:, 0:4], dn[:, 0:4], 1.0 / 32, 1e-30, ALU.mult, ALU.max, [dn], [dn])
                            fw.op("dve", lambda e: e.reciprocal(out=rd_[:, 0:4], in_=dn[:, 0:4]), [dn], [rd_])
                        else:
                            fw.op("dve", lambda e: e.reciprocal(out=rd_[:, 0:4], in_=accv[:, :, 64]), [accT], [rd_])
                        tt("dve", cf_[:, 0:4], rd_[:, 0:4], G[:, 4 * Q:4 * Q + 4, branch * 4 + h], ALU.mult, [rd_, G], [cf_])
                        for s in range(4):
                            ydv = yd[:, 4 * Q + s, h * 64:(h + 1) * 64]
                            if first:
                                ts("dve", ydv, accv[:, s, 0:64], cf_[:, s:s + 1], None, ALU.mult, None, [accT, cf_], [yd])
                            else:
                                stt(ydv, accv[:, s, 0:64], cf_[:, s:s + 1], ydv, ALU.mult, ALU.add, [accT, cf_, yd], [yd])
                        if branch == 0:
                            for s in range(4):
                                if h == 0:
                                    ts("dve", impacc[:, s, :], accv[:, s, 64:128], rd_[:, s:s + 1], None, ALU.mult, None, [accT, rd_], [impacc])
                                else:
                                    stt(impacc[:, s, :], accv[:, s, 64:128], rd_[:, s:s + 1], impacc[:, s, :], ALU.mult, ALU.add, [accT, rd_, impacc], [impacc])

                    tiles = []
                    for h in range(4):
                        accT = PS[3 + accn % 2]
                        dn, rd_, cf_ = den[accn % 2], rden[accn % 2], coef[accn % 2]
                        accn += 1
                        chunks = [0, 1] if Q >= 4 else [0]
                        for c in chunks:
                            bt = btile[bn % 3]
                            bn += 1
                            off = h * 8208 + ZC + q0 - 31 - 16 * (c * 128 + 127)
                            src = bass.AP(tensor=ftc_h, offset=off, ap=[[16, 128], [1, 512]])
                            tl = dict(np=128, n=512, pv=[])
                            tl["pre"] = (bt, src)
                            tl["mms"] = [(0, 512, KcT[:, c * 128:(c + 1) * 128], Qn[h][:, q0:q0 + 512], [KcT, Qn[h]]),
                                         (0, 512, Jm[:], bt[:], [Jm, bt])]
                            for s in range(4):
                                tl["pv"].append((accT, accT[:, s * 128:(s + 1) * 128], s * 128, VC1[:, c, :], [VC1]))
                            if c == 0:
                                tl["init"] = (accT, 512)
                            if c == chunks[-1]:
                                tl["fin"] = (lambda accT=accT, h=h, dn=dn, rd_=rd_, cf_=cf_: fin_generic(accT, 128, h, 0, True, dn, rd_, cf_))
                            tiles.append(tl)
                    run_attention(tiles, pts)

                    tiles = []
                    for h in range(4):
                        accT = PS[3 + accn % 2]
                        dn, rd_, cf_ = den[accn % 2], rden[accn % 2], coef[accn % 2]
                        accn += 1
                        kts = [kt for kt in range(4 * Q - 4, 4 * Q + 4) if kt >= 0]
                        for kt in kts:
                            k0 = kt * 128
                            d = kt - 4 * Q
                            dd = max(d, 0)
                            n = 512 - 128 * dd
                            sc0 = 128 * (-d) if d < 0 else 0
                            tl = dict(np=128, n=n, pv=[])
                            tl["mms"] = [(0, n, KwT[:, k0:k0 + 128], Qn[h][:, q0 + 128 * dd:q0 + 512], [KwT, Qn[h]]),
                                         (0, n, Jm[:], slab_w[:, h, sc0:sc0 + n], [Jm, slab_w])]
                            for s in range(dd, 4):
                                tl["pv"].append((accT, accT[:, s * 65:(s + 1) * 65], (s - dd) * 128, VW1[:, kt, :], [VW1]))
                            if kt == kts[0]:
                                tl["init"] = (accT, 260)
                            if kt == kts[-1]:
                                tl["fin"] = (lambda accT=accT, h=h, dn=dn, rd_=rd_, cf_=cf_: fin_generic(accT, 65, h, 2, False, dn, rd_, cf_))
                            tiles.append(tl)
                    run_attention(tiles, pts)

                    for s in range(4):
                        i_ = 4 * Q + s
                        iv = impacc[:, s, :]
                        if 2 * i_ + 2 < 64:
                            mset("dve", impacc[:, s, 2 * i_ + 2:64], -1e30, [impacc])
                        mset("dve", impacc[0:64, s, 2 * i_ + 1:2 * i_ + 2], -1e30, [impacc])
                        mset("dve", impacc[64:128, s, 2 * i_ + 1:2 * i_ + 2], 1e30, [impacc])
                        mset("dve", impacc[:, s, 2 * i_:2 * i_ + 1], 1e30, [impacc])
                        if i_ >= 1:
                            mset("dve", impacc[0:64, s, 2 * i_ - 1:2 * i_], 1e30, [impacc])
                        mset("dve", impacc[:, s, 0:1], 1e30, [impacc])
                        fw.op("dve", lambda e, iv=iv: e.max(out=m8[:], in_=iv), [impacc], [m8])
                        fw.op("dve", lambda e, iv=iv: e.match_replace(out=imp2[:], in_to_replace=m8[:], in_values=iv, imm_value=-3e38), [impacc, m8], [imp2])
                        fw.op("dve", lambda e: e.max(out=m8b[:], in_=imp2[:]), [imp2], [m8b])
                        ts("dve", self_[:], iv, m8b[:, 7:8], -1.0, ALU.is_ge, ALU.add, [impacc, m8b], [self_])
                        ts("dve", selb[:, 64:128], self_[:], -NEGM, None, ALU.mult, None, [self_], [selb])
                        fw.op("pe", lambda e, s=s: e.transpose(out=PSB[:, s * 128:(s + 1) * 128], in_=selb[:], identity=ident[:]), [selb, ident], [PSB])
                    for h in range(4):
                        cp("dve", Qn[h][64:128, q0:q0 + 512], PSB[64:128, 0:512], [PSB], [Qn[h]])

                    tiles = []
                    for h in range(4):
                        accT = PS[3 + accn % 2]
                        dn, rd_, cf_ = den[accn % 2], rden[accn % 2], coef[accn % 2]
                        accn += 1
                        nk = 4 * Q + 4
                        for kt in range(nk):
                            k0 = kt * 128
                            d = kt - 4 * Q
                            dd = max(d, 0)
                            n = 512 - 128 * dd
                            tl = dict(np=128, n=n, pv=[])
                            tl["mms"] = [(0, n, KsT[:, k0:k0 + 128], Qn[h][:, q0 + 128 * dd:q0 + 512], [KsT, Qn[h]])]
                            if d >= -1:
                                sc0 = 128 if d == -1 else 0
                                tl["mms"].append((0, n, Jm[:], slab_s[:, h, sc0:sc0 + n], [Jm, slab_s]))
                            else:
                                tl["bias"] = rb31[:, h:h + 1]
                                tl["bias_reads"] = [rb31]
                            for s in range(dd, 4):
                                tl["pv"].append((accT, accT[:, s * 65:(s + 1) * 65], (s - dd) * 128, VS1[:, kt, :], [VS1]))
                            if kt == 0:
                                tl["init"] = (accT, 260)
                            if kt == nk - 1:
                                tl["fin"] = (lambda accT=accT, h=h, dn=dn, rd_=rd_, cf_=cf_: fin_generic(accT, 65, h, 1, False, dn, rd_, cf_))
                            tiles.append(tl)
                    run_attention(tiles, pts)
                    outnorm_tok(on_tl, Q, lambda s, Q=Q: yd[:, 4 * Q + s, :], yd, brow_all[:, l, 256:512], 768)
                fw.barrier()
            if stop <= 4:
                break

            with ExitStack() as ph:
                wgu = sb(ph, "wgu", [128, 8, 2 * DFF], BF16)
                wdn = sb(ph, "wdn", [128, 22, 1024], BF16)
                wgu_u = [[U(), U()] for _ in range(8)]
                wdn_u = [U() for _ in range(22)]
                with ExitStack() as ph2:
                    wo = sb(ph2, "wo", [128, 8, 1024], BF16)
                    for c in range(8):
                        fw.dma("pool", wo[:, c, :], w_out[l, c * 128:(c + 1) * 128, :], writes=[wo])
                    xt = [sb(ph2, "xto%d" % i, [128, 8, 512]) for i in range(1)]
                    mt = [sb(ph2, "mt%d" % i, [128, 8, 512], BF16) for i in range(1)]
                    xo = [sb(ph2, "xo%d" % i, [128, 512]) for i in range(4)]
                    ev = 0
                    for tt_ in range(8):
                        t0 = tt_ * 512
                        xt_, mt_ = xt[0], mt[0]
                        rd = [u_xres[tt_]] if l > 0 else []
                        fw.dma("sp", xt_[:], x_src[:, t0:t0 + 512].rearrange("(c p) t -> p c t", p=128), reads=rd, writes=[xt_])
                        fw.dma("sp", mt_[:], mixT[:, t0:t0 + 512].rearrange("(c p) t -> p c t", p=128), reads=[u_mix[c][tt_] for c in range(8)], writes=[mt_])
                        if tt_ == 0:
                            for c in range(8):
                                for hh in range(2):
                                    fw.dma("pool", wgu[:, c, hh * DFF:(hh + 1) * DFF], w_gu[l, c * 128:(c + 1) * 128, hh * DFF:(hh + 1) * DFF], writes=[wgu_u[c][hh]])
                            for c in range(22):
                                fw.dma("pool", wdn[:, c, :], w_dn[l, c * 128:(c + 1) * 128, :], writes=[wdn_u[c]])
                        for n_ in range(8):
                            ps = PS[ev % 3]
                            for c in range(8):
                                mm(ps[:], wo[:, c, n_ * 128:(n_ + 1) * 128], mt_[:, c, :], c == 0, c == 7, [wo, mt_], [ps])
                            xo_ = xo[ev % 4]
                            tt("dve", xo_[:], xt_[:, n_, :], ps[:], ALU.add, [xt_, ps], [xo_])
                            fw.dma("sp", xres[n_ * 128:(n_ + 1) * 128, t0:t0 + 512], xo_[:], reads=[xo_], writes=[u_xres[tt_]])
                            ev += 1
                    fw.barrier()
                if stop <= 5:
                    break
                NT = 256
                xt = [sb(ph, "xtf%d" % i, [128, 8, NT]) for i in range(2)]
                xn = sb(ph, "xnf", [128, 8, NT], BF16)
                sq = sb(ph, "sqf", [128, 8, NT], BF16)
                lnb = sb(ph, "lnbf", [128, NT])
                rstd = sb(ph, "rstdf", [128, NT])
                hT = sb(ph, "hT", [128, 22, NT], BF16)
                sg = [sb(ph, "sg%d" % i, [128, NT]) for i in range(2)]
                xo = [sb(ph, "xof%d" % i, [128, NT]) for i in range(4)]
                ev = 0
                for tt_ in range(S // NT):
                    t0 = tt_ * NT
                    xt_ = xt[tt_ % 2]
                    ux = u_xres[t0 // 512]
                    fw.dma("sp", xt_[:], xres[:, t0:t0 + NT].rearrange("(c p) t -> p c t", p=128), reads=[ux], writes=[xt_])
                    act(sq[:], xt_[:], AF.Square, [xt_], [sq])
                    for c in range(8):
                        mm(PS[6][:, 0:NT], ones1024[:], sq[:, c, :], c == 0, c == 7, [ones1024, sq], [PS[6]])
                    rstd_from(PS[6][:, 0:NT], rstd[:], lnb[:], [PS[6]], lnb, rstd)
                    for c in range(8):
                        stt(xn[:, c, :], xt_[:, c, :], pv(8 + c), rstd[:], ALU.mult, ALU.mult, [xt_, rstd, pv_all], [xn])
                    for j in range(22):
                        psg = PS[(2 * j) % 4]
                        psu = PS[(2 * j + 1) % 4]
                        for c in range(8):
                            mm(psg[:, 0:NT], wgu[:, c, j * 128:(j + 1) * 128], xn[:, c, :], c == 0, c == 7, [wgu_u[c][0], xn], [psg])
                        for c in range(8):
                            mm(psu[:, 0:NT], wgu[:, c, DFF + j * 128:DFF + (j + 1) * 128], xn[:, c, :], c == 0, c == 7, [wgu_u[c][1], xn], [psu])
                        sg_ = sg[j % 2]
                        act(sg_[:], psg[:, 0:NT], AF.Silu, [psg], [sg_])
                        tt("dve", hT[:, j, :], sg_[:], psu[:, 0:NT], ALU.mult, [sg_, psu], [hT])
                    for n_ in range(8):
                        ps = PS[4 + n_ % 2]
                        for j in range(22):
                            mm(ps[:, 0:NT], wdn[:, j, n_ * 128:(n_ + 1) * 128], hT[:, j, :], j == 0, j == 21, [wdn_u[j], hT], [ps])
                        xo_ = xo[ev % 4]
                        tt("dve", xo_[:], xt_[:, n_, :], ps[:, 0:NT], ALU.add, [xt_, ps], [xo_])
                        fw.dma("sp", x_dst[n_ * 128:(n_ + 1) * 128, t0:t0 + NT], xo_[:], reads=[xo_], writes=[ux] if x_dst is xres else [])
                        ev += 1
                fw.barrier()
        fw.barrier()
    return nc


def _bucket(d):
    d = np.maximum(d, 0)
    large = 16 + (np.log(np.maximum(d, 1).astype(np.float32) / np.float32(16)) / np.float32(np.log(128 / 16)) * np.float32(16)).astype(np.int32)
    large = np.minimum(large, 31)
    return np.where(d < 16, d, large)


def _host_consts():
    bf = ml_dtypes.bfloat16
    c = {}
    c["cident"] = np.eye(128, dtype=np.float32).astype(bf)
    c["cJ"] = np.eye(128, dtype=np.float32)[::-1].copy().astype(bf)
    kl = np.arange(128)[:, None]
    ql = np.arange(128)[None, :]
    c["ctri"] = np.where(ql >= kl, 0.0, NEGM).astype(np.float32).astype(bf)
    E = np.zeros((64, S), np.float32)
    E[np.arange(S) // 64, np.arange(S)] = 1.0
    c["cE"] = E.astype(bf)
    n = np.arange(256)
    m = np.arange(64)
    cs = n[:, None] * 16
    ss = m[None, :] * 64
    ov = np.clip(np.minimum(cs + 32, ss + 64) - np.maximum(cs, ss), 0, 32).astype(np.float32)
    ov[255] = 0
    c["covl"] = np.concatenate([ov[0:128], ov[128:256]], axis=1).astype(bf)
    d = np.arange(8208) - ZC
    oh = np.zeros((33, 8208), np.float32)
    b = _bucket(d)
    oh[b[d >= 0], np.nonzero(d >= 0)[0]] = 1.0
    oh[32, d < 0] = 1.0
    c["ohc"] = oh.astype(bf)
    d = np.arange(1152) - ZW
    oh = np.zeros((33, 1152), np.float32)
    b = _bucket(d)
    ok = (d >= 0) & (d < 512)
    oh[b[ok], np.nonzero(ok)[0]] = 1.0
    oh[32, ~ok] = 1.0
    c["ohw"] = oh.astype(bf)
    return c


def _host_layout(inp):
    f = np.float32
    C_LRU_X, C_LRU_G, C_SC_B, C_SC_C, C_SC_X = 0, 256, 512, 768, 1024
    C_FOX_Q, C_FOX_K, C_FOX_V, C_FOX_F, C_NSA_Q, C_NSA_KV, C_NSA_G = 1280, 1536, 1792, 2048, 2052, 2308, 2692
    r = lambda a, n: list(range(a, a + n))
    perm = (r(C_LRU_X, 256) + r(C_LRU_G, 256) + r(C_SC_B, 256) + r(C_SC_C, 256) + r(C_SC_X, 256)
            + r(C_FOX_Q, 256) + r(C_FOX_K, 256) + r(C_NSA_Q, 256)
            + r(C_NSA_KV + 0, 64) + r(C_NSA_KV + 64, 64) + r(C_NSA_KV + 128, 64) + r(C_NSA_KV + 256, 64)
            + r(C_FOX_F, 4)
            + r(C_FOX_V, 256) + r(C_NSA_KV + 192, 64) + r(C_NSA_KV + 320, 64) + r(C_NSA_G, 12))
    assert len(perm) == 2704 and len(set(perm)) == 2704
    m = {}
    m["w_in"] = np.ascontiguousarray(np.asarray(inp["w_in"], f)[:, :, perm])
    m["w_out"] = np.ascontiguousarray(np.asarray(inp["w_out"], f))
    m["w_gu"] = np.ascontiguousarray(np.asarray(inp["w_gate_up"], f))
    m["w_dn"] = np.ascontiguousarray(np.asarray(inp["w_down"], f))
    pvec = np.zeros((NL, 128, 64), f)
    brow = np.zeros((NL, 1, 524), f)
    wbd = np.zeros((NL, 2, 2, 128, 128), f)
    for l in range(NL):
        pvec[l, :, 0:8] = np.asarray(inp["norm_mix"][l]).reshape(8, 128).T
        pvec[l, :, 8:16] = np.asarray(inp["norm_ffn"][l]).reshape(8, 128).T
        for j in range(2):
            sl = slice(j * 128, (j + 1) * 128)
            pvec[l, :, 16 + j * 4:20 + j * 4] = np.asarray(inp["lru_conv_w"][l])[:, sl].T
            pvec[l, :, 24 + j] = np.asarray(inp["lru_conv_b"][l])[sl]
            for g in range(2):
                pvec[l, :, 26 + g * 2 + j] = np.asarray(inp["lru_b_gates"][l])[g, sl]
                for bb in range(2):
                    wbd[l, g, j, bb * 64:(bb + 1) * 64, bb * 64:(bb + 1) * 64] = np.asarray(inp["lru_w_gates"][l])[g, 2 * j + bb]
            pvec[l, :, 30 + j] = np.asarray(inp["lru_lambda"][l])[sl]
            pvec[l, :, 32 + j * 3:35 + j * 3] = np.asarray(inp["sc_conv_w"][l])[:, sl].T
            for grp in range(2):
                pvec[l, :, 38 + grp * 2 + j] = np.asarray(inp["out_norm"][l])[grp, sl]
        pvec[l, 0:64, 42] = np.asarray(inp["fox_qk_norm"][l])[0]
        pvec[l, 0:64, 43] = np.asarray(inp["fox_qk_norm"][l])[1]
        for i in range(4):
            pvec[l, 0:64, 44 + i] = np.asarray(inp["nsa_qk_norm"][l])[i]
        pvec[l, 0:4, 48] = np.asarray(inp["fox_f_bias"][l])
        brow[l, 0, 0:256] = np.asarray(inp["out_norm"][l])[2]
        brow[l, 0, 256:512] = np.asarray(inp["out_norm"][l])[3]
        brow[l, 0, 512:524] = np.asarray(inp["nsa_gate_bias"][l])
    m["pvec"] = pvec
    m["brow"] = brow
    m["wbd"] = wbd
    m["cw1"] = np.ascontiguousarray(np.asarray(inp["nsa_cmp_w1"], f))
    m["cw2"] = np.ascontiguousarray(np.asarray(inp["nsa_cmp_w2"], f))
    m["cposT"] = np.ascontiguousarray(np.asarray(inp["nsa_cmp_pos"], f).transpose(0, 1, 3, 2))
    rb = np.zeros((33, 4), f)
    rb[0:32] = np.asarray(inp["rel_bias"], f)
    rb[32] = NEGM
    m["rb33"] = rb
    m.update(_host_consts())
    return m


def kernel(**inputs):
    x = np.asarray(inputs["x"], np.float32)
    shared = _host_layout(inputs)
    nc = build()
    in_maps = []
    for b in range(8):
        d = dict(shared)
        d["xT"] = np.ascontiguousarray(x[b].T)
        in_maps.append(d)
    res = run_bass_kernel_spmd(nc, in_maps, core_ids=list(range(8)))
    out = np.stack([np.ascontiguousarray(res.results[b]["out"].T) for b in range(8)], axis=0)
    return out.astype(np.float32)
```

```python
import numpy as np
import ml_dtypes
from contextlib import ExitStack
import concourse.bass as bass
import concourse.mybir as mybir
from concourse.bass_utils import run_bass_kernel_spmd

F32 = mybir.dt.float32
BF16 = mybir.dt.bfloat16
AF = mybir.ActivationFunctionType
ALU = mybir.AluOpType

S = 4096
D = 1024
NL = 2
DFF = 2816
NFM = 2308
NTM = 396
EPS = 1e-6
NEGM = -30000.0
R_LRU_X, R_LRU_G, R_SC_B, R_SC_C, R_SC_X = 0, 256, 512, 768, 1024
R_FOX_Q, R_FOX_K, R_NSA_Q, R_KC, R_VC, R_KS, R_KW, R_FOX_F = 1280, 1536, 1792, 2048, 2112, 2176, 2240, 2304
T_FOX_V, T_VS, T_VW, T_G = 0, 256, 320, 384
ZC = 4112
ZW = 128


class U:
    __slots__ = ("lastw", "readers")

    def __init__(self):
        self.lastw = None
        self.readers = []


class T:
    def __init__(self, t):
        self.t = t
        self.u = U()

    def __getitem__(self, k):
        return self.t[k]


class FW:
    def __init__(self, nc, es, ndma=32):
        self.nc = nc
        self.engs = {}
        for name, h in [("pe", nc.tensor), ("dve", nc.vector), ("act", nc.scalar),
                        ("pool", nc.gpsimd), ("sp", nc.sync)]:
            sem = es.enter_context(nc.semaphore("sem_" + name))
            self.engs[name] = dict(h=h, sem=sem, count=0, known={})
        self.dsems = [[es.enter_context(nc.semaphore("dq%d" % i)), 0] for i in range(ndma)]
        self.drr = 0
        self.ninstr = 0

    def _wait(self, en, toks):
        e = self.engs[en]
        need = {}
        for (sem, val) in toks:
            k = id(sem)
            if k not in need or need[k][1] < val:
                need[k] = (sem, val)
        for k, (sem, val) in need.items():
            if e["known"].get(k, 0) < val:
                e["h"].wait_ge(sem, val)
                e["known"][k] = val
                self.ninstr += 1

    def _deps(self, en, reads, writes):
        toks = []
        for u in reads:
            t = u.lastw
            if t is not None:
                if t[2] == en and en == "pe":
                    continue
                toks.append((t[0], t[1]))
        for u in writes:
            t = u.lastw
            if t is not None and (t[2] != en or en == "dma"):
                toks.append((t[0], t[1]))
            for r in u.readers:
                if r[2] != en or en == "dma":
                    toks.append((r[0], r[1]))
        return toks

    def _commit(self, tok, reads, writes):
        for u in writes:
            u.lastw = tok
            u.readers = []
        for u in reads:
            if u in writes:
                continue
            u.readers.append(tok)
            if len(u.readers) > 48:
                best = {}
                for r in u.readers:
                    k = id(r[0])
                    if k not in best or best[k][1] < r[1]:
                        best[k] = r
                u.readers = list(best.values())

    def op(self, en, fn, reads=(), writes=()):
        reads = [x.u if isinstance(x, T) else x for x in reads]
        writes = [x.u if isinstance(x, T) else x for x in writes]
        e = self.engs[en]
        self._wait(en, self._deps(en, reads, writes))
        ins = fn(e["h"])
        e["count"] += 1
        ins.then_inc(e["sem"], 1)
        self.ninstr += 1
        tok = (e["sem"], e["count"], en)
        self._commit(tok, reads, writes)
        return tok

    def dma(self, en, out, in_, reads=(), writes=(), **kw):
        reads = [x.u if isinstance(x, T) else x for x in reads]
        writes = [x.u if isinstance(x, T) else x for x in writes]
        e = self.engs[en]
        slot = self.dsems[self.drr % len(self.dsems)]
        self.drr += 1
        toks = self._deps("dma", reads, writes)
        toks.append((slot[0], slot[1]))
        self._wait(en, toks)
        ins = e["h"].dma_start(out=out, in_=in_, **kw)
        slot[1] += 16
        ins.then_inc(slot[0], 16)
        self.ninstr += 1
        tok = (slot[0], slot[1], "dma")
        self._commit(tok, reads, writes)
        return tok

    def barrier(self):
        toks = []
        for n, e in self.engs.items():
            if e["count"] > 0:
                toks.append((e["sem"], e["count"]))
        for s in self.dsems:
            if s[1] > 0:
                toks.append((s[0], s[1]))
        for n in self.engs:
            self._wait(n, toks)


def build(dbg=False, nlayers=NL, stop=99):
    nc = bass.Bass("TRN2", target_bir_lowering=False)

    def din(name, shape, dt=F32):
        return nc.dram_tensor(name, list(shape), dt, kind="ExternalInput").ap()

    kind_s = "ExternalOutput" if dbg else "Internal"

    def dscr(name, shape, dt=F32):
        return nc.dram_tensor(name, list(shape), dt, kind=kind_s)

    xT = din("xT", [D, S])
    w_in = din("w_in", [NL, D, 2704])
    w_out = din("w_out", [NL, D, D])
    w_gu = din("w_gu", [NL, D, 2 * DFF])
    w_dn = din("w_dn", [NL, DFF, D])
    pvec_d = din("pvec", [NL, 128, 64])
    brow_d = din("brow", [NL, 1, 524])
    wbd_d = din("wbd", [NL, 2, 2, 128, 128])
    cw1_d = din("cw1", [NL, 2, 32, 64, 128])
    cw2_d = din("cw2", [NL, 2, 128, 64])
    cposT_d = din("cposT", [NL, 2, 64, 32])
    rb33_d = din("rb33", [33, 4])
    ohc_d = din("ohc", [33, 8208], BF16)
    ohw_d = din("ohw", [33, 1152], BF16)
    cident_d = din("cident", [128, 128], BF16)
    cJ_d = din("cJ", [128, 128], BF16)
    ctri_d = din("ctri", [128, 128], BF16)
    cE_d = din("cE", [64, S], BF16)
    covl_d = din("covl", [128, 128], BF16)
    out_d = nc.dram_tensor("out", [D, S], F32, kind="ExternalOutput").ap()
    zT_h = dscr("zT", [NFM, S])
    ztok_h = dscr("ztok", [S, NTM])
    mixT_h = dscr("mixT", [D, S], BF16)
    xres_h = dscr("xres", [D, S])
    ftc_h = dscr("ftc", [4, 8208], BF16)
    ftw_h = dscr("ftw", [4, 1152], BF16)
    zT, ztok, mixT, xres, ftc, ftw = (h.ap() for h in (zT_h, ztok_h, mixT_h, xres_h, ftc_h, ftw_h))
    u_zT = [[U() for _ in range(8)] for _ in range(19)]
    u_ztok = [U() for _ in range(32)]
    u_mix = [[U() for _ in range(8)] for _ in range(8)]
    u_xres = [U() for _ in range(8)]
    u_ft = U()

    with ExitStack() as es:
        fw = FW(nc, es)

        uid = [0]

        def sb(st, name, shape, dt=F32):
            uid[0] += 1
            return T(st.enter_context(nc.sbuf_tensor("%s_%d" % (name, uid[0]), list(shape), dt)))

        def psm(st, name, shape, dt=F32):
            return T(st.enter_context(nc.psum_tensor(name, list(shape), dt)))

        def act(out, in_, func, reads, writes, **kw):
            return fw.op("act", lambda e: e.activation(out=out, in_=in_, func=func, **kw), reads, writes)

        def mm(out, lhsT, rhs, start, stop, reads, writes, **kw):
            return fw.op("pe", lambda e: e.matmul(out, lhsT=lhsT, rhs=rhs, start=start, stop=stop, **kw), reads, writes)

        def tt(en, out, in0, in1, op, reads, writes):
            return fw.op(en, lambda e: e.tensor_tensor(out=out, in0=in0, in1=in1, op=op), reads, writes)

        def ts(en, out, in0, s1, s2, op0, op1, reads, writes):
            if op1 is None:
                return fw.op(en, lambda e: e.tensor_scalar(out=out, in0=in0, scalar1=s1, scalar2=None, op0=op0), reads, writes)
            return fw.op(en, lambda e: e.tensor_scalar(out=out, in0=in0, scalar1=s1, scalar2=s2, op0=op0, op1=op1), reads, writes)

        def stt(out, in0, scalar, in1, op0, op1, reads, writes):
            return fw.op("dve", lambda e: e.scalar_tensor_tensor(out=out, in0=in0, scalar=scalar, in1=in1, op0=op0, op1=op1), reads, writes)

        def cp(en, out, in_, reads, writes):
            return fw.op(en, lambda e: e.tensor_copy(out=out, in_=in_), reads, writes)

        def mset(en, ap, val, writes):
            return fw.op(en, lambda e: e.memset(ap, val), (), writes)

        ident = sb(es, "ident", [128, 128], BF16)
        Jm = sb(es, "Jm", [128, 128], BF16)
        tri = sb(es, "tri", [128, 128], BF16)
        ovl = sb(es, "ovl", [128, 128], BF16)
        ones1024 = sb(es, "ones1024", [128, 128], BF16)
        ones256 = sb(es, "ones256", [128, 128], BF16)
        ones64 = sb(es, "ones64", [64, 64], BF16)
        zeros = sb(es, "zeros", [128, 512], BF16)
        slab_s = sb(es, "slab_s", [128, 4, 640], BF16)
        slab_w = sb(es, "slab_w", [128, 4, 1024], BF16)
        pv_all = sb(es, "pv_all", [128, NL, 64])
        brow_all = sb(es, "brow_all", [128, NL, 524])
        fw.dma("sp", ident[:], cident_d[:, :], writes=[ident])
        fw.dma("sp", Jm[:], cJ_d[:, :], writes=[Jm])
        fw.dma("sp", tri[:], ctri_d[:, :], writes=[tri])
        fw.dma("sp", ovl[:], covl_d[:, :], writes=[ovl])
        for l in range(NL):
            fw.dma("sp", pv_all[:, l, :], pvec_d[l], writes=[pv_all])
            fw.dma("sp", brow_all[:, l, :], brow_d[l].to_broadcast([128, 524]), writes=[brow_all])
        mset("pool", ones1024[:], 1.0 / 1024, [ones1024])
        mset("pool", ones256[:], 1.0 / 256, [ones256])
        mset("pool", ones64[:], 1.0 / 64, [ones64])
        mset("pool", zeros[:], 0.0, [zeros])

        PS = [psm(es, "ps%d" % i, [128, 512]) for i in range(7)]
        PSB = psm(es, "psb", [128, 1024], BF16)
        rb31 = sb(es, "rb31", [128, 4])
        fw.dma("sp", rb31[:], rb33_d[31:32, :].to_broadcast([128, 4]), writes=[rb31])

        with ExitStack() as ph:
            rb_f = sb(ph, "rb_f", [33, 4])
            rb_b = sb(ph, "rb_b", [33, 4], BF16)
            oh = sb(ph, "oh", [33, 8208], BF16)
            ohw_s = sb(ph, "ohw_s", [33, 1152], BF16)
            ftab = sb(ph, "ftab", [4, 8208 + 1152], BF16)
            fw.dma("sp", rb_f[:], rb33_d[:, :], writes=[rb_f])
            fw.dma("sp", oh[:], ohc_d[:, :], writes=[oh])
            fw.dma("sp", ohw_s[:], ohw_d[:, :], writes=[ohw_s])
            cp("dve", rb_b[:], rb_f[:], [rb_f], [rb_b])
            pieces = [(oh, c0, min(512, 8208 - c0), c0) for c0 in range(0, 8208, 512)]
            pieces += [(ohw_s, c0, min(512, 1152 - c0), 8208 + c0) for c0 in range(0, 1152, 512)]
            for i, (src, c0, n, dst0) in enumerate(pieces):
                ps = PS[i % 2]
                mm(ps[0:4, 0:n], rb_b[:, 0:4], src[:, c0:c0 + n], True, True, [rb_b, src], [ps])
                cp("dve", ftab[:, dst0:dst0 + n], ps[0:4, 0:n], [ps], [ftab])
            fw.dma("sp", ftc[:, :], ftab[:, 0:8208], reads=[ftab], writes=[u_ft])
            fw.dma("sp", ftw[:, :], ftab[:, 8208:8208 + 1152], reads=[ftab], writes=[u_ft])
            for h in range(4):
                src = bass.AP(tensor=ftc_h, offset=h * 8208 + ZC - 127, ap=[[1, 128], [1, 640]])
                fw.dma("sp", slab_s[:, h, :], src, reads=[u_ft], writes=[slab_s])
                src = bass.AP(tensor=ftw_h, offset=h * 1152 + ZW - 127, ap=[[1, 128], [1, 1024]])
                fw.dma("sp", slab_w[:, h, :], src, reads=[u_ft], writes=[slab_w])
            fw.barrier()

        def rstd_from(ps_ap, out_ap, tmp_ap, reads, tmpT, outT, scale=1.0):
            act(tmp_ap, ps_ap, AF.Ln, reads, [tmpT], bias=EPS, scale=scale)
            act(out_ap, tmp_ap, AF.Exp, [tmpT], [outT], scale=-0.5)

        for l in range(nlayers):
            x_src = xT if l == 0 else xres
            x_dst = out_d if l == nlayers - 1 else xres
            pv = lambda c0, c1=None, p0=0, p1=128: pv_all[p0:p1, l, c0:(c0 + 1 if c1 is None else c1)]

            with ExitStack() as ph:
                w_sb = sb(ph, "w_sb", [128, 8, 2704], BF16)
                w_u = [U() for _ in range(8)]
                for c in range(8):
                    fw.dma("pool", w_sb[:, c, :], w_in[l, c * 128:(c + 1) * 128, :], writes=[w_u[c]])
                xt = [sb(ph, "xt%d" % i, [128, 8, 512]) for i in range(2)]
                xn = [sb(ph, "xn%d" % i, [128, 8, 512], BF16) for i in range(2)]
                sq = sb(ph, "sq", [128, 8, 512], BF16)
                lnb = sb(ph, "lnb", [128, 512])
                rstd = sb(ph, "rstd", [128, 512])
                zst = [sb(ph, "zst%d" % i, [128, 512]) for i in range(4)]
                ev = 0

                def norm1(tt_):
                    t0 = tt_ * 512
                    xt_, xn_ = xt[tt_ % 2], xn[tt_ % 2]
                    rd = [u_xres[tt_]] if l > 0 else []
                    fw.dma("sp", xt_[:], x_src[:, t0:t0 + 512].rearrange("(c p) t -> p c t", p=128), reads=rd, writes=[xt_])
                    act(sq[:], xt_[:], AF.Square, [xt_], [sq])
                    for c in range(8):
                        mm(PS[0][:], ones1024[:], sq[:, c, :], c == 0, c == 7, [ones1024, sq], [PS[0]])
                    rstd_from(PS[0][:], rstd[:], lnb[:], [PS[0]], lnb, rstd)
                    for c in range(8):
                        stt(xn_[:, c, :], xt_[:, c, :], pv(c), rstd[:], ALU.mult, ALU.mult, [xt_, rstd, pv_all], [xn_])

                norm1(0)
                for tt_ in range(8):
                    t0 = tt_ * 512
                    xt_, xn_ = xt[tt_ % 2], xn[tt_ % 2]
                    for oc in range(19):
                        if oc == 6 and tt_ + 1 < 8:
                            norm1(tt_ + 1)
                        m = 128 if oc < 18 else 4
                        ps = PS[1 + ev % 3]
                        for c in range(8):
                            mm(ps[0:m, :], w_sb[:, c, oc * 128:oc * 128 + m], xn_[:, c, :], c == 0, c == 7, [w_u[c], xn_], [ps])
                        st_ = zst[ev % 4]
                        if ev % 2 == 0:
                            act(st_[0:m, :], ps[0:m, :], AF.Copy, [ps], [st_])
                        else:
                            cp("dve", st_[0:m, :], ps[0:m, :], [ps], [st_])
                        fw.dma("pool", zT[oc * 128:oc * 128 + m, t0:t0 + 512], st_[0:m, :], reads=[st_], writes=[u_zT[oc][tt_]])
                        ev += 1
                    for s in range(4):
                        ps = PS[1 + ev % 3]
                        for c in range(8):
                            mm(ps[:, 0:NTM], xn_[:, c, s * 128:(s + 1) * 128], w_sb[:, c, NFM:NFM + NTM], c == 0, c == 7, [w_u[c], xn_], [ps])
                        st_ = zst[ev % 4]
                        if ev % 2 == 0:
                            act(st_[:, 0:NTM], ps[:, 0:NTM], AF.Copy, [ps], [st_])
                        else:
                            cp("dve", st_[:, 0:NTM], ps[:, 0:NTM], [ps], [st_])
                        fw.dma("pool", ztok[t0 + s * 128:t0 + (s + 1) * 128, :], st_[:, 0:NTM], reads=[st_], writes=[u_ztok[tt_ * 4 + s]])
                        ev += 1
                fw.barrier()
            if stop <= 1:
                break

            with ExitStack() as ph:
                NB = 4100
                X = sb(ph, "X", [128, NB])
                C = sb(ph, "C", [128, NB])
                R = sb(ph, "R", [128, NB])
                I = sb(ph, "I", [128, NB])
                A2 = sb(ph, "A2", [128, NB])
                Y0 = sb(ph, "Y0", [128, S])
                xcb = sb(ph, "xcb", [128, S], BF16)
                sq0 = sb(ph, "sq0", [128, S], BF16)
                sq1 = sb(ph, "sq1", [128, S], BF16)
                wg_f = sb(ph, "wg_f", [128, 4, 128])
                wg_b = sb(ph, "wg_b", [128, 4, 128], BF16)
                cst = sb(ph, "cst", [128, 8])
                lnb = sb(ph, "lnb2", [128, 512])
                rstd = sb(ph, "rstd2", [128, 512])
                mst = [sb(ph, "mst%d" % i, [128, 512], BF16) for i in range(3)]
                for g in range(2):
                    for j in range(2):
                        fw.dma("sp", wg_f[:, g * 2 + j, :], wbd_d[l, g, j], writes=[wg_f])
                cp("dve", wg_b[:], wg_f[:], [wg_f], [wg_b])
                act(cst[:, 4:6], pv(30, 32), AF.Exp, [pv_all], [cst], scale=-1.0)
                act(cst[:, 6:8], cst[:, 4:6], AF.Ln, [cst], [cst], bias=1.0)
                ts("dve", cst[:, 0:2], cst[:, 6:8], -8.0, None, ALU.mult, None, [cst], [cst])
                ts("dve", cst[:, 2:4], cst[:, 6:8], -16.0, None, ALU.mult, None, [cst], [cst])

                def outnorm_fm(Ys, sqs, grp):
                    for j in range(2):
                        act(sqs[j][:], Ys[j][:, 0:S], AF.Square, [Ys[j]], [sqs[j]])
                    for tt_ in range(8):
                        t0 = tt_ * 512
                        ps = PS[tt_ % 2]
                        mm(ps[:], ones256[:], sqs[0][:, t0:t0 + 512], True, False, [ones256, sqs[0]], [ps])
                        mm(ps[:], ones256[:], sqs[1][:, t0:t0 + 512], False, True, [ones256, sqs[1]], [ps])
                        rstd_from(ps[:], rstd[:], lnb[:], [ps], lnb, rstd)
                        for j in range(2):
                            m_ = mst[(tt_ * 2 + j) % 3]
                            stt(m_[:], Ys[j][:, t0:t0 + 512], pv(38 + grp * 2 + j), rstd[:], ALU.mult, ALU.mult, [Ys[j], pv_all, rstd], [m_])
                            fw.dma("pool", mixT[grp * 256 + j * 128:grp * 256 + (j + 1) * 128, t0:t0 + 512], m_[:], reads=[m_], writes=[u_mix[grp * 2 + j][tt_]])

                for j in range(2):
                    mset("pool", X[:, 0:3], 0.0, [X])
                    fw.dma("sp", X[:, 3:3 + S], zT[R_LRU_X + j * 128:R_LRU_X + (j + 1) * 128, :], reads=u_zT[0 + j], writes=[X])
                    wc = 16 + j * 4
                    ts("dve", C[:, 0:S], X[:, 0:S], pv(wc), pv(24 + j), ALU.mult, ALU.add, [X, pv_all], [C])
                    for k in range(1, 4):
                        stt(C[:, 0:S], X[:, k:k + S], pv(wc + k), C[:, 0:S], ALU.mult, ALU.add, [X, pv_all, C], [C])
                    cp("pool", xcb[:], C[:, 0:S], [C], [xcb])
                    for tt_ in range(8):
                        t0 = tt_ * 512
                        for g, dst in ((0, R), (1, I)):
                            ps = PS[(tt_ * 2 + g) % 4]
                            mm(ps[:], wg_b[:, g * 2 + j, :], xcb[:, t0:t0 + 512], True, True, [wg_b, xcb], [ps])
                            act(dst[:, t0:t0 + 512], ps[:], AF.Sigmoid, [ps, pv_all], [dst], bias=pv(26 + g * 2 + j))
                    act(A2[:, 0:S], R[:, 0:S], AF.Exp, [R, cst], [A2], scale=cst[:, 2 + j:3 + j])
                    act(R[:, 0:S], R[:, 0:S], AF.Exp, [R, cst], [R], scale=cst[:, j:j + 1])
                    act(A2[:, 0:S], A2[:, 0:S], AF.Sqrt, [A2], [A2], scale=-1.0, bias=1.0)
                    tt("dve", I[:, 0:S], I[:, 0:S], C[:, 0:S], ALU.mult, [I, C], [I])
                    tt("dve", I[:, 0:S], I[:, 0:S], A2[:, 0:S], ALU.mult, [I, A2], [I])
                    fw.op("dve", lambda e: e.tensor_tensor_scan(out=C[:, 0:S], data0=R[:, 0:S], data1=I[:, 0:S], initial=0.0, op0=ALU.mult, op1=ALU.add), [R, I], [C])
                    fw.dma("sp", X[:, 0:S], zT[R_LRU_G + j * 128:R_LRU_G + (j + 1) * 128, :], reads=u_zT[2 + j], writes=[X])
                    act(A2[:, 0:S], X[:, 0:S], AF.Gelu_apprx_tanh, [X], [A2])
                    Yd = Y0 if j == 0 else C
                    tt("dve", Yd[:, 0:S], C[:, 0:S], A2[:, 0:S], ALU.mult, [C, A2], [Yd])
                outnorm_fm([Y0, C], [sq0, sq1], 0)
                for j in range(2):
                    Yd = Y0 if j == 0 else C
                    mset("pool", X[:, 0:2], 0.0, [X])
                    fw.dma("sp", R[:, 0:S], zT[R_SC_C + j * 128:R_SC_C + (j + 1) * 128, :], reads=u_zT[6 + j], writes=[R])
                    fw.dma("sp", I[:, 0:S], zT[R_SC_X + j * 128:R_SC_X + (j + 1) * 128, :], reads=u_zT[8 + j], writes=[I])
                    fw.dma("sp", A2[:, 0:S], zT[R_SC_B + j * 128:R_SC_B + (j + 1) * 128, :], reads=u_zT[4 + j], writes=[A2])
                    tt("dve", X[:, 2:2 + S], R[:, 0:S], I[:, 0:S], ALU.mult, [R, I], [X])
                    wc = 32 + j * 3
                    ts("dve", R[:, 0:S], X[:, 0:S], pv(wc), None, ALU.mult, None, [X, pv_all], [R])
                    for k in range(1, 3):
                        stt(R[:, 0:S], X[:, k:k + S], pv(wc + k), R[:, 0:S], ALU.mult, ALU.add, [X, pv_all, R], [R])
                    tt("dve", Yd[:, 0:S], R[:, 0:S], A2[:, 0:S], ALU.mult, [R, A2], [Yd])
                outnorm_fm([Y0, C], [sq0, sq1], 1)
                fw.barrier()
            if stop <= 2:
                break

            def headnorm(tl, src_ap, src_units, gcol, scale, dst, ntok):
                hq, hsq, gs, lnb, rstd = tl
                if src_ap is not None:
                    fw.dma("sp", hq[0:64, 0:ntok], src_ap, reads=src_units, writes=[hq])
                ts("dve", gs[:, 0:1], pv(gcol, p1=64), float(scale), None, ALU.mult, None, [pv_all], [gs])
                act(hsq[0:64, 0:ntok], hq[0:64, 0:ntok], AF.Square, [hq], [hsq])
                for t0 in range(0, ntok, 512):
                    n = min(512, ntok - t0)
                    ps = PS[5 + (t0 // 512) % 2]
                    mm(ps[0:64, 0:n], ones64[:], hsq[0:64, t0:t0 + n], True, True, [ones64, hsq], [ps])
                    rstd_from(ps[0:64, 0:n], rstd[0:64, 0:n], lnb[0:64, 0:n], [ps], lnb, rstd)
                    stt(dst[0:64, t0:t0 + n], hq[0:64, t0:t0 + n], gs[:, 0:1], rstd[0:64, 0:n], ALU.mult, ALU.mult, [hq, gs, rstd], [dst])

            def run_attention(tiles, pts):
                n_t = len(tiles)
                ring = PS[0:3]

                def emit_pre(i):
                    if i < n_t and tiles[i].get("pre") is not None:
                        bt, src = tiles[i]["pre"]
                        fw.dma("sp", bt[:], src, reads=[u_ft], writes=[bt])

                emit_pre(0)

                def emit_qk(i):
                    tl = tiles[i]
                    ps = ring[i % 3]
                    emit_pre(i + 1)
                    np_ = tl["np"]
                    last = len(tl["mms"]) - 1
                    for idx, (c0, ncol, lhsT, rhs, rds) in enumerate(tl["mms"]):
                        mm(ps[0:np_, c0:c0 + ncol], lhsT, rhs, idx == 0, idx == last, rds, [ps], skip_group_check=True)

                def emit_rest(i):
                    tl = tiles[i]
                    ps = ring[i % 3]
                    pt = pts[i % len(pts)]
                    np_, n = tl["np"], tl["n"]
                    if tl.get("bias") is not None:
                        act(pt[0:np_, 0:n], ps[0:np_, 0:n], AF.Exp, [ps] + tl["bias_reads"], [pt], bias=tl["bias"])
                    else:
                        act(pt[0:np_, 0:n], ps[0:np_, 0:n], AF.Exp, [ps], [pt])
                    if tl.get("init") is not None:
                        accT, w = tl["init"]
                        mm(accT[:, 0:w], zeros[:, 0:128], zeros[:, 0:w], True, True, [zeros], [accT])
                    for (accT, acc_ap, pc0, V_ap, rds) in tl["pv"]:
                        mm(acc_ap, pt[0:np_, pc0:pc0 + 128], V_ap, False, True, [pt] + rds, [accT], skip_group_check=True)
                    if tl.get("fin") is not None:
                        tl["fin"]()

                LA = 2
                for i in range(n_t + LA):
                    if i < n_t:
                        emit_qk(i)
                    if i >= LA:
                        emit_rest(i - LA)

            def outnorm_tok(tl, Q, yv, yT, gain_ap, rowbase):
                junk, ssq, lnq, rs, ym, mst2 = tl
                q0 = Q * 512
                for s in range(4):
                    act(junk[:, 0:256], yv(s), AF.Square, [yT], [junk, ssq], accum_out=ssq[:, s:s + 1])
                act(lnq[:, 0:4], ssq[:, 0:4], AF.Ln, [ssq], [lnq], scale=1.0 / 256, bias=EPS)
                act(rs[:, 0:4], lnq[:, 0:4], AF.Exp, [lnq], [rs], scale=-0.5)
                for s in range(4):
                    stt(ym[:, s, :], yv(s), rs[:, s:s + 1], gain_ap, ALU.mult, ALU.mult, [yT, rs, brow_all], [ym])
                for j in range(2):
                    for s in range(4):
                        fw.op("pe", lambda e, s=s, j=j: e.transpose(out=PSB[:, s * 128:(s + 1) * 128], in_=ym[:, s, j * 128:(j + 1) * 128], identity=ident[:]), [ym, ident], [PSB])
                    m_ = mst2[j]
                    cp("dve", m_[:], PSB[:, 0:512], [PSB], [m_])
                    fw.dma("pool", mixT[rowbase + j * 128:rowbase + (j + 1) * 128, q0:q0 + 512], m_[:], reads=[m_], writes=[u_mix[rowbase // 128 + j][Q]])

            with ExitStack() as ph:
                Qp = [sb(ph, "Qp%d" % h, [128, S], BF16) for h in range(4)]
                Kp = [sb(ph, "Kp%d" % h, [128, S], BF16) for h in range(4)]
                with ExitStack() as ph2:
                    fr = sb(ph2, "fr", [4, S])
                    e1 = sb(ph2, "e1", [4, S])
                    cc = sb(ph2, "cc", [4, S])
                    ones4 = sb(ph2, "ones4", [4, S], BF16)
                    nb = sb(ph2, "nb", [4, 1])
                    csp = sb(ph2, "csp", [4, 3, S], BF16)
                    ncsp = sb(ph2, "ncsp", [4, 3, S], BF16)
                    fw.dma("sp", fr[:], zT[R_FOX_F:R_FOX_F + 4, :], reads=u_zT[18], writes=[fr])
                    ts("dve", nb[:], pv(48, p1=4), -1.0, None, ALU.mult, None, [pv_all], [nb])
                    act(e1[:], fr[:], AF.Exp, [fr, nb], [e1], scale=-1.0, bias=nb[:])
                    act(e1[:], e1[:], AF.Ln, [e1], [e1], bias=1.0)
                    ts("dve", e1[:], e1[:], -1.0, None, ALU.mult, None, [e1], [e1])
                    mset("pool", ones4[:], 1.0, [ones4])
                    fw.op("dve", lambda e: e.tensor_tensor_scan(out=cc[:], data0=ones4[:], data1=e1[:], initial=0.0, op0=ALU.mult, op1=ALU.add), [ones4, e1], [cc])
                    cp("dve", csp[:, 0, :], cc[:], [cc], [csp])
                    cp("dve", fr[:], csp[:, 0, :], [csp], [fr])
                    tt("dve", cc[:], cc[:], fr[:], ALU.subtract, [cc, fr], [cc])
                    cp("dve", csp[:, 1, :], cc[:], [cc], [csp])
                    cp("dve", fr[:], csp[:, 1, :], [csp], [fr])
                    tt("dve", cc[:], cc[:], fr[:], ALU.subtract, [cc, fr], [cc])
                    cp("dve", csp[:, 2, :], cc[:], [cc], [csp])
                    ts("dve", ncsp[:], csp[:], -1.0, None, ALU.mult, None, [csp], [ncsp])
                    for h in range(4):
                        mset("pool", Qp[h][64:128, :], 0.0, [Qp[h]])
                        mset("pool", Kp[h][64:128, :], 0.0, [Kp[h]])
                        for i in range(3):
                            fw.dma("sp", Qp[h][64 + i:65 + i, :], csp[h:h + 1, i, :], reads=[csp], writes=[Qp[h]])
                            fw.dma("sp", Kp[h][96 + i:97 + i, :], ncsp[h:h + 1, i, :], reads=[ncsp], writes=[Kp[h]])
                        fw.dma("sp", Qp[h][96:99, :], ones4[0:3, :], reads=[ones4], writes=[Qp[h]])
                        fw.dma("sp", Kp[h][64:67, :], ones4[0:3, :], reads=[ones4], writes=[Kp[h]])
                    fw.barrier()
                V1 = sb(ph, "V1", [128, 32, 4, 65], BF16)
                hq = sb(ph, "hq", [64, S])
                hsq = sb(ph, "hsq", [64, S], BF16)
                gs = sb(ph, "gs", [64, 1])
                lnb = sb(ph, "lnb3", [64, 512])
                rstd = sb(ph, "rstd3", [64, 512])
                hn = (hq, hsq, gs, lnb, rstd)
                vst = sb(ph, "vst", [128, 8, 256])
                pts = [sb(ph, "pt%d" % i, [128, 512], BF16) for i in range(3)]
                rden = [sb(ph, "rden%d" % i, [128, 4]) for i in range(2)]
                ytk = [sb(ph, "ytk%d" % i, [128, 4, 256]) for i in range(2)]
                on_tl = (sb(ph, "junk", [128, 256]), sb(ph, "ssq", [128, 4]), sb(ph, "lnq", [128, 4]), sb(ph, "rs", [128, 4]),
                         sb(ph, "ym", [128, 4, 256], BF16), [sb(ph, "mst2_%d" % i, [128, 512], BF16) for i in range(2)])
                for h in range(4):
                    headnorm(hn, zT[R_FOX_Q + h * 64:R_FOX_Q + (h + 1) * 64, :], u_zT[10 + h // 2], 42, 0.125, Qp[h], S)
                    headnorm(hn, zT[R_FOX_K + h * 64:R_FOX_K + (h + 1) * 64, :], u_zT[12 + h // 2], 43, 1.0, Kp[h], S)
                mset("pool", V1[:, :, :, 64:65], 1.0, [V1])
                for g in range(4):
                    fw.dma("sp", vst[:], ztok[g * 1024:(g + 1) * 1024, T_FOX_V:T_FOX_V + 256].rearrange("(k p) c -> p k c", p=128), reads=u_ztok[g * 8:(g + 1) * 8], writes=[vst])
                    for h in range(4):
                        cp("pool" if h % 2 else "dve", V1[:, g * 8:(g + 1) * 8, h, 0:64], vst[:, :, h * 64:(h + 1) * 64], [vst], [V1])
                accn = 0
                for Q in range(8):
                    q0 = Q * 512
                    yT = ytk[Q % 2]
                    tiles = []
                    for h in range(4):
                        accT = PS[3 + accn % 2]
                        rd_ = rden[accn % 2]
                        accn += 1
                        nk = 4 * Q + 4
                        for kt in range(nk):
                            k0 = kt * 128
                            d = kt - 4 * Q
                            dd = max(d, 0)
                            n = 512 - 128 * dd
                            mms = [(0, n, Kp[h][:, k0:k0 + 128], Qp[h][:, q0 + 128 * dd:q0 + 512], [Kp[h], Qp[h]])]
                            if d >= 0:
                                mms.append((0, 128, ident[:], tri[:], [ident, tri]))
                            tl = dict(np=128, n=n, mms=mms, pv=[])
                            for s in range(dd, 4):
                                tl["pv"].append((accT, accT[:, s * 65:(s + 1) * 65], (s - dd) * 128, V1[:, kt, h, :], [V1]))
                            if kt == 0:
                                tl["init"] = (accT, 260)
                            if kt == nk - 1:
                                def fin(accT=accT, rd_=rd_, h=h, yT=yT):
                                    accv = accT[:, 0:260].rearrange("p (s c) -> p s c", c=65)
                                    fw.op("dve", lambda e: e.reciprocal(out=rd_[:, 0:4], in_=accv[:, :, 64]), [accT], [rd_])
                                    for s in range(4):
                                        ts("dve", yT[:, s, h * 64:(h + 1) * 64], accv[:, s, 0:64], rd_[:, s:s + 1], None, ALU.mult, None, [accT, rd_], [yT])
                                tl["fin"] = fin
                            tiles.append(tl)
                    run_attention(tiles, pts)
                    outnorm_tok(on_tl, Q, lambda s, yT=yT: yT[:, s, :], yT, brow_all[:, l, 0:256], 512)
                fw.barrier()
            if stop <= 3:
                break

            with ExitStack() as ph:
                Qn = [sb(ph, "Qn%d" % h, [128, S], BF16) for h in range(4)]
                KsT = sb(ph, "KsT", [128, S], BF16)
                KwT = sb(ph, "KwT", [128, S], BF16)
                KcT = sb(ph, "KcT", [128, 256], BF16)
                for h in range(4):
                    mset("pool", Qn[h][64:128, :], 0.0, [Qn[h]])
                mset("pool", KwT[64:128, :], 0.0, [KwT])
                mset("pool", KcT[64:128, :], 0.0, [KcT])
                fw.dma("sp", KsT[64:128, :], cE_d[:, :], writes=[KsT])
                VS1 = sb(ph, "VS1", [128, 32, 65], BF16)
                VW1 = sb(ph, "VW1", [128, 32, 65], BF16)
                VC1 = sb(ph, "VC1", [128, 2, 128], BF16)
                G = sb(ph, "G", [128, 32, 12])
                yd = sb(ph, "yd", [128, 32, 256])
                hq = sb(ph, "hq5", [64, 4112])
                hsq = sb(ph, "hsq5", [64, 4112], BF16)
                gs = sb(ph, "gs5", [64, 1])
                lnb = sb(ph, "lnb5", [64, 512])
                rstd = sb(ph, "rstd5", [64, 512])
                hn = (hq, hsq, gs, lnb, rstd)
                w1b = sb(ph, "w1b", [64, 32, 128], BF16)
                posb = sb(ph, "posb", [64, 32], BF16)
                w2b = sb(ph, "w2b", [128, 64], BF16)
                hbias = sb(ph, "hbias", [128, 1])
                hidb = sb(ph, "hidb", [128, 256], BF16)
                vst2 = sb(ph, "vst2", [128, 32, 64])
                gst = sb(ph, "gst", [128, 32, 12])
                pts = [sb(ph, "ptn%d" % i, [128, 512], BF16) for i in range(3)]
                btile = [sb(ph, "btile%d" % i, [128, 512], BF16) for i in range(3)]
                den = [sb(ph, "den%d" % i, [128, 4]) for i in range(2)]
                rden = [sb(ph, "rdenn%d" % i, [128, 4]) for i in range(2)]
                coef = [sb(ph, "coef%d" % i, [128, 4]) for i in range(2)]
                impacc = sb(ph, "impacc", [128, 4, 64])
                imp2 = sb(ph, "imp2", [128, 64])
                m8 = sb(ph, "m8", [128, 8])
                m8b = sb(ph, "m8b", [128, 8])
                self_ = sb(ph, "self", [128, 64])
                selb = sb(ph, "selb", [128, 128], BF16)
                on_tl = (sb(ph, "junk5", [128, 256]), sb(ph, "ssq5", [128, 4]), sb(ph, "lnq5", [128, 4]), sb(ph, "rs5", [128, 4]),
                         sb(ph, "ym5", [128, 4, 256], BF16), [sb(ph, "mst5_%d" % i, [128, 512], BF16) for i in range(2)])
                for h in range(4):
                    headnorm(hn, zT[R_NSA_Q + h * 64:R_NSA_Q + (h + 1) * 64, :], u_zT[14 + h // 2], 44, 0.125, Qn[h], S)
                headnorm(hn, zT[R_KS:R_KS + 64, :], u_zT[17], 46, 1.0, KsT, S)
                headnorm(hn, zT[R_KW:R_KW + 64, :], u_zT[17], 47, 1.0, KwT, S)
                for kv in range(2):
                    rows = R_KC if kv == 0 else R_VC
                    fw.dma("sp", hq[0:64, 0:S], zT[rows:rows + 64, :], reads=u_zT[16], writes=[hq])
                    mset("pool", hq[0:64, S:4112], 0.0, [hq])
                    cp("dve", hsq[0:64, :], hq[0:64, :], [hq], [hsq])
                    fw.dma("pool", w1b[:], cw1_d[l, kv].rearrange("l d m -> d l m"), writes=[w1b])
                    fw.dma("pool", posb[:], cposT_d[l, kv], writes=[posb])
                    fw.dma("pool", w2b[:], cw2_d[l, kv], writes=[w2b])
                    ps = PS[5]
                    for li in range(32):
                        mm(ps[:, 0:1], w1b[:, li, :], posb[:, li:li + 1], li == 0, li == 31, [w1b, posb], [ps])
                    cp("dve", hbias[:], ps[:, 0:1], [ps], [hbias])
                    ps = PS[6]
                    for li in range(32):
                        if li < 16:
                            rhs = hsq[0:64, 0:4096].rearrange("p (n r) -> p n r", r=16)[:, :, li]
                        else:
                            rhs = hsq[0:64, 16:4112].rearrange("p (n r) -> p n r", r=16)[:, :, li - 16]
                        mm(ps[:, 0:256], w1b[:, li, :], rhs, li == 0, li == 31, [w1b, hsq], [ps])
                    act(hidb[:], ps[:, 0:256], AF.Gelu_apprx_tanh, [ps, hbias], [hidb], bias=hbias[:])
                    if kv == 0:
                        ps = PS[5]
                        mm(ps[0:64, 0:256], w2b[:], hidb[:], True, True, [w2b, hidb], [ps])
                        cp("dve", hq[0:64, 0:256], ps[0:64, 0:256], [ps], [hq])
                        headnorm(hn, None, None, 45, 1.0, KcT, 256)
                    else:
                        for c in range(2):
                            ps = PS[5]
                            mm(ps[:, 0:64], hidb[:, c * 128:(c + 1) * 128], w2b[:], True, True, [w2b, hidb], [ps])
                            cp("dve", VC1[:, c, 0:64], ps[:, 0:64], [ps], [VC1])
                            cp("dve", VC1[:, c, 64:128], ovl[:, c * 64:(c + 1) * 64], [ovl], [VC1])
                for (VT, col) in ((VS1, T_VS), (VW1, T_VW)):
                    fw.dma("sp", vst2[:], ztok[:, col:col + 64].rearrange("(k p) c -> p k c", p=128), reads=u_ztok, writes=[vst2])
                    cp("dve", VT[:, :, 0:64], vst2[:], [vst2], [VT])
                    mset("pool", VT[:, :, 64:65], 1.0, [VT])
                fw.dma("sp", gst[:], ztok[:, T_G:T_G + 12].rearrange("(k p) c -> p k c", p=128), reads=u_ztok, writes=[gst])
                tt("dve", gst[:], gst[:], brow_all[:, l, 512:524].unsqueeze(1).to_broadcast([128, 32, 12]), ALU.add, [gst, brow_all], [gst])
                act(G[:], gst[:], AF.Sigmoid, [gst], [G])
                mset("pool", selb[:], 0.0, [selb])

                accn = 0
                bn = 0
                for Q in range(8):
                    q0 = Q * 512

                    def fin_generic(accT, w, h, branch, first, dn, rd_, cf_, Q=Q):
                        accv = accT[:, 0:4 * w].rearrange("p (s c) -> p s c", c=w)
                        if branch == 0:
                            fw.op("dve", lambda e: e.tensor_reduce(out=dn[:, 0:4], in_=accv[:, :, 64:128], axis=mybir.AxisListType.X, op=ALU.add), [accT], [dn])
                            ts("dve", dn[:, 0:4], dn[:, 0:4], 1.0 / 32, 1e-30, ALU.mult, ALU.max, [dn], [dn])
                            fw.op("dve", lambda e: e.reciprocal(out=rd_[:, 0:4], in_=dn[:, 0:4]), [dn], [rd_])
                        else:
                            fw.op("dve", lambda e: e.reciprocal(out=rd_[:, 0:4], in_=accv[:, :, 64]), [accT], [rd_])
                        tt("dve", cf_[:, 0:4], rd_[:, 0:4], G[:, 4 * Q:4 * Q + 4, branch * 4 + h], ALU.mult, [rd_, G], [cf_])
                        for s in range(4):
                            ydv = yd[:, 4 * Q + s, h * 64:(h + 1) * 64]
                            if first:
                                ts("dve", ydv, accv[:, s, 0:64], cf_[:, s:s + 1], None, ALU.mult, None, [accT, cf_], [yd])
                            else:
                                stt(ydv, accv[:, s, 0:64], cf_[:, s:s + 1], ydv, ALU.mult, ALU.add, [accT, cf_, yd], [yd])
                        if branch == 0:
                            for s in range(4):
                                if h == 0:
                                    ts("dve", impacc[:, s, :], accv[:, s, 64:128], rd_[:, s:s + 1], None, ALU.mult, None, [accT, rd_], [impacc])
                                else:
                                    stt(impacc[:, s, :], accv[:, s, 64:128], rd_[:, s:s + 1], impacc[:, s, :], ALU.mult, ALU.add, [accT, rd_, impacc], [impacc])

                    tiles = []
                    for h in range(4):
                        accT = PS[3 + accn % 2]
                        dn, rd_, cf_ = den[accn % 2], rden[accn % 2], coef[accn % 2]
                        accn += 1
                        chunks = [0, 1] if Q >= 4 else [0]
                        for c in chunks:
                            bt = btile[bn % 3]
                            bn += 1
                            off = h * 8208 + ZC + q0 - 31 - 16 * (c * 128 + 127)
                            src = bass.AP(tensor=ftc_h, offset=off, ap=[[16, 128], [1, 512]])
                            tl = dict(np=128, n=512, pv=[])
                            tl["pre"] = (bt, src)
                            tl["mms"] = [(0, 512, KcT[:, c * 128:(c + 1) * 128], Qn[h][:, q0:q0 + 512], [KcT, Qn[h]]),
                                         (0, 512, Jm[:], bt[:], [Jm, bt])]
                            for s in range(4):
                                tl["pv"].append((accT, accT[:, s * 128:(s + 1) * 128], s * 128, VC1[:, c, :], [VC1]))
                            if c == 0:
                                tl["init"] = (accT, 512)
                            if c == chunks[-1]:
                                tl["fin"] = (lambda accT=accT, h=h, dn=dn, rd_=rd_, cf_=cf_: fin_generic(accT, 128, h, 0, True, dn, rd_, cf_))
                            tiles.append(tl)
                    run_attention(tiles, pts)

                    tiles = []
                    for h in range(4):
                        accT = PS[3 + accn % 2]
                        dn, rd_, cf_ = den[accn % 2], rden[accn % 2], coef[accn % 2]
                        accn += 1
                        kts = [kt for kt in range(4 * Q - 4, 4 * Q + 4) if kt >= 0]
                        for kt in kts:
                            k0 = kt * 128
                            d = kt - 4 * Q
                            dd = max(d, 0)
                            n = 512 - 128 * dd
                            sc0 = 128 * (-d) if d < 0 else 0
                            tl = dict(np=128, n=n, pv=[])
                            tl["mms"] = [(0, n, KwT[:, k0:k0 + 128], Qn[h][:, q0 + 128 * dd:q0 + 512], [KwT, Qn[h]]),
                                         (0, n, Jm[:], slab_w[:, h, sc0:sc0 + n], [Jm, slab_w])]
                            for s in range(dd, 4):
                                tl["pv"].append((accT, accT[:, s * 65:(s + 1) * 65], (s - dd) * 128, VW1[:, kt, :], [VW1]))
                            if kt == kts[0]:
                                tl["init"] = (accT, 260)
                            if kt == kts[-1]:
                                tl["fin"] = (lambda accT=accT, h=h, dn=dn, rd_=rd_, cf_=cf_: fin_generic(accT, 65, h, 2, False, dn, rd_, cf_))
                            tiles.append(tl)
                    run_attention(tiles, pts)

                    for s in range(4):
                        i_ = 4 * Q + s
                        iv = impacc[:, s, :]
                        if 2 * i_ + 2 < 64:
                            mset("dve", impacc[:, s, 2 * i_ + 2:64], -1e30, [impacc])
                        mset("dve", impacc[0:64, s, 2 * i_ + 1:2 * i_ + 2], -1e30, [impacc])
                        mset("dve", impacc[64:128, s, 2 * i_ + 1:2 * i_ + 2], 1e30, [impacc])
                        mset("dve", impacc[:, s, 2 * i_:2 * i_ + 1], 1e30, [impacc])
                        if i_ >= 1:
                            mset("dve", impacc[0:64, s, 2 * i_ - 1:2 * i_], 1e30, [impacc])
                        mset("dve", impacc[:, s, 0:1], 1e30, [impacc])
                        fw.op("dve", lambda e, iv=iv: e.max(out=m8[:], in_=iv), [impacc], [m8])
                        fw.op("dve", lambda e, iv=iv: e.match_replace(out=imp2[:], in_to_replace=m8[:], in_values=iv, imm_value=-3e38), [impacc, m8], [imp2])
                        fw.op("dve", lambda e: e.max(out=m8b[:], in_=imp2[:]), [imp2], [m8b])
                        ts("dve", self_[:], iv, m8b[:, 7:8], -1.0, ALU.is_ge, ALU.add, [impacc, m8b], [self_])
                        ts("dve", selb[:, 64:128], self_[:], -NEGM, None, ALU.mult, None, [self_], [selb])
                        fw.op("pe", lambda e, s=s: e.transpose(out=PSB[:, s * 128:(s + 1) * 128], in_=selb[:], identity=ident[:]), [selb, ident], [PSB])
                    for h in range(4):
                        cp("dve", Qn[h][64:128, q0:q0 + 512], PSB[64:128, 0:512], [PSB], [Qn[h]])

                    tiles = []
                    for h in range(4):
                        accT = PS[3 + accn % 2]
                        dn, rd_, cf_ = den[accn % 2], rden[accn % 2], coef[accn % 2]
                        accn += 1
                        nk = 4 * Q + 4
                        for kt in range(nk):
                            k0 = kt * 128
                            d = kt - 4 * Q
                            dd = max(d, 0)
                            n = 512 - 128 * dd
                            tl = dict(np=128, n=n, pv=[])
                            tl["mms"] = [(0, n, KsT[:, k0:k0 + 128], Qn[h][:, q0 + 128 * dd:q0 + 512], [KsT, Qn[h]])]
                            if d >= -1:
                                sc0 = 128 if d == -1 else 0
                                tl["mms"].append((0, n, Jm[:], slab_s[:, h, sc0:sc0 + n], [Jm, slab_s]))
                            else:
                                tl["bias"] = rb31[:, h:h + 1]
                                tl["bias_reads"] = [rb31]
                            for s in range(dd, 4):
                                tl["pv"].append((accT, accT[:, s * 65:(s + 1) * 65], (s - dd) * 128, VS1[:, kt, :], [VS1]))
                            if kt == 0:
                                tl["init"] = (accT, 260)
                            if kt == nk - 1:
                                tl["fin"] = (lambda accT=accT, h=h, dn=dn, rd_=rd_, cf_=cf_: fin_generic(accT, 65, h, 1, False, dn, rd_, cf_))
                            tiles.append(tl)
                    run_attention(tiles, pts)
                    outnorm_tok(on_tl, Q, lambda s, Q=Q: yd[:, 4 * Q + s, :], yd, brow_all[:, l, 256:512], 768)
                fw.barrier()
            if stop <= 4:
                break

            with ExitStack() as ph:
                wgu = sb(ph, "wgu", [128, 8, 2 * DFF], BF16)
                wdn = sb(ph, "wdn", [128, 22, 1024], BF16)
                wgu_u = [[U(), U()] for _ in range(8)]
                wdn_u = [U() for _ in range(22)]
                with ExitStack() as ph2:
                    wo = sb(ph2, "wo", [128, 8, 1024], BF16)
                    for c in range(8):
                        fw.dma("pool", wo[:, c, :], w_out[l, c * 128:(c + 1) * 128, :], writes=[wo])
                    xt = [sb(ph2, "xto%d" % i, [128, 8, 512]) for i in range(1)]
                    mt = [sb(ph2, "mt%d" % i, [128, 8, 512], BF16) for i in range(1)]
                    xo = [sb(ph2, "xo%d" % i, [128, 512]) for i in range(4)]
                    ev = 0
                    for tt_ in range(8):
                        t0 = tt_ * 512
                        xt_, mt_ = xt[0], mt[0]
                        rd = [u_xres[tt_]] if l > 0 else []
                        fw.dma("sp", xt_[:], x_src[:, t0:t0 + 512].rearrange("(c p) t -> p c t", p=128), reads=rd, writes=[xt_])
                        fw.dma("sp", mt_[:], mixT[:, t0:t0 + 512].rearrange("(c p) t -> p c t", p=128), reads=[u_mix[c][tt_] for c in range(8)], writes=[mt_])
                        if tt_ == 0:
                            for c in range(8):
                                for hh in range(2):
                                    fw.dma("pool", wgu[:, c, hh * DFF:(hh + 1) * DFF], w_gu[l, c * 128:(c + 1) * 128, hh * DFF:(hh + 1) * DFF], writes=[wgu_u[c][hh]])
                            for c in range(22):
                                fw.dma("pool", wdn[:, c, :], w_dn[l, c * 128:(c + 1) * 128, :], writes=[wdn_u[c]])
                        for n_ in range(8):
                            ps = PS[ev % 3]
                            for c in range(8):
                                mm(ps[:], wo[:, c, n_ * 128:(n_ + 1) * 128], mt_[:, c, :], c == 0, c == 7, [wo, mt_], [ps])
                            xo_ = xo[ev % 4]
                            tt("dve", xo_[:], xt_[:, n_, :], ps[:], ALU.add, [xt_, ps], [xo_])
                            fw.dma("sp", xres[n_ * 128:(n_ + 1) * 128, t0:t0 + 512], xo_[:], reads=[xo_], writes=[u_xres[tt_]])
                            ev += 1
                    fw.barrier()
                if stop <= 5:
                    break
                NT = 256
                xt = [sb(ph, "xtf%d" % i, [128, 8, NT]) for i in range(2)]
                xn = [sb(ph, "xnf%d" % i, [128, 8, NT], BF16) for i in range(2)]
                sq = sb(ph, "sqf", [128, 8, NT], BF16)
                lnb = sb(ph, "lnbf", [128, NT])
                rstd = sb(ph, "rstdf", [128, NT])
                hT = sb(ph, "hT", [128, 22, NT], BF16)
                sg = [sb(ph, "sg%d" % i, [128, NT]) for i in range(2)]
                xo = [sb(ph, "xof%d" % i, [128, NT]) for i in range(4)]
                ev = 0
                NTL = S // NT

                def norm9(tt_):
                    t0 = tt_ * NT
                    xt_, xn_ = xt[tt_ % 2], xn[tt_ % 2]
                    fw.dma("sp", xt_[:], xres[:, t0:t0 + NT].rearrange("(c p) t -> p c t", p=128), reads=[u_xres[t0 // 512]], writes=[xt_])
                    act(sq[:], xt_[:], AF.Square, [xt_], [sq])
                    for c in range(8):
                        mm(PS[6][:, 0:NT], ones1024[:], sq[:, c, :], c == 0, c == 7, [ones1024, sq], [PS[6]])
                    rstd_from(PS[6][:, 0:NT], rstd[:], lnb[:], [PS[6]], lnb, rstd)
                    for c in range(8):
                        stt(xn_[:, c, :], xt_[:, c, :], pv(8 + c), rstd[:], ALU.mult, ALU.mult, [xt_, rstd, pv_all], [xn_])

                norm9(0)
                for tt_ in range(NTL):
                    t0 = tt_ * NT
                    xt_, xn_ = xt[tt_ % 2], xn[tt_ % 2]
                    ux = u_xres[t0 // 512]
                    for j in range(22):
                        psg = PS[(2 * j) % 4]
                        psu = PS[(2 * j + 1) % 4]
                        for c in range(8):
                            mm(psg[:, 0:NT], wgu[:, c, j * 128:(j + 1) * 128], xn_[:, c, :], c == 0, c == 7, [wgu_u[c][0], xn_], [psg])
                        for c in range(8):
                            mm(psu[:, 0:NT], wgu[:, c, DFF + j * 128:DFF + (j + 1) * 128], xn_[:, c, :], c == 0, c == 7, [wgu_u[c][1], xn_], [psu])
                        sg_ = sg[j % 2]
                        act(sg_[:], psg[:, 0:NT], AF.Silu, [psg], [sg_])
                        tt("dve", hT[:, j, :], sg_[:], psu[:, 0:NT], ALU.mult, [sg_, psu], [hT])
                    if tt_ + 1 < NTL:
                        norm9(tt_ + 1)
                    for n_ in range(8):
                        ps = PS[4 + n_ % 2]
                        for j in range(22):
                            mm(ps[:, 0:NT], wdn[:, j, n_ * 128:(n_ + 1) * 128], hT[:, j, :], j == 0, j == 21, [wdn_u[j], hT], [ps])
                        xo_ = xo[ev % 4]
                        tt("dve", xo_[:], xt_[:, n_, :], ps[:, 0:NT], ALU.add, [xt_, ps], [xo_])
                        fw.dma("sp", x_dst[n_ * 128:(n_ + 1) * 128, t0:t0 + NT], xo_[:], reads=[xo_], writes=[ux] if x_dst is xres else [])
                        ev += 1
                fw.barrier()
        fw.barrier()
    return nc


def _bucket(d):
    d = np.maximum(d, 0)
    large = 16 + (np.log(np.maximum(d, 1).astype(np.float32) / np.float32(16)) / np.float32(np.log(128 / 16)) * np.float32(16)).astype(np.int32)
    large = np.minimum(large, 31)
    return np.where(d < 16, d, large)


def _host_consts():
    bf = ml_dtypes.bfloat16
    c = {}
    c["cident"] = np.eye(128, dtype=np.float32).astype(bf)
    c["cJ"] = np.eye(128, dtype=np.float32)[::-1].copy().astype(bf)
    kl = np.arange(128)[:, None]
    ql = np.arange(128)[None, :]
    c["ctri"] = np.where(ql >= kl, 0.0, NEGM).astype(np.float32).astype(bf)
    E = np.zeros((64, S), np.float32)
    E[np.arange(S) // 64, np.arange(S)] = 1.0
    c["cE"] = E.astype(bf)
    n = np.arange(256)
    m = np.arange(64)
    cs = n[:, None] * 16
    ss = m[None, :] * 64
    ov = np.clip(np.minimum(cs + 32, ss + 64) - np.maximum(cs, ss), 0, 32).astype(np.float32)
    ov[255] = 0
    c["covl"] = np.concatenate([ov[0:128], ov[128:256]], axis=1).astype(bf)
    d = np.arange(8208) - ZC
    oh = np.zeros((33, 8208), np.float32)
    b = _bucket(d)
    oh[b[d >= 0], np.nonzero(d >= 0)[0]] = 1.0
    oh[32, d < 0] = 1.0
    c["ohc"] = oh.astype(bf)
    d = np.arange(1152) - ZW
    oh = np.zeros((33, 1152), np.float32)
    b = _bucket(d)
    ok = (d >= 0) & (d < 512)
    oh[b[ok], np.nonzero(ok)[0]] = 1.0
    oh[32, ~ok] = 1.0
    c["ohw"] = oh.astype(bf)
    return c


def _host_layout(inp):
    f = np.float32
    C_LRU_X, C_LRU_G, C_SC_B, C_SC_C, C_SC_X = 0, 256, 512, 768, 1024
    C_FOX_Q, C_FOX_K, C_FOX_V, C_FOX_F, C_NSA_Q, C_NSA_KV, C_NSA_G = 1280, 1536, 1792, 2048, 2052, 2308, 2692
    r = lambda a, n: list(range(a, a + n))
    perm = (r(C_LRU_X, 256) + r(C_LRU_G, 256) + r(C_SC_B, 256) + r(C_SC_C, 256) + r(C_SC_X, 256)
            + r(C_FOX_Q, 256) + r(C_FOX_K, 256) + r(C_NSA_Q, 256)
            + r(C_NSA_KV + 0, 64) + r(C_NSA_KV + 64, 64) + r(C_NSA_KV + 128, 64) + r(C_NSA_KV + 256, 64)
            + r(C_FOX_F, 4)
            + r(C_FOX_V, 256) + r(C_NSA_KV + 192, 64) + r(C_NSA_KV + 320, 64) + r(C_NSA_G, 12))
    assert len(perm) == 2704 and len(set(perm)) == 2704
    m = {}
    m["w_in"] = np.ascontiguousarray(np.asarray(inp["w_in"], f)[:, :, perm])
    m["w_out"] = np.ascontiguousarray(np.asarray(inp["w_out"], f))
    m["w_gu"] = np.ascontiguousarray(np.asarray(inp["w_gate_up"], f))
    m["w_dn"] = np.ascontiguousarray(np.asarray(inp["w_down"], f))
    pvec = np.zeros((NL, 128, 64), f)
    brow = np.zeros((NL, 1, 524), f)
    wbd = np.zeros((NL, 2, 2, 128, 128), f)
    for l in range(NL):
        pvec[l, :, 0:8] = np.asarray(inp["norm_mix"][l]).reshape(8, 128).T
        pvec[l, :, 8:16] = np.asarray(inp["norm_ffn"][l]).reshape(8, 128).T
        for j in range(2):
            sl = slice(j * 128, (j + 1) * 128)
            pvec[l, :, 16 + j * 4:20 + j * 4] = np.asarray(inp["lru_conv_w"][l])[:, sl].T
            pvec[l, :, 24 + j] = np.asarray(inp["lru_conv_b"][l])[sl]
            for g in range(2):
                pvec[l, :, 26 + g * 2 + j] = np.asarray(inp["lru_b_gates"][l])[g, sl]
                for bb in range(2):
                    wbd[l, g, j, bb * 64:(bb + 1) * 64, bb * 64:(bb + 1) * 64] = np.asarray(inp["lru_w_gates"][l])[g, 2 * j + bb]
            pvec[l, :, 30 + j] = np.asarray(inp["lru_lambda"][l])[sl]
            pvec[l, :, 32 + j * 3:35 + j * 3] = np.asarray(inp["sc_conv_w"][l])[:, sl].T
            for grp in range(2):
                pvec[l, :, 38 + grp * 2 + j] = np.asarray(inp["out_norm"][l])[grp, sl]
        pvec[l, 0:64, 42] = np.asarray(inp["fox_qk_norm"][l])[0]
        pvec[l, 0:64, 43] = np.asarray(inp["fox_qk_norm"][l])[1]
        for i in range(4):
            pvec[l, 0:64, 44 + i] = np.asarray(inp["nsa_qk_norm"][l])[i]
        pvec[l, 0:4, 48] = np.asarray(inp["fox_f_bias"][l])
        brow[l, 0, 0:256] = np.asarray(inp["out_norm"][l])[2]
        brow[l, 0, 256:512] = np.asarray(inp["out_norm"][l])[3]
        brow[l, 0, 512:524] = np.asarray(inp["nsa_gate_bias"][l])
    m["pvec"] = pvec
    m["brow"] = brow
    m["wbd"] = wbd
    m["cw1"] = np.ascontiguousarray(np.asarray(inp["nsa_cmp_w1"], f))
    m["cw2"] = np.ascontiguousarray(np.asarray(inp["nsa_cmp_w2"], f))
    m["cposT"] = np.ascontiguousarray(np.asarray(inp["nsa_cmp_pos"], f).transpose(0, 1, 3, 2))
    rb = np.zeros((33, 4), f)
    rb[0:32] = np.asarray(inp["rel_bias"], f)
    rb[32] = NEGM
    m["rb33"] = rb
    m.update(_host_consts())
    return m


def kernel(**inputs):
    x = np.asarray(inputs["x"], np.float32)
    shared = _host_layout(inputs)
    nc = build()
    in_maps = []
    for b in range(8):
        d = dict(shared)
        d["xT"] = np.ascontiguousarray(x[b].T)
        in_maps.append(d)
    res = run_bass_kernel_spmd(nc, in_maps, core_ids=list(range(8)))
    out = np.stack([np.ascontiguousarray(res.results[b]["out"].T) for b in range(8)], axis=0)
    return out.astype(np.float32)
```

```python
import numpy as np
import ml_dtypes
from contextlib import ExitStack
import concourse.bass as bass
import concourse.mybir as mybir
from concourse.bass_utils import run_bass_kernel_spmd

F32 = mybir.dt.float32
BF16 = mybir.dt.bfloat16
AF = mybir.ActivationFunctionType
ALU = mybir.AluOpType

S = 4096
D = 1024
NL = 2
DFF = 2816
NFM = 2308
NTM = 396
EPS = 1e-6
NEGM = -30000.0
R_LRU_X, R_LRU_G, R_SC_B, R_SC_C, R_SC_X = 0, 256, 512, 768, 1024
R_FOX_Q, R_FOX_K, R_NSA_Q, R_KC, R_VC, R_KS, R_KW, R_FOX_F = 1280, 1536, 1792, 2048, 2112, 2176, 2240, 2304
T_FOX_V, T_VS, T_VW, T_G = 0, 256, 320, 384
ZC = 4112
ZW = 128


class U:
    __slots__ = ("lastw", "readers")

    def __init__(self):
        self.lastw = None
        self.readers = []


class T:
    def __init__(self, t):
        self.t = t
        self.u = U()

    def __getitem__(self, k):
        return self.t[k]


class FW:
    def __init__(self, nc, es, ndma=24):
        self.nc = nc
        self.engs = {}
        for name, h in [("pe", nc.tensor), ("dve", nc.vector), ("act", nc.scalar),
                        ("pool", nc.gpsimd), ("sp", nc.sync)]:
            sem = es.enter_context(nc.semaphore("sem_" + name))
            self.engs[name] = dict(h=h, sem=sem, count=0, known={})
        self.dsems = {"sp": [[es.enter_context(nc.semaphore("dqs%d" % i)), 0] for i in range(ndma)],
                      "pool": [[es.enter_context(nc.semaphore("dqp%d" % i)), 0] for i in range(ndma)]}
        self.drr = {"sp": 0, "pool": 0}
        self.ninstr = 0

    def _wait(self, en, toks):
        e = self.engs[en]
        need = {}
        for (sem, val) in toks:
            k = id(sem)
            if k not in need or need[k][1] < val:
                need[k] = (sem, val)
        for k, (sem, val) in need.items():
            if e["known"].get(k, 0) < val:
                e["h"].wait_ge(sem, val)
                e["known"][k] = val
                self.ninstr += 1

    def _deps(self, en, reads, writes):
        toks = []
        for u in reads:
            t = u.lastw
            if t is not None:
                if t[2] == en and en == "pe":
                    continue
                toks.append((t[0], t[1]))
        for u in writes:
            t = u.lastw
            same_ok = en in ("pe", "act", "dve")
            if t is not None and (t[2] != en or not same_ok):
                toks.append((t[0], t[1]))
            for r in u.readers:
                if r[2] != en or not same_ok:
                    toks.append((r[0], r[1]))
        return toks

    def _commit(self, tok, reads, writes):
        for u in writes:
            u.lastw = tok
            u.readers = []
        for u in reads:
            if u in writes:
                continue
            u.readers.append(tok)
            if len(u.readers) > 48:
                best = {}
                for r in u.readers:
                    k = id(r[0])
                    if k not in best or best[k][1] < r[1]:
                        best[k] = r
                u.readers = list(best.values())

    def op(self, en, fn, reads=(), writes=()):
        reads = [x.u if isinstance(x, T) else x for x in reads]
        writes = [x.u if isinstance(x, T) else x for x in writes]
        e = self.engs[en]
        self._wait(en, self._deps(en, reads, writes))
        ins = fn(e["h"])
        e["count"] += 1
        ins.then_inc(e["sem"], 1)
        self.ninstr += 1
        tok = (e["sem"], e["count"], en)
        self._commit(tok, reads, writes)
        return tok

    def dma(self, en, out, in_, reads=(), writes=(), **kw):
        reads = [x.u if isinstance(x, T) else x for x in reads]
        writes = [x.u if isinstance(x, T) else x for x in writes]
        e = self.engs[en]
        pool_ = self.dsems[en]
        slot = pool_[self.drr[en] % len(pool_)]
        self.drr[en] += 1
        toks = self._deps("dma", reads, writes)
        toks.append((slot[0], slot[1]))
        self._wait(en, toks)
        ins = e["h"].dma_start(out=out, in_=in_, **kw)
        slot[1] += 16
        ins.then_inc(slot[0], 16)
        self.ninstr += 1
        tok = (slot[0], slot[1], "dma")
        self._commit(tok, reads, writes)
        return tok

    def barrier(self):
        toks = []
        for n, e in self.engs.items():
            if e["count"] > 0:
                toks.append((e["sem"], e["count"]))
        for pl in self.dsems.values():
            for s in pl:
                if s[1] > 0:
                    toks.append((s[0], s[1]))
        for n in self.engs:
            self._wait(n, toks)


def build(dbg=False, nlayers=NL, stop=99):
    nc = bass.Bass("TRN2", target_bir_lowering=False)

    def din(name, shape, dt=F32):
        return nc.dram_tensor(name, list(shape), dt, kind="ExternalInput").ap()

    kind_s = "ExternalOutput" if dbg else "Internal"

    def dscr(name, shape, dt=F32):
        return nc.dram_tensor(name, list(shape), dt, kind=kind_s)

    xT = din("xT", [D, S])
    w_in = din("w_in", [NL, D, 2704])
    w_out = din("w_out", [NL, D, D])
    w_gu = din("w_gu", [NL, D, 2 * DFF])
    w_dn = din("w_dn", [NL, DFF, D])
    pvec_d = din("pvec", [NL, 128, 64])
    brow_d = din("brow", [NL, 1, 524])
    wbd_d = din("wbd", [NL, 2, 2, 128, 128])
    cw1_d = din("cw1", [NL, 2, 32, 64, 128])
    cw2_d = din("cw2", [NL, 2, 128, 64])
    cposT_d = din("cposT", [NL, 2, 64, 32])
    rb33_d = din("rb33", [33, 4])
    ohc_d = din("ohc", [33, 8208], BF16)
    ohw_d = din("ohw", [33, 1152], BF16)
    cident_d = din("cident", [128, 128], BF16)
    cJ_d = din("cJ", [128, 128], BF16)
    ctri_d = din("ctri", [128, 128], BF16)
    cE_d = din("cE", [64, S], BF16)
    covl_d = din("covl", [128, 128], BF16)
    cbd64_d = din("cbd64", [128, 128], BF16)
    out_d = nc.dram_tensor("out", [D, S], F32, kind="ExternalOutput").ap()
    zT_h = dscr("zT", [NFM, S])
    ztok_h = dscr("ztok", [S, NTM])
    mixT_h = dscr("mixT", [D, S], BF16)
    xres_h = dscr("xres", [D, S])
    ftc_h = dscr("ftc", [4, 8208], BF16)
    ftw_h = dscr("ftw", [4, 1152], BF16)
    zT, ztok, mixT, xres, ftc, ftw = (h.ap() for h in (zT_h, ztok_h, mixT_h, xres_h, ftc_h, ftw_h))
    u_zT = [[U() for _ in range(8)] for _ in range(19)]
    u_ztok = [U() for _ in range(32)]
    u_mix = [[U() for _ in range(8)] for _ in range(8)]
    u_xres = [U() for _ in range(8)]
    u_ft = U()

    with ExitStack() as es:
        fw = FW(nc, es)

        uid = [0]

        def sb(st, name, shape, dt=F32):
            uid[0] += 1
            return T(st.enter_context(nc.sbuf_tensor("%s_%d" % (name, uid[0]), list(shape), dt)))

        def psm(st, name, shape, dt=F32):
            return T(st.enter_context(nc.psum_tensor(name, list(shape), dt)))

        def act(out, in_, func, reads, writes, **kw):
            return fw.op("act", lambda e: e.activation(out=out, in_=in_, func=func, **kw), reads, writes)

        def mm(out, lhsT, rhs, start, stop, reads, writes, **kw):
            return fw.op("pe", lambda e: e.matmul(out, lhsT=lhsT, rhs=rhs, start=start, stop=stop, **kw), reads, writes)

        def tt(en, out, in0, in1, op, reads, writes):
            return fw.op(en, lambda e: e.tensor_tensor(out=out, in0=in0, in1=in1, op=op), reads, writes)

        def ts(en, out, in0, s1, s2, op0, op1, reads, writes):
            if op1 is None:
                return fw.op(en, lambda e: e.tensor_scalar(out=out, in0=in0, scalar1=s1, scalar2=None, op0=op0), reads, writes)
            return fw.op(en, lambda e: e.tensor_scalar(out=out, in0=in0, scalar1=s1, scalar2=s2, op0=op0, op1=op1), reads, writes)

        def stt(out, in0, scalar, in1, op0, op1, reads, writes):
            return fw.op("dve", lambda e: e.scalar_tensor_tensor(out=out, in0=in0, scalar=scalar, in1=in1, op0=op0, op1=op1), reads, writes)

        def cp(en, out, in_, reads, writes):
            return fw.op(en, lambda e: e.tensor_copy(out=out, in_=in_), reads, writes)

        def mset(en, ap, val, writes):
            return fw.op(en, lambda e: e.memset(ap, val), (), writes)

        ident = sb(es, "ident", [128, 128], BF16)
        Jm = sb(es, "Jm", [128, 128], BF16)
        tri = sb(es, "tri", [128, 128], BF16)
        ovl = sb(es, "ovl", [128, 128], BF16)
        bd64 = sb(es, "bd64", [128, 128], BF16)
        ones1024 = sb(es, "ones1024", [128, 128], BF16)
        ones256 = sb(es, "ones256", [128, 128], BF16)
        ones64 = sb(es, "ones64", [64, 64], BF16)
        zeros = sb(es, "zeros", [128, 512], BF16)
        slab_s = sb(es, "slab_s", [128, 4, 640], BF16)
        slab_w = sb(es, "slab_w", [128, 4, 1024], BF16)
        pv_all = sb(es, "pv_all", [128, NL, 64])
        brow_all = sb(es, "brow_all", [128, NL, 524])
        fw.dma("sp", ident[:], cident_d[:, :], writes=[ident])
        fw.dma("sp", Jm[:], cJ_d[:, :], writes=[Jm])
        fw.dma("sp", tri[:], ctri_d[:, :], writes=[tri])
        fw.dma("sp", ovl[:], covl_d[:, :], writes=[ovl])
        fw.dma("sp", bd64[:], cbd64_d[:, :], writes=[bd64])
        for l in range(NL):
            fw.dma("sp", pv_all[:, l, :], pvec_d[l], writes=[pv_all])
            fw.dma("sp", brow_all[:, l, :], brow_d[l].to_broadcast([128, 524]), writes=[brow_all])
        mset("pool", ones1024[:], 1.0 / 1024, [ones1024])
        mset("pool", ones256[:], 1.0 / 256, [ones256])
        mset("pool", ones64[:], 1.0 / 64, [ones64])
        mset("pool", zeros[:], 0.0, [zeros])

        PS = [psm(es, "ps%d" % i, [128, 512]) for i in range(7)]
        PSB = psm(es, "psb", [128, 1024], BF16)
        rb31 = sb(es, "rb31", [128, 4])
        fw.dma("sp", rb31[:], rb33_d[31:32, :].to_broadcast([128, 4]), writes=[rb31])

        with ExitStack() as ph:
            rb_f = sb(ph, "rb_f", [33, 4])
            rb_b = sb(ph, "rb_b", [33, 4], BF16)
            oh = sb(ph, "oh", [33, 8208], BF16)
            ohw_s = sb(ph, "ohw_s", [33, 1152], BF16)
            ftab = sb(ph, "ftab", [4, 8208 + 1152], BF16)
            fw.dma("sp", rb_f[:], rb33_d[:, :], writes=[rb_f])
            fw.dma("sp", oh[:], ohc_d[:, :], writes=[oh])
            fw.dma("sp", ohw_s[:], ohw_d[:, :], writes=[ohw_s])
            cp("dve", rb_b[:], rb_f[:], [rb_f], [rb_b])
            pieces = [(oh, c0, min(512, 8208 - c0), c0) for c0 in range(0, 8208, 512)]
            pieces += [(ohw_s, c0, min(512, 1152 - c0), 8208 + c0) for c0 in range(0, 1152, 512)]
            for i, (src, c0, n, dst0) in enumerate(pieces):
                ps = PS[i % 2]
                mm(ps[0:4, 0:n], rb_b[:, 0:4], src[:, c0:c0 + n], True, True, [rb_b, src], [ps])
                cp("dve", ftab[:, dst0:dst0 + n], ps[0:4, 0:n], [ps], [ftab])
            fw.dma("sp", ftc[:, :], ftab[:, 0:8208], reads=[ftab], writes=[u_ft])
            fw.dma("sp", ftw[:, :], ftab[:, 8208:8208 + 1152], reads=[ftab], writes=[u_ft])
            for h in range(4):
                src = bass.AP(tensor=ftc_h, offset=h * 8208 + ZC - 127, ap=[[1, 128], [1, 640]])
                fw.dma("sp", slab_s[:, h, :], src, reads=[u_ft], writes=[slab_s])
                src = bass.AP(tensor=ftw_h, offset=h * 1152 + ZW - 127, ap=[[1, 128], [1, 1024]])
                fw.dma("sp", slab_w[:, h, :], src, reads=[u_ft], writes=[slab_w])
            fw.barrier()

        def rstd_from(ps_ap, out_ap, tmp_ap, reads, tmpT, outT, scale=1.0):
            act(tmp_ap, ps_ap, AF.Ln, reads, [tmpT], bias=EPS, scale=scale)
            act(out_ap, tmp_ap, AF.Exp, [tmpT], [outT], scale=-0.5)

        for l in range(nlayers):
            x_src = xT if l == 0 else xres
            x_dst = out_d if l == nlayers - 1 else xres
            pv = lambda c0, c1=None, p0=0, p1=128: pv_all[p0:p1, l, c0:(c0 + 1 if c1 is None else c1)]

            with ExitStack() as ph:
                w_sb = sb(ph, "w_sb", [128, 8, 2704], BF16)
                w_u = [U() for _ in range(8)]
                for c in range(8):
                    fw.dma("pool", w_sb[:, c, :], w_in[l, c * 128:(c + 1) * 128, :], writes=[w_u[c]])
                xt = [sb(ph, "xt%d" % i, [128, 8, 512]) for i in range(2)]
                xn = [sb(ph, "xn%d" % i, [128, 8, 512], BF16) for i in range(2)]
                sq = sb(ph, "sq", [128, 8, 512], BF16)
                lnb = sb(ph, "lnb", [128, 512])
                rstd = sb(ph, "rstd", [128, 512])
                zst = [sb(ph, "zst%d" % i, [128, 512]) for i in range(4)]
                ev = 0
                f2 = lambda nm, w=512, dt=F32: [sb(ph, "%s%d" % (nm, j), [128, w], dt) for j in range(2)]
                Xh, CXh, Cb, Rr, Ii, A2 = f2("Xh", 515), f2("CXh", 514), f2("Cb"), f2("Rr"), f2("Ii"), f2("A2")
                xcb, GE, Yl, SBb, SCc, Oo, Ys = f2("xcb", 512, BF16), f2("GE"), f2("Yl"), f2("SBb"), f2("SCc"), f2("Oo"), f2("Ys")
                Hh = [f2("Hh%d_" % j) for j in range(2)]
                sqA = f2("sqA", 512, BF16)
                lnbA = sb(ph, "lnbA", [128, 512])
                rstdA = sb(ph, "rstdA", [128, 512])
                mstA = [sb(ph, "mstA%d" % i, [128, 512], BF16) for i in range(4)]
                wg_b = sb(ph, "wg_b", [128, 4, 128], BF16)
                cst = sb(ph, "cst", [128, 8])
                for g in range(2):
                    for j in range(2):
                        fw.dma("pool", wg_b[:, g * 2 + j, :], wbd_d[l, g, j], writes=[wg_b])
                act(cst[:, 4:6], pv(30, 32), AF.Exp, [pv_all], [cst], scale=-1.0)
                act(cst[:, 6:8], cst[:, 4:6], AF.Ln, [cst], [cst], bias=1.0)
                ts("dve", cst[:, 0:2], cst[:, 6:8], -8.0, None, ALU.mult, None, [cst], [cst])
                ts("dve", cst[:, 2:4], cst[:, 6:8], -16.0, None, ALU.mult, None, [cst], [cst])
                for j in range(2):
                    mset("pool", Xh[j][:, 0:3], 0.0, [Xh[j]])
                    mset("pool", CXh[j][:, 0:2], 0.0, [CXh[j]])
                mctr = [0]

                def lru_part1(tt_, j, ps):
                    if tt_ > 0:
                        cp("dve", Xh[j][:, 0:3], Xh[j][:, 512:515], [Xh[j]], [Xh[j]])
                    act(Xh[j][:, 3:515], ps[:, :], AF.Copy, [ps], [Xh[j]])
                    wc = 16 + j * 4
                    ts("dve", Cb[j][:], Xh[j][:, 0:512], pv(wc), pv(24 + j), ALU.mult, ALU.add, [Xh[j], pv_all], [Cb[j]])
                    for k in range(1, 4):
                        stt(Cb[j][:], Xh[j][:, k:k + 512], pv(wc + k), Cb[j][:], ALU.mult, ALU.add, [Xh[j], pv_all, Cb[j]], [Cb[j]])
                    act(xcb[j][:], Cb[j][:], AF.Copy, [Cb[j]], [xcb[j]])

                def lru_part2(tt_, j):
                    for g, dst in ((0, Rr[j]), (1, Ii[j])):
                        ps = PS[4 + g]
                        mm(ps[:], wg_b[:, g * 2 + j, :], xcb[j][:], True, True, [wg_b, xcb[j]], [ps])
                        act(dst[:], ps[:], AF.Sigmoid, [ps, pv_all], [dst], bias=pv(26 + g * 2 + j))
                    act(A2[j][:], Rr[j][:], AF.Exp, [Rr[j], cst], [A2[j]], scale=cst[:, 2 + j:3 + j])
                    act(Rr[j][:], Rr[j][:], AF.Exp, [Rr[j], cst], [Rr[j]], scale=cst[:, j:j + 1])
                    act(A2[j][:], A2[j][:], AF.Sqrt, [A2[j]], [A2[j]], scale=-1.0, bias=1.0)

                def lru_part2b(tt_, j):
                    tt("dve", Ii[j][:], Ii[j][:], Cb[j][:], ALU.mult, [Ii[j], Cb[j]], [Ii[j]])
                    tt("dve", Ii[j][:], Ii[j][:], A2[j][:], ALU.mult, [Ii[j], A2[j]], [Ii[j]])
                    Hc = Hh[j][tt_ % 2]
                    if tt_ == 0:
                        fw.op("dve", lambda e: e.tensor_tensor_scan(out=Hc[:], data0=Rr[j][:], data1=Ii[j][:], initial=0.0, op0=ALU.mult, op1=ALU.add), [Rr[j], Ii[j]], [Hc])
                    else:
                        Hp = Hh[j][(tt_ - 1) % 2]
                        fw.op("dve", lambda e: e.tensor_tensor_scan(out=Hc[:], data0=Rr[j][:], data1=Ii[j][:], initial=Hp[:, 511:512], op0=ALU.mult, op1=ALU.add), [Rr[j], Ii[j], Hp], [Hc])
                    tt("dve", Yl[j][:], Hc[:], GE[j][:], ALU.mult, [Hc, GE[j]], [Yl[j]])

                def sc_chain(tt_, j, ps):
                    if tt_ > 0:
                        cp("dve", CXh[j][:, 0:2], CXh[j][:, 512:514], [CXh[j]], [CXh[j]])
                    tt("dve", CXh[j][:, 2:514], SCc[j][:], ps[:, :], ALU.mult, [SCc[j], ps], [CXh[j]])
                    wc = 32 + j * 3
                    ts("dve", Oo[j][:], CXh[j][:, 0:512], pv(wc), None, ALU.mult, None, [CXh[j], pv_all], [Oo[j]])
                    for k in range(1, 3):
                        stt(Oo[j][:], CXh[j][:, k:k + 512], pv(wc + k), Oo[j][:], ALU.mult, ALU.add, [CXh[j], pv_all, Oo[j]], [Oo[j]])
                    tt("dve", Ys[j][:], Oo[j][:], SBb[j][:], ALU.mult, [Oo[j], SBb[j]], [Ys[j]])

                def outnorm_fused(tt_, Yv, grp):
                    t0 = tt_ * 512
                    for j in range(2):
                        act(sqA[j][:], Yv[j][:], AF.Square, [Yv[j]], [sqA[j]])
                    mm(PS[6][:], ones256[:], sqA[0][:], True, False, [ones256, sqA[0]], [PS[6]])
                    mm(PS[6][:], ones256[:], sqA[1][:], False, True, [ones256, sqA[1]], [PS[6]])
                    rstd_from(PS[6][:], rstdA[:], lnbA[:], [PS[6]], lnbA, rstdA)
                    for j in range(2):
                        m_ = mstA[mctr[0] % 4]
                        mctr[0] += 1
                        stt(m_[:], Yv[j][:], pv(38 + grp * 2 + j), rstdA[:], ALU.mult, ALU.mult, [Yv[j], pv_all, rstdA], [m_])
                        fw.dma("pool", mixT[grp * 256 + j * 128:grp * 256 + (j + 1) * 128, t0:t0 + 512], m_[:], reads=[m_], writes=[u_mix[grp * 2 + j][tt_]])

                def norm1(tt_):
                    t0 = tt_ * 512
                    xt_, xn_ = xt[tt_ % 2], xn[tt_ % 2]
                    rd = [u_xres[tt_]] if l > 0 else []
                    fw.dma("sp", xt_[:], x_src[:, t0:t0 + 512].rearrange("(c p) t -> p c t", p=128), reads=rd, writes=[xt_])
                    act(sq[:], xt_[:], AF.Square, [xt_], [sq])
                    for c in range(8):
                        mm(PS[0][:], ones1024[:], sq[:, c, :], c == 0, c == 7, [ones1024, sq], [PS[0]])
                    rstd_from(PS[0][:], rstd[:], lnb[:], [PS[0]], lnb, rstd)
                    for c in range(8):
                        stt(xn_[:, c, :], xt_[:, c, :], pv(c), rstd[:], ALU.mult, ALU.mult, [xt_, rstd, pv_all], [xn_])

                norm1(0)
                for tt_ in range(8):
                    t0 = tt_ * 512
                    xt_, xn_ = xt[tt_ % 2], xn[tt_ % 2]
                    for oc in range(19):
                        if oc == 6 and tt_ + 1 < 8:
                            norm1(tt_ + 1)
                        m = 128 if oc < 18 else 4
                        ps = PS[1 + ev % 3]
                        for c in range(8):
                            mm(ps[0:m, :], w_sb[:, c, oc * 128:oc * 128 + m], xn_[:, c, :], c == 0, c == 7, [w_u[c], xn_], [ps])
                        if oc == 10 or oc == 11:
                            lru_part2b(tt_, oc - 10)
                        if oc == 13:
                            outnorm_fused(tt_, Yl, 0)
                        if oc == 16:
                            outnorm_fused(tt_, Ys, 1)
                        if oc < 10:
                            j = oc % 2
                            if oc < 2:
                                lru_part1(tt_, j, ps)
                            elif oc < 4:
                                act(GE[j][:], ps[:, :], AF.Gelu_apprx_tanh, [ps], [GE[j]])
                            elif oc < 6:
                                cp("dve", SBb[j][:], ps[:, :], [ps], [SBb[j]])
                                lru_part2(tt_, j)
                            elif oc < 8:
                                act(SCc[j][:], ps[:, :], AF.Copy, [ps], [SCc[j]])
                            else:
                                sc_chain(tt_, j, ps)
                            ev += 1
                            continue
                        st_ = zst[ev % 4]
                        if ev % 2 == 0:
                            act(st_[0:m, :], ps[0:m, :], AF.Copy, [ps], [st_])
                        else:
                            cp("dve", st_[0:m, :], ps[0:m, :], [ps], [st_])
                        fw.dma("pool", zT[oc * 128:oc * 128 + m, t0:t0 + 512], st_[0:m, :], reads=[st_], writes=[u_zT[oc][tt_]])
                        ev += 1
                    for s in range(4):
                        ps = PS[1 + ev % 3]
                        for c in range(8):
                            mm(ps[:, 0:NTM], xn_[:, c, s * 128:(s + 1) * 128], w_sb[:, c, NFM:NFM + NTM], c == 0, c == 7, [w_u[c], xn_], [ps])
                        st_ = zst[ev % 4]
                        if ev % 2 == 0:
                            act(st_[:, 0:NTM], ps[:, 0:NTM], AF.Copy, [ps], [st_])
                        else:
                            cp("dve", st_[:, 0:NTM], ps[:, 0:NTM], [ps], [st_])
                        fw.dma("pool", ztok[t0 + s * 128:t0 + (s + 1) * 128, :], st_[:, 0:NTM], reads=[st_], writes=[u_ztok[tt_ * 4 + s]])
                        ev += 1
                fw.barrier()
            if stop <= 1:
                break

            for _unused in []:
              with ExitStack() as ph:
                  NB = 4100
                  X = sb(ph, "X", [128, NB])
                  C = sb(ph, "C", [128, NB])
                  R = sb(ph, "R", [128, NB])
                  I = sb(ph, "I", [128, NB])
                  A2 = sb(ph, "A2", [128, NB])
                  Y0 = sb(ph, "Y0", [128, S])
                  xcb = sb(ph, "xcb", [128, S], BF16)
                  sq0 = sb(ph, "sq0", [128, S], BF16)
                  sq1 = sb(ph, "sq1", [128, S], BF16)
                  wg_f = sb(ph, "wg_f", [128, 4, 128])
                  wg_b = sb(ph, "wg_b", [128, 4, 128], BF16)
                  cst = sb(ph, "cst", [128, 8])
                  lnb = sb(ph, "lnb2", [128, 512])
                  rstd = sb(ph, "rstd2", [128, 512])
                  mst = [sb(ph, "mst%d" % i, [128, 512], BF16) for i in range(3)]
                  for g in range(2):
                      for j in range(2):
                          fw.dma("sp", wg_f[:, g * 2 + j, :], wbd_d[l, g, j], writes=[wg_f])
                  cp("dve", wg_b[:], wg_f[:], [wg_f], [wg_b])
                  act(cst[:, 4:6], pv(30, 32), AF.Exp, [pv_all], [cst], scale=-1.0)
                  act(cst[:, 6:8], cst[:, 4:6], AF.Ln, [cst], [cst], bias=1.0)
                  ts("dve", cst[:, 0:2], cst[:, 6:8], -8.0, None, ALU.mult, None, [cst], [cst])
                  ts("dve", cst[:, 2:4], cst[:, 6:8], -16.0, None, ALU.mult, None, [cst], [cst])

                  def outnorm_fm(Ys, sqs, grp):
                      for j in range(2):
                          act(sqs[j][:], Ys[j][:, 0:S], AF.Square, [Ys[j]], [sqs[j]])
                      for tt_ in range(8):
                          t0 = tt_ * 512
                          ps = PS[tt_ % 2]
                          mm(ps[:], ones256[:], sqs[0][:, t0:t0 + 512], True, False, [ones256, sqs[0]], [ps])
                          mm(ps[:], ones256[:], sqs[1][:, t0:t0 + 512], False, True, [ones256, sqs[1]], [ps])
                          rstd_from(ps[:], rstd[:], lnb[:], [ps], lnb, rstd)
                          for j in range(2):
                              m_ = mst[(tt_ * 2 + j) % 3]
                              stt(m_[:], Ys[j][:, t0:t0 + 512], pv(38 + grp * 2 + j), rstd[:], ALU.mult, ALU.mult, [Ys[j], pv_all, rstd], [m_])
                              fw.dma("pool", mixT[grp * 256 + j * 128:grp * 256 + (j + 1) * 128, t0:t0 + 512], m_[:], reads=[m_], writes=[u_mix[grp * 2 + j][tt_]])

                  for j in range(2):
                      mset("pool", X[:, 0:3], 0.0, [X])
                      fw.dma("sp", X[:, 3:3 + S], zT[R_LRU_X + j * 128:R_LRU_X + (j + 1) * 128, :], reads=u_zT[0 + j], writes=[X])
                      wc = 16 + j * 4
                      ts("dve", C[:, 0:S], X[:, 0:S], pv(wc), pv(24 + j), ALU.mult, ALU.add, [X, pv_all], [C])
                      for k in range(1, 4):
                          stt(C[:, 0:S], X[:, k:k + S], pv(wc + k), C[:, 0:S], ALU.mult, ALU.add, [X, pv_all, C], [C])
                      cp("pool", xcb[:], C[:, 0:S], [C], [xcb])
                      for tt_ in range(8):
                          t0 = tt_ * 512
                          for g, dst in ((0, R), (1, I)):
                              ps = PS[(tt_ * 2 + g) % 4]
                              mm(ps[:], wg_b[:, g * 2 + j, :], xcb[:, t0:t0 + 512], True, True, [wg_b, xcb], [ps])
                              act(dst[:, t0:t0 + 512], ps[:], AF.Sigmoid, [ps, pv_all], [dst], bias=pv(26 + g * 2 + j))
                      act(A2[:, 0:S], R[:, 0:S], AF.Exp, [R, cst], [A2], scale=cst[:, 2 + j:3 + j])
                      act(R[:, 0:S], R[:, 0:S], AF.Exp, [R, cst], [R], scale=cst[:, j:j + 1])
                      act(A2[:, 0:S], A2[:, 0:S], AF.Sqrt, [A2], [A2], scale=-1.0, bias=1.0)
                      tt("dve", I[:, 0:S], I[:, 0:S], C[:, 0:S], ALU.mult, [I, C], [I])
                      tt("dve", I[:, 0:S], I[:, 0:S], A2[:, 0:S], ALU.mult, [I, A2], [I])
                      fw.op("dve", lambda e: e.tensor_tensor_scan(out=C[:, 0:S], data0=R[:, 0:S], data1=I[:, 0:S], initial=0.0, op0=ALU.mult, op1=ALU.add), [R, I], [C])
                      fw.dma("sp", X[:, 0:S], zT[R_LRU_G + j * 128:R_LRU_G + (j + 1) * 128, :], reads=u_zT[2 + j], writes=[X])
                      act(A2[:, 0:S], X[:, 0:S], AF.Gelu_apprx_tanh, [X], [A2])
                      Yd = Y0 if j == 0 else C
                      tt("dve", Yd[:, 0:S], C[:, 0:S], A2[:, 0:S], ALU.mult, [C, A2], [Yd])
                  outnorm_fm([Y0, C], [sq0, sq1], 0)
                  for j in range(2):
                      Yd = Y0 if j == 0 else C
                      mset("pool", X[:, 0:2], 0.0, [X])
                      fw.dma("sp", R[:, 0:S], zT[R_SC_C + j * 128:R_SC_C + (j + 1) * 128, :], reads=u_zT[6 + j], writes=[R])
                      fw.dma("sp", I[:, 0:S], zT[R_SC_X + j * 128:R_SC_X + (j + 1) * 128, :], reads=u_zT[8 + j], writes=[I])
                      fw.dma("sp", A2[:, 0:S], zT[R_SC_B + j * 128:R_SC_B + (j + 1) * 128, :], reads=u_zT[4 + j], writes=[A2])
                      tt("dve", X[:, 2:2 + S], R[:, 0:S], I[:, 0:S], ALU.mult, [R, I], [X])
                      wc = 32 + j * 3
                      ts("dve", R[:, 0:S], X[:, 0:S], pv(wc), None, ALU.mult, None, [X, pv_all], [R])
                      for k in range(1, 3):
                          stt(R[:, 0:S], X[:, k:k + S], pv(wc + k), R[:, 0:S], ALU.mult, ALU.add, [X, pv_all, R], [R])
                      tt("dve", Yd[:, 0:S], R[:, 0:S], A2[:, 0:S], ALU.mult, [R, A2], [Yd])
                  outnorm_fm([Y0, C], [sq0, sq1], 1)
                  fw.barrier()
            if stop <= 2:
                break

            def headnorm(tl, src_ap, src_units, gcol, scale, dst, ntok):
                hq, hsq, gs, lnb, rstd = tl
                if src_ap is not None:
                    fw.dma("sp", hq[0:64, 0:ntok], src_ap, reads=src_units, writes=[hq])
                ts("dve", gs[:, 0:1], pv(gcol, p1=64), float(scale), None, ALU.mult, None, [pv_all], [gs])
                act(hsq[0:64, 0:ntok], hq[0:64, 0:ntok], AF.Square, [hq], [hsq])
                for t0 in range(0, ntok, 512):
                    n = min(512, ntok - t0)
                    ps = PS[5 + (t0 // 512) % 2]
                    mm(ps[0:64, 0:n], ones64[:], hsq[0:64, t0:t0 + n], True, True, [ones64, hsq], [ps])
                    rstd_from(ps[0:64, 0:n], rstd[0:64, 0:n], lnb[0:64, 0:n], [ps], lnb, rstd)
                    stt(dst[0:64, t0:t0 + n], hq[0:64, t0:t0 + n], gs[:, 0:1], rstd[0:64, 0:n], ALU.mult, ALU.mult, [hq, gs, rstd], [dst])

            def headnorm2(tl, k, src_ap, src_units, gcol, scale, dstA, dstB):
                hq2s, hsq2, lnall, hb, gs2 = tl
                hq2 = hq2s[k % len(hq2s)]
                fw.dma("sp", hq2[:, 0:S], src_ap, reads=src_units, writes=[hq2])
                ts("dve", gs2[:, 0:1], pv(gcol), float(scale), None, ALU.mult, None, [pv_all], [gs2])
                act(hsq2[:, 0:S], hq2[:, 0:S], AF.Square, [hq2], [hsq2])
                for t in range(8):
                    ps = PS[5 + t % 2]
                    mm(ps[:, :], bd64[:], hsq2[:, t * 512:(t + 1) * 512], True, True, [bd64, hsq2], [ps])
                    act(lnall[:, t * 512:(t + 1) * 512], ps[:, :], AF.Ln, [ps], [lnall], bias=EPS)
                act(lnall[:, :], lnall[:, :], AF.Exp, [lnall], [lnall], scale=-0.5)
                stt(hb[:, :], hq2[:, 0:S], gs2[:, 0:1], lnall[:, :], ALU.mult, ALU.mult, [hq2, gs2, lnall], [hb])
                fw.dma("sp", dstA[0:64, :], hb[0:64, :], reads=[hb], writes=[dstA])
                fw.dma("sp", dstB[0:64, :], hb[64:128, :], reads=[hb], writes=[dstB])

            def run_attention(tiles, pts):
                n_t = len(tiles)
                ring = PS[0:3]

                def emit_pre(i):
                    if i < n_t and tiles[i].get("pre") is not None:
                        bt, src = tiles[i]["pre"]
                        fw.dma("sp", bt[:], src, reads=[u_ft], writes=[bt])

                emit_pre(0)

                def emit_qk(i):
                    tl = tiles[i]
                    ps = ring[i % 3]
                    emit_pre(i + 1)
                    np_ = tl["np"]
                    last = len(tl["mms"]) - 1
                    for idx, (c0, ncol, lhsT, rhs, rds) in enumerate(tl["mms"]):
                        mm(ps[0:np_, c0:c0 + ncol], lhsT, rhs, idx == 0, idx == last, rds, [ps], skip_group_check=True)

                def emit_rest(i):
                    tl = tiles[i]
                    ps = ring[i % 3]
                    pt = pts[i % len(pts)]
                    np_, n = tl["np"], tl["n"]
                    if tl.get("bias") is not None:
                        act(pt[0:np_, 0:n], ps[0:np_, 0:n], AF.Exp, [ps] + tl["bias_reads"], [pt], bias=tl["bias"])
                    else:
                        act(pt[0:np_, 0:n], ps[0:np_, 0:n], AF.Exp, [ps], [pt])
                    if tl.get("init") is not None:
                        accT, w = tl["init"]
                        mm(accT[:, 0:w], zeros[:, 0:128], zeros[:, 0:w], True, True, [zeros], [accT])
                    for (accT, acc_ap, pc0, V_ap, rds) in tl["pv"]:
                        mm(acc_ap, pt[0:np_, pc0:pc0 + 128], V_ap, False, True, [pt] + rds, [accT], skip_group_check=True)
                    if tl.get("fin") is not None:
                        tl["fin"]()

                LA = 2
                for i in range(n_t + LA):
                    if i < n_t:
                        emit_qk(i)
                    if i >= LA:
                        emit_rest(i - LA)

            def outnorm_tok(tl, Q, yv, yT, gain_ap, rowbase):
                junk, ssq, lnq, rs, ym, mst2 = tl
                q0 = Q * 512
                for s in range(4):
                    act(junk[:, 0:256], yv(s), AF.Square, [yT], [junk, ssq], accum_out=ssq[:, s:s + 1])
                act(lnq[:, 0:4], ssq[:, 0:4], AF.Ln, [ssq], [lnq], scale=1.0 / 256, bias=EPS)
                act(rs[:, 0:4], lnq[:, 0:4], AF.Exp, [lnq], [rs], scale=-0.5)
                for s in range(4):
                    stt(ym[:, s, :], yv(s), rs[:, s:s + 1], gain_ap, ALU.mult, ALU.mult, [yT, rs, brow_all], [ym])
                for j in range(2):
                    for s in range(4):
                        fw.op("pe", lambda e, s=s, j=j: e.transpose(out=PSB[:, s * 128:(s + 1) * 128], in_=ym[:, s, j * 128:(j + 1) * 128], identity=ident[:]), [ym, ident], [PSB])
                    m_ = mst2[j]
                    cp("dve", m_[:], PSB[:, 0:512], [PSB], [m_])
                    fw.dma("pool", mixT[rowbase + j * 128:rowbase + (j + 1) * 128, q0:q0 + 512], m_[:], reads=[m_], writes=[u_mix[rowbase // 128 + j][Q]])

            with ExitStack() as ph:
                Qp = [sb(ph, "Qp%d" % h, [128, S], BF16) for h in range(4)]
                Kp = [sb(ph, "Kp%d" % h, [128, S], BF16) for h in range(4)]
                with ExitStack() as ph2:
                    fr = sb(ph2, "fr", [4, S])
                    e1 = sb(ph2, "e1", [4, S])
                    cc = sb(ph2, "cc", [4, S])
                    ones4 = sb(ph2, "ones4", [4, S], BF16)
                    nb = sb(ph2, "nb", [4, 1])
                    csp = sb(ph2, "csp", [4, 3, S], BF16)
                    ncsp = sb(ph2, "ncsp", [4, 3, S], BF16)
                    fw.dma("sp", fr[:], zT[R_FOX_F:R_FOX_F + 4, :], reads=u_zT[18], writes=[fr])
                    ts("dve", nb[:], pv(48, p1=4), -1.0, None, ALU.mult, None, [pv_all], [nb])
                    act(e1[:], fr[:], AF.Exp, [fr, nb], [e1], scale=-1.0, bias=nb[:])
                    act(e1[:], e1[:], AF.Ln, [e1], [e1], bias=1.0)
                    ts("dve", e1[:], e1[:], -1.0, None, ALU.mult, None, [e1], [e1])
                    mset("pool", ones4[:], 1.0, [ones4])
                    fw.op("dve", lambda e: e.tensor_tensor_scan(out=cc[:], data0=ones4[:], data1=e1[:], initial=0.0, op0=ALU.mult, op1=ALU.add), [ones4, e1], [cc])
                    cp("dve", csp[:, 0, :], cc[:], [cc], [csp])
                    cp("dve", fr[:], csp[:, 0, :], [csp], [fr])
                    tt("dve", cc[:], cc[:], fr[:], ALU.subtract, [cc, fr], [cc])
                    cp("dve", csp[:, 1, :], cc[:], [cc], [csp])
                    cp("dve", fr[:], csp[:, 1, :], [csp], [fr])
                    tt("dve", cc[:], cc[:], fr[:], ALU.subtract, [cc, fr], [cc])
                    cp("dve", csp[:, 2, :], cc[:], [cc], [csp])
                    ts("dve", ncsp[:], csp[:], -1.0, None, ALU.mult, None, [csp], [ncsp])
                    for h in range(4):
                        mset("pool", Qp[h][64:128, :], 0.0, [Qp[h]])
                        mset("pool", Kp[h][64:128, :], 0.0, [Kp[h]])
                        for i in range(3):
                            fw.dma("sp", Qp[h][64 + i:65 + i, :], csp[h:h + 1, i, :], reads=[csp], writes=[Qp[h]])
                            fw.dma("sp", Kp[h][96 + i:97 + i, :], ncsp[h:h + 1, i, :], reads=[ncsp], writes=[Kp[h]])
                        fw.dma("sp", Qp[h][96:99, :], ones4[0:3, :], reads=[ones4], writes=[Qp[h]])
                        fw.dma("sp", Kp[h][64:67, :], ones4[0:3, :], reads=[ones4], writes=[Kp[h]])
                    fw.barrier()
                V1 = sb(ph, "V1", [128, 32, 4, 65], BF16)
                hn2 = ([sb(ph, "hq2_%d" % i, [128, S]) for i in range(2)], sb(ph, "hsq2", [128, S], BF16),
                       sb(ph, "lnall", [128, S]), sb(ph, "hb", [128, S], BF16), sb(ph, "gs2", [128, 1]))
                vst = sb(ph, "vst", [128, 8, 256])
                pts = [sb(ph, "pt%d" % i, [128, 512], BF16) for i in range(3)]
                rden = [sb(ph, "rden%d" % i, [128, 4]) for i in range(2)]
                ytk = [sb(ph, "ytk%d" % i, [128, 4, 256]) for i in range(2)]
                on_tl = (sb(ph, "junk", [128, 256]), sb(ph, "ssq", [128, 4]), sb(ph, "lnq", [128, 4]), sb(ph, "rs", [128, 4]),
                         sb(ph, "ym", [128, 4, 256], BF16), [sb(ph, "mst2_%d" % i, [128, 512], BF16) for i in range(2)])
                for j in range(2):
                    headnorm2(hn2, 2 * j, zT[R_FOX_Q + j * 128:R_FOX_Q + (j + 1) * 128, :], u_zT[10 + j], 50, 0.125, Qp[2 * j], Qp[2 * j + 1])
                    headnorm2(hn2, 2 * j + 1, zT[R_FOX_K + j * 128:R_FOX_K + (j + 1) * 128, :], u_zT[12 + j], 51, 1.0, Kp[2 * j], Kp[2 * j + 1])
                mset("pool", V1[:, :, :, 64:65], 1.0, [V1])
                for g in range(4):
                    fw.dma("sp", vst[:], ztok[g * 1024:(g + 1) * 1024, T_FOX_V:T_FOX_V + 256].rearrange("(k p) c -> p k c", p=128), reads=u_ztok[g * 8:(g + 1) * 8], writes=[vst])
                    for h in range(4):
                        cp("pool" if h % 2 else "dve", V1[:, g * 8:(g + 1) * 8, h, 0:64], vst[:, :, h * 64:(h + 1) * 64], [vst], [V1])
                accn = 0
                for Q in range(8):
                    q0 = Q * 512
                    yT = ytk[Q % 2]
                    tiles = []
                    for h in range(4):
                        accT = PS[3 + accn % 2]
                        rd_ = rden[accn % 2]
                        accn += 1
                        nk = 4 * Q + 4
                        for kt in range(nk):
                            k0 = kt * 128
                            d = kt - 4 * Q
                            dd = max(d, 0)
                            n = 512 - 128 * dd
                            mms = [(0, n, Kp[h][:, k0:k0 + 128], Qp[h][:, q0 + 128 * dd:q0 + 512], [Kp[h], Qp[h]])]
                            if d >= 0:
                                mms.append((0, 128, ident[:], tri[:], [ident, tri]))
                            tl = dict(np=128, n=n, mms=mms, pv=[])
                            for s in range(dd, 4):
                                tl["pv"].append((accT, accT[:, s * 65:(s + 1) * 65], (s - dd) * 128, V1[:, kt, h, :], [V1]))
                            if kt == 0:
                                tl["init"] = (accT, 260)
                            if kt == nk - 1:
                                def fin(accT=accT, rd_=rd_, h=h, yT=yT):
                                    accv = accT[:, 0:260].rearrange("p (s c) -> p s c", c=65)
                                    fw.op("dve", lambda e: e.reciprocal(out=rd_[:, 0:4], in_=accv[:, :, 64]), [accT], [rd_])
                                    for s in range(4):
                                        ts("dve", yT[:, s, h * 64:(h + 1) * 64], accv[:, s, 0:64], rd_[:, s:s + 1], None, ALU.mult, None, [accT, rd_], [yT])
                                tl["fin"] = fin
                            tiles.append(tl)
                    run_attention(tiles, pts)
                    outnorm_tok(on_tl, Q, lambda s, yT=yT: yT[:, s, :], yT, brow_all[:, l, 0:256], 512)
                fw.barrier()
            if stop <= 3:
                break

            with ExitStack() as ph:
                Qn = [sb(ph, "Qn%d" % h, [128, S], BF16) for h in range(4)]
                KsT = sb(ph, "KsT", [128, S], BF16)
                KwT = sb(ph, "KwT", [128, S], BF16)
                KcT = sb(ph, "KcT", [128, 256], BF16)
                for h in range(4):
                    mset("pool", Qn[h][64:128, :], 0.0, [Qn[h]])
                mset("pool", KwT[64:128, :], 0.0, [KwT])
                mset("pool", KcT[64:128, :], 0.0, [KcT])
                fw.dma("sp", KsT[64:128, :], cE_d[:, :], writes=[KsT])
                VS1 = sb(ph, "VS1", [128, 32, 65], BF16)
                VW1 = sb(ph, "VW1", [128, 32, 65], BF16)
                VC1 = sb(ph, "VC1", [128, 2, 128], BF16)
                G = sb(ph, "G", [128, 32, 12])
                yd = sb(ph, "yd", [128, 32, 256])
                hq = sb(ph, "hq5", [128, 4112])
                hsq = sb(ph, "hsq5", [128, 4112], BF16)
                hn2 = ([hq], hsq, sb(ph, "lnall5", [128, S]), sb(ph, "hb5", [128, S], BF16), sb(ph, "gs25", [128, 1]))
                gs = sb(ph, "gs5", [64, 1])
                lnb = sb(ph, "lnb5", [64, 512])
                rstd = sb(ph, "rstd5", [64, 512])
                hn = (hq, hsq, gs, lnb, rstd)
                w1b = sb(ph, "w1b", [64, 32, 128], BF16)
                posb = sb(ph, "posb", [64, 32], BF16)
                w2b = sb(ph, "w2b", [128, 64], BF16)
                hbias = sb(ph, "hbias", [128, 1])
                hidb = sb(ph, "hidb", [128, 256], BF16)
                vst2 = sb(ph, "vst2", [128, 32, 64])
                gst = sb(ph, "gst", [128, 32, 12])
                pts = [sb(ph, "ptn%d" % i, [128, 512], BF16) for i in range(3)]
                btile = [sb(ph, "btile%d" % i, [128, 512], BF16) for i in range(3)]
                den = [sb(ph, "den%d" % i, [128, 4]) for i in range(2)]
                rden = [sb(ph, "rdenn%d" % i, [128, 4]) for i in range(2)]
                coef = [sb(ph, "coef%d" % i, [128, 4]) for i in range(2)]
                impacc = sb(ph, "impacc", [128, 4, 64])
                imp2 = sb(ph, "imp2", [128, 64])
                m8 = sb(ph, "m8", [128, 8])
                m8b = sb(ph, "m8b", [128, 8])
                self_ = sb(ph, "self", [128, 64])
                selb = sb(ph, "selb", [128, 128], BF16)
                on_tl = (sb(ph, "junk5", [128, 256]), sb(ph, "ssq5", [128, 4]), sb(ph, "lnq5", [128, 4]), sb(ph, "rs5", [128, 4]),
                         sb(ph, "ym5", [128, 4, 256], BF16), [sb(ph, "mst5_%d" % i, [128, 512], BF16) for i in range(2)])
                for j in range(2):
                    headnorm2(hn2, 0, zT[R_NSA_Q + j * 128:R_NSA_Q + (j + 1) * 128, :], u_zT[14 + j], 52, 0.125, Qn[2 * j], Qn[2 * j + 1])
                headnorm2(hn2, 0, zT[R_KS:R_KS + 128, :], u_zT[17], 53, 1.0, KsT, KwT)
                for kv in range(2):
                    rows = R_KC if kv == 0 else R_VC
                    fw.dma("sp", hq[0:64, 0:S], zT[rows:rows + 64, :], reads=u_zT[16], writes=[hq])
                    mset("pool", hq[0:64, S:4112], 0.0, [hq])
                    cp("dve", hsq[0:64, :], hq[0:64, :], [hq], [hsq])
                    fw.dma("pool", w1b[:], cw1_d[l, kv].rearrange("l d m -> d l m"), writes=[w1b])
                    fw.dma("pool", posb[:], cposT_d[l, kv], writes=[posb])
                    fw.dma("pool", w2b[:], cw2_d[l, kv], writes=[w2b])
                    ps = PS[5]
                    for li in range(32):
                        mm(ps[:, 0:1], w1b[:, li, :], posb[:, li:li + 1], li == 0, li == 31, [w1b, posb], [ps])
                    cp("dve", hbias[:], ps[:, 0:1], [ps], [hbias])
                    ps = PS[6]
                    for li in range(32):
                        if li < 16:
                            rhs = hsq[0:64, 0:4096].rearrange("p (n r) -> p n r", r=16)[:, :, li]
                        else:
                            rhs = hsq[0:64, 16:4112].rearrange("p (n r) -> p n r", r=16)[:, :, li - 16]
                        mm(ps[:, 0:256], w1b[:, li, :], rhs, li == 0, li == 31, [w1b, hsq], [ps])
                    act(hidb[:], ps[:, 0:256], AF.Gelu_apprx_tanh, [ps, hbias], [hidb], bias=hbias[:])
                    if kv == 0:
                        ps = PS[5]
                        mm(ps[0:64, 0:256], w2b[:], hidb[:], True, True, [w2b, hidb], [ps])
                        cp("dve", hq[0:64, 0:256], ps[0:64, 0:256], [ps], [hq])
                        headnorm(hn, None, None, 45, 1.0, KcT, 256)
                    else:
                        for c in range(2):
                            ps = PS[5]
                            mm(ps[:, 0:64], hidb[:, c * 128:(c + 1) * 128], w2b[:], True, True, [w2b, hidb], [ps])
                            cp("dve", VC1[:, c, 0:64], ps[:, 0:64], [ps], [VC1])
                            cp("dve", VC1[:, c, 64:128], ovl[:, c * 64:(c + 1) * 64], [ovl], [VC1])
                for (VT, col) in ((VS1, T_VS), (VW1, T_VW)):
                    fw.dma("sp", vst2[:], ztok[:, col:col + 64].rearrange("(k p) c -> p k c", p=128), reads=u_ztok, writes=[vst2])
                    cp("dve", VT[:, :, 0:64], vst2[:], [vst2], [VT])
                    mset("pool", VT[:, :, 64:65], 1.0, [VT])
                fw.dma("sp", gst[:], ztok[:, T_G:T_G + 12].rearrange("(k p) c -> p k c", p=128), reads=u_ztok, writes=[gst])
                tt("dve", gst[:], gst[:], brow_all[:, l, 512:524].unsqueeze(1).to_broadcast([128, 32, 12]), ALU.add, [gst, brow_all], [gst])
                act(G[:], gst[:], AF.Sigmoid, [gst], [G])
                mset("pool", selb[:], 0.0, [selb])

                accn = 0
                bn = 0
                for Q in range(8):
                    q0 = Q * 512

                    def fin_generic(accT, w, h, branch, first, dn, rd_, cf_, Q=Q):
                        accv = accT[:, 0:4 * w].rearrange("p (s c) -> p s c", c=w)
                        if branch == 0:
                            fw.op("dve", lambda e: e.tensor_reduce(out=dn[:, 0:4], in_=accv[:, :, 64:128], axis=mybir.AxisListType.X, op=ALU.add), [accT], [dn])
                            ts("dve", dn[:, 0:4], dn[:, 0:4], 1.0 / 32, 1e-30, ALU.mult, ALU.max, [dn], [dn])
                            fw.op("dve", lambda e: e.reciprocal(out=rd_[:, 0:4], in_=dn[:, 0:4]), [dn], [rd_])
                        else:
                            fw.op("dve", lambda e: e.reciprocal(out=rd_[:, 0:4], in_=accv[:, :, 64]), [accT], [rd_])
                        tt("dve", cf_[:, 0:4], rd_[:, 0:4], G[:, 4 * Q:4 * Q + 4, branch * 4 + h], ALU.mult, [rd_, G], [cf_])
                        for s in range(4):
                            ydv = yd[:, 4 * Q + s, h * 64:(h + 1) * 64]
                            if first:
                                ts("dve", ydv, accv[:, s, 0:64], cf_[:, s:s + 1], None, ALU.mult, None, [accT, cf_], [yd])
                            else:
                                stt(ydv, accv[:, s, 0:64], cf_[:, s:s + 1], ydv, ALU.mult, ALU.add, [accT, cf_, yd], [yd])
                        if branch == 0:
                            for s in range(4):
                                if h == 0:
                                    ts("dve", impacc[:, s, :], accv[:, s, 64:128], rd_[:, s:s + 1], None, ALU.mult, None, [accT, rd_], [impacc])
                                else:
                                    stt(impacc[:, s, :], accv[:, s, 64:128], rd_[:, s:s + 1], impacc[:, s, :], ALU.mult, ALU.add, [accT, rd_, impacc], [impacc])

                    tiles = []
                    for h in range(4):
                        accT = PS[3 + accn % 2]
                        dn, rd_, cf_ = den[accn % 2], rden[accn % 2], coef[accn % 2]
                        accn += 1
                        chunks = [0, 1] if Q >= 4 else [0]
                        for c in chunks:
                            bt = btile[bn % 3]
                            bn += 1
                            off = h * 8208 + ZC + q0 - 31 - 16 * (c * 128 + 127)
                            src = bass.AP(tensor=ftc_h, offset=off, ap=[[16, 128], [1, 512]])
                            tl = dict(np=128, n=512, pv=[])
                            tl["pre"] = (bt, src)
                            tl["mms"] = [(0, 512, KcT[:, c * 128:(c + 1) * 128], Qn[h][:, q0:q0 + 512], [KcT, Qn[h]]),
                                         (0, 512, Jm[:], bt[:], [Jm, bt])]
                            for s in range(4):
                                tl["pv"].append((accT, accT[:, s * 128:(s + 1) * 128], s * 128, VC1[:, c, :], [VC1]))
                            if c == 0:
                                tl["init"] = (accT, 512)
                            if c == chunks[-1]:
                                tl["fin"] = (lambda accT=accT, h=h, dn=dn, rd_=rd_, cf_=cf_: fin_generic(accT, 128, h, 0, True, dn, rd_, cf_))
                            tiles.append(tl)
                    run_attention(tiles, pts)

                    tiles = []
                    for h in range(4):
                        accT = PS[3 + accn % 2]
                        dn, rd_, cf_ = den[accn % 2], rden[accn % 2], coef[accn % 2]
                        accn += 1
                        kts = [kt for kt in range(4 * Q - 4, 4 * Q + 4) if kt >= 0]
                        for kt in kts:
                            k0 = kt * 128
                            d = kt - 4 * Q
                            dd = max(d, 0)
                            n = 512 - 128 * dd
                            sc0 = 128 * (-d) if d < 0 else 0
                            tl = dict(np=128, n=n, pv=[])
                            tl["mms"] = [(0, n, KwT[:, k0:k0 + 128], Qn[h][:, q0 + 128 * dd:q0 + 512], [KwT, Qn[h]]),
                                         (0, n, Jm[:], slab_w[:, h, sc0:sc0 + n], [Jm, slab_w])]
                            for s in range(dd, 4):
                                tl["pv"].append((accT, accT[:, s * 65:(s + 1) * 65], (s - dd) * 128, VW1[:, kt, :], [VW1]))
                            if kt == kts[0]:
                                tl["init"] = (accT, 260)
                            if kt == kts[-1]:
                                tl["fin"] = (lambda accT=accT, h=h, dn=dn, rd_=rd_, cf_=cf_: fin_generic(accT, 65, h, 2, False, dn, rd_, cf_))
                            tiles.append(tl)
                    run_attention(tiles, pts)

                    for s in range(4):
                        i_ = 4 * Q + s
                        iv = impacc[:, s, :]
                        if 2 * i_ + 2 < 64:
                            mset("dve", impacc[:, s, 2 * i_ + 2:64], -1e30, [impacc])
                        mset("dve", impacc[0:64, s, 2 * i_ + 1:2 * i_ + 2], -1e30, [impacc])
                        mset("dve", impacc[64:128, s, 2 * i_ + 1:2 * i_ + 2], 1e30, [impacc])
                        mset("dve", impacc[:, s, 2 * i_:2 * i_ + 1], 1e30, [impacc])
                        if i_ >= 1:
                            mset("dve", impacc[0:64, s, 2 * i_ - 1:2 * i_], 1e30, [impacc])
                        mset("dve", impacc[:, s, 0:1], 1e30, [impacc])
                        fw.op("dve", lambda e, iv=iv: e.max(out=m8[:], in_=iv), [impacc], [m8])
                        fw.op("dve", lambda e, iv=iv: e.match_replace(out=imp2[:], in_to_replace=m8[:], in_values=iv, imm_value=-3e38), [impacc, m8], [imp2])
                        fw.op("dve", lambda e: e.max(out=m8b[:], in_=imp2[:]), [imp2], [m8b])
                        ts("dve", self_[:], iv, m8b[:, 7:8], -1.0, ALU.is_ge, ALU.add, [impacc, m8b], [self_])
                        ts("dve", selb[:, 64:128], self_[:], -NEGM, None, ALU.mult, None, [self_], [selb])
                        fw.op("pe", lambda e, s=s: e.transpose(out=PSB[:, s * 128:(s + 1) * 128], in_=selb[:], identity=ident[:]), [selb, ident], [PSB])
                    for h in range(4):
                        cp("dve", Qn[h][64:128, q0:q0 + 512], PSB[64:128, 0:512], [PSB], [Qn[h]])

                    tiles = []
                    for h in range(4):
                        accT = PS[3 + accn % 2]
                        dn, rd_, cf_ = den[accn % 2], rden[accn % 2], coef[accn % 2]
                        accn += 1
                        nk = 4 * Q + 4
                        for kt in range(nk):
                            k0 = kt * 128
                            d = kt - 4 * Q
                            dd = max(d, 0)
                            n = 512 - 128 * dd
                            tl = dict(np=128, n=n, pv=[])
                            tl["mms"] = [(0, n, KsT[:, k0:k0 + 128], Qn[h][:, q0 + 128 * dd:q0 + 512], [KsT, Qn[h]])]
                            if d >= -1:
                                sc0 = 128 if d == -1 else 0
                                tl["mms"].append((0, n, Jm[:], slab_s[:, h, sc0:sc0 + n], [Jm, slab_s]))
                            else:
                                tl["bias"] = rb31[:, h:h + 1]
                                tl["bias_reads"] = [rb31]
                            for s in range(dd, 4):
                                tl["pv"].append((accT, accT[:, s * 65:(s + 1) * 65], (s - dd) * 128, VS1[:, kt, :], [VS1]))
                            if kt == 0:
                                tl["init"] = (accT, 260)
                            if kt == nk - 1:
                                tl["fin"] = (lambda accT=accT, h=h, dn=dn, rd_=rd_, cf_=cf_: fin_generic(accT, 65, h, 1, False, dn, rd_, cf_))
                            tiles.append(tl)
                    run_attention(tiles, pts)
                    outnorm_tok(on_tl, Q, lambda s, Q=Q: yd[:, 4 * Q + s, :], yd, brow_all[:, l, 256:512], 768)
                fw.barrier()
            if stop <= 4:
                break

            with ExitStack() as ph:
                wgu = sb(ph, "wgu", [128, 8, 2 * DFF], BF16)
                wdn = sb(ph, "wdn", [128, 22, 1024], BF16)
                wgu_u = [[U(), U()] for _ in range(8)]
                wdn_u = [U() for _ in range(22)]
                with ExitStack() as ph2:
                    wo = sb(ph2, "wo", [128, 8, 1024], BF16)
                    for c in range(8):
                        fw.dma("pool", wo[:, c, :], w_out[l, c * 128:(c + 1) * 128, :], writes=[wo])
                    xt = [sb(ph2, "xto%d" % i, [128, 8, 512]) for i in range(1)]
                    mt = [sb(ph2, "mt%d" % i, [128, 8, 512], BF16) for i in range(1)]
                    xo = [sb(ph2, "xo%d" % i, [128, 512]) for i in range(4)]
                    ev = 0
                    for tt_ in range(8):
                        t0 = tt_ * 512
                        xt_, mt_ = xt[0], mt[0]
                        rd = [u_xres[tt_]] if l > 0 else []
                        fw.dma("sp", xt_[:], x_src[:, t0:t0 + 512].rearrange("(c p) t -> p c t", p=128), reads=rd, writes=[xt_])
                        fw.dma("sp", mt_[:], mixT[:, t0:t0 + 512].rearrange("(c p) t -> p c t", p=128), reads=[u_mix[c][tt_] for c in range(8)], writes=[mt_])
                        if tt_ == 0:
                            for c in range(8):
                                for hh in range(2):
                                    fw.dma("pool", wgu[:, c, hh * DFF:(hh + 1) * DFF], w_gu[l, c * 128:(c + 1) * 128, hh * DFF:(hh + 1) * DFF], writes=[wgu_u[c][hh]])
                            for c in range(22):
                                fw.dma("pool", wdn[:, c, :], w_dn[l, c * 128:(c + 1) * 128, :], writes=[wdn_u[c]])
                        for n_ in range(8):
                            ps = PS[ev % 3]
                            for c in range(8):
                                mm(ps[:], wo[:, c, n_ * 128:(n_ + 1) * 128], mt_[:, c, :], c == 0, c == 7, [wo, mt_], [ps])
                            xo_ = xo[ev % 4]
                            tt("dve", xo_[:], xt_[:, n_, :], ps[:], ALU.add, [xt_, ps], [xo_])
                            fw.dma("sp", xres[n_ * 128:(n_ + 1) * 128, t0:t0 + 512], xo_[:], reads=[xo_], writes=[u_xres[tt_]])
                            ev += 1
                    fw.barrier()
                if stop <= 5:
                    break
                NT = 256
                xt = [sb(ph, "xtf%d" % i, [128, 8, NT]) for i in range(2)]
                xn = [sb(ph, "xnf%d" % i, [128, 8, NT], BF16) for i in range(2)]
                sq = sb(ph, "sqf", [128, 8, NT], BF16)
                lnb = sb(ph, "lnbf", [128, NT])
                rstd = sb(ph, "rstdf", [128, NT])
                hT = sb(ph, "hT", [128, 22, NT], BF16)
                sg = [sb(ph, "sg%d" % i, [128, NT]) for i in range(2)]
                xo = [sb(ph, "xof%d" % i, [128, NT]) for i in range(4)]
                ev = 0
                NTL = S // NT

                def norm9(tt_):
                    t0 = tt_ * NT
                    xt_, xn_ = xt[tt_ % 2], xn[tt_ % 2]
                    fw.dma("sp", xt_[:], xres[:, t0:t0 + NT].rearrange("(c p) t -> p c t", p=128), reads=[u_xres[t0 // 512]], writes=[xt_])
                    act(sq[:], xt_[:], AF.Square, [xt_], [sq])
                    for c in range(8):
                        mm(PS[6][:, 0:NT], ones1024[:], sq[:, c, :], c == 0, c == 7, [ones1024, sq], [PS[6]])
                    rstd_from(PS[6][:, 0:NT], rstd[:], lnb[:], [PS[6]], lnb, rstd)
                    for c in range(8):
                        stt(xn_[:, c, :], xt_[:, c, :], pv(8 + c), rstd[:], ALU.mult, ALU.mult, [xt_, rstd, pv_all], [xn_])

                norm9(0)
                for tt_ in range(NTL):
                    t0 = tt_ * NT
                    xt_, xn_ = xt[tt_ % 2], xn[tt_ % 2]
                    ux = u_xres[t0 // 512]
                    for j in range(22):
                        psg = PS[(2 * j) % 4]
                        psu = PS[(2 * j + 1) % 4]
                        for c in range(8):
                            mm(psg[:, 0:NT], wgu[:, c, j * 128:(j + 1) * 128], xn_[:, c, :], c == 0, c == 7, [wgu_u[c][0], xn_], [psg])
                        for c in range(8):
                            mm(psu[:, 0:NT], wgu[:, c, DFF + j * 128:DFF + (j + 1) * 128], xn_[:, c, :], c == 0, c == 7, [wgu_u[c][1], xn_], [psu])
                        sg_ = sg[j % 2]
                        act(sg_[:], psg[:, 0:NT], AF.Silu, [psg], [sg_])
                        tt("dve", hT[:, j, :], sg_[:], psu[:, 0:NT], ALU.mult, [sg_, psu], [hT])
                    if tt_ + 1 < NTL:
                        norm9(tt_ + 1)
                    for n_ in range(8):
                        ps = PS[4 + n_ % 2]
                        for j in range(22):
                            mm(ps[:, 0:NT], wdn[:, j, n_ * 128:(n_ + 1) * 128], hT[:, j, :], j == 0, j == 21, [wdn_u[j], hT], [ps])
                        xo_ = xo[ev % 4]
                        tt("dve", xo_[:], xt_[:, n_, :], ps[:, 0:NT], ALU.add, [xt_, ps], [xo_])
                        fw.dma("sp", x_dst[n_ * 128:(n_ + 1) * 128, t0:t0 + NT], xo_[:], reads=[xo_], writes=[ux] if x_dst is xres else [])
                        ev += 1
                fw.barrier()
        fw.barrier()
    return nc


def _bucket(d):
    d = np.maximum(d, 0)
    large = 16 + (np.log(np.maximum(d, 1).astype(np.float32) / np.float32(16)) / np.float32(np.log(128 / 16)) * np.float32(16)).astype(np.int32)
    large = np.minimum(large, 31)
    return np.where(d < 16, d, large)


def _host_consts():
    bf = ml_dtypes.bfloat16
    c = {}
    c["cident"] = np.eye(128, dtype=np.float32).astype(bf)
    c["cJ"] = np.eye(128, dtype=np.float32)[::-1].copy().astype(bf)
    kl = np.arange(128)[:, None]
    ql = np.arange(128)[None, :]
    c["ctri"] = np.where(ql >= kl, 0.0, NEGM).astype(np.float32).astype(bf)
    bd = np.zeros((128, 128), np.float32)
    bd[0:64, 0:64] = 1.0 / 64
    bd[64:128, 64:128] = 1.0 / 64
    c["cbd64"] = bd.astype(bf)
    E = np.zeros((64, S), np.float32)
    E[np.arange(S) // 64, np.arange(S)] = 1.0
    c["cE"] = E.astype(bf)
    n = np.arange(256)
    m = np.arange(64)
    cs = n[:, None] * 16
    ss = m[None, :] * 64
    ov = np.clip(np.minimum(cs + 32, ss + 64) - np.maximum(cs, ss), 0, 32).astype(np.float32)
    ov[255] = 0
    c["covl"] = np.concatenate([ov[0:128], ov[128:256]], axis=1).astype(bf)
    d = np.arange(8208) - ZC
    oh = np.zeros((33, 8208), np.float32)
    b = _bucket(d)
    oh[b[d >= 0], np.nonzero(d >= 0)[0]] = 1.0
    oh[32, d < 0] = 1.0
    c["ohc"] = oh.astype(bf)
    d = np.arange(1152) - ZW
    oh = np.zeros((33, 1152), np.float32)
    b = _bucket(d)
    ok = (d >= 0) & (d < 512)
    oh[b[ok], np.nonzero(ok)[0]] = 1.0
    oh[32, ~ok] = 1.0
    c["ohw"] = oh.astype(bf)
    return c


def _host_layout(inp):
    f = np.float32
    C_LRU_X, C_LRU_G, C_SC_B, C_SC_C, C_SC_X = 0, 256, 512, 768, 1024
    C_FOX_Q, C_FOX_K, C_FOX_V, C_FOX_F, C_NSA_Q, C_NSA_KV, C_NSA_G = 1280, 1536, 1792, 2048, 2052, 2308, 2692
    r = lambda a, n: list(range(a, a + n))
    perm = (r(C_LRU_X, 256) + r(C_LRU_G, 256) + r(C_SC_B, 256) + r(C_SC_C, 256) + r(C_SC_X, 256)
            + r(C_FOX_Q, 256) + r(C_FOX_K, 256) + r(C_NSA_Q, 256)
            + r(C_NSA_KV + 0, 64) + r(C_NSA_KV + 64, 64) + r(C_NSA_KV + 128, 64) + r(C_NSA_KV + 256, 64)
            + r(C_FOX_F, 4)
            + r(C_FOX_V, 256) + r(C_NSA_KV + 192, 64) + r(C_NSA_KV + 320, 64) + r(C_NSA_G, 12))
    assert len(perm) == 2704 and len(set(perm)) == 2704
    m = {}
    m["w_in"] = np.ascontiguousarray(np.asarray(inp["w_in"], f)[:, :, perm])
    m["w_out"] = np.ascontiguousarray(np.asarray(inp["w_out"], f))
    m["w_gu"] = np.ascontiguousarray(np.asarray(inp["w_gate_up"], f))
    m["w_dn"] = np.ascontiguousarray(np.asarray(inp["w_down"], f))
    pvec = np.zeros((NL, 128, 64), f)
    brow = np.zeros((NL, 1, 524), f)
    wbd = np.zeros((NL, 2, 2, 128, 128), f)
    for l in range(NL):
        pvec[l, :, 0:8] = np.asarray(inp["norm_mix"][l]).reshape(8, 128).T
        pvec[l, :, 8:16] = np.asarray(inp["norm_ffn"][l]).reshape(8, 128).T
        for j in range(2):
            sl = slice(j * 128, (j + 1) * 128)
            pvec[l, :, 16 + j * 4:20 + j * 4] = np.asarray(inp["lru_conv_w"][l])[:, sl].T
            pvec[l, :, 24 + j] = np.asarray(inp["lru_conv_b"][l])[sl]
            for g in range(2):
                pvec[l, :, 26 + g * 2 + j] = np.asarray(inp["lru_b_gates"][l])[g, sl]
                for bb in range(2):
                    wbd[l, g, j, bb * 64:(bb + 1) * 64, bb * 64:(bb + 1) * 64] = np.asarray(inp["lru_w_gates"][l])[g, 2 * j + bb]
            pvec[l, :, 30 + j] = np.asarray(inp["lru_lambda"][l])[sl]
            pvec[l, :, 32 + j * 3:35 + j * 3] = np.asarray(inp["sc_conv_w"][l])[:, sl].T
            for grp in range(2):
                pvec[l, :, 38 + grp * 2 + j] = np.asarray(inp["out_norm"][l])[grp, sl]
        pvec[l, 0:64, 42] = np.asarray(inp["fox_qk_norm"][l])[0]
        pvec[l, 0:64, 43] = np.asarray(inp["fox_qk_norm"][l])[1]
        for i in range(4):
            pvec[l, 0:64, 44 + i] = np.asarray(inp["nsa_qk_norm"][l])[i]
        pvec[l, 0:4, 48] = np.asarray(inp["fox_f_bias"][l])
        for hf in range(2):
            pvec[l, hf * 64:(hf + 1) * 64, 50] = np.asarray(inp["fox_qk_norm"][l])[0]
            pvec[l, hf * 64:(hf + 1) * 64, 51] = np.asarray(inp["fox_qk_norm"][l])[1]
            pvec[l, hf * 64:(hf + 1) * 64, 52] = np.asarray(inp["nsa_qk_norm"][l])[0]
            pvec[l, hf * 64:(hf + 1) * 64, 53] = np.asarray(inp["nsa_qk_norm"][l])[2 + hf]
        brow[l, 0, 0:256] = np.asarray(inp["out_norm"][l])[2]
        brow[l, 0, 256:512] = np.asarray(inp["out_norm"][l])[3]
        brow[l, 0, 512:524] = np.asarray(inp["nsa_gate_bias"][l])
    m["pvec"] = pvec
    m["brow"] = brow
    m["wbd"] = wbd
    m["cw1"] = np.ascontiguousarray(np.asarray(inp["nsa_cmp_w1"], f))
    m["cw2"] = np.ascontiguousarray(np.asarray(inp["nsa_cmp_w2"], f))
    m["cposT"] = np.ascontiguousarray(np.asarray(inp["nsa_cmp_pos"], f).transpose(0, 1, 3, 2))
    rb = np.zeros((33, 4), f)
    rb[0:32] = np.asarray(inp["rel_bias"], f)
    rb[32] = NEGM
    m["rb33"] = rb
    m.update(_host_consts())
    return m


def kernel(**inputs):
    x = np.asarray(inputs["x"], np.float32)
    shared = _host_layout(inputs)
    nc = build()
    in_maps = []
    for b in range(8):
        d = dict(shared)
        d["xT"] = np.ascontiguousarray(x[b].T)
        in_maps.append(d)
    res = run_bass_kernel_spmd(nc, in_maps, core_ids=list(range(8)))
    out = np.stack([np.ascontiguousarray(res.results[b]["out"].T) for b in range(8)], axis=0)
    return out.astype(np.float32)
```

```python
import numpy as np
import ml_dtypes
from contextlib import ExitStack
import concourse.bass as bass
import concourse.mybir as mybir
from concourse.bass_utils import run_bass_kernel_spmd

F32 = mybir.dt.float32
BF16 = mybir.dt.bfloat16
AF = mybir.ActivationFunctionType
ALU = mybir.AluOpType

S = 4096
D = 1024
NL = 2
DFF = 2816
NFM = 2308
NTM = 396
EPS = 1e-6
NEGM = -30000.0
R_LRU_X, R_LRU_G, R_SC_B, R_SC_C, R_SC_X = 0, 256, 512, 768, 1024
R_FOX_Q, R_FOX_K, R_NSA_Q, R_KC, R_VC, R_KS, R_KW, R_FOX_F = 1280, 1536, 1792, 2048, 2112, 2176, 2240, 2304
T_FOX_V, T_VS, T_VW, T_G = 0, 256, 320, 384
ZC = 4112
ZW = 128


class U:
    __slots__ = ("lastw", "readers")

    def __init__(self):
        self.lastw = None
        self.readers = []


class T:
    def __init__(self, t):
        self.t = t
        self.u = U()

    def __getitem__(self, k):
        return self.t[k]


class FW:
    def __init__(self, nc, es, ndma=24):
        self.nc = nc
        self.engs = {}
        for name, h in [("pe", nc.tensor), ("dve", nc.vector), ("act", nc.scalar),
                        ("pool", nc.gpsimd), ("sp", nc.sync)]:
            sem = es.enter_context(nc.semaphore("sem_" + name))
            self.engs[name] = dict(h=h, sem=sem, count=0, known={})
        self.dsems = {"sp": [[es.enter_context(nc.semaphore("dqs%d" % i)), 0] for i in range(ndma)],
                      "pool": [[es.enter_context(nc.semaphore("dqp%d" % i)), 0] for i in range(ndma)]}
        self.drr = {"sp": 0, "pool": 0}
        self.ninstr = 0

    def _wait(self, en, toks):
        e = self.engs[en]
        need = {}
        for (sem, val) in toks:
            k = id(sem)
            if k not in need or need[k][1] < val:
                need[k] = (sem, val)
        for k, (sem, val) in need.items():
            if e["known"].get(k, 0) < val:
                e["h"].wait_ge(sem, val)
                e["known"][k] = val
                self.ninstr += 1

    def _deps(self, en, reads, writes):
        toks = []
        for u in reads:
            t = u.lastw
            if t is not None:
                if t[2] == en and en == "pe":
                    continue
                toks.append((t[0], t[1]))
        for u in writes:
            t = u.lastw
            same_ok = en == "pe"
            if t is not None and (t[2] != en or not same_ok):
                toks.append((t[0], t[1]))
            for r in u.readers:
                if r[2] != en or not same_ok:
                    toks.append((r[0], r[1]))
        return toks

    def _commit(self, tok, reads, writes):
        for u in writes:
            u.lastw = tok
            u.readers = []
        for u in reads:
            if u in writes:
                continue
            u.readers.append(tok)
            if len(u.readers) > 48:
                best = {}
                for r in u.readers:
                    k = id(r[0])
                    if k not in best or best[k][1] < r[1]:
                        best[k] = r
                u.readers = list(best.values())

    def op(self, en, fn, reads=(), writes=()):
        reads = [x.u if isinstance(x, T) else x for x in reads]
        writes = [x.u if isinstance(x, T) else x for x in writes]
        e = self.engs[en]
        self._wait(en, self._deps(en, reads, writes))
        ins = fn(e["h"])
        e["count"] += 1
        ins.then_inc(e["sem"], 1)
        self.ninstr += 1
        tok = (e["sem"], e["count"], en)
        self._commit(tok, reads, writes)
        return tok

    def dma(self, en, out, in_, reads=(), writes=(), **kw):
        reads = [x.u if isinstance(x, T) else x for x in reads]
        writes = [x.u if isinstance(x, T) else x for x in writes]
        e = self.engs[en]
        pool_ = self.dsems[en]
        slot = pool_[self.drr[en] % len(pool_)]
        self.drr[en] += 1
        toks = self._deps("dma", reads, writes)
        toks.append((slot[0], slot[1]))
        self._wait(en, toks)
        ins = e["h"].dma_start(out=out, in_=in_, **kw)
        slot[1] += 16
        ins.then_inc(slot[0], 16)
        self.ninstr += 1
        tok = (slot[0], slot[1], "dma")
        self._commit(tok, reads, writes)
        return tok

    def barrier(self):
        toks = []
        for n, e in self.engs.items():
            if e["count"] > 0:
                toks.append((e["sem"], e["count"]))
        for pl in self.dsems.values():
            for s in pl:
                if s[1] > 0:
                    toks.append((s[0], s[1]))
        for n in self.engs:
            self._wait(n, toks)


def build(dbg=False, nlayers=NL, stop=99):
    nc = bass.Bass("TRN2", target_bir_lowering=False)

    def din(name, shape, dt=F32):
        return nc.dram_tensor(name, list(shape), dt, kind="ExternalInput").ap()

    kind_s = "ExternalOutput" if dbg else "Internal"

    def dscr(name, shape, dt=F32):
        return nc.dram_tensor(name, list(shape), dt, kind=kind_s)

    xT = din("xT", [D, S])
    w_in = din("w_in", [NL, D, 2704])
    w_out = din("w_out", [NL, D, D])
    w_gu = din("w_gu", [NL, D, 2 * DFF])
    w_dn = din("w_dn", [NL, DFF, D])
    pvec_d = din("pvec", [NL, 128, 64])
    brow_d = din("brow", [NL, 1, 524])
    wbd_d = din("wbd", [NL, 2, 2, 128, 128])
    cw1_d = din("cw1", [NL, 2, 32, 64, 128])
    cw2_d = din("cw2", [NL, 2, 128, 64])
    cposT_d = din("cposT", [NL, 2, 64, 32])
    rb33_d = din("rb33", [33, 4])
    ohc_d = din("ohc", [33, 8208], BF16)
    ohw_d = din("ohw", [33, 1152], BF16)
    cident_d = din("cident", [128, 128], BF16)
    cJ_d = din("cJ", [128, 128], BF16)
    ctri_d = din("ctri", [128, 128], BF16)
    cE_d = din("cE", [64, S], BF16)
    covl_d = din("covl", [128, 128], BF16)
    cbd64_d = din("cbd64", [128, 128], BF16)
    out_d = nc.dram_tensor("out", [D, S], F32, kind="ExternalOutput").ap()
    zT_h = dscr("zT", [NFM, S])
    ztok_h = dscr("ztok", [S, NTM])
    mixT_h = dscr("mixT", [D, S], BF16)
    xres_h = dscr("xres", [D, S])
    ftc_h = dscr("ftc", [4, 8208], BF16)
    ftw_h = dscr("ftw", [4, 1152], BF16)
    zT, ztok, mixT, xres, ftc, ftw = (h.ap() for h in (zT_h, ztok_h, mixT_h, xres_h, ftc_h, ftw_h))
    u_zT = [[U() for _ in range(8)] for _ in range(19)]
    u_ztok = [U() for _ in range(32)]
    u_mix = [[U() for _ in range(8)] for _ in range(8)]
    u_xres = [U() for _ in range(8)]
    u_ft = U()

    with ExitStack() as es:
        fw = FW(nc, es)

        uid = [0]

        def sb(st, name, shape, dt=F32):
            uid[0] += 1
            return T(st.enter_context(nc.sbuf_tensor("%s_%d" % (name, uid[0]), list(shape), dt)))

        def psm(st, name, shape, dt=F32):
            return T(st.enter_context(nc.psum_tensor(name, list(shape), dt)))

        def act(out, in_, func, reads, writes, **kw):
            return fw.op("act", lambda e: e.activation(out=out, in_=in_, func=func, **kw), reads, writes)

        def mm(out, lhsT, rhs, start, stop, reads, writes, **kw):
            return fw.op("pe", lambda e: e.matmul(out, lhsT=lhsT, rhs=rhs, start=start, stop=stop, **kw), reads, writes)

        def tt(en, out, in0, in1, op, reads, writes):
            return fw.op(en, lambda e: e.tensor_tensor(out=out, in0=in0, in1=in1, op=op), reads, writes)

        def ts(en, out, in0, s1, s2, op0, op1, reads, writes):
            if op1 is None:
                return fw.op(en, lambda e: e.tensor_scalar(out=out, in0=in0, scalar1=s1, scalar2=None, op0=op0), reads, writes)
            return fw.op(en, lambda e: e.tensor_scalar(out=out, in0=in0, scalar1=s1, scalar2=s2, op0=op0, op1=op1), reads, writes)

        def stt(out, in0, scalar, in1, op0, op1, reads, writes):
            return fw.op("dve", lambda e: e.scalar_tensor_tensor(out=out, in0=in0, scalar=scalar, in1=in1, op0=op0, op1=op1), reads, writes)

        def cp(en, out, in_, reads, writes):
            return fw.op(en, lambda e: e.tensor_copy(out=out, in_=in_), reads, writes)

        def mset(en, ap, val, writes):
            return fw.op(en, lambda e: e.memset(ap, val), (), writes)

        ident = sb(es, "ident", [128, 128], BF16)
        Jm = sb(es, "Jm", [128, 128], BF16)
        tri = sb(es, "tri", [128, 128], BF16)
        ovl = sb(es, "ovl", [128, 128], BF16)
        bd64 = sb(es, "bd64", [128, 128], BF16)
        ones1024 = sb(es, "ones1024", [128, 128], BF16)
        ones256 = sb(es, "ones256", [128, 128], BF16)
        ones64 = sb(es, "ones64", [64, 64], BF16)
        zeros = sb(es, "zeros", [128, 512], BF16)
        slab_s = sb(es, "slab_s", [128, 4, 640], BF16)
        slab_w = sb(es, "slab_w", [128, 4, 1024], BF16)
        pv_all = sb(es, "pv_all", [128, NL, 64])
        brow_all = sb(es, "brow_all", [128, NL, 524])
        fw.dma("sp", ident[:], cident_d[:, :], writes=[ident])
        fw.dma("sp", Jm[:], cJ_d[:, :], writes=[Jm])
        fw.dma("sp", tri[:], ctri_d[:, :], writes=[tri])
        fw.dma("sp", ovl[:], covl_d[:, :], writes=[ovl])
        fw.dma("sp", bd64[:], cbd64_d[:, :], writes=[bd64])
        for l in range(NL):
            fw.dma("sp", pv_all[:, l, :], pvec_d[l], writes=[pv_all])
            fw.dma("sp", brow_all[:, l, :], brow_d[l].to_broadcast([128, 524]), writes=[brow_all])
        mset("pool", ones1024[:], 1.0 / 1024, [ones1024])
        mset("pool", ones256[:], 1.0 / 256, [ones256])
        mset("pool", ones64[:], 1.0 / 64, [ones64])
        mset("pool", zeros[:], 0.0, [zeros])

        PS = [psm(es, "ps%d" % i, [128, 512]) for i in range(7)]
        PSB = psm(es, "psb", [128, 1024], BF16)
        rb31 = sb(es, "rb31", [128, 4])
        fw.dma("sp", rb31[:], rb33_d[31:32, :].to_broadcast([128, 4]), writes=[rb31])

        with ExitStack() as ph:
            rb_f = sb(ph, "rb_f", [33, 4])
            rb_b = sb(ph, "rb_b", [33, 4], BF16)
            oh = sb(ph, "oh", [33, 8208], BF16)
            ohw_s = sb(ph, "ohw_s", [33, 1152], BF16)
            ftab = sb(ph, "ftab", [4, 8208 + 1152], BF16)
            fw.dma("sp", rb_f[:], rb33_d[:, :], writes=[rb_f])
            fw.dma("sp", oh[:], ohc_d[:, :], writes=[oh])
            fw.dma("sp", ohw_s[:], ohw_d[:, :], writes=[ohw_s])
            cp("dve", rb_b[:], rb_f[:], [rb_f], [rb_b])
            pieces = [(oh, c0, min(512, 8208 - c0), c0) for c0 in range(0, 8208, 512)]
            pieces += [(ohw_s, c0, min(512, 1152 - c0), 8208 + c0) for c0 in range(0, 1152, 512)]
            for i, (src, c0, n, dst0) in enumerate(pieces):
                ps = PS[i % 2]
                mm(ps[0:4, 0:n], rb_b[:, 0:4], src[:, c0:c0 + n], True, True, [rb_b, src], [ps])
                cp("dve", ftab[:, dst0:dst0 + n], ps[0:4, 0:n], [ps], [ftab])
            fw.dma("sp", ftc[:, :], ftab[:, 0:8208], reads=[ftab], writes=[u_ft])
            fw.dma("sp", ftw[:, :], ftab[:, 8208:8208 + 1152], reads=[ftab], writes=[u_ft])
            for h in range(4):
                src = bass.AP(tensor=ftc_h, offset=h * 8208 + ZC - 127, ap=[[1, 128], [1, 640]])
                fw.dma("sp", slab_s[:, h, :], src, reads=[u_ft], writes=[slab_s])
                src = bass.AP(tensor=ftw_h, offset=h * 1152 + ZW - 127, ap=[[1, 128], [1, 1024]])
                fw.dma("sp", slab_w[:, h, :], src, reads=[u_ft], writes=[slab_w])
            fw.barrier()

        def rstd_from(ps_ap, out_ap, tmp_ap, reads, tmpT, outT, scale=1.0):
            act(tmp_ap, ps_ap, AF.Ln, reads, [tmpT], bias=EPS, scale=scale)
            act(out_ap, tmp_ap, AF.Exp, [tmpT], [outT], scale=-0.5)

        for l in range(nlayers):
            x_src = xT if l == 0 else xres
            x_dst = out_d if l == nlayers - 1 else xres
            pv = lambda c0, c1=None, p0=0, p1=128: pv_all[p0:p1, l, c0:(c0 + 1 if c1 is None else c1)]

            with ExitStack() as ph:
                w_sb = sb(ph, "w_sb", [128, 8, 2704], BF16)
                w_u = [U() for _ in range(8)]
                for c in range(8):
                    fw.dma("pool", w_sb[:, c, :], w_in[l, c * 128:(c + 1) * 128, :], writes=[w_u[c]])
                xt = [sb(ph, "xt%d" % i, [128, 8, 512]) for i in range(2)]
                xn = [sb(ph, "xn%d" % i, [128, 8, 512], BF16) for i in range(2)]
                sq = sb(ph, "sq", [128, 8, 512], BF16)
                lnb = sb(ph, "lnb", [128, 512])
                rstd = sb(ph, "rstd", [128, 512])
                zst = [sb(ph, "zst%d" % i, [128, 512]) for i in range(4)]
                ev = 0
                f2 = lambda nm, w=512, dt=F32: [sb(ph, "%s%d" % (nm, j), [128, w], dt) for j in range(2)]
                Xh, CXh, Cb, Rr, Ii, A2 = f2("Xh", 515), f2("CXh", 514), f2("Cb"), f2("Rr"), f2("Ii"), f2("A2")
                xcb, GE, Yl, SBb, SCc, Oo, Ys = f2("xcb", 512, BF16), f2("GE"), f2("Yl"), f2("SBb"), f2("SCc"), f2("Oo"), f2("Ys")
                Hh = [f2("Hh%d_" % j) for j in range(2)]
                sqA = f2("sqA", 512, BF16)
                lnbA = sb(ph, "lnbA", [128, 512])
                rstdA = sb(ph, "rstdA", [128, 512])
                mstA = [sb(ph, "mstA%d" % i, [128, 512], BF16) for i in range(4)]
                wg_b = sb(ph, "wg_b", [128, 4, 128], BF16)
                cst = sb(ph, "cst", [128, 8])
                for g in range(2):
                    for j in range(2):
                        fw.dma("pool", wg_b[:, g * 2 + j, :], wbd_d[l, g, j], writes=[wg_b])
                act(cst[:, 4:6], pv(30, 32), AF.Exp, [pv_all], [cst], scale=-1.0)
                act(cst[:, 6:8], cst[:, 4:6], AF.Ln, [cst], [cst], bias=1.0)
                ts("dve", cst[:, 0:2], cst[:, 6:8], -8.0, None, ALU.mult, None, [cst], [cst])
                ts("dve", cst[:, 2:4], cst[:, 6:8], -16.0, None, ALU.mult, None, [cst], [cst])
                for j in range(2):
                    mset("pool", Xh[j][:, 0:3], 0.0, [Xh[j]])
                    mset("pool", CXh[j][:, 0:2], 0.0, [CXh[j]])
                mctr = [0]

                def lru_part1(tt_, j, ps):
                    if tt_ > 0:
                        cp("dve", Xh[j][:, 0:3], Xh[j][:, 512:515], [Xh[j]], [Xh[j]])
                    act(Xh[j][:, 3:515], ps[:, :], AF.Copy, [ps], [Xh[j]])
                    wc = 16 + j * 4
                    ts("dve", Cb[j][:], Xh[j][:, 0:512], pv(wc), pv(24 + j), ALU.mult, ALU.add, [Xh[j], pv_all], [Cb[j]])
                    for k in range(1, 4):
                        stt(Cb[j][:], Xh[j][:, k:k + 512], pv(wc + k), Cb[j][:], ALU.mult, ALU.add, [Xh[j], pv_all, Cb[j]], [Cb[j]])
                    act(xcb[j][:], Cb[j][:], AF.Copy, [Cb[j]], [xcb[j]])

                def lru_part2(tt_, j):
                    for g, dst in ((0, Rr[j]), (1, Ii[j])):
                        ps = PS[4 + g]
                        mm(ps[:], wg_b[:, g * 2 + j, :], xcb[j][:], True, True, [wg_b, xcb[j]], [ps])
                        act(dst[:], ps[:], AF.Sigmoid, [ps, pv_all], [dst], bias=pv(26 + g * 2 + j))
                    act(A2[j][:], Rr[j][:], AF.Exp, [Rr[j], cst], [A2[j]], scale=cst[:, 2 + j:3 + j])
                    act(Rr[j][:], Rr[j][:], AF.Exp, [Rr[j], cst], [Rr[j]], scale=cst[:, j:j + 1])
                    act(A2[j][:], A2[j][:], AF.Sqrt, [A2[j]], [A2[j]], scale=-1.0, bias=1.0)

                def lru_part2b(tt_, j):
                    tt("dve", Ii[j][:], Ii[j][:], Cb[j][:], ALU.mult, [Ii[j], Cb[j]], [Ii[j]])
                    tt("dve", Ii[j][:], Ii[j][:], A2[j][:], ALU.mult, [Ii[j], A2[j]], [Ii[j]])
                    Hc = Hh[j][tt_ % 2]
                    if tt_ == 0:
                        fw.op("dve", lambda e: e.tensor_tensor_scan(out=Hc[:], data0=Rr[j][:], data1=Ii[j][:], initial=0.0, op0=ALU.mult, op1=ALU.add), [Rr[j], Ii[j]], [Hc])
                    else:
                        Hp = Hh[j][(tt_ - 1) % 2]
                        fw.op("dve", lambda e: e.tensor_tensor_scan(out=Hc[:], data0=Rr[j][:], data1=Ii[j][:], initial=Hp[:, 511:512], op0=ALU.mult, op1=ALU.add), [Rr[j], Ii[j], Hp], [Hc])
                    tt("dve", Yl[j][:], Hc[:], GE[j][:], ALU.mult, [Hc, GE[j]], [Yl[j]])

                def sc_chain(tt_, j, ps):
                    if tt_ > 0:
                        cp("dve", CXh[j][:, 0:2], CXh[j][:, 512:514], [CXh[j]], [CXh[j]])
                    tt("dve", CXh[j][:, 2:514], SCc[j][:], ps[:, :], ALU.mult, [SCc[j], ps], [CXh[j]])
                    wc = 32 + j * 3
                    ts("dve", Oo[j][:], CXh[j][:, 0:512], pv(wc), None, ALU.mult, None, [CXh[j], pv_all], [Oo[j]])
                    for k in range(1, 3):
                        stt(Oo[j][:], CXh[j][:, k:k + 512], pv(wc + k), Oo[j][:], ALU.mult, ALU.add, [CXh[j], pv_all, Oo[j]], [Oo[j]])
                    tt("dve", Ys[j][:], Oo[j][:], SBb[j][:], ALU.mult, [Oo[j], SBb[j]], [Ys[j]])

                def outnorm_fused(tt_, Yv, grp):
                    t0 = tt_ * 512
                    for j in range(2):
                        act(sqA[j][:], Yv[j][:], AF.Square, [Yv[j]], [sqA[j]])
                    mm(PS[6][:], ones256[:], sqA[0][:], True, False, [ones256, sqA[0]], [PS[6]])
                    mm(PS[6][:], ones256[:], sqA[1][:], False, True, [ones256, sqA[1]], [PS[6]])
                    rstd_from(PS[6][:], rstdA[:], lnbA[:], [PS[6]], lnbA, rstdA)
                    for j in range(2):
                        m_ = mstA[mctr[0] % 4]
                        mctr[0] += 1
                        stt(m_[:], Yv[j][:], pv(38 + grp * 2 + j), rstdA[:], ALU.mult, ALU.mult, [Yv[j], pv_all, rstdA], [m_])
                        fw.dma("pool", mixT[grp * 256 + j * 128:grp * 256 + (j + 1) * 128, t0:t0 + 512], m_[:], reads=[m_], writes=[u_mix[grp * 2 + j][tt_]])

                xn1_u = [[U() for _ in range(8)] for _ in range(2)]

                def norm1(tt_):
                    t0 = tt_ * 512
                    xt_, xn_ = xt[tt_ % 2], xn[tt_ % 2]
                    rd = [u_xres[tt_]] if l > 0 else []
                    fw.dma("sp", xt_[:], x_src[:, t0:t0 + 512].rearrange("(c p) t -> p c t", p=128), reads=rd, writes=[xt_])
                    act(sq[:], xt_[:], AF.Square, [xt_], [sq])
                    for c in range(8):
                        mm(PS[0][:], ones1024[:], sq[:, c, :], c == 0, c == 7, [ones1024, sq], [PS[0]])
                    rstd_from(PS[0][:], rstd[:], lnb[:], [PS[0]], lnb, rstd)
                    for c in range(8):
                        stt(xn_[:, c, :], xt_[:, c, :], pv(c), rstd[:], ALU.mult, ALU.mult, [xt_, rstd, pv_all], [xn1_u[tt_ % 2][c]])

                norm1(0)
                for tt_ in range(8):
                    t0 = tt_ * 512
                    xt_, xn_ = xt[tt_ % 2], xn[tt_ % 2]
                    for oc in range(19):
                        if oc == 6 and tt_ + 1 < 8:
                            norm1(tt_ + 1)
                        m = 128 if oc < 18 else 4
                        ps = PS[1 + ev % 3]
                        for c in range(8):
                            mm(ps[0:m, :], w_sb[:, c, oc * 128:oc * 128 + m], xn_[:, c, :], c == 0, c == 7, [w_u[c], xn1_u[tt_ % 2][c]], [ps])
                        if oc == 10 or oc == 11:
                            lru_part2b(tt_, oc - 10)
                        if oc == 13:
                            outnorm_fused(tt_, Yl, 0)
                        if oc == 16:
                            outnorm_fused(tt_, Ys, 1)
                        if oc < 10:
                            j = oc % 2
                            if oc < 2:
                                lru_part1(tt_, j, ps)
                            elif oc < 4:
                                act(GE[j][:], ps[:, :], AF.Gelu_apprx_tanh, [ps], [GE[j]])
                            elif oc < 6:
                                cp("dve", SBb[j][:], ps[:, :], [ps], [SBb[j]])
                                lru_part2(tt_, j)
                            elif oc < 8:
                                act(SCc[j][:], ps[:, :], AF.Copy, [ps], [SCc[j]])
                            else:
                                sc_chain(tt_, j, ps)
                            ev += 1
                            continue
                        st_ = zst[ev % 4]
                        if ev % 2 == 0:
                            act(st_[0:m, :], ps[0:m, :], AF.Copy, [ps], [st_])
                        else:
                            cp("dve", st_[0:m, :], ps[0:m, :], [ps], [st_])
                        fw.dma("pool", zT[oc * 128:oc * 128 + m, t0:t0 + 512], st_[0:m, :], reads=[st_], writes=[u_zT[oc][tt_]])
                        ev += 1
                    for s in range(4):
                        ps = PS[1 + ev % 3]
                        for c in range(8):
                            mm(ps[:, 0:NTM], xn_[:, c, s * 128:(s + 1) * 128], w_sb[:, c, NFM:NFM + NTM], c == 0, c == 7, [w_u[c], xn1_u[tt_ % 2][c]], [ps])
                        st_ = zst[ev % 4]
                        if ev % 2 == 0:
                            act(st_[:, 0:NTM], ps[:, 0:NTM], AF.Copy, [ps], [st_])
                        else:
                            cp("dve", st_[:, 0:NTM], ps[:, 0:NTM], [ps], [st_])
                        fw.dma("pool", ztok[t0 + s * 128:t0 + (s + 1) * 128, :], st_[:, 0:NTM], reads=[st_], writes=[u_ztok[tt_ * 4 + s]])
                        ev += 1
                fw.barrier()
            if stop <= 1:
                break

            for _unused in []:
              with ExitStack() as ph:
                  NB = 4100
                  X = sb(ph, "X", [128, NB])
                  C = sb(ph, "C", [128, NB])
                  R = sb(ph, "R", [128, NB])
                  I = sb(ph, "I", [128, NB])
                  A2 = sb(ph, "A2", [128, NB])
                  Y0 = sb(ph, "Y0", [128, S])
                  xcb = sb(ph, "xcb", [128, S], BF16)
                  sq0 = sb(ph, "sq0", [128, S], BF16)
                  sq1 = sb(ph, "sq1", [128, S], BF16)
                  wg_f = sb(ph, "wg_f", [128, 4, 128])
                  wg_b = sb(ph, "wg_b", [128, 4, 128], BF16)
                  cst = sb(ph, "cst", [128, 8])
                  lnb = sb(ph, "lnb2", [128, 512])
                  rstd = sb(ph, "rstd2", [128, 512])
                  mst = [sb(ph, "mst%d" % i, [128, 512], BF16) for i in range(3)]
                  for g in range(2):
                      for j in range(2):
                          fw.dma("sp", wg_f[:, g * 2 + j, :], wbd_d[l, g, j], writes=[wg_f])
                  cp("dve", wg_b[:], wg_f[:], [wg_f], [wg_b])
                  act(cst[:, 4:6], pv(30, 32), AF.Exp, [pv_all], [cst], scale=-1.0)
                  act(cst[:, 6:8], cst[:, 4:6], AF.Ln, [cst], [cst], bias=1.0)
                  ts("dve", cst[:, 0:2], cst[:, 6:8], -8.0, None, ALU.mult, None, [cst], [cst])
                  ts("dve", cst[:, 2:4], cst[:, 6:8], -16.0, None, ALU.mult, None, [cst], [cst])

                  def outnorm_fm(Ys, sqs, grp):
                      for j in range(2):
                          act(sqs[j][:], Ys[j][:, 0:S], AF.Square, [Ys[j]], [sqs[j]])
                      for tt_ in range(8):
                          t0 = tt_ * 512
                          ps = PS[tt_ % 2]
                          mm(ps[:], ones256[:], sqs[0][:, t0:t0 + 512], True, False, [ones256, sqs[0]], [ps])
                          mm(ps[:], ones256[:], sqs[1][:, t0:t0 + 512], False, True, [ones256, sqs[1]], [ps])
                          rstd_from(ps[:], rstd[:], lnb[:], [ps], lnb, rstd)
                          for j in range(2):
                              m_ = mst[(tt_ * 2 + j) % 3]
                              stt(m_[:], Ys[j][:, t0:t0 + 512], pv(38 + grp * 2 + j), rstd[:], ALU.mult, ALU.mult, [Ys[j], pv_all, rstd], [m_])
                              fw.dma("pool", mixT[grp * 256 + j * 128:grp * 256 + (j + 1) * 128, t0:t0 + 512], m_[:], reads=[m_], writes=[u_mix[grp * 2 + j][tt_]])

                  for j in range(2):
                      mset("pool", X[:, 0:3], 0.0, [X])
                      fw.dma("sp", X[:, 3:3 + S], zT[R_LRU_X + j * 128:R_LRU_X + (j + 1) * 128, :], reads=u_zT[0 + j], writes=[X])
                      wc = 16 + j * 4
                      ts("dve", C[:, 0:S], X[:, 0:S], pv(wc), pv(24 + j), ALU.mult, ALU.add, [X, pv_all], [C])
                      for k in range(1, 4):
                          stt(C[:, 0:S], X[:, k:k + S], pv(wc + k), C[:, 0:S], ALU.mult, ALU.add, [X, pv_all, C], [C])
                      cp("pool", xcb[:], C[:, 0:S], [C], [xcb])
                      for tt_ in range(8):
                          t0 = tt_ * 512
                          for g, dst in ((0, R), (1, I)):
                              ps = PS[(tt_ * 2 + g) % 4]
                              mm(ps[:], wg_b[:, g * 2 + j, :], xcb[:, t0:t0 + 512], True, True, [wg_b, xcb], [ps])
                              act(dst[:, t0:t0 + 512], ps[:], AF.Sigmoid, [ps, pv_all], [dst], bias=pv(26 + g * 2 + j))
                      act(A2[:, 0:S], R[:, 0:S], AF.Exp, [R, cst], [A2], scale=cst[:, 2 + j:3 + j])
                      act(R[:, 0:S], R[:, 0:S], AF.Exp, [R, cst], [R], scale=cst[:, j:j + 1])
                      act(A2[:, 0:S], A2[:, 0:S], AF.Sqrt, [A2], [A2], scale=-1.0, bias=1.0)
                      tt("dve", I[:, 0:S], I[:, 0:S], C[:, 0:S], ALU.mult, [I, C], [I])
                      tt("dve", I[:, 0:S], I[:, 0:S], A2[:, 0:S], ALU.mult, [I, A2], [I])
                      fw.op("dve", lambda e: e.tensor_tensor_scan(out=C[:, 0:S], data0=R[:, 0:S], data1=I[:, 0:S], initial=0.0, op0=ALU.mult, op1=ALU.add), [R, I], [C])
                      fw.dma("sp", X[:, 0:S], zT[R_LRU_G + j * 128:R_LRU_G + (j + 1) * 128, :], reads=u_zT[2 + j], writes=[X])
                      act(A2[:, 0:S], X[:, 0:S], AF.Gelu_apprx_tanh, [X], [A2])
                      Yd = Y0 if j == 0 else C
                      tt("dve", Yd[:, 0:S], C[:, 0:S], A2[:, 0:S], ALU.mult, [C, A2], [Yd])
                  outnorm_fm([Y0, C], [sq0, sq1], 0)
                  for j in range(2):
                      Yd = Y0 if j == 0 else C
                      mset("pool", X[:, 0:2], 0.0, [X])
                      fw.dma("sp", R[:, 0:S], zT[R_SC_C + j * 128:R_SC_C + (j + 1) * 128, :], reads=u_zT[6 + j], writes=[R])
                      fw.dma("sp", I[:, 0:S], zT[R_SC_X + j * 128:R_SC_X + (j + 1) * 128, :], reads=u_zT[8 + j], writes=[I])
                      fw.dma("sp", A2[:, 0:S], zT[R_SC_B + j * 128:R_SC_B + (j + 1) * 128, :], reads=u_zT[4 + j], writes=[A2])
                      tt("dve", X[:, 2:2 + S], R[:, 0:S], I[:, 0:S], ALU.mult, [R, I], [X])
                      wc = 32 + j * 3
                      ts("dve", R[:, 0:S], X[:, 0:S], pv(wc), None, ALU.mult, None, [X, pv_all], [R])
                      for k in range(1, 3):
                          stt(R[:, 0:S], X[:, k:k + S], pv(wc + k), R[:, 0:S], ALU.mult, ALU.add, [X, pv_all, R], [R])
                      tt("dve", Yd[:, 0:S], R[:, 0:S], A2[:, 0:S], ALU.mult, [R, A2], [Yd])
                  outnorm_fm([Y0, C], [sq0, sq1], 1)
                  fw.barrier()
            if stop <= 2:
                break

            def headnorm(tl, src_ap, src_units, gcol, scale, dst, ntok):
                hq, hsq, gs, lnb, rstd = tl
                if src_ap is not None:
                    fw.dma("sp", hq[0:64, 0:ntok], src_ap, reads=src_units, writes=[hq])
                ts("dve", gs[:, 0:1], pv(gcol, p1=64), float(scale), None, ALU.mult, None, [pv_all], [gs])
                act(hsq[0:64, 0:ntok], hq[0:64, 0:ntok], AF.Square, [hq], [hsq])
                for t0 in range(0, ntok, 512):
                    n = min(512, ntok - t0)
                    ps = PS[5 + (t0 // 512) % 2]
                    mm(ps[0:64, 0:n], ones64[:], hsq[0:64, t0:t0 + n], True, True, [ones64, hsq], [ps])
                    rstd_from(ps[0:64, 0:n], rstd[0:64, 0:n], lnb[0:64, 0:n], [ps], lnb, rstd)
                    stt(dst[0:64, t0:t0 + n], hq[0:64, t0:t0 + n], gs[:, 0:1], rstd[0:64, 0:n], ALU.mult, ALU.mult, [hq, gs, rstd], [dst])

            def headnorm2(tl, k, src_ap, src_units, gcol, scale, dstA, dstB):
                hq2s, hsq2, lnall, hb, gs2 = tl
                hq2 = hq2s[k % len(hq2s)]
                fw.dma("sp", hq2[:, 0:S], src_ap, reads=src_units, writes=[hq2])
                ts("dve", gs2[:, 0:1], pv(gcol), float(scale), None, ALU.mult, None, [pv_all], [gs2])
                act(hsq2[:, 0:S], hq2[:, 0:S], AF.Square, [hq2], [hsq2])
                for t in range(8):
                    ps = PS[5 + t % 2]
                    mm(ps[:, :], bd64[:], hsq2[:, t * 512:(t + 1) * 512], True, True, [bd64, hsq2], [ps])
                    act(lnall[:, t * 512:(t + 1) * 512], ps[:, :], AF.Ln, [ps], [lnall], bias=EPS)
                act(lnall[:, :], lnall[:, :], AF.Exp, [lnall], [lnall], scale=-0.5)
                stt(hb[:, :], hq2[:, 0:S], gs2[:, 0:1], lnall[:, :], ALU.mult, ALU.mult, [hq2, gs2, lnall], [hb])
                fw.dma("sp", dstA[0:64, :], hb[0:64, :], reads=[hb], writes=[dstA])
                fw.dma("sp", dstB[0:64, :], hb[64:128, :], reads=[hb], writes=[dstB])

            def run_attention(tiles, pts):
                n_t = len(tiles)
                ring = PS[0:3]

                def emit_pre(i):
                    if i < n_t and tiles[i].get("pre") is not None:
                        bt, src = tiles[i]["pre"]
                        fw.dma("sp", bt[:], src, reads=[u_ft], writes=[bt])

                emit_pre(0)

                def emit_qk(i):
                    tl = tiles[i]
                    ps = ring[i % 3]
                    emit_pre(i + 1)
                    np_ = tl["np"]
                    last = len(tl["mms"]) - 1
                    for idx, (c0, ncol, lhsT, rhs, rds) in enumerate(tl["mms"]):
                        mm(ps[0:np_, c0:c0 + ncol], lhsT, rhs, idx == 0, idx == last, rds, [ps], skip_group_check=True)

                def emit_rest(i):
                    tl = tiles[i]
                    ps = ring[i % 3]
                    pt = pts[i % len(pts)]
                    np_, n = tl["np"], tl["n"]
                    if tl.get("bias") is not None:
                        act(pt[0:np_, 0:n], ps[0:np_, 0:n], AF.Exp, [ps] + tl["bias_reads"], [pt], bias=tl["bias"])
                    else:
                        act(pt[0:np_, 0:n], ps[0:np_, 0:n], AF.Exp, [ps], [pt])
                    if tl.get("init") is not None:
                        accT, w = tl["init"]
                        mm(accT[:, 0:w], zeros[:, 0:128], zeros[:, 0:w], True, True, [zeros], [accT])
                    for (accT, acc_ap, pc0, V_ap, rds) in tl["pv"]:
                        mm(acc_ap, pt[0:np_, pc0:pc0 + 128], V_ap, False, True, [pt] + rds, [accT], skip_group_check=True)
                    if tl.get("fin") is not None:
                        tl["fin"]()

                LA = 2
                for i in range(n_t + LA):
                    if i < n_t:
                        emit_qk(i)
                    if i >= LA:
                        emit_rest(i - LA)

            def outnorm_tok(tl, Q, yv, yT, gain_ap, rowbase):
                junk, ssq, lnq, rs, ym, mst2 = tl
                q0 = Q * 512
                for s in range(4):
                    act(junk[:, 0:256], yv(s), AF.Square, [yT], [junk, ssq], accum_out=ssq[:, s:s + 1])
                act(lnq[:, 0:4], ssq[:, 0:4], AF.Ln, [ssq], [lnq], scale=1.0 / 256, bias=EPS)
                act(rs[:, 0:4], lnq[:, 0:4], AF.Exp, [lnq], [rs], scale=-0.5)
                for s in range(4):
                    stt(ym[:, s, :], yv(s), rs[:, s:s + 1], gain_ap, ALU.mult, ALU.mult, [yT, rs, brow_all], [ym])
                for j in range(2):
                    for s in range(4):
                        fw.op("pe", lambda e, s=s, j=j: e.transpose(out=PSB[:, s * 128:(s + 1) * 128], in_=ym[:, s, j * 128:(j + 1) * 128], identity=ident[:]), [ym, ident], [PSB])
                    m_ = mst2[j]
                    cp("dve", m_[:], PSB[:, 0:512], [PSB], [m_])
                    fw.dma("pool", mixT[rowbase + j * 128:rowbase + (j + 1) * 128, q0:q0 + 512], m_[:], reads=[m_], writes=[u_mix[rowbase // 128 + j][Q]])

            with ExitStack() as ph:
                Qp = [sb(ph, "Qp%d" % h, [128, S], BF16) for h in range(4)]
                Kp = [sb(ph, "Kp%d" % h, [128, S], BF16) for h in range(4)]
                with ExitStack() as ph2:
                    fr = sb(ph2, "fr", [4, S])
                    e1 = sb(ph2, "e1", [4, S])
                    cc = sb(ph2, "cc", [4, S])
                    ones4 = sb(ph2, "ones4", [4, S], BF16)
                    nb = sb(ph2, "nb", [4, 1])
                    csp = sb(ph2, "csp", [4, 3, S], BF16)
                    ncsp = sb(ph2, "ncsp", [4, 3, S], BF16)
                    fw.dma("sp", fr[:], zT[R_FOX_F:R_FOX_F + 4, :], reads=u_zT[18], writes=[fr])
                    ts("dve", nb[:], pv(48, p1=4), -1.0, None, ALU.mult, None, [pv_all], [nb])
                    act(e1[:], fr[:], AF.Exp, [fr, nb], [e1], scale=-1.0, bias=nb[:])
                    act(e1[:], e1[:], AF.Ln, [e1], [e1], bias=1.0)
                    ts("dve", e1[:], e1[:], -1.0, None, ALU.mult, None, [e1], [e1])
                    mset("pool", ones4[:], 1.0, [ones4])
                    fw.op("dve", lambda e: e.tensor_tensor_scan(out=cc[:], data0=ones4[:], data1=e1[:], initial=0.0, op0=ALU.mult, op1=ALU.add), [ones4, e1], [cc])
                    cp("dve", csp[:, 0, :], cc[:], [cc], [csp])
                    cp("dve", fr[:], csp[:, 0, :], [csp], [fr])
                    tt("dve", cc[:], cc[:], fr[:], ALU.subtract, [cc, fr], [cc])
                    cp("dve", csp[:, 1, :], cc[:], [cc], [csp])
                    cp("dve", fr[:], csp[:, 1, :], [csp], [fr])
                    tt("dve", cc[:], cc[:], fr[:], ALU.subtract, [cc, fr], [cc])
                    cp("dve", csp[:, 2, :], cc[:], [cc], [csp])
                    ts("dve", ncsp[:], csp[:], -1.0, None, ALU.mult, None, [csp], [ncsp])
                    for h in range(4):
                        mset("pool", Qp[h][64:128, :], 0.0, [Qp[h]])
                        mset("pool", Kp[h][64:128, :], 0.0, [Kp[h]])
                        for i in range(3):
                            fw.dma("sp", Qp[h][64 + i:65 + i, :], csp[h:h + 1, i, :], reads=[csp], writes=[Qp[h]])
                            fw.dma("sp", Kp[h][96 + i:97 + i, :], ncsp[h:h + 1, i, :], reads=[ncsp], writes=[Kp[h]])
                        fw.dma("sp", Qp[h][96:99, :], ones4[0:3, :], reads=[ones4], writes=[Qp[h]])
                        fw.dma("sp", Kp[h][64:67, :], ones4[0:3, :], reads=[ones4], writes=[Kp[h]])
                    fw.barrier()
                V1 = sb(ph, "V1", [128, 32, 4, 65], BF16)
                hn2 = ([sb(ph, "hq2_%d" % i, [128, S]) for i in range(2)], sb(ph, "hsq2", [128, S], BF16),
                       sb(ph, "lnall", [128, S]), sb(ph, "hb", [128, S], BF16), sb(ph, "gs2", [128, 1]))
                vst = sb(ph, "vst", [128, 8, 256])
                pts = [sb(ph, "pt%d" % i, [128, 512], BF16) for i in range(3)]
                rden = [sb(ph, "rden%d" % i, [128, 4]) for i in range(2)]
                ytk = [sb(ph, "ytk%d" % i, [128, 4, 256]) for i in range(2)]
                on_tl = (sb(ph, "junk", [128, 256]), sb(ph, "ssq", [128, 4]), sb(ph, "lnq", [128, 4]), sb(ph, "rs", [128, 4]),
                         sb(ph, "ym", [128, 4, 256], BF16), [sb(ph, "mst2_%d" % i, [128, 512], BF16) for i in range(2)])
                for j in range(2):
                    headnorm2(hn2, 2 * j, zT[R_FOX_Q + j * 128:R_FOX_Q + (j + 1) * 128, :], u_zT[10 + j], 50, 0.125, Qp[2 * j], Qp[2 * j + 1])
                    headnorm2(hn2, 2 * j + 1, zT[R_FOX_K + j * 128:R_FOX_K + (j + 1) * 128, :], u_zT[12 + j], 51, 1.0, Kp[2 * j], Kp[2 * j + 1])
                mset("pool", V1[:, :, :, 64:65], 1.0, [V1])
                for g in range(4):
                    fw.dma("sp", vst[:], ztok[g * 1024:(g + 1) * 1024, T_FOX_V:T_FOX_V + 256].rearrange("(k p) c -> p k c", p=128), reads=u_ztok[g * 8:(g + 1) * 8], writes=[vst])
                    for h in range(4):
                        cp("pool" if h % 2 else "dve", V1[:, g * 8:(g + 1) * 8, h, 0:64], vst[:, :, h * 64:(h + 1) * 64], [vst], [V1])
                accn = 0
                for Q in range(8):
                    q0 = Q * 512
                    yT = ytk[Q % 2]
                    tiles = []
                    for h in range(4):
                        accT = PS[3 + accn % 2]
                        rd_ = rden[accn % 2]
                        accn += 1
                        nk = 4 * Q + 4
                        for kt in range(nk):
                            k0 = kt * 128
                            d = kt - 4 * Q
                            dd = max(d, 0)
                            n = 512 - 128 * dd
                            mms = [(0, n, Kp[h][:, k0:k0 + 128], Qp[h][:, q0 + 128 * dd:q0 + 512], [Kp[h], Qp[h]])]
                            if d >= 0:
                                mms.append((0, 128, ident[:], tri[:], [ident, tri]))
                            tl = dict(np=128, n=n, mms=mms, pv=[])
                            for s in range(dd, 4):
                                tl["pv"].append((accT, accT[:, s * 65:(s + 1) * 65], (s - dd) * 128, V1[:, kt, h, :], [V1]))
                            if kt == 0:
                                tl["init"] = (accT, 260)
                            if kt == nk - 1:
                                def fin(accT=accT, rd_=rd_, h=h, yT=yT):
                                    accv = accT[:, 0:260].rearrange("p (s c) -> p s c", c=65)
                                    fw.op("dve", lambda e: e.reciprocal(out=rd_[:, 0:4], in_=accv[:, :, 64]), [accT], [rd_])
                                    for s in range(4):
                                        ts("dve", yT[:, s, h * 64:(h + 1) * 64], accv[:, s, 0:64], rd_[:, s:s + 1], None, ALU.mult, None, [accT, rd_], [yT])
                                tl["fin"] = fin
                            tiles.append(tl)
                    run_attention(tiles, pts)
                    outnorm_tok(on_tl, Q, lambda s, yT=yT: yT[:, s, :], yT, brow_all[:, l, 0:256], 512)
                fw.barrier()
            if stop <= 3:
                break

            with ExitStack() as ph:
                Qn = [sb(ph, "Qn%d" % h, [128, S], BF16) for h in range(4)]
                KsT = sb(ph, "KsT", [128, S], BF16)
                KwT = sb(ph, "KwT", [128, S], BF16)
                KcT = sb(ph, "KcT", [128, 256], BF16)
                for h in range(4):
                    mset("pool", Qn[h][64:128, :], 0.0, [Qn[h]])
                mset("pool", KwT[64:128, :], 0.0, [KwT])
                mset("pool", KcT[64:128, :], 0.0, [KcT])
                fw.dma("sp", KsT[64:128, :], cE_d[:, :], writes=[KsT])
                VS1 = sb(ph, "VS1", [128, 32, 65], BF16)
                VW1 = sb(ph, "VW1", [128, 32, 65], BF16)
                VC1 = sb(ph, "VC1", [128, 2, 128], BF16)
                G = sb(ph, "G", [128, 32, 12])
                yd = sb(ph, "yd", [128, 32, 256])
                hq = sb(ph, "hq5", [128, 4112])
                hsq = sb(ph, "hsq5", [128, 4112], BF16)
                hn2 = ([hq], hsq, sb(ph, "lnall5", [128, S]), sb(ph, "hb5", [128, S], BF16), sb(ph, "gs25", [128, 1]))
                gs = sb(ph, "gs5", [64, 1])
                lnb = sb(ph, "lnb5", [64, 512])
                rstd = sb(ph, "rstd5", [64, 512])
                hn = (hq, hsq, gs, lnb, rstd)
                w1b = sb(ph, "w1b", [64, 32, 128], BF16)
                posb = sb(ph, "posb", [64, 32], BF16)
                w2b = sb(ph, "w2b", [128, 64], BF16)
                hbias = sb(ph, "hbias", [128, 1])
                hidb = sb(ph, "hidb", [128, 256], BF16)
                vst2 = sb(ph, "vst2", [128, 32, 64])
                gst = sb(ph, "gst", [128, 32, 12])
                pts = [sb(ph, "ptn%d" % i, [128, 512], BF16) for i in range(3)]
                btile = [sb(ph, "btile%d" % i, [128, 512], BF16) for i in range(3)]
                den = [sb(ph, "den%d" % i, [128, 4]) for i in range(2)]
                rden = [sb(ph, "rdenn%d" % i, [128, 4]) for i in range(2)]
                coef = [sb(ph, "coef%d" % i, [128, 4]) for i in range(2)]
                impacc = sb(ph, "impacc", [128, 4, 64])
                imp2 = sb(ph, "imp2", [128, 64])
                m8 = sb(ph, "m8", [128, 8])
                m8b = sb(ph, "m8b", [128, 8])
                self_ = sb(ph, "self", [128, 64])
                selb = sb(ph, "selb", [128, 128], BF16)
                on_tl = (sb(ph, "junk5", [128, 256]), sb(ph, "ssq5", [128, 4]), sb(ph, "lnq5", [128, 4]), sb(ph, "rs5", [128, 4]),
                         sb(ph, "ym5", [128, 4, 256], BF16), [sb(ph, "mst5_%d" % i, [128, 512], BF16) for i in range(2)])
                for j in range(2):
                    headnorm2(hn2, 0, zT[R_NSA_Q + j * 128:R_NSA_Q + (j + 1) * 128, :], u_zT[14 + j], 52, 0.125, Qn[2 * j], Qn[2 * j + 1])
                headnorm2(hn2, 0, zT[R_KS:R_KS + 128, :], u_zT[17], 53, 1.0, KsT, KwT)
                for kv in range(2):
                    rows = R_KC if kv == 0 else R_VC
                    fw.dma("sp", hq[0:64, 0:S], zT[rows:rows + 64, :], reads=u_zT[16], writes=[hq])
                    mset("pool", hq[0:64, S:4112], 0.0, [hq])
                    cp("dve", hsq[0:64, :], hq[0:64, :], [hq], [hsq])
                    fw.dma("pool", w1b[:], cw1_d[l, kv].rearrange("l d m -> d l m"), writes=[w1b])
                    fw.dma("pool", posb[:], cposT_d[l, kv], writes=[posb])
                    fw.dma("pool", w2b[:], cw2_d[l, kv], writes=[w2b])
                    ps = PS[5]
                    for li in range(32):
                        mm(ps[:, 0:1], w1b[:, li, :], posb[:, li:li + 1], li == 0, li == 31, [w1b, posb], [ps])
                    cp("dve", hbias[:], ps[:, 0:1], [ps], [hbias])
                    ps = PS[6]
                    for li in range(32):
                        if li < 16:
                            rhs = hsq[0:64, 0:4096].rearrange("p (n r) -> p n r", r=16)[:, :, li]
                        else:
                            rhs = hsq[0:64, 16:4112].rearrange("p (n r) -> p n r", r=16)[:, :, li - 16]
                        mm(ps[:, 0:256], w1b[:, li, :], rhs, li == 0, li == 31, [w1b, hsq], [ps])
                    act(hidb[:], ps[:, 0:256], AF.Gelu_apprx_tanh, [ps, hbias], [hidb], bias=hbias[:])
                    if kv == 0:
                        ps = PS[5]
                        mm(ps[0:64, 0:256], w2b[:], hidb[:], True, True, [w2b, hidb], [ps])
                        cp("dve", hq[0:64, 0:256], ps[0:64, 0:256], [ps], [hq])
                        headnorm(hn, None, None, 45, 1.0, KcT, 256)
                    else:
                        for c in range(2):
                            ps = PS[5]
                            mm(ps[:, 0:64], hidb[:, c * 128:(c + 1) * 128], w2b[:], True, True, [w2b, hidb], [ps])
                            cp("dve", VC1[:, c, 0:64], ps[:, 0:64], [ps], [VC1])
                            cp("dve", VC1[:, c, 64:128], ovl[:, c * 64:(c + 1) * 64], [ovl], [VC1])
                for (VT, col) in ((VS1, T_VS), (VW1, T_VW)):
                    fw.dma("sp", vst2[:], ztok[:, col:col + 64].rearrange("(k p) c -> p k c", p=128), reads=u_ztok, writes=[vst2])
                    cp("dve", VT[:, :, 0:64], vst2[:], [vst2], [VT])
                    mset("pool", VT[:, :, 64:65], 1.0, [VT])
                fw.dma("sp", gst[:], ztok[:, T_G:T_G + 12].rearrange("(k p) c -> p k c", p=128), reads=u_ztok, writes=[gst])
                tt("dve", gst[:], gst[:], brow_all[:, l, 512:524].unsqueeze(1).to_broadcast([128, 32, 12]), ALU.add, [gst, brow_all], [gst])
                act(G[:], gst[:], AF.Sigmoid, [gst], [G])
                mset("pool", selb[:], 0.0, [selb])

                accn = 0
                bn = 0
                for Q in range(8):
                    q0 = Q * 512

                    def fin_generic(accT, w, h, branch, first, dn, rd_, cf_, Q=Q):
                        accv = accT[:, 0:4 * w].rearrange("p (s c) -> p s c", c=w)
                        if branch == 0:
                            fw.op("dve", lambda e: e.tensor_reduce(out=dn[:, 0:4], in_=accv[:, :, 64:128], axis=mybir.AxisListType.X, op=ALU.add), [accT], [dn])
                            ts("dve", dn[:, 0:4], dn[:, 0:4], 1.0 / 32, 1e-30, ALU.mult, ALU.max, [dn], [dn])
                            fw.op("dve", lambda e: e.reciprocal(out=rd_[:, 0:4], in_=dn[:, 0:4]), [dn], [rd_])
                        else:
                            fw.op("dve", lambda e: e.reciprocal(out=rd_[:, 0:4], in_=accv[:, :, 64]), [accT], [rd_])
                        tt("dve", cf_[:, 0:4], rd_[:, 0:4], G[:, 4 * Q:4 * Q + 4, branch * 4 + h], ALU.mult, [rd_, G], [cf_])
                        for s in range(4):
                            ydv = yd[:, 4 * Q + s, h * 64:(h + 1) * 64]
                            if first:
                                ts("dve", ydv, accv[:, s, 0:64], cf_[:, s:s + 1], None, ALU.mult, None, [accT, cf_], [yd])
                            else:
                                stt(ydv, accv[:, s, 0:64], cf_[:, s:s + 1], ydv, ALU.mult, ALU.add, [accT, cf_, yd], [yd])
                        if branch == 0:
                            for s in range(4):
                                if h == 0:
                                    ts("dve", impacc[:, s, :], accv[:, s, 64:128], rd_[:, s:s + 1], None, ALU.mult, None, [accT, rd_], [impacc])
                                else:
                                    stt(impacc[:, s, :], accv[:, s, 64:128], rd_[:, s:s + 1], impacc[:, s, :], ALU.mult, ALU.add, [accT, rd_, impacc], [impacc])

                    tiles = []
                    for h in range(4):
                        accT = PS[3 + accn % 2]
                        dn, rd_, cf_ = den[accn % 2], rden[accn % 2], coef[accn % 2]
                        accn += 1
                        chunks = [0, 1] if Q >= 4 else [0]
                        for c in chunks:
                            bt = btile[bn % 3]
                            bn += 1
                            off = h * 8208 + ZC + q0 - 31 - 16 * (c * 128 + 127)
                            src = bass.AP(tensor=ftc_h, offset=off, ap=[[16, 128], [1, 512]])
                            tl = dict(np=128, n=512, pv=[])
                            tl["pre"] = (bt, src)
                            tl["mms"] = [(0, 512, KcT[:, c * 128:(c + 1) * 128], Qn[h][:, q0:q0 + 512], [KcT, Qn[h]]),
                                         (0, 512, Jm[:], bt[:], [Jm, bt])]
                            for s in range(4):
                                tl["pv"].append((accT, accT[:, s * 128:(s + 1) * 128], s * 128, VC1[:, c, :], [VC1]))
                            if c == 0:
                                tl["init"] = (accT, 512)
                            if c == chunks[-1]:
                                tl["fin"] = (lambda accT=accT, h=h, dn=dn, rd_=rd_, cf_=cf_: fin_generic(accT, 128, h, 0, True, dn, rd_, cf_))
                            tiles.append(tl)
                    run_attention(tiles, pts)

                    tiles = []
                    for h in range(4):
                        accT = PS[3 + accn % 2]
                        dn, rd_, cf_ = den[accn % 2], rden[accn % 2], coef[accn % 2]
                        accn += 1
                        kts = [kt for kt in range(4 * Q - 4, 4 * Q + 4) if kt >= 0]
                        for kt in kts:
                            k0 = kt * 128
                            d = kt - 4 * Q
                            dd = max(d, 0)
                            n = 512 - 128 * dd
                            sc0 = 128 * (-d) if d < 0 else 0
                            tl = dict(np=128, n=n, pv=[])
                            tl["mms"] = [(0, n, KwT[:, k0:k0 + 128], Qn[h][:, q0 + 128 * dd:q0 + 512], [KwT, Qn[h]]),
                                         (0, n, Jm[:], slab_w[:, h, sc0:sc0 + n], [Jm, slab_w])]
                            for s in range(dd, 4):
                                tl["pv"].append((accT, accT[:, s * 65:(s + 1) * 65], (s - dd) * 128, VW1[:, kt, :], [VW1]))
                            if kt == kts[0]:
                                tl["init"] = (accT, 260)
                            if kt == kts[-1]:
                                tl["fin"] = (lambda accT=accT, h=h, dn=dn, rd_=rd_, cf_=cf_: fin_generic(accT, 65, h, 2, False, dn, rd_, cf_))
                            tiles.append(tl)
                    run_attention(tiles, pts)

                    for s in range(4):
                        i_ = 4 * Q + s
                        iv = impacc[:, s, :]
                        if 2 * i_ + 2 < 64:
                            mset("dve", impacc[:, s, 2 * i_ + 2:64], -1e30, [impacc])
                        mset("dve", impacc[0:64, s, 2 * i_ + 1:2 * i_ + 2], -1e30, [impacc])
                        mset("dve", impacc[64:128, s, 2 * i_ + 1:2 * i_ + 2], 1e30, [impacc])
                        mset("dve", impacc[:, s, 2 * i_:2 * i_ + 1], 1e30, [impacc])
                        if i_ >= 1:
                            mset("dve", impacc[0:64, s, 2 * i_ - 1:2 * i_], 1e30, [impacc])
                        mset("dve", impacc[:, s, 0:1], 1e30, [impacc])
                        fw.op("dve", lambda e, iv=iv: e.max(out=m8[:], in_=iv), [impacc], [m8])
                        fw.op("dve", lambda e, iv=iv: e.match_replace(out=imp2[:], in_to_replace=m8[:], in_values=iv, imm_value=-3e38), [impacc, m8], [imp2])
                        fw.op("dve", lambda e: e.max(out=m8b[:], in_=imp2[:]), [imp2], [m8b])
                        ts("dve", self_[:], iv, m8b[:, 7:8], -1.0, ALU.is_ge, ALU.add, [impacc, m8b], [self_])
                        ts("dve", selb[:, 64:128], self_[:], -NEGM, None, ALU.mult, None, [self_], [selb])
                        fw.op("pe", lambda e, s=s: e.transpose(out=PSB[:, s * 128:(s + 1) * 128], in_=selb[:], identity=ident[:]), [selb, ident], [PSB])
                    for h in range(4):
                        cp("dve", Qn[h][64:128, q0:q0 + 512], PSB[64:128, 0:512], [PSB], [Qn[h]])

                    tiles = []
                    for h in range(4):
                        accT = PS[3 + accn % 2]
                        dn, rd_, cf_ = den[accn % 2], rden[accn % 2], coef[accn % 2]
                        accn += 1
                        nk = 4 * Q + 4
                        for kt in range(nk):
                            k0 = kt * 128
                            d = kt - 4 * Q
                            dd = max(d, 0)
                            n = 512 - 128 * dd
                            tl = dict(np=128, n=n, pv=[])
                            tl["mms"] = [(0, n, KsT[:, k0:k0 + 128], Qn[h][:, q0 + 128 * dd:q0 + 512], [KsT, Qn[h]])]
                            if d >= -1:
                                sc0 = 128 if d == -1 else 0
                                tl["mms"].append((0, n, Jm[:], slab_s[:, h, sc0:sc0 + n], [Jm, slab_s]))
                            else:
                                tl["bias"] = rb31[:, h:h + 1]
                                tl["bias_reads"] = [rb31]
                            for s in range(dd, 4):
                                tl["pv"].append((accT, accT[:, s * 65:(s + 1) * 65], (s - dd) * 128, VS1[:, kt, :], [VS1]))
                            if kt == 0:
                                tl["init"] = (accT, 260)
                            if kt == nk - 1:
                                tl["fin"] = (lambda accT=accT, h=h, dn=dn, rd_=rd_, cf_=cf_: fin_generic(accT, 65, h, 1, False, dn, rd_, cf_))
                            tiles.append(tl)
                    run_attention(tiles, pts)
                    outnorm_tok(on_tl, Q, lambda s, Q=Q: yd[:, 4 * Q + s, :], yd, brow_all[:, l, 256:512], 768)
                fw.barrier()
            if stop <= 4:
                break

            with ExitStack() as ph:
                wgu = sb(ph, "wgu", [128, 8, 2 * DFF], BF16)
                wdn = sb(ph, "wdn", [128, 22, 1024], BF16)
                wgu_u = [[U(), U()] for _ in range(8)]
                wdn_u = [U() for _ in range(22)]
                with ExitStack() as ph2:
                    wo = sb(ph2, "wo", [128, 8, 1024], BF16)
                    for c in range(8):
                        fw.dma("pool", wo[:, c, :], w_out[l, c * 128:(c + 1) * 128, :], writes=[wo])
                    xt = [sb(ph2, "xto%d" % i, [128, 8, 512]) for i in range(1)]
                    mt = [sb(ph2, "mt%d" % i, [128, 8, 512], BF16) for i in range(1)]
                    xo = [sb(ph2, "xo%d" % i, [128, 512]) for i in range(4)]
                    ev = 0
                    for tt_ in range(8):
                        t0 = tt_ * 512
                        xt_, mt_ = xt[0], mt[0]
                        rd = [u_xres[tt_]] if l > 0 else []
                        fw.dma("sp", xt_[:], x_src[:, t0:t0 + 512].rearrange("(c p) t -> p c t", p=128), reads=rd, writes=[xt_])
                        fw.dma("sp", mt_[:], mixT[:, t0:t0 + 512].rearrange("(c p) t -> p c t", p=128), reads=[u_mix[c][tt_] for c in range(8)], writes=[mt_])
                        if tt_ == 0:
                            for c in range(8):
                                for hh in range(2):
                                    fw.dma("pool", wgu[:, c, hh * DFF:(hh + 1) * DFF], w_gu[l, c * 128:(c + 1) * 128, hh * DFF:(hh + 1) * DFF], writes=[wgu_u[c][hh]])
                            for c in range(22):
                                fw.dma("pool", wdn[:, c, :], w_dn[l, c * 128:(c + 1) * 128, :], writes=[wdn_u[c]])
                        for n_ in range(8):
                            ps = PS[ev % 3]
                            for c in range(8):
                                mm(ps[:], wo[:, c, n_ * 128:(n_ + 1) * 128], mt_[:, c, :], c == 0, c == 7, [wo, mt_], [ps])
                            xo_ = xo[ev % 4]
                            tt("dve", xo_[:], xt_[:, n_, :], ps[:], ALU.add, [xt_, ps], [xo_])
                            fw.dma("sp", xres[n_ * 128:(n_ + 1) * 128, t0:t0 + 512], xo_[:], reads=[xo_], writes=[u_xres[tt_]])
                            ev += 1
                    fw.barrier()
                if stop <= 5:
                    break
                NT = 256
                xt = [sb(ph, "xtf%d" % i, [128, 8, NT]) for i in range(2)]
                xn = [sb(ph, "xnf%d" % i, [128, 8, NT], BF16) for i in range(2)]
                sq = sb(ph, "sqf", [128, 8, NT], BF16)
                lnb = sb(ph, "lnbf", [128, NT])
                rstd = sb(ph, "rstdf", [128, NT])
                hT = sb(ph, "hT", [128, 22, NT], BF16)
                hT_u = [U() for _ in range(22)]
                xn_u = [[U() for _ in range(8)] for _ in range(2)]
                sg = [sb(ph, "sg%d" % i, [128, NT]) for i in range(2)]
                xo = [sb(ph, "xof%d" % i, [128, NT]) for i in range(4)]
                ev = 0
                NTL = S // NT

                def norm9(tt_):
                    t0 = tt_ * NT
                    xt_, xn_ = xt[tt_ % 2], xn[tt_ % 2]
                    fw.dma("sp", xt_[:], xres[:, t0:t0 + NT].rearrange("(c p) t -> p c t", p=128), reads=[u_xres[t0 // 512]], writes=[xt_])
                    act(sq[:], xt_[:], AF.Square, [xt_], [sq])
                    for c in range(8):
                        mm(PS[6][:, 0:NT], ones1024[:], sq[:, c, :], c == 0, c == 7, [ones1024, sq], [PS[6]])
                    rstd_from(PS[6][:, 0:NT], rstd[:], lnb[:], [PS[6]], lnb, rstd)
                    for c in range(8):
                        stt(xn_[:, c, :], xt_[:, c, :], pv(8 + c), rstd[:], ALU.mult, ALU.mult, [xt_, rstd, pv_all], [xn_u[tt_ % 2][c]])

                norm9(0)
                for tt_ in range(NTL):
                    t0 = tt_ * NT
                    xt_, xn_ = xt[tt_ % 2], xn[tt_ % 2]
                    ux = u_xres[t0 // 512]
                    for j in range(22):
                        psg = PS[(2 * j) % 4]
                        psu = PS[(2 * j + 1) % 4]
                        for c in range(8):
                            mm(psg[:, 0:NT], wgu[:, c, j * 128:(j + 1) * 128], xn_[:, c, :], c == 0, c == 7, [wgu_u[c][0], xn_u[tt_ % 2][c]], [psg])
                        for c in range(8):
                            mm(psu[:, 0:NT], wgu[:, c, DFF + j * 128:DFF + (j + 1) * 128], xn_[:, c, :], c == 0, c == 7, [wgu_u[c][1], xn_u[tt_ % 2][c]], [psu])
                        sg_ = sg[j % 2]
                        act(sg_[:], psg[:, 0:NT], AF.Silu, [psg], [sg_])
                        tt("dve", hT[:, j, :], sg_[:], psu[:, 0:NT], ALU.mult, [sg_, psu], [hT_u[j]])
                    if tt_ + 1 < NTL:
                        norm9(tt_ + 1)
                    for n_ in range(8):
                        ps = PS[4 + n_ % 2]
                        for j in range(22):
                            mm(ps[:, 0:NT], wdn[:, j, n_ * 128:(n_ + 1) * 128], hT[:, j, :], j == 0, j == 21, [wdn_u[j], hT_u[j]], [ps])
                        xo_ = xo[ev % 4]
                        tt("dve", xo_[:], xt_[:, n_, :], ps[:, 0:NT], ALU.add, [xt_, ps], [xo_])
                        fw.dma("sp", x_dst[n_ * 128:(n_ + 1) * 128, t0:t0 + NT], xo_[:], reads=[xo_], writes=[ux] if x_dst is xres else [])
                        ev += 1
                fw.barrier()
        fw.barrier()
    return nc


def _bucket(d):
    d = np.maximum(d, 0)
    large = 16 + (np.log(np.maximum(d, 1).astype(np.float32) / np.float32(16)) / np.float32(np.log(128 / 16)) * np.float32(16)).astype(np.int32)
    large = np.minimum(large, 31)
    return np.where(d < 16, d, large)


def _host_consts():
    bf = ml_dtypes.bfloat16
    c = {}
    c["cident"] = np.eye(128, dtype=np.float32).astype(bf)
    c["cJ"] = np.eye(128, dtype=np.float32)[::-1].copy().astype(bf)
    kl = np.arange(128)[:, None]
    ql = np.arange(128)[None, :]
    c["ctri"] = np.where(ql >= kl, 0.0, NEGM).astype(np.float32).astype(bf)
    bd = np.zeros((128, 128), np.float32)
    bd[0:64, 0:64] = 1.0 / 64
    bd[64:128, 64:128] = 1.0 / 64
    c["cbd64"] = bd.astype(bf)
    E = np.zeros((64, S), np.float32)
    E[np.arange(S) // 64, np.arange(S)] = 1.0
    c["cE"] = E.astype(bf)
    n = np.arange(256)
    m = np.arange(64)
    cs = n[:, None] * 16
    ss = m[None, :] * 64
    ov = np.clip(np.minimum(cs + 32, ss + 64) - np.maximum(cs, ss), 0, 32).astype(np.float32)
    ov[255] = 0
    c["covl"] = np.concatenate([ov[0:128], ov[128:256]], axis=1).astype(bf)
    d = np.arange(8208) - ZC
    oh = np.zeros((33, 8208), np.float32)
    b = _bucket(d)
    oh[b[d >= 0], np.nonzero(d >= 0)[0]] = 1.0
    oh[32, d < 0] = 1.0
    c["ohc"] = oh.astype(bf)
    d = np.arange(1152) - ZW
    oh = np.zeros((33, 1152), np.float32)
    b = _bucket(d)
    ok = (d >= 0) & (d < 512)
    oh[b[ok], np.nonzero(ok)[0]] = 1.0
    oh[32, ~ok] = 1.0
    c["ohw"] = oh.astype(bf)
    return c


def _host_layout(inp):
    f = np.float32
    C_LRU_X, C_LRU_G, C_SC_B, C_SC_C, C_SC_X = 0, 256, 512, 768, 1024
    C_FOX_Q, C_FOX_K, C_FOX_V, C_FOX_F, C_NSA_Q, C_NSA_KV, C_NSA_G = 1280, 1536, 1792, 2048, 2052, 2308, 2692
    r = lambda a, n: list(range(a, a + n))
    perm = (r(C_LRU_X, 256) + r(C_LRU_G, 256) + r(C_SC_B, 256) + r(C_SC_C, 256) + r(C_SC_X, 256)
            + r(C_FOX_Q, 256) + r(C_FOX_K, 256) + r(C_NSA_Q, 256)
            + r(C_NSA_KV + 0, 64) + r(C_NSA_KV + 64, 64) + r(C_NSA_KV + 128, 64) + r(C_NSA_KV + 256, 64)
            + r(C_FOX_F, 4)
            + r(C_FOX_V, 256) + r(C_NSA_KV + 192, 64) + r(C_NSA_KV + 320, 64) + r(C_NSA_G, 12))
    assert len(perm) == 2704 and len(set(perm)) == 2704
    m = {}
    m["w_in"] = np.ascontiguousarray(np.asarray(inp["w_in"], f)[:, :, perm])
    m["w_out"] = np.ascontiguousarray(np.asarray(inp["w_out"], f))
    m["w_gu"] = np.ascontiguousarray(np.asarray(inp["w_gate_up"], f))
    m["w_dn"] = np.ascontiguousarray(np.asarray(inp["w_down"], f))
    pvec = np.zeros((NL, 128, 64), f)
    brow = np.zeros((NL, 1, 524), f)
    wbd = np.zeros((NL, 2, 2, 128, 128), f)
    for l in range(NL):
        pvec[l, :, 0:8] = np.asarray(inp["norm_mix"][l]).reshape(8, 128).T
        pvec[l, :, 8:16] = np.asarray(inp["norm_ffn"][l]).reshape(8, 128).T
        for j in range(2):
            sl = slice(j * 128, (j + 1) * 128)
            pvec[l, :, 16 + j * 4:20 + j * 4] = np.asarray(inp["lru_conv_w"][l])[:, sl].T
            pvec[l, :, 24 + j] = np.asarray(inp["lru_conv_b"][l])[sl]
            for g in range(2):
                pvec[l, :, 26 + g * 2 + j] = np.asarray(inp["lru_b_gates"][l])[g, sl]
                for bb in range(2):
                    wbd[l, g, j, bb * 64:(bb + 1) * 64, bb * 64:(bb + 1) * 64] = np.asarray(inp["lru_w_gates"][l])[g, 2 * j + bb]
            pvec[l, :, 30 + j] = np.asarray(inp["lru_lambda"][l])[sl]
            pvec[l, :, 32 + j * 3:35 + j * 3] = np.asarray(inp["sc_conv_w"][l])[:, sl].T
            for grp in range(2):
                pvec[l, :, 38 + grp * 2 + j] = np.asarray(inp["out_norm"][l])[grp, sl]
        pvec[l, 0:64, 42] = np.asarray(inp["fox_qk_norm"][l])[0]
        pvec[l, 0:64, 43] = np.asarray(inp["fox_qk_norm"][l])[1]
        for i in range(4):
            pvec[l, 0:64, 44 + i] = np.asarray(inp["nsa_qk_norm"][l])[i]
        pvec[l, 0:4, 48] = np.asarray(inp["fox_f_bias"][l])
        for hf in range(2):
            pvec[l, hf * 64:(hf + 1) * 64, 50] = np.asarray(inp["fox_qk_norm"][l])[0]
            pvec[l, hf * 64:(hf + 1) * 64, 51] = np.asarray(inp["fox_qk_norm"][l])[1]
            pvec[l, hf * 64:(hf + 1) * 64, 52] = np.asarray(inp["nsa_qk_norm"][l])[0]
            pvec[l, hf * 64:(hf + 1) * 64, 53] = np.asarray(inp["nsa_qk_norm"][l])[2 + hf]
        brow[l, 0, 0:256] = np.asarray(inp["out_norm"][l])[2]
        brow[l, 0, 256:512] = np.asarray(inp["out_norm"][l])[3]
        brow[l, 0, 512:524] = np.asarray(inp["nsa_gate_bias"][l])
    m["pvec"] = pvec
    m["brow"] = brow
    m["wbd"] = wbd
    m["cw1"] = np.ascontiguousarray(np.asarray(inp["nsa_cmp_w1"], f))
    m["cw2"] = np.ascontiguousarray(np.asarray(inp["nsa_cmp_w2"], f))
    m["cposT"] = np.ascontiguousarray(np.asarray(inp["nsa_cmp_pos"], f).transpose(0, 1, 3, 2))
    rb = np.zeros((33, 4), f)
    rb[0:32] = np.asarray(inp["rel_bias"], f)
    rb[32] = NEGM
    m["rb33"] = rb
    m.update(_host_consts())
    return m


def kernel(**inputs):
    x = np.asarray(inputs["x"], np.float32)
    shared = _host_layout(inputs)
    nc = build()
    in_maps = []
    for b in range(8):
        d = dict(shared)
        d["xT"] = np.ascontiguousarray(x[b].T)
        in_maps.append(d)
    res = run_bass_kernel_spmd(nc, in_maps, core_ids=list(range(8)))
    out = np.stack([np.ascontiguousarray(res.results[b]["out"].T) for b in range(8)], axis=0)
    return out.astype(np.float32)
```

```python
import numpy as np
import ml_dtypes
from contextlib import ExitStack
import concourse.bass as bass
import concourse.mybir as mybir
from concourse.bass_utils import run_bass_kernel_spmd

F32 = mybir.dt.float32
BF16 = mybir.dt.bfloat16
AF = mybir.ActivationFunctionType
ALU = mybir.AluOpType

S = 4096
D = 1024
NL = 2
DFF = 2816
NFM = 2308
NTM = 396
EPS = 1e-6
NEGM = -30000.0
R_LRU_X, R_LRU_G, R_SC_B, R_SC_C, R_SC_X = 0, 256, 512, 768, 1024
R_FOX_Q, R_FOX_K, R_NSA_Q, R_KC, R_VC, R_KS, R_KW, R_FOX_F = 1280, 1536, 1792, 2048, 2112, 2176, 2240, 2304
T_FOX_V, T_VS, T_VW, T_G = 0, 256, 320, 384
ZC = 4112
ZW = 128


class U:
    __slots__ = ("lastw", "readers")

    def __init__(self):
        self.lastw = None
        self.readers = []


class T:
    def __init__(self, t):
        self.t = t
        self.u = U()

    def __getitem__(self, k):
        return self.t[k]


class FW:
    def __init__(self, nc, es, ndma=24):
        self.nc = nc
        self.engs = {}
        for name, h in [("pe", nc.tensor), ("dve", nc.vector), ("act", nc.scalar),
                        ("pool", nc.gpsimd), ("sp", nc.sync)]:
            sem = es.enter_context(nc.semaphore("sem_" + name))
            self.engs[name] = dict(h=h, sem=sem, count=0, known={})
        self.dsems = {"sp": [[es.enter_context(nc.semaphore("dqs%d" % i)), 0] for i in range(ndma)],
                      "pool": [[es.enter_context(nc.semaphore("dqp%d" % i)), 0] for i in range(ndma)]}
        self.drr = {"sp": 0, "pool": 0}
        self.ninstr = 0

    def _wait(self, en, toks):
        e = self.engs[en]
        need = {}
        for (sem, val) in toks:
            k = id(sem)
            if k not in need or need[k][1] < val:
                need[k] = (sem, val)
        for k, (sem, val) in need.items():
            if e["known"].get(k, 0) < val:
                e["h"].wait_ge(sem, val)
                e["known"][k] = val
                self.ninstr += 1

    def _deps(self, en, reads, writes):
        toks = []
        for u in reads:
            t = u.lastw
            if t is not None:
                if t[2] == en and en == "pe":
                    continue
                toks.append((t[0], t[1]))
        for u in writes:
            t = u.lastw
            if t is not None and (t[2] != en or en != "pe"):
                toks.append((t[0], t[1]))
            for r in u.readers:
                if r[2] != en or en != "pe":
                    toks.append((r[0], r[1]))
        return toks

    def _commit(self, tok, reads, writes):
        for u in writes:
            u.lastw = tok
            u.readers = []
        for u in reads:
            if u in writes:
                continue
            u.readers.append(tok)
            if len(u.readers) > 48:
                best = {}
                for r in u.readers:
                    k = id(r[0])
                    if k not in best or best[k][1] < r[1]:
                        best[k] = r
                u.readers = list(best.values())

    def op(self, en, fn, reads=(), writes=()):
        reads = [x.u if isinstance(x, T) else x for x in reads]
        writes = [x.u if isinstance(x, T) else x for x in writes]
        e = self.engs[en]
        self._wait(en, self._deps(en, reads, writes))
        ins = fn(e["h"])
        e["count"] += 1
        ins.then_inc(e["sem"], 1)
        self.ninstr += 1
        tok = (e["sem"], e["count"], en)
        self._commit(tok, reads, writes)
        return tok

    def dma(self, en, out, in_, reads=(), writes=(), **kw):
        reads = [x.u if isinstance(x, T) else x for x in reads]
        writes = [x.u if isinstance(x, T) else x for x in writes]
        e = self.engs[en]
        pool_ = self.dsems[en]
        slot = pool_[self.drr[en] % len(pool_)]
        self.drr[en] += 1
        toks = self._deps("dma", reads, writes)
        toks.append((slot[0], slot[1]))
        self._wait(en, toks)
        ins = e["h"].dma_start(out=out, in_=in_, **kw)
        slot[1] += 16
        ins.then_inc(slot[0], 16)
        self.ninstr += 1
        tok = (slot[0], slot[1], "dma")
        self._commit(tok, reads, writes)
        return tok

    def barrier(self):
        toks = []
        for n, e in self.engs.items():
            if e["count"] > 0:
                toks.append((e["sem"], e["count"]))
        for pl in self.dsems.values():
            for s in pl:
                if s[1] > 0:
                    toks.append((s[0], s[1]))
        for n in self.engs:
            self._wait(n, toks)


def build(dbg=False, nlayers=NL, stop=99):
    nc = bass.Bass("TRN2", target_bir_lowering=False)

    def din(name, shape, dt=F32):
        return nc.dram_tensor(name, list(shape), dt, kind="ExternalInput").ap()

    kind_s = "ExternalOutput" if dbg else "Internal"

    def dscr(name, shape, dt=F32):
        return nc.dram_tensor(name, list(shape), dt, kind=kind_s)

    xT = din("xT", [D, S])
    w_in = din("w_in", [NL, D, 2704])
    w_out = din("w_out", [NL, D, D])
    w_gu = din("w_gu", [NL, D, 2 * DFF])
    w_dn = din("w_dn", [NL, DFF, D])
    pvec_d = din("pvec", [NL, 128, 64])
    brow_d = din("brow", [NL, 1, 524])
    wbd_d = din("wbd", [NL, 2, 2, 128, 128])
    cw1_d = din("cw1", [NL, 2, 32, 64, 128])
    cw2_d = din("cw2", [NL, 2, 128, 64])
    cposT_d = din("cposT", [NL, 2, 64, 32])
    rb33_d = din("rb33", [33, 4])
    ohc_d = din("ohc", [33, 8208], BF16)
    ohw_d = din("ohw", [33, 1152], BF16)
    cident_d = din("cident", [128, 128], BF16)
    cJ_d = din("cJ", [128, 128], BF16)
    ctri_d = din("ctri", [128, 128], BF16)
    cE_d = din("cE", [64, S], BF16)
    covl_d = din("covl", [128, 128], BF16)
    cbd64_d = din("cbd64", [128, 128], BF16)
    out_d = nc.dram_tensor("out", [D, S], F32, kind="ExternalOutput").ap()
    zT_h = dscr("zT", [NFM, S])
    ztok_h = dscr("ztok", [S, NTM])
    mixT_h = dscr("mixT", [D, S], BF16)
    xres_h = dscr("xres", [D, S])
    ftc_h = dscr("ftc", [4, 8208], BF16)
    ftw_h = dscr("ftw", [4, 1152], BF16)
    zT, ztok, mixT, xres, ftc, ftw = (h.ap() for h in (zT_h, ztok_h, mixT_h, xres_h, ftc_h, ftw_h))
    u_zT = [[U() for _ in range(8)] for _ in range(19)]
    u_ztok = [U() for _ in range(32)]
    u_mix = [[U() for _ in range(8)] for _ in range(8)]
    u_xres = [U() for _ in range(8)]
    u_ft = U()

    with ExitStack() as es:
        fw = FW(nc, es)

        uid = [0]

        def sb(st, name, shape, dt=F32):
            uid[0] += 1
            return T(st.enter_context(nc.sbuf_tensor("%s_%d" % (name, uid[0]), list(shape), dt)))

        def psm(st, name, shape, dt=F32):
            return T(st.enter_context(nc.psum_tensor(name, list(shape), dt)))

        def act(out, in_, func, reads, writes, **kw):
            return fw.op("act", lambda e: e.activation(out=out, in_=in_, func=func, **kw), reads, writes)

        def mm(out, lhsT, rhs, start, stop, reads, writes, **kw):
            return fw.op("pe", lambda e: e.matmul(out, lhsT=lhsT, rhs=rhs, start=start, stop=stop, **kw), reads, writes)

        def tt(en, out, in0, in1, op, reads, writes):
            return fw.op(en, lambda e: e.tensor_tensor(out=out, in0=in0, in1=in1, op=op), reads, writes)

        def ts(en, out, in0, s1, s2, op0, op1, reads, writes):
            if op1 is None:
                return fw.op(en, lambda e: e.tensor_scalar(out=out, in0=in0, scalar1=s1, scalar2=None, op0=op0), reads, writes)
            return fw.op(en, lambda e: e.tensor_scalar(out=out, in0=in0, scalar1=s1, scalar2=s2, op0=op0, op1=op1), reads, writes)

        def stt(out, in0, scalar, in1, op0, op1, reads, writes):
            return fw.op("dve", lambda e: e.scalar_tensor_tensor(out=out, in0=in0, scalar=scalar, in1=in1, op0=op0, op1=op1), reads, writes)

        def cp(en, out, in_, reads, writes):
            return fw.op(en, lambda e: e.tensor_copy(out=out, in_=in_), reads, writes)

        def mset(en, ap, val, writes):
            return fw.op(en, lambda e: e.memset(ap, val), (), writes)

        ident = sb(es, "ident", [128, 128], BF16)
        Jm = sb(es, "Jm", [128, 128], BF16)
        tri = sb(es, "tri", [128, 128], BF16)
        ovl = sb(es, "ovl", [128, 128], BF16)
        bd64 = sb(es, "bd64", [128, 128], BF16)
        ones1024 = sb(es, "ones1024", [128, 128], BF16)
        ones256 = sb(es, "ones256", [128, 128], BF16)
        ones64 = sb(es, "ones64", [64, 64], BF16)
        zeros = sb(es, "zeros", [128, 512], BF16)
        slab_s = sb(es, "slab_s", [128, 4, 640], BF16)
        slab_w = sb(es, "slab_w", [128, 4, 1024], BF16)
        pv_all = sb(es, "pv_all", [128, NL, 64])
        brow_all = sb(es, "brow_all", [128, NL, 524])
        fw.dma("sp", ident[:], cident_d[:, :], writes=[ident])
        fw.dma("sp", Jm[:], cJ_d[:, :], writes=[Jm])
        fw.dma("sp", tri[:], ctri_d[:, :], writes=[tri])
        fw.dma("sp", ovl[:], covl_d[:, :], writes=[ovl])
        fw.dma("sp", bd64[:], cbd64_d[:, :], writes=[bd64])
        for l in range(NL):
            fw.dma("sp", pv_all[:, l, :], pvec_d[l], writes=[pv_all])
            fw.dma("sp", brow_all[:, l, :], brow_d[l].to_broadcast([128, 524]), writes=[brow_all])
        mset("pool", ones1024[:], 1.0 / 1024, [ones1024])
        mset("pool", ones256[:], 1.0 / 256, [ones256])
        mset("pool", ones64[:], 1.0 / 64, [ones64])
        mset("pool", zeros[:], 0.0, [zeros])

        PS = [psm(es, "ps%d" % i, [128, 512]) for i in range(7)]
        PSB = psm(es, "psb", [128, 1024], BF16)
        rb31 = sb(es, "rb31", [128, 4])
        fw.dma("sp", rb31[:], rb33_d[31:32, :].to_broadcast([128, 4]), writes=[rb31])

        with ExitStack() as ph:
            rb_f = sb(ph, "rb_f", [33, 4])
            rb_b = sb(ph, "rb_b", [33, 4], BF16)
            oh = sb(ph, "oh", [33, 8208], BF16)
            ohw_s = sb(ph, "ohw_s", [33, 1152], BF16)
            ftab = sb(ph, "ftab", [4, 8208 + 1152], BF16)
            fw.dma("sp", rb_f[:], rb33_d[:, :], writes=[rb_f])
            fw.dma("sp", oh[:], ohc_d[:, :], writes=[oh])
            fw.dma("sp", ohw_s[:], ohw_d[:, :], writes=[ohw_s])
            cp("dve", rb_b[:], rb_f[:], [rb_f], [rb_b])
            pieces = [(oh, c0, min(512, 8208 - c0), c0) for c0 in range(0, 8208, 512)]
            pieces += [(ohw_s, c0, min(512, 1152 - c0), 8208 + c0) for c0 in range(0, 1152, 512)]
            for i, (src, c0, n, dst0) in enumerate(pieces):
                ps = PS[i % 2]
                mm(ps[0:4, 0:n], rb_b[:, 0:4], src[:, c0:c0 + n], True, True, [rb_b, src], [ps])
                cp("dve", ftab[:, dst0:dst0 + n], ps[0:4, 0:n], [ps], [ftab])
            fw.dma("sp", ftc[:, :], ftab[:, 0:8208], reads=[ftab], writes=[u_ft])
            fw.dma("sp", ftw[:, :], ftab[:, 8208:8208 + 1152], reads=[ftab], writes=[u_ft])
            for h in range(4):
                src = bass.AP(tensor=ftc_h, offset=h * 8208 + ZC - 127, ap=[[1, 128], [1, 640]])
                fw.dma("sp", slab_s[:, h, :], src, reads=[u_ft], writes=[slab_s])
                src = bass.AP(tensor=ftw_h, offset=h * 1152 + ZW - 127, ap=[[1, 128], [1, 1024]])
                fw.dma("sp", slab_w[:, h, :], src, reads=[u_ft], writes=[slab_w])
            fw.barrier()

        def rstd_from(ps_ap, out_ap, tmp_ap, reads, tmpT, outT, scale=1.0):
            act(tmp_ap, ps_ap, AF.Ln, reads, [tmpT], bias=EPS, scale=scale)
            act(out_ap, tmp_ap, AF.Exp, [tmpT], [outT], scale=-0.5)

        for l in range(nlayers):
            x_src = xT if l == 0 else xres
            x_dst = out_d if l == nlayers - 1 else xres
            pv = lambda c0, c1=None, p0=0, p1=128: pv_all[p0:p1, l, c0:(c0 + 1 if c1 is None else c1)]

            with ExitStack() as ph:
                w_sb = sb(ph, "w_sb", [128, 8, 2704], BF16)
                w_u = [U() for _ in range(8)]
                for c in range(8):
                    fw.dma("pool", w_sb[:, c, :], w_in[l, c * 128:(c + 1) * 128, :], writes=[w_u[c]])
                xt = [sb(ph, "xt%d" % i, [128, 8, 512]) for i in range(2)]
                xn = [sb(ph, "xn%d" % i, [128, 8, 512], BF16) for i in range(2)]
                sq = sb(ph, "sq", [128, 8, 512], BF16)
                lnb = sb(ph, "lnb", [128, 512])
                rstd = sb(ph, "rstd", [128, 512])
                zst = [sb(ph, "zst%d" % i, [128, 512]) for i in range(4)]
                ev = 0
                f2 = lambda nm, w=512, dt=F32: [sb(ph, "%s%d" % (nm, j), [128, w], dt) for j in range(2)]
                Xh, CXh, Cb, Rr, Ii, A2 = f2("Xh", 515), f2("CXh", 514), f2("Cb"), f2("Rr"), f2("Ii"), f2("A2")
                xcb, GE, Yl, SBb, SCc, Oo, Ys = f2("xcb", 512, BF16), f2("GE"), f2("Yl"), f2("SBb"), f2("SCc"), f2("Oo"), f2("Ys")
                Hh = [f2("Hh%d_" % j) for j in range(2)]
                sqA = f2("sqA", 512, BF16)
                lnbA = sb(ph, "lnbA", [128, 512])
                rstdA = sb(ph, "rstdA", [128, 512])
                mstA = [sb(ph, "mstA%d" % i, [128, 512], BF16) for i in range(4)]
                wg_b = sb(ph, "wg_b", [128, 4, 128], BF16)
                cst = sb(ph, "cst", [128, 8])
                for g in range(2):
                    for j in range(2):
                        fw.dma("pool", wg_b[:, g * 2 + j, :], wbd_d[l, g, j], writes=[wg_b])
                act(cst[:, 4:6], pv(30, 32), AF.Exp, [pv_all], [cst], scale=-1.0)
                act(cst[:, 6:8], cst[:, 4:6], AF.Ln, [cst], [cst], bias=1.0)
                ts("dve", cst[:, 0:2], cst[:, 6:8], -8.0, None, ALU.mult, None, [cst], [cst])
                ts("dve", cst[:, 2:4], cst[:, 6:8], -16.0, None, ALU.mult, None, [cst], [cst])
                for j in range(2):
                    mset("pool", Xh[j][:, 0:3], 0.0, [Xh[j]])
                    mset("pool", CXh[j][:, 0:2], 0.0, [CXh[j]])
                mctr = [0]

                def lru_part1(tt_, j, ps):
                    if tt_ > 0:
                        cp("dve", Xh[j][:, 0:3], Xh[j][:, 512:515], [Xh[j]], [Xh[j]])
                    act(Xh[j][:, 3:515], ps[:, :], AF.Copy, [ps], [Xh[j]])
                    wc = 16 + j * 4
                    ts("dve", Cb[j][:], Xh[j][:, 0:512], pv(wc), pv(24 + j), ALU.mult, ALU.add, [Xh[j], pv_all], [Cb[j]])
                    for k in range(1, 4):
                        stt(Cb[j][:], Xh[j][:, k:k + 512], pv(wc + k), Cb[j][:], ALU.mult, ALU.add, [Xh[j], pv_all, Cb[j]], [Cb[j]])
                    act(xcb[j][:], Cb[j][:], AF.Copy, [Cb[j]], [xcb[j]])

                def lru_part2(tt_, j):
                    for g, dst in ((0, Rr[j]), (1, Ii[j])):
                        ps = PS[4 + g]
                        mm(ps[:], wg_b[:, g * 2 + j, :], xcb[j][:], True, True, [wg_b, xcb[j]], [ps])
                        act(dst[:], ps[:], AF.Sigmoid, [ps, pv_all], [dst], bias=pv(26 + g * 2 + j))
                    act(A2[j][:], Rr[j][:], AF.Exp, [Rr[j], cst], [A2[j]], scale=cst[:, 2 + j:3 + j])
                    act(Rr[j][:], Rr[j][:], AF.Exp, [Rr[j], cst], [Rr[j]], scale=cst[:, j:j + 1])
                    act(A2[j][:], A2[j][:], AF.Sqrt, [A2[j]], [A2[j]], scale=-1.0, bias=1.0)
                    tt("dve", Ii[j][:], Ii[j][:], Cb[j][:], ALU.mult, [Ii[j], Cb[j]], [Ii[j]])
                    tt("dve", Ii[j][:], Ii[j][:], A2[j][:], ALU.mult, [Ii[j], A2[j]], [Ii[j]])
                    Hc = Hh[j][tt_ % 2]
                    if tt_ == 0:
                        fw.op("dve", lambda e: e.tensor_tensor_scan(out=Hc[:], data0=Rr[j][:], data1=Ii[j][:], initial=0.0, op0=ALU.mult, op1=ALU.add), [Rr[j], Ii[j]], [Hc])
                    else:
                        Hp = Hh[j][(tt_ - 1) % 2]
                        fw.op("dve", lambda e: e.tensor_tensor_scan(out=Hc[:], data0=Rr[j][:], data1=Ii[j][:], initial=Hp[:, 511:512], op0=ALU.mult, op1=ALU.add), [Rr[j], Ii[j], Hp], [Hc])
                    tt("dve", Yl[j][:], Hc[:], GE[j][:], ALU.mult, [Hc, GE[j]], [Yl[j]])

                def sc_chain(tt_, j, ps):
                    if tt_ > 0:
                        cp("dve", CXh[j][:, 0:2], CXh[j][:, 512:514], [CXh[j]], [CXh[j]])
                    tt("dve", CXh[j][:, 2:514], SCc[j][:], ps[:, :], ALU.mult, [SCc[j], ps], [CXh[j]])
                    wc = 32 + j * 3
                    ts("dve", Oo[j][:], CXh[j][:, 0:512], pv(wc), None, ALU.mult, None, [CXh[j], pv_all], [Oo[j]])
                    for k in range(1, 3):
                        stt(Oo[j][:], CXh[j][:, k:k + 512], pv(wc + k), Oo[j][:], ALU.mult, ALU.add, [CXh[j], pv_all, Oo[j]], [Oo[j]])
                    tt("dve", Ys[j][:], Oo[j][:], SBb[j][:], ALU.mult, [Oo[j], SBb[j]], [Ys[j]])

                def outnorm_fused(tt_, Yv, grp):
                    t0 = tt_ * 512
                    for j in range(2):
                        act(sqA[j][:], Yv[j][:], AF.Square, [Yv[j]], [sqA[j]])
                    mm(PS[6][:], ones256[:], sqA[0][:], True, False, [ones256, sqA[0]], [PS[6]])
                    mm(PS[6][:], ones256[:], sqA[1][:], False, True, [ones256, sqA[1]], [PS[6]])
                    rstd_from(PS[6][:], rstdA[:], lnbA[:], [PS[6]], lnbA, rstdA)
                    for j in range(2):
                        m_ = mstA[mctr[0] % 4]
                        mctr[0] += 1
                        stt(m_[:], Yv[j][:], pv(38 + grp * 2 + j), rstdA[:], ALU.mult, ALU.mult, [Yv[j], pv_all, rstdA], [m_])
                        fw.dma("pool", mixT[grp * 256 + j * 128:grp * 256 + (j + 1) * 128, t0:t0 + 512], m_[:], reads=[m_], writes=[u_mix[grp * 2 + j][tt_]])

                def norm1(tt_):
                    t0 = tt_ * 512
                    xt_, xn_ = xt[tt_ % 2], xn[tt_ % 2]
                    rd = [u_xres[tt_]] if l > 0 else []
                    fw.dma("sp", xt_[:], x_src[:, t0:t0 + 512].rearrange("(c p) t -> p c t", p=128), reads=rd, writes=[xt_])
                    act(sq[:], xt_[:], AF.Square, [xt_], [sq])
                    for c in range(8):
                        mm(PS[0][:], ones1024[:], sq[:, c, :], c == 0, c == 7, [ones1024, sq], [PS[0]])
                    rstd_from(PS[0][:], rstd[:], lnb[:], [PS[0]], lnb, rstd)
                    for c in range(8):
                        stt(xn_[:, c, :], xt_[:, c, :], pv(c), rstd[:], ALU.mult, ALU.mult, [xt_, rstd, pv_all], [xn_])

                norm1(0)
                for tt_ in range(8):
                    t0 = tt_ * 512
                    xt_, xn_ = xt[tt_ % 2], xn[tt_ % 2]
                    for oc in range(19):
                        if oc == 6 and tt_ + 1 < 8:
                            norm1(tt_ + 1)
                        m = 128 if oc < 18 else 4
                        ps = PS[1 + ev % 3]
                        for c in range(8):
                            mm(ps[0:m, :], w_sb[:, c, oc * 128:oc * 128 + m], xn_[:, c, :], c == 0, c == 7, [w_u[c], xn_], [ps])
                        if oc == 12:
                            outnorm_fused(tt_, Yl, 0)
                        if oc == 15:
                            outnorm_fused(tt_, Ys, 1)
                        if oc < 10:
                            j = oc % 2
                            if oc < 2:
                                lru_part1(tt_, j, ps)
                            elif oc < 4:
                                act(GE[j][:], ps[:, :], AF.Gelu_apprx_tanh, [ps], [GE[j]])
                            elif oc < 6:
                                cp("dve", SBb[j][:], ps[:, :], [ps], [SBb[j]])
                                lru_part2(tt_, j)
                            elif oc < 8:
                                act(SCc[j][:], ps[:, :], AF.Copy, [ps], [SCc[j]])
                            else:
                                sc_chain(tt_, j, ps)
                            ev += 1
                            continue
                        st_ = zst[ev % 4]
                        if ev % 2 == 0:
                            act(st_[0:m, :], ps[0:m, :], AF.Copy, [ps], [st_])
                        else:
                            cp("dve", st_[0:m, :], ps[0:m, :], [ps], [st_])
                        fw.dma("pool", zT[oc * 128:oc * 128 + m, t0:t0 + 512], st_[0:m, :], reads=[st_], writes=[u_zT[oc][tt_]])
                        ev += 1
                    for s in range(4):
                        ps = PS[1 + ev % 3]
                        for c in range(8):
                            mm(ps[:, 0:NTM], xn_[:, c, s * 128:(s + 1) * 128], w_sb[:, c, NFM:NFM + NTM], c == 0, c == 7, [w_u[c], xn_], [ps])
                        st_ = zst[ev % 4]
                        if ev % 2 == 0:
                            act(st_[:, 0:NTM], ps[:, 0:NTM], AF.Copy, [ps], [st_])
                        else:
                            cp("dve", st_[:, 0:NTM], ps[:, 0:NTM], [ps], [st_])
                        fw.dma("pool", ztok[t0 + s * 128:t0 + (s + 1) * 128, :], st_[:, 0:NTM], reads=[st_], writes=[u_ztok[tt_ * 4 + s]])
                        ev += 1
                fw.barrier()
            if stop <= 1:
                break

            for _unused in []:
              with ExitStack() as ph:
                  NB = 4100
                  X = sb(ph, "X", [128, NB])
                  C = sb(ph, "C", [128, NB])
                  R = sb(ph, "R", [128, NB])
                  I = sb(ph, "I", [128, NB])
                  A2 = sb(ph, "A2", [128, NB])
                  Y0 = sb(ph, "Y0", [128, S])
                  xcb = sb(ph, "xcb", [128, S], BF16)
                  sq0 = sb(ph, "sq0", [128, S], BF16)
                  sq1 = sb(ph, "sq1", [128, S], BF16)
                  wg_f = sb(ph, "wg_f", [128, 4, 128])
                  wg_b = sb(ph, "wg_b", [128, 4, 128], BF16)
                  cst = sb(ph, "cst", [128, 8])
                  lnb = sb(ph, "lnb2", [128, 512])
                  rstd = sb(ph, "rstd2", [128, 512])
                  mst = [sb(ph, "mst%d" % i, [128, 512], BF16) for i in range(3)]
                  for g in range(2):
                      for j in range(2):
                          fw.dma("sp", wg_f[:, g * 2 + j, :], wbd_d[l, g, j], writes=[wg_f])
                  cp("dve", wg_b[:], wg_f[:], [wg_f], [wg_b])
                  act(cst[:, 4:6], pv(30, 32), AF.Exp, [pv_all], [cst], scale=-1.0)
                  act(cst[:, 6:8], cst[:, 4:6], AF.Ln, [cst], [cst], bias=1.0)
                  ts("dve", cst[:, 0:2], cst[:, 6:8], -8.0, None, ALU.mult, None, [cst], [cst])
                  ts("dve", cst[:, 2:4], cst[:, 6:8], -16.0, None, ALU.mult, None, [cst], [cst])

                  def outnorm_fm(Ys, sqs, grp):
                      for j in range(2):
                          act(sqs[j][:], Ys[j][:, 0:S], AF.Square, [Ys[j]], [sqs[j]])
                      for tt_ in range(8):
                          t0 = tt_ * 512
                          ps = PS[tt_ % 2]
                          mm(ps[:], ones256[:], sqs[0][:, t0:t0 + 512], True, False, [ones256, sqs[0]], [ps])
                          mm(ps[:], ones256[:], sqs[1][:, t0:t0 + 512], False, True, [ones256, sqs[1]], [ps])
                          rstd_from(ps[:], rstd[:], lnb[:], [ps], lnb, rstd)
                          for j in range(2):
                              m_ = mst[(tt_ * 2 + j) % 3]
                              stt(m_[:], Ys[j][:, t0:t0 + 512], pv(38 + grp * 2 + j), rstd[:], ALU.mult, ALU.mult, [Ys[j], pv_all, rstd], [m_])
                              fw.dma("pool", mixT[grp * 256 + j * 128:grp * 256 + (j + 1) * 128, t0:t0 + 512], m_[:], reads=[m_], writes=[u_mix[grp * 2 + j][tt_]])

                  for j in range(2):
                      mset("pool", X[:, 0:3], 0.0, [X])
                      fw.dma("sp", X[:, 3:3 + S], zT[R_LRU_X + j * 128:R_LRU_X + (j + 1) * 128, :], reads=u_zT[0 + j], writes=[X])
                      wc = 16 + j * 4
                      ts("dve", C[:, 0:S], X[:, 0:S], pv(wc), pv(24 + j), ALU.mult, ALU.add, [X, pv_all], [C])
                      for k in range(1, 4):
                          stt(C[:, 0:S], X[:, k:k + S], pv(wc + k), C[:, 0:S], ALU.mult, ALU.add, [X, pv_all, C], [C])
                      cp("pool", xcb[:], C[:, 0:S], [C], [xcb])
                      for tt_ in range(8):
                          t0 = tt_ * 512
                          for g, dst in ((0, R), (1, I)):
                              ps = PS[(tt_ * 2 + g) % 4]
                              mm(ps[:], wg_b[:, g * 2 + j, :], xcb[:, t0:t0 + 512], True, True, [wg_b, xcb], [ps])
                              act(dst[:, t0:t0 + 512], ps[:], AF.Sigmoid, [ps, pv_all], [dst], bias=pv(26 + g * 2 + j))
                      act(A2[:, 0:S], R[:, 0:S], AF.Exp, [R, cst], [A2], scale=cst[:, 2 + j:3 + j])
                      act(R[:, 0:S], R[:, 0:S], AF.Exp, [R, cst], [R], scale=cst[:, j:j + 1])
                      act(A2[:, 0:S], A2[:, 0:S], AF.Sqrt, [A2], [A2], scale=-1.0, bias=1.0)
                      tt("dve", I[:, 0:S], I[:, 0:S], C[:, 0:S], ALU.mult, [I, C], [I])
                      tt("dve", I[:, 0:S], I[:, 0:S], A2[:, 0:S], ALU.mult, [I, A2], [I])
                      fw.op("dve", lambda e: e.tensor_tensor_scan(out=C[:, 0:S], data0=R[:, 0:S], data1=I[:, 0:S], initial=0.0, op0=ALU.mult, op1=ALU.add), [R, I], [C])
                      fw.dma("sp", X[:, 0:S], zT[R_LRU_G + j * 128:R_LRU_G + (j + 1) * 128, :], reads=u_zT[2 + j], writes=[X])
                      act(A2[:, 0:S], X[:, 0:S], AF.Gelu_apprx_tanh, [X], [A2])
                      Yd = Y0 if j == 0 else C
                      tt("dve", Yd[:, 0:S], C[:, 0:S], A2[:, 0:S], ALU.mult, [C, A2], [Yd])
                  outnorm_fm([Y0, C], [sq0, sq1], 0)
                  for j in range(2):
                      Yd = Y0 if j == 0 else C
                      mset("pool", X[:, 0:2], 0.0, [X])
                      fw.dma("sp", R[:, 0:S], zT[R_SC_C + j * 128:R_SC_C + (j + 1) * 128, :], reads=u_zT[6 + j], writes=[R])
                      fw.dma("sp", I[:, 0:S], zT[R_SC_X + j * 128:R_SC_X + (j + 1) * 128, :], reads=u_zT[8 + j], writes=[I])
                      fw.dma("sp", A2[:, 0:S], zT[R_SC_B + j * 128:R_SC_B + (j + 1) * 128, :], reads=u_zT[4 + j], writes=[A2])
                      tt("dve", X[:, 2:2 + S], R[:, 0:S], I[:, 0:S], ALU.mult, [R, I], [X])
                      wc = 32 + j * 3
                      ts("dve", R[:, 0:S], X[:, 0:S], pv(wc), None, ALU.mult, None, [X, pv_all], [R])
                      for k in range(1, 3):
                          stt(R[:, 0:S], X[:, k:k + S], pv(wc + k), R[:, 0:S], ALU.mult, ALU.add, [X, pv_all, R], [R])
                      tt("dve", Yd[:, 0:S], R[:, 0:S], A2[:, 0:S], ALU.mult, [R, A2], [Yd])
                  outnorm_fm([Y0, C], [sq0, sq1], 1)
                  fw.barrier()
            if stop <= 2:
                break

            def headnorm(tl, src_ap, src_units, gcol, scale, dst, ntok):
                hq, hsq, gs, lnb, rstd = tl
                if src_ap is not None:
                    fw.dma("sp", hq[0:64, 0:ntok], src_ap, reads=src_units, writes=[hq])
                ts("dve", gs[:, 0:1], pv(gcol, p1=64), float(scale), None, ALU.mult, None, [pv_all], [gs])
                act(hsq[0:64, 0:ntok], hq[0:64, 0:ntok], AF.Square, [hq], [hsq])
                for t0 in range(0, ntok, 512):
                    n = min(512, ntok - t0)
                    ps = PS[5 + (t0 // 512) % 2]
                    mm(ps[0:64, 0:n], ones64[:], hsq[0:64, t0:t0 + n], True, True, [ones64, hsq], [ps])
                    rstd_from(ps[0:64, 0:n], rstd[0:64, 0:n], lnb[0:64, 0:n], [ps], lnb, rstd)
                    stt(dst[0:64, t0:t0 + n], hq[0:64, t0:t0 + n], gs[:, 0:1], rstd[0:64, 0:n], ALU.mult, ALU.mult, [hq, gs, rstd], [dst])

            def headnorm2(tl, k, src_ap, src_units, gcol, scale, dstA, dstB):
                hq2s, hsq2, lnall, hb, gs2 = tl
                hq2 = hq2s[k % len(hq2s)]
                fw.dma("sp", hq2[:, 0:S], src_ap, reads=src_units, writes=[hq2])
                ts("dve", gs2[:, 0:1], pv(gcol), float(scale), None, ALU.mult, None, [pv_all], [gs2])
                act(hsq2[:, 0:S], hq2[:, 0:S], AF.Square, [hq2], [hsq2])
                for t in range(8):
                    ps = PS[5 + t % 2]
                    mm(ps[:, :], bd64[:], hsq2[:, t * 512:(t + 1) * 512], True, True, [bd64, hsq2], [ps])
                    act(lnall[:, t * 512:(t + 1) * 512], ps[:, :], AF.Ln, [ps], [lnall], bias=EPS)
                act(lnall[:, :], lnall[:, :], AF.Exp, [lnall], [lnall], scale=-0.5)
                stt(hb[:, :], hq2[:, 0:S], gs2[:, 0:1], lnall[:, :], ALU.mult, ALU.mult, [hq2, gs2, lnall], [hb])
                fw.dma("sp", dstA[0:64, :], hb[0:64, :], reads=[hb], writes=[dstA])
                fw.dma("sp", dstB[0:64, :], hb[64:128, :], reads=[hb], writes=[dstB])

            def run_attention(tiles, pts):
                n_t = len(tiles)
                ring = PS[0:3]

                def emit_pre(i):
                    if i < n_t and tiles[i].get("pre") is not None:
                        bt, src = tiles[i]["pre"]
                        fw.dma("sp", bt[:], src, reads=[u_ft], writes=[bt])

                emit_pre(0)

                def emit_qk(i):
                    tl = tiles[i]
                    ps = ring[i % 3]
                    emit_pre(i + 1)
                    np_ = tl["np"]
                    last = len(tl["mms"]) - 1
                    for idx, (c0, ncol, lhsT, rhs, rds) in enumerate(tl["mms"]):
                        mm(ps[0:np_, c0:c0 + ncol], lhsT, rhs, idx == 0, idx == last, rds, [ps], skip_group_check=True)

                def emit_rest(i):
                    tl = tiles[i]
                    ps = ring[i % 3]
                    pt = pts[i % len(pts)]
                    np_, n = tl["np"], tl["n"]
                    if tl.get("bias") is not None:
                        act(pt[0:np_, 0:n], ps[0:np_, 0:n], AF.Exp, [ps] + tl["bias_reads"], [pt], bias=tl["bias"])
                    else:
                        act(pt[0:np_, 0:n], ps[0:np_, 0:n], AF.Exp, [ps], [pt])
                    if tl.get("init") is not None:
                        accT, w = tl["init"]
                        mm(accT[:, 0:w], zeros[:, 0:128], zeros[:, 0:w], True, True, [zeros], [accT])
                    for (accT, acc_ap, pc0, V_ap, rds) in tl["pv"]:
                        mm(acc_ap, pt[0:np_, pc0:pc0 + 128], V_ap, False, True, [pt] + rds, [accT], skip_group_check=True)
                    if tl.get("fin") is not None:
                        tl["fin"]()

                LA = 2
                for i in range(n_t + LA):
                    if i < n_t:
                        emit_qk(i)
                    if i >= LA:
                        emit_rest(i - LA)

            def outnorm_tok(tl, Q, yv, yT, gain_ap, rowbase):
                junk, ssq, lnq, rs, ym, mst2 = tl
                q0 = Q * 512
                for s in range(4):
                    act(junk[:, 0:256], yv(s), AF.Square, [yT], [junk, ssq], accum_out=ssq[:, s:s + 1])
                act(lnq[:, 0:4], ssq[:, 0:4], AF.Ln, [ssq], [lnq], scale=1.0 / 256, bias=EPS)
                act(rs[:, 0:4], lnq[:, 0:4], AF.Exp, [lnq], [rs], scale=-0.5)
                for s in range(4):
                    stt(ym[:, s, :], yv(s), rs[:, s:s + 1], gain_ap, ALU.mult, ALU.mult, [yT, rs, brow_all], [ym])

                def finish():
                    for j in range(2):
                        for s in range(4):
                            fw.op("pe", lambda e, s=s, j=j: e.transpose(out=PSB[:, s * 128:(s + 1) * 128], in_=ym[:, s, j * 128:(j + 1) * 128], identity=ident[:]), [ym, ident], [PSB])
                        m_ = mst2[j]
                        cp("dve", m_[:], PSB[:, 0:512], [PSB], [m_])
                        fw.dma("pool", mixT[rowbase + j * 128:rowbase + (j + 1) * 128, q0:q0 + 512], m_[:], reads=[m_], writes=[u_mix[rowbase // 128 + j][Q]])
                return finish

            with ExitStack() as ph:
                Qp = [sb(ph, "Qp%d" % h, [128, S], BF16) for h in range(4)]
                Kp = [sb(ph, "Kp%d" % h, [128, S], BF16) for h in range(4)]
                with ExitStack() as ph2:
                    fr = sb(ph2, "fr", [4, S])
                    e1 = sb(ph2, "e1", [4, S])
                    cc = sb(ph2, "cc", [4, S])
                    ones4 = sb(ph2, "ones4", [4, S], BF16)
                    nb = sb(ph2, "nb", [4, 1])
                    csp = sb(ph2, "csp", [4, 3, S], BF16)
                    ncsp = sb(ph2, "ncsp", [4, 3, S], BF16)
                    fw.dma("sp", fr[:], zT[R_FOX_F:R_FOX_F + 4, :], reads=u_zT[18], writes=[fr])
                    ts("dve", nb[:], pv(48, p1=4), -1.0, None, ALU.mult, None, [pv_all], [nb])
                    act(e1[:], fr[:], AF.Exp, [fr, nb], [e1], scale=-1.0, bias=nb[:])
                    act(e1[:], e1[:], AF.Ln, [e1], [e1], bias=1.0)
                    ts("dve", e1[:], e1[:], -1.0, None, ALU.mult, None, [e1], [e1])
                    mset("pool", ones4[:], 1.0, [ones4])
                    fw.op("dve", lambda e: e.tensor_tensor_scan(out=cc[:], data0=ones4[:], data1=e1[:], initial=0.0, op0=ALU.mult, op1=ALU.add), [ones4, e1], [cc])
                    cp("dve", csp[:, 0, :], cc[:], [cc], [csp])
                    cp("dve", fr[:], csp[:, 0, :], [csp], [fr])
                    tt("dve", cc[:], cc[:], fr[:], ALU.subtract, [cc, fr], [cc])
                    cp("dve", csp[:, 1, :], cc[:], [cc], [csp])
                    cp("dve", fr[:], csp[:, 1, :], [csp], [fr])
                    tt("dve", cc[:], cc[:], fr[:], ALU.subtract, [cc, fr], [cc])
                    cp("dve", csp[:, 2, :], cc[:], [cc], [csp])
                    ts("dve", ncsp[:], csp[:], -1.0, None, ALU.mult, None, [csp], [ncsp])
                    for h in range(4):
                        mset("pool", Qp[h][64:128, :], 0.0, [Qp[h]])
                        mset("pool", Kp[h][64:128, :], 0.0, [Kp[h]])
                        for i in range(3):
                            fw.dma("sp", Qp[h][64 + i:65 + i, :], csp[h:h + 1, i, :], reads=[csp], writes=[Qp[h]])
                            fw.dma("sp", Kp[h][96 + i:97 + i, :], ncsp[h:h + 1, i, :], reads=[ncsp], writes=[Kp[h]])
                        fw.dma("sp", Qp[h][96:99, :], ones4[0:3, :], reads=[ones4], writes=[Qp[h]])
                        fw.dma("sp", Kp[h][64:67, :], ones4[0:3, :], reads=[ones4], writes=[Kp[h]])
                    fw.barrier()
                V1 = sb(ph, "V1", [128, 32, 4, 65], BF16)
                hn2 = ([sb(ph, "hq2_%d" % i, [128, S]) for i in range(2)], sb(ph, "hsq2", [128, S], BF16),
                       sb(ph, "lnall", [128, S]), sb(ph, "hb", [128, S], BF16), sb(ph, "gs2", [128, 1]))
                vst = sb(ph, "vst", [128, 8, 256])
                pts = [sb(ph, "pt%d" % i, [128, 512], BF16) for i in range(3)]
                rden = [sb(ph, "rden%d" % i, [128, 4]) for i in range(2)]
                ytk = [sb(ph, "ytk%d" % i, [128, 4, 256]) for i in range(2)]
                on_tl = (sb(ph, "junk", [128, 256]), sb(ph, "ssq", [128, 4]), sb(ph, "lnq", [128, 4]), sb(ph, "rs", [128, 4]),
                         sb(ph, "ym", [128, 4, 256], BF16), [sb(ph, "mst2_%d" % i, [128, 512], BF16) for i in range(2)])
                for j in range(2):
                    headnorm2(hn2, 2 * j, zT[R_FOX_Q + j * 128:R_FOX_Q + (j + 1) * 128, :], u_zT[10 + j], 50, 0.125, Qp[2 * j], Qp[2 * j + 1])
                    headnorm2(hn2, 2 * j + 1, zT[R_FOX_K + j * 128:R_FOX_K + (j + 1) * 128, :], u_zT[12 + j], 51, 1.0, Kp[2 * j], Kp[2 * j + 1])
                mset("pool", V1[:, :, :, 64:65], 1.0, [V1])
                for g in range(4):
                    fw.dma("sp", vst[:], ztok[g * 1024:(g + 1) * 1024, T_FOX_V:T_FOX_V + 256].rearrange("(k p) c -> p k c", p=128), reads=u_ztok[g * 8:(g + 1) * 8], writes=[vst])
                    for h in range(4):
                        cp("pool" if h % 2 else "dve", V1[:, g * 8:(g + 1) * 8, h, 0:64], vst[:, :, h * 64:(h + 1) * 64], [vst], [V1])
                accn = 0
                pend_fin = [None]
                for Q in range(8):
                    q0 = Q * 512
                    yT = ytk[Q % 2]
                    tiles = []
                    for h in range(4):
                        accT = PS[3 + accn % 2]
                        rd_ = rden[accn % 2]
                        accn += 1
                        nk = 4 * Q + 4
                        for kt in range(nk):
                            k0 = kt * 128
                            d = kt - 4 * Q
                            dd = max(d, 0)
                            n = 512 - 128 * dd
                            mms = [(0, n, Kp[h][:, k0:k0 + 128], Qp[h][:, q0 + 128 * dd:q0 + 512], [Kp[h], Qp[h]])]
                            if d >= 0:
                                mms.append((0, 128, ident[:], tri[:], [ident, tri]))
                            tl = dict(np=128, n=n, mms=mms, pv=[])
                            for s in range(dd, 4):
                                tl["pv"].append((accT, accT[:, s * 65:(s + 1) * 65], (s - dd) * 128, V1[:, kt, h, :], [V1]))
                            if kt == 0:
                                tl["init"] = (accT, 260)
                            if kt == nk - 1:
                                def fin(accT=accT, rd_=rd_, h=h, yT=yT):
                                    accv = accT[:, 0:260].rearrange("p (s c) -> p s c", c=65)
                                    fw.op("dve", lambda e: e.reciprocal(out=rd_[:, 0:4], in_=accv[:, :, 64]), [accT], [rd_])
                                    for s in range(4):
                                        ts("dve", yT[:, s, h * 64:(h + 1) * 64], accv[:, s, 0:64], rd_[:, s:s + 1], None, ALU.mult, None, [accT, rd_], [yT])
                                tl["fin"] = fin
                            tiles.append(tl)
                    run_attention(tiles, pts)
                    if pend_fin[0] is not None:
                        pend_fin[0]()
                    pend_fin[0] = outnorm_tok(on_tl, Q, lambda s, yT=yT: yT[:, s, :], yT, brow_all[:, l, 0:256], 512)
                pend_fin[0]()
                fw.barrier()
            if stop <= 3:
                break

            with ExitStack() as ph:
                Qn = [sb(ph, "Qn%d" % h, [128, S], BF16) for h in range(4)]
                KsT = sb(ph, "KsT", [128, S], BF16)
                KwT = sb(ph, "KwT", [128, S], BF16)
                KcT = sb(ph, "KcT", [128, 256], BF16)
                for h in range(4):
                    mset("pool", Qn[h][64:128, :], 0.0, [Qn[h]])
                mset("pool", KwT[64:128, :], 0.0, [KwT])
                mset("pool", KcT[64:128, :], 0.0, [KcT])
                fw.dma("sp", KsT[64:128, :], cE_d[:, :], writes=[KsT])
                VS1 = sb(ph, "VS1", [128, 32, 65], BF16)
                VW1 = sb(ph, "VW1", [128, 32, 65], BF16)
                VC1 = sb(ph, "VC1", [128, 2, 128], BF16)
                G = sb(ph, "G", [128, 32, 12])
                yd = sb(ph, "yd", [128, 32, 256])
                hq = sb(ph, "hq5", [128, 4112])
                hsq = sb(ph, "hsq5", [128, 4112], BF16)
                hn2 = ([hq], hsq, sb(ph, "lnall5", [128, S]), sb(ph, "hb5", [128, S], BF16), sb(ph, "gs25", [128, 1]))
                gs = sb(ph, "gs5", [64, 1])
                lnb = sb(ph, "lnb5", [64, 512])
                rstd = sb(ph, "rstd5", [64, 512])
                hn = (hq, hsq, gs, lnb, rstd)
                w1b = sb(ph, "w1b", [64, 32, 128], BF16)
                posb = sb(ph, "posb", [64, 32], BF16)
                w2b = sb(ph, "w2b", [128, 64], BF16)
                hbias = sb(ph, "hbias", [128, 1])
                hidb = sb(ph, "hidb", [128, 256], BF16)
                vst2 = sb(ph, "vst2", [128, 32, 64])
                gst = sb(ph, "gst", [128, 32, 12])
                pts = [sb(ph, "ptn%d" % i, [128, 512], BF16) for i in range(3)]
                btile = [sb(ph, "btile%d" % i, [128, 512], BF16) for i in range(3)]
                den = [sb(ph, "den%d" % i, [128, 4]) for i in range(2)]
                rden = [sb(ph, "rdenn%d" % i, [128, 4]) for i in range(2)]
                coef = [sb(ph, "coef%d" % i, [128, 4]) for i in range(2)]
                impacc = sb(ph, "impacc", [128, 4, 64])
                imp2 = sb(ph, "imp2", [128, 64])
                m8 = sb(ph, "m8", [128, 8])
                m8b = sb(ph, "m8b", [128, 8])
                self_ = sb(ph, "self", [128, 64])
                selb = sb(ph, "selb", [128, 4, 128], BF16)
                on_tl = (sb(ph, "junk5", [128, 256]), sb(ph, "ssq5", [128, 4]), sb(ph, "lnq5", [128, 4]), sb(ph, "rs5", [128, 4]),
                         sb(ph, "ym5", [128, 4, 256], BF16), [sb(ph, "mst5_%d" % i, [128, 512], BF16) for i in range(2)])
                for j in range(2):
                    headnorm2(hn2, 0, zT[R_NSA_Q + j * 128:R_NSA_Q + (j + 1) * 128, :], u_zT[14 + j], 52, 0.125, Qn[2 * j], Qn[2 * j + 1])
                headnorm2(hn2, 0, zT[R_KS:R_KS + 128, :], u_zT[17], 53, 1.0, KsT, KwT)
                for kv in range(2):
                    rows = R_KC if kv == 0 else R_VC
                    fw.dma("sp", hq[0:64, 0:S], zT[rows:rows + 64, :], reads=u_zT[16], writes=[hq])
                    mset("pool", hq[0:64, S:4112], 0.0, [hq])
                    cp("dve", hsq[0:64, :], hq[0:64, :], [hq], [hsq])
                    fw.dma("pool", w1b[:], cw1_d[l, kv].rearrange("l d m -> d l m"), writes=[w1b])
                    fw.dma("pool", posb[:], cposT_d[l, kv], writes=[posb])
                    fw.dma("pool", w2b[:], cw2_d[l, kv], writes=[w2b])
                    ps = PS[5]
                    for li in range(32):
                        mm(ps[:, 0:1], w1b[:, li, :], posb[:, li:li + 1], li == 0, li == 31, [w1b, posb], [ps])
                    cp("dve", hbias[:], ps[:, 0:1], [ps], [hbias])
                    ps = PS[6]
                    for li in range(32):
                        if li < 16:
                            rhs = hsq[0:64, 0:4096].rearrange("p (n r) -> p n r", r=16)[:, :, li]
                        else:
                            rhs = hsq[0:64, 16:4112].rearrange("p (n r) -> p n r", r=16)[:, :, li - 16]
                        mm(ps[:, 0:256], w1b[:, li, :], rhs, li == 0, li == 31, [w1b, hsq], [ps])
                    act(hidb[:], ps[:, 0:256], AF.Gelu_apprx_tanh, [ps, hbias], [hidb], bias=hbias[:])
                    if kv == 0:
                        ps = PS[5]
                        mm(ps[0:64, 0:256], w2b[:], hidb[:], True, True, [w2b, hidb], [ps])
                        cp("dve", hq[0:64, 0:256], ps[0:64, 0:256], [ps], [hq])
                        headnorm(hn, None, None, 45, 1.0, KcT, 256)
                    else:
                        for c in range(2):
                            ps = PS[5]
                            mm(ps[:, 0:64], hidb[:, c * 128:(c + 1) * 128], w2b[:], True, True, [w2b, hidb], [ps])
                            cp("dve", VC1[:, c, 0:64], ps[:, 0:64], [ps], [VC1])
                            cp("dve", VC1[:, c, 64:128], ovl[:, c * 64:(c + 1) * 64], [ovl], [VC1])
                for (VT, col) in ((VS1, T_VS), (VW1, T_VW)):
                    fw.dma("sp", vst2[:], ztok[:, col:col + 64].rearrange("(k p) c -> p k c", p=128), reads=u_ztok, writes=[vst2])
                    cp("dve", VT[:, :, 0:64], vst2[:], [vst2], [VT])
                    mset("pool", VT[:, :, 64:65], 1.0, [VT])
                fw.dma("sp", gst[:], ztok[:, T_G:T_G + 12].rearrange("(k p) c -> p k c", p=128), reads=u_ztok, writes=[gst])
                tt("dve", gst[:], gst[:], brow_all[:, l, 512:524].unsqueeze(1).to_broadcast([128, 32, 12]), ALU.add, [gst, brow_all], [gst])
                act(G[:], gst[:], AF.Sigmoid, [gst], [G])
                mset("pool", selb[:], 0.0, [selb])

                accn = 0
                bn = 0
                pend_fin = [None]
                for Q in range(8):
                    q0 = Q * 512

                    def fin_generic(accT, w, h, branch, first, dn, rd_, cf_, Q=Q):
                        accv = accT[:, 0:4 * w].rearrange("p (s c) -> p s c", c=w)
                        if branch == 0:
                            fw.op("dve", lambda e: e.tensor_reduce(out=dn[:, 0:4], in_=accv[:, :, 64:128], axis=mybir.AxisListType.X, op=ALU.add), [accT], [dn])
                            ts("dve", dn[:, 0:4], dn[:, 0:4], 1.0 / 32, 1e-30, ALU.mult, ALU.max, [dn], [dn])
                            fw.op("dve", lambda e: e.reciprocal(out=rd_[:, 0:4], in_=dn[:, 0:4]), [dn], [rd_])
                        else:
                            fw.op("dve", lambda e: e.reciprocal(out=rd_[:, 0:4], in_=accv[:, :, 64]), [accT], [rd_])
                        tt("dve", cf_[:, 0:4], rd_[:, 0:4], G[:, 4 * Q:4 * Q + 4, branch * 4 + h], ALU.mult, [rd_, G], [cf_])
                        for s in range(4):
                            ydv = yd[:, 4 * Q + s, h * 64:(h + 1) * 64]
                            if first:
                                ts("dve", ydv, accv[:, s, 0:64], cf_[:, s:s + 1], None, ALU.mult, None, [accT, cf_], [yd])
                            else:
                                stt(ydv, accv[:, s, 0:64], cf_[:, s:s + 1], ydv, ALU.mult, ALU.add, [accT, cf_, yd], [yd])
                        if branch == 0:
                            for s in range(4):
                                if h == 0:
                                    ts("dve", impacc[:, s, :], accv[:, s, 64:128], rd_[:, s:s + 1], None, ALU.mult, None, [accT, rd_], [impacc])
                                else:
                                    stt(impacc[:, s, :], accv[:, s, 64:128], rd_[:, s:s + 1], impacc[:, s, :], ALU.mult, ALU.add, [accT, rd_, impacc], [impacc])

                    tiles = []
                    for h in range(4):
                        accT = PS[3 + accn % 2]
                        dn, rd_, cf_ = den[accn % 2], rden[accn % 2], coef[accn % 2]
                        accn += 1
                        chunks = [0, 1] if Q >= 4 else [0]
                        for c in chunks:
                            bt = btile[bn % 3]
                            bn += 1
                            off = h * 8208 + ZC + q0 - 31 - 16 * (c * 128 + 127)
                            src = bass.AP(tensor=ftc_h, offset=off, ap=[[16, 128], [1, 512]])
                            tl = dict(np=128, n=512, pv=[])
                            tl["pre"] = (bt, src)
                            tl["mms"] = [(0, 512, KcT[:, c * 128:(c + 1) * 128], Qn[h][:, q0:q0 + 512], [KcT, Qn[h]]),
                                         (0, 512, Jm[:], bt[:], [Jm, bt])]
                            for s in range(4):
                                tl["pv"].append((accT, accT[:, s * 128:(s + 1) * 128], s * 128, VC1[:, c, :], [VC1]))
                            if c == 0:
                                tl["init"] = (accT, 512)
                            if c == chunks[-1]:
                                tl["fin"] = (lambda accT=accT, h=h, dn=dn, rd_=rd_, cf_=cf_: fin_generic(accT, 128, h, 0, True, dn, rd_, cf_))
                            tiles.append(tl)
                    run_attention(tiles, pts)

                    if pend_fin[0] is not None:
                        pend_fin[0]()
                        pend_fin[0] = None
                    for s in range(4):
                        i_ = 4 * Q + s
                        iv = impacc[:, s, :]
                        if 2 * i_ + 2 < 64:
                            mset("dve", impacc[:, s, 2 * i_ + 2:64], -1e30, [impacc])
                        mset("dve", impacc[0:64, s, 2 * i_ + 1:2 * i_ + 2], -1e30, [impacc])
                        mset("dve", impacc[64:128, s, 2 * i_ + 1:2 * i_ + 2], 1e30, [impacc])
                        mset("dve", impacc[:, s, 2 * i_:2 * i_ + 1], 1e30, [impacc])
                        if i_ >= 1:
                            mset("dve", impacc[0:64, s, 2 * i_ - 1:2 * i_], 1e30, [impacc])
                        mset("dve", impacc[:, s, 0:1], 1e30, [impacc])
                        fw.op("dve", lambda e, iv=iv: e.max(out=m8[:], in_=iv), [impacc], [m8])
                        fw.op("dve", lambda e, iv=iv: e.match_replace(out=imp2[:], in_to_replace=m8[:], in_values=iv, imm_value=-3e38), [impacc, m8], [imp2])
                        fw.op("dve", lambda e: e.max(out=m8b[:], in_=imp2[:]), [imp2], [m8b])
                        ts("dve", self_[:], iv, m8b[:, 7:8], -1.0, ALU.is_ge, ALU.add, [impacc, m8b], [self_])
                        ts("dve", selb[:, s, 64:128], self_[:], -NEGM, None, ALU.mult, None, [self_], [selb])

                    tiles = []
                    for h in range(4):
                        accT = PS[3 + accn % 2]
                        dn, rd_, cf_ = den[accn % 2], rden[accn % 2], coef[accn % 2]
                        accn += 1
                        kts = [kt for kt in range(4 * Q - 4, 4 * Q + 4) if kt >= 0]
                        for kt in kts:
                            k0 = kt * 128
                            d = kt - 4 * Q
                            dd = max(d, 0)
                            n = 512 - 128 * dd
                            sc0 = 128 * (-d) if d < 0 else 0
                            tl = dict(np=128, n=n, pv=[])
                            tl["mms"] = [(0, n, KwT[:, k0:k0 + 128], Qn[h][:, q0 + 128 * dd:q0 + 512], [KwT, Qn[h]]),
                                         (0, n, Jm[:], slab_w[:, h, sc0:sc0 + n], [Jm, slab_w])]
                            for s in range(dd, 4):
                                tl["pv"].append((accT, accT[:, s * 65:(s + 1) * 65], (s - dd) * 128, VW1[:, kt, :], [VW1]))
                            if kt == kts[0]:
                                tl["init"] = (accT, 260)
                            if kt == kts[-1]:
                                tl["fin"] = (lambda accT=accT, h=h, dn=dn, rd_=rd_, cf_=cf_: fin_generic(accT, 65, h, 2, False, dn, rd_, cf_))
                            tiles.append(tl)
                    run_attention(tiles, pts)

                    for s in range(4):
                        fw.op("pe", lambda e, s=s: e.transpose(out=PSB[:, s * 128:(s + 1) * 128], in_=selb[:, s, :], identity=ident[:]), [selb, ident], [PSB])
                    for h in range(4):
                        cp("dve", Qn[h][64:128, q0:q0 + 512], PSB[64:128, 0:512], [PSB], [Qn[h]])

                    tiles = []
                    for h in range(4):
                        accT = PS[3 + accn % 2]
                        dn, rd_, cf_ = den[accn % 2], rden[accn % 2], coef[accn % 2]
                        accn += 1
                        nk = 4 * Q + 4
                        for kt in range(nk):
                            k0 = kt * 128
                            d = kt - 4 * Q
                            dd = max(d, 0)
                            n = 512 - 128 * dd
                            tl = dict(np=128, n=n, pv=[])
                            tl["mms"] = [(0, n, KsT[:, k0:k0 + 128], Qn[h][:, q0 + 128 * dd:q0 + 512], [KsT, Qn[h]])]
                            if d >= -1:
                                sc0 = 128 if d == -1 else 0
                                tl["mms"].append((0, n, Jm[:], slab_s[:, h, sc0:sc0 + n], [Jm, slab_s]))
                            else:
                                tl["bias"] = rb31[:, h:h + 1]
                                tl["bias_reads"] = [rb31]
                            for s in range(dd, 4):
                                tl["pv"].append((accT, accT[:, s * 65:(s + 1) * 65], (s - dd) * 128, VS1[:, kt, :], [VS1]))
                            if kt == 0:
                                tl["init"] = (accT, 260)
                            if kt == nk - 1:
                                tl["fin"] = (lambda accT=accT, h=h, dn=dn, rd_=rd_, cf_=cf_: fin_generic(accT, 65, h, 1, False, dn, rd_, cf_))
                            tiles.append(tl)
                    run_attention(tiles, pts)
                    pend_fin[0] = outnorm_tok(on_tl, Q, lambda s, Q=Q: yd[:, 4 * Q + s, :], yd, brow_all[:, l, 256:512], 768)
                pend_fin[0]()
                fw.barrier()
            if stop <= 4:
                break

            with ExitStack() as ph:
                wgu = sb(ph, "wgu", [128, 8, 2 * DFF], BF16)
                wdn = sb(ph, "wdn", [128, 22, 1024], BF16)
                wgu_u = [[U(), U()] for _ in range(8)]
                wdn_u = [U() for _ in range(22)]
                with ExitStack() as ph2:
                    wo = sb(ph2, "wo", [128, 8, 1024], BF16)
                    for c in range(8):
                        fw.dma("pool", wo[:, c, :], w_out[l, c * 128:(c + 1) * 128, :], writes=[wo])
                    xt = [sb(ph2, "xto%d" % i, [128, 8, 512]) for i in range(1)]
                    mt = [sb(ph2, "mt%d" % i, [128, 8, 512], BF16) for i in range(1)]
                    xo = [sb(ph2, "xo%d" % i, [128, 512]) for i in range(4)]
                    ev = 0
                    for tt_ in range(8):
                        t0 = tt_ * 512
                        xt_, mt_ = xt[0], mt[0]
                        rd = [u_xres[tt_]] if l > 0 else []
                        fw.dma("sp", xt_[:], x_src[:, t0:t0 + 512].rearrange("(c p) t -> p c t", p=128), reads=rd, writes=[xt_])
                        fw.dma("sp", mt_[:], mixT[:, t0:t0 + 512].rearrange("(c p) t -> p c t", p=128), reads=[u_mix[c][tt_] for c in range(8)], writes=[mt_])
                        if tt_ == 0:
                            for c in range(8):
                                for hh in range(2):
                                    fw.dma("pool", wgu[:, c, hh * DFF:(hh + 1) * DFF], w_gu[l, c * 128:(c + 1) * 128, hh * DFF:(hh + 1) * DFF], writes=[wgu_u[c][hh]])
                            for c in range(22):
                                fw.dma("pool", wdn[:, c, :], w_dn[l, c * 128:(c + 1) * 128, :], writes=[wdn_u[c]])
                        for n_ in range(8):
                            ps = PS[ev % 3]
                            for c in range(8):
                                mm(ps[:], wo[:, c, n_ * 128:(n_ + 1) * 128], mt_[:, c, :], c == 0, c == 7, [wo, mt_], [ps])
                            xo_ = xo[ev % 4]
                            tt("dve", xo_[:], xt_[:, n_, :], ps[:], ALU.add, [xt_, ps], [xo_])
                            fw.dma("sp", xres[n_ * 128:(n_ + 1) * 128, t0:t0 + 512], xo_[:], reads=[xo_], writes=[u_xres[tt_]])
                            ev += 1
                    fw.barrier()
                if stop <= 5:
                    break
                NT = 256
                xt = [sb(ph, "xtf%d" % i, [128, 8, NT]) for i in range(2)]
                xn = [sb(ph, "xnf%d" % i, [128, 8, NT], BF16) for i in range(2)]
                sq = sb(ph, "sqf", [128, 8, NT], BF16)
                lnb = sb(ph, "lnbf", [128, NT])
                rstd = sb(ph, "rstdf", [128, NT])
                hT = sb(ph, "hT", [128, 22, NT], BF16)
                sg = [sb(ph, "sg%d" % i, [128, NT]) for i in range(2)]
                xo = [sb(ph, "xof%d" % i, [128, NT]) for i in range(4)]
                ev = 0
                NTL = S // NT

                def norm9(tt_):
                    t0 = tt_ * NT
                    xt_, xn_ = xt[tt_ % 2], xn[tt_ % 2]
                    fw.dma("sp", xt_[:], xres[:, t0:t0 + NT].rearrange("(c p) t -> p c t", p=128), reads=[u_xres[t0 // 512]], writes=[xt_])
                    act(sq[:], xt_[:], AF.Square, [xt_], [sq])
                    for c in range(8):
                        mm(PS[6][:, 0:NT], ones1024[:], sq[:, c, :], c == 0, c == 7, [ones1024, sq], [PS[6]])
                    rstd_from(PS[6][:, 0:NT], rstd[:], lnb[:], [PS[6]], lnb, rstd)
                    for c in range(8):
                        stt(xn_[:, c, :], xt_[:, c, :], pv(8 + c), rstd[:], ALU.mult, ALU.mult, [xt_, rstd, pv_all], [xn_])

                norm9(0)
                for tt_ in range(NTL):
                    t0 = tt_ * NT
                    xt_, xn_ = xt[tt_ % 2], xn[tt_ % 2]
                    ux = u_xres[t0 // 512]
                    for j in range(22):
                        psg = PS[(2 * j) % 4]
                        psu = PS[(2 * j + 1) % 4]
                        for c in range(8):
                            mm(psg[:, 0:NT], wgu[:, c, j * 128:(j + 1) * 128], xn_[:, c, :], c == 0, c == 7, [wgu_u[c][0], xn_], [psg])
                        for c in range(8):
                            mm(psu[:, 0:NT], wgu[:, c, DFF + j * 128:DFF + (j + 1) * 128], xn_[:, c, :], c == 0, c == 7, [wgu_u[c][1], xn_], [psu])
                        sg_ = sg[j % 2]
                        act(sg_[:], psg[:, 0:NT], AF.Silu, [psg], [sg_])
                        tt("dve", hT[:, j, :], sg_[:], psu[:, 0:NT], ALU.mult, [sg_, psu], [hT])
                    if tt_ + 1 < NTL:
                        norm9(tt_ + 1)
                    for n_ in range(8):
                        ps = PS[4 + n_ % 2]
                        for j in range(22):
                            mm(ps[:, 0:NT], wdn[:, j, n_ * 128:(n_ + 1) * 128], hT[:, j, :], j == 0, j == 21, [wdn_u[j], hT], [ps])
                        xo_ = xo[ev % 4]
                        tt("dve", xo_[:], xt_[:, n_, :], ps[:, 0:NT], ALU.add, [xt_, ps], [xo_])
                        fw.dma("sp", x_dst[n_ * 128:(n_ + 1) * 128, t0:t0 + NT], xo_[:], reads=[xo_], writes=[ux] if x_dst is xres else [])
                        ev += 1
                fw.barrier()
        fw.barrier()
    return nc


def _bucket(d):
    d = np.maximum(d, 0)
    large = 16 + (np.log(np.maximum(d, 1).astype(np.float32) / np.float32(16)) / np.float32(np.log(128 / 16)) * np.float32(16)).astype(np.int32)
    large = np.minimum(large, 31)
    return np.where(d < 16, d, large)


def _host_consts():
    bf = ml_dtypes.bfloat16
    c = {}
    c["cident"] = np.eye(128, dtype=np.float32).astype(bf)
    c["cJ"] = np.eye(128, dtype=np.float32)[::-1].copy().astype(bf)
    kl = np.arange(128)[:, None]
    ql = np.arange(128)[None, :]
    c["ctri"] = np.where(ql >= kl, 0.0, NEGM).astype(np.float32).astype(bf)
    bd = np.zeros((128, 128), np.float32)
    bd[0:64, 0:64] = 1.0 / 64
    bd[64:128, 64:128] = 1.0 / 64
    c["cbd64"] = bd.astype(bf)
    E = np.zeros((64, S), np.float32)
    E[np.arange(S) // 64, np.arange(S)] = 1.0
    c["cE"] = E.astype(bf)
    n = np.arange(256)
    m = np.arange(64)
    cs = n[:, None] * 16
    ss = m[None, :] * 64
    ov = np.clip(np.minimum(cs + 32, ss + 64) - np.maximum(cs, ss), 0, 32).astype(np.float32)
    ov[255] = 0
    c["covl"] = np.concatenate([ov[0:128], ov[128:256]], axis=1).astype(bf)
    d = np.arange(8208) - ZC
    oh = np.zeros((33, 8208), np.float32)
    b = _bucket(d)
    oh[b[d >= 0], np.nonzero(d >= 0)[0]] = 1.0
    oh[32, d < 0] = 1.0
    c["ohc"] = oh.astype(bf)
    d = np.arange(1152) - ZW
    oh = np.zeros((33, 1152), np.float32)
    b = _bucket(d)
    ok = (d >= 0) & (d < 512)
    oh[b[ok], np.nonzero(ok)[0]] = 1.0
    oh[32, ~ok] = 1.0
    c["ohw"] = oh.astype(bf)
    return c


def _host_layout(inp):
    f = np.float32
    C_LRU_X, C_LRU_G, C_SC_B, C_SC_C, C_SC_X = 0, 256, 512, 768, 1024
    C_FOX_Q, C_FOX_K, C_FOX_V, C_FOX_F, C_NSA_Q, C_NSA_KV, C_NSA_G = 1280, 1536, 1792, 2048, 2052, 2308, 2692
    r = lambda a, n: list(range(a, a + n))
    perm = (r(C_LRU_X, 256) + r(C_LRU_G, 256) + r(C_SC_B, 256) + r(C_SC_C, 256) + r(C_SC_X, 256)
            + r(C_FOX_Q, 256) + r(C_FOX_K, 256) + r(C_NSA_Q, 256)
            + r(C_NSA_KV + 0, 64) + r(C_NSA_KV + 64, 64) + r(C_NSA_KV + 128, 64) + r(C_NSA_KV + 256, 64)
            + r(C_FOX_F, 4)
            + r(C_FOX_V, 256) + r(C_NSA_KV + 192, 64) + r(C_NSA_KV + 320, 64) + r(C_NSA_G, 12))
    assert len(perm) == 2704 and len(set(perm)) == 2704
    m = {}
    m["w_in"] = np.ascontiguousarray(np.asarray(inp["w_in"], f)[:, :, perm])
    m["w_out"] = np.ascontiguousarray(np.asarray(inp["w_out"], f))
    m["w_gu"] = np.ascontiguousarray(np.asarray(inp["w_gate_up"], f))
    m["w_dn"] = np.ascontiguousarray(np.asarray(inp["w_down"], f))
    pvec = np.zeros((NL, 128, 64), f)
    brow = np.zeros((NL, 1, 524), f)
    wbd = np.zeros((NL, 2, 2, 128, 128), f)
    for l in range(NL):
        pvec[l, :, 0:8] = np.asarray(inp["norm_mix"][l]).reshape(8, 128).T
        pvec[l, :, 8:16] = np.asarray(inp["norm_ffn"][l]).reshape(8, 128).T
        for j in range(2):
            sl = slice(j * 128, (j + 1) * 128)
            pvec[l, :, 16 + j * 4:20 + j * 4] = np.asarray(inp["lru_conv_w"][l])[:, sl].T
            pvec[l, :, 24 + j] = np.asarray(inp["lru_conv_b"][l])[sl]
            for g in range(2):
                pvec[l, :, 26 + g * 2 + j] = np.asarray(inp["lru_b_gates"][l])[g, sl]
                for bb in range(2):
                    wbd[l, g, j, bb * 64:(bb + 1) * 64, bb * 64:(bb + 1) * 64] = np.asarray(inp["lru_w_gates"][l])[g, 2 * j + bb]
            pvec[l, :, 30 + j] = np.asarray(inp["lru_lambda"][l])[sl]
            pvec[l, :, 32 + j * 3:35 + j * 3] = np.asarray(inp["sc_conv_w"][l])[:, sl].T
            for grp in range(2):
                pvec[l, :, 38 + grp * 2 + j] = np.asarray(inp["out_norm"][l])[grp, sl]
        pvec[l, 0:64, 42] = np.asarray(inp["fox_qk_norm"][l])[0]
        pvec[l, 0:64, 43] = np.asarray(inp["fox_qk_norm"][l])[1]
        for i in range(4):
            pvec[l, 0:64, 44 + i] = np.asarray(inp["nsa_qk_norm"][l])[i]
        pvec[l, 0:4, 48] = np.asarray(inp["fox_f_bias"][l])
        for hf in range(2):
            pvec[l, hf * 64:(hf + 1) * 64, 50] = np.asarray(inp["fox_qk_norm"][l])[0]
            pvec[l, hf * 64:(hf + 1) * 64, 51] = np.asarray(inp["fox_qk_norm"][l])[1]
            pvec[l, hf * 64:(hf + 1) * 64, 52] = np.asarray(inp["nsa_qk_norm"][l])[0]
            pvec[l, hf * 64:(hf + 1) * 64, 53] = np.asarray(inp["nsa_qk_norm"][l])[2 + hf]
        brow[l, 0, 0:256] = np.asarray(inp["out_norm"][l])[2]
        brow[l, 0, 256:512] = np.asarray(inp["out_norm"][l])[3]
        brow[l, 0, 512:524] = np.asarray(inp["nsa_gate_bias"][l])
    m["pvec"] = pvec
    m["brow"] = brow
    m["wbd"] = wbd
    m["cw1"] = np.ascontiguousarray(np.asarray(inp["nsa_cmp_w1"], f))
    m["cw2"] = np.ascontiguousarray(np.asarray(inp["nsa_cmp_w2"], f))
    m["cposT"] = np.ascontiguousarray(np.asarray(inp["nsa_cmp_pos"], f).transpose(0, 1, 3, 2))
    rb = np.zeros((33, 4), f)
    rb[0:32] = np.asarray(inp["rel_bias"], f)
    rb[32] = NEGM
    m["rb33"] = rb
    m.update(_host_consts())
    return m


def kernel(**inputs):
    x = np.asarray(inputs["x"], np.float32)
    shared = _host_layout(inputs)
    nc = build()
    in_maps = []
    for b in range(8):
        d = dict(shared)
        d["xT"] = np.ascontiguousarray(x[b].T)
        in_maps.append(d)
    res = run_bass_kernel_spmd(nc, in_maps, core_ids=list(range(8)))
    out = np.stack([np.ascontiguousarray(res.results[b]["out"].T) for b in range(8)], axis=0)
    return out.astype(np.float32)
```
